# Optimizing a Trainium2 kernel written in Bass

```python
import jax
import jax.numpy as jnp
from jax import lax
import numpy as np

D_MODEL = 2048
BATCH = 16
SEQ = 256
DEPTH = 1
DEC_BATCH = 8
DEC_SEQ = 1024
PAST_LEN = 512

GRID_W = 64
HG_HEADS = 8
HG_DK = 128
HG_DV = 128
HG_KW = HG_HEADS * HG_DK
HG_W = HG_HEADS * HG_DV
RET_HEADS = 8
RET_DK = 128
RET_DV = 256
RET_QK_W = RET_HEADS * RET_DK
RET_W = RET_HEADS * RET_DV
IN_COLS = 3 * HG_KW + 2 * HG_W + 2 * RET_QK_W + 2 * RET_W + 2 * D_MODEL
CHUNK = 32
ROPE_PAIRS = RET_DK // 4
ROPE_BASE = 10000.0
N_EXPERTS = 32
TOP_K = 4
D_FF = D_MODEL
SWIGLU_LIMIT = 7.0
SWIGLU_ALPHA = 1.702
MOE_BLOCK = 128
N_MOD = 6
EPS = 1e-6

kernel_name = "hybrid_hgrn2_retention_moe_dit_step"


def rmsnorm(x, g):
    xf = x.astype(jnp.float32)
    return xf * lax.rsqrt(jnp.mean(xf * xf, axis=-1, keepdims=True) + EPS) * g.astype(jnp.float32)


def head_layernorm(x, g):
    xf = x.astype(jnp.float32)
    xc = xf - jnp.mean(xf, axis=-1, keepdims=True)
    return xc * lax.rsqrt(jnp.mean(xc * xc, axis=-1, keepdims=True) + EPS) * g.astype(jnp.float32)


def to_heads(a, n_heads):
    b, t, _ = a.shape
    return a.reshape(b, t, n_heads, -1).transpose(0, 2, 1, 3)


def from_heads(a):
    b, h, t, d = a.shape
    return a.transpose(0, 2, 1, 3).reshape(b, t, h * d)


def chunk_gated_linear_scan(q, k, v, log_f, s0):
    f32 = jnp.float32
    q, k, v = q.astype(f32), k.astype(f32), v.astype(f32)
    b, h, t, _ = q.shape
    n = t // CHUNK

    def blocks(a):
        return a.reshape(a.shape[:2] + (n, CHUNK) + a.shape[3:])

    qc, kc, vc = blocks(q), blocks(k), blocks(v)
    g = jnp.cumsum(blocks(log_f.astype(f32)), axis=3)
    g_last = g[:, :, :, -1:]
    q_in = qc * jnp.exp(g)
    k_in = kc * jnp.exp(-g)
    k_end = kc * jnp.exp(g_last - g)
    tri = jnp.tril(jnp.ones((CHUNK, CHUNK), f32))
    scores = jnp.einsum("bhntd,bhnsd->bhnts", q_in, k_in) * tri
    o_intra = jnp.einsum("bhnts,bhnsv->bhntv", scores, vc)
    u = jnp.einsum("bhnsd,bhnsv->bhndv", k_end, vc)
    decay = jnp.exp(g_last[:, :, :, 0])

    def step(s, inp):
        d_c, u_c = inp
        return d_c[..., None] * s + u_c, s

    s_fin, s_start = lax.scan(step, s0.astype(f32),
                              (jnp.moveaxis(decay, 2, 0), jnp.moveaxis(u, 2, 0)))
    s_start = jnp.moveaxis(s_start, 0, 2)
    o_inter = jnp.einsum("bhntd,bhndv->bhntv", q_in, s_start)
    return (o_intra + o_inter).reshape(b, h, t, -1), s_fin


def latent_rope_tables(n_tokens):
    n_rows = n_tokens // GRID_W
    rows = jnp.repeat(jnp.arange(n_rows, dtype=jnp.float32), GRID_W)
    cols = jnp.tile(jnp.arange(GRID_W, dtype=jnp.float32), n_rows)
    inv = ROPE_BASE ** (-jnp.arange(ROPE_PAIRS, dtype=jnp.float32) / ROPE_PAIRS)
    ang = jnp.stack([rows, cols], axis=-1)[:, :, None] * inv
    return jnp.cos(ang), jnp.sin(ang)


def apply_axial_rope(x, cos, sin):
    b, h, t, _ = x.shape
    xa = x.astype(jnp.float32).reshape(b, h, t, 2, 2, ROPE_PAIRS)
    x1, x2 = xa[..., 0, :], xa[..., 1, :]
    return jnp.stack([x1 * cos - x2 * sin, x2 * cos + x1 * sin], axis=-2).reshape(b, h, t, RET_DK)


def token_mixer(xm, s_hf, s_hb, s_rf, s_rb, rope, w_in, lb_f, lb_b, hg_norm_g,
                ret_pf, ret_pb, ret_norm_g, w_pa, w_pb, w_out):
    b, t, _ = xm.shape
    sizes = (HG_KW, HG_KW, HG_KW, HG_W, HG_W, RET_QK_W, RET_QK_W, RET_W, RET_W, D_MODEL, D_MODEL)
    cuts = []
    acc = 0
    for s in sizes[:-1]:
        acc += s
        cuts.append(acc)
    proj = jnp.einsum("btd,dn->btn", xm, w_in)
    (hq, hzf, hzb, hi, hgate, rq, rk, rv, rgate, mga, mgb) = jnp.split(proj, cuts, axis=-1)

    q_h = to_heads(jax.nn.silu(hq), HG_HEADS)
    v_h = to_heads(hi, HG_HEADS)

    def hgrn_direction(zf, lb, s0, reverse):
        f = lb + (1.0 - lb) * jax.nn.sigmoid(zf.astype(jnp.float32))
        k_h = to_heads(1.0 - f, HG_HEADS)
        lf = to_heads(jnp.log(f), HG_HEADS)
        qq, vv = q_h, v_h
        if reverse:
            qq, k_h, vv, lf = (jnp.flip(a, axis=2) for a in (qq, k_h, vv, lf))
        o, s = chunk_gated_linear_scan(qq, k_h, vv, lf, s0)
        return (jnp.flip(o, axis=2) if reverse else o), s

    oa_f, f_hf = hgrn_direction(hzf, lb_f, s_hf, False)
    oa_b, f_hb = hgrn_direction(hzb, lb_b, s_hb, True)
    oa_sum = oa_f + oa_b
    oa = oa_sum * lax.rsqrt(jnp.mean(oa_sum * oa_sum, axis=-1, keepdims=True) + EPS) * hg_norm_g
    oa = from_heads(oa) * jax.nn.sigmoid(hgate)

    rq_h = to_heads(rq, RET_HEADS)
    rk_h = to_heads(rk, RET_HEADS) * (RET_DK ** -0.5)
    rv_h = to_heads(rv, RET_HEADS)
    if rope is not None:
        rq_h = apply_axial_rope(rq_h, rope[0], rope[1])
        rk_h = apply_axial_rope(rk_h, rope[0], rope[1])

    def ret_direction(p, s0, reverse):
        log_gamma = jnp.log1p(-jnp.exp2(p.astype(jnp.float32)))
        lf = jnp.broadcast_to(log_gamma[None, :, None, None], (1, RET_HEADS, t, 1))
        qq, kk, vv = rq_h, rk_h, rv_h
        if reverse:
            qq, kk, vv = (jnp.flip(a, axis=2) for a in (qq, kk, vv))
        o, s = chunk_gated_linear_scan(qq, kk, vv, lf, s0)
        return (jnp.flip(o, axis=2) if reverse else o), s

    ob_f, f_rf = ret_direction(ret_pf, s_rf, False)
    ob_b, f_rb = ret_direction(ret_pb, s_rb, True)
    ob = from_heads(head_layernorm(ob_f + ob_b, ret_norm_g)) * jax.nn.silu(rgate)

    branch_a = jnp.einsum("btc,cd->btd", oa, w_pa)
    branch_b = jnp.einsum("btc,cd->btd", ob, w_pb)
    merged = jax.nn.sigmoid(mga) * branch_a + jax.nn.sigmoid(mgb) * branch_b
    out = jnp.einsum("btd,de->bte", merged, w_out)
    return out, f_hf, f_hb, f_rf, f_rb


def moe_ffn(x, router_w, router_b, w_gu, b_gu, w_dn, b_dn):
    shp = x.shape
    xt = x.reshape(-1, D_MODEL)
    t = xt.shape[0]
    logits = (jnp.einsum("td,de->te", xt, router_w) + router_b).astype(jnp.float32)
    top_val, top_idx = lax.top_k(logits, TOP_K)
    top_w = jax.nn.softmax(top_val, axis=-1)
    a = t * TOP_K
    flat_e = top_idx.reshape(-1)
    flat_tok = jnp.arange(a, dtype=jnp.int32) // TOP_K
    flat_w = top_w.reshape(-1)
    order = jnp.argsort(flat_e)
    e_sorted = flat_e[order]
    counts = jnp.bincount(flat_e, length=N_EXPERTS)
    padded = (counts + MOE_BLOCK - 1) // MOE_BLOCK * MOE_BLOCK
    pad_end = jnp.cumsum(padded)
    pad_start = pad_end - padded
    start = jnp.cumsum(counts) - counts
    dest = pad_start[e_sorted] + jnp.arange(a) - start[e_sorted]
    n_blocks = -(-a // MOE_BLOCK) + N_EXPERTS
    rows = n_blocks * MOE_BLOCK
    row_tok = jnp.full((rows,), t, jnp.int32).at[dest].set(flat_tok[order])
    row_w = jnp.zeros((rows,), jnp.float32).at[dest].set(flat_w[order])
    block_e = jnp.minimum(
        jnp.searchsorted(pad_end, jnp.arange(n_blocks) * MOE_BLOCK, side="right"), N_EXPERTS - 1)
    x_pad = jnp.concatenate([xt, jnp.zeros((1, D_MODEL), xt.dtype)], axis=0)

    def run_block(args):
        tok, e = args
        xb = x_pad[tok]
        hgu = jnp.einsum("td,df->tf", xb, w_gu[e]) + b_gu[e]
        gate = jnp.minimum(hgu[:, 0::2], SWIGLU_LIMIT)
        up = jnp.clip(hgu[:, 1::2], -SWIGLU_LIMIT, SWIGLU_LIMIT)
        act = (up + 1.0) * gate * jax.nn.sigmoid(SWIGLU_ALPHA * gate)
        return jnp.einsum("tf,fd->td", act, w_dn[e]) + b_dn[e]

    y_rows = lax.map(run_block, (row_tok.reshape(n_blocks, MOE_BLOCK), block_e))
    y_rows = y_rows.reshape(rows, D_MODEL) * row_w[:, None]
    y = jax.ops.segment_sum(y_rows, row_tok, num_segments=t + 1)[:t]
    return y.reshape(shp)


def trunk_layer(x, cond, s_hf, s_hb, s_rf, s_rb, rope, ada_w, ada_b, norm1_g, norm2_g, w_in,
                lb_f, lb_b, hg_norm_g, ret_pf, ret_pb, ret_norm_g, w_pa, w_pb, w_out,
                router_w, router_b, w_gu, b_gu, w_dn, b_dn):
    mod = jnp.einsum("bd,dm->bm", jax.nn.silu(cond), ada_w) + ada_b
    sh1, sc1, g1, sh2, sc2, g2 = jnp.split(mod[:, None, :], N_MOD, axis=-1)
    xm = rmsnorm(x, norm1_g) * (1.0 + sc1) + sh1
    mix, f_hf, f_hb, f_rf, f_rb = token_mixer(xm, s_hf, s_hb, s_rf, s_rb, rope, w_in, lb_f, lb_b,
                                              hg_norm_g, ret_pf, ret_pb, ret_norm_g, w_pa, w_pb, w_out)
    x = x + g1 * mix
    xm = rmsnorm(x, norm2_g) * (1.0 + sc2) + sh2
    x = x + g2 * moe_ffn(xm, router_w, router_b, w_gu, b_gu, w_dn, b_dn)
    return x, f_hf, f_hb, f_rf, f_rb


def setup_inputs(seed: int = 0) -> dict:
    key = jax.random.key(seed)
    ks = jax.random.split(key, 32)
    f32 = jnp.float32

    def nrm(k, shape, scale):
        return jax.random.normal(k, shape, f32) * scale

    log2_base = -5.0 - jnp.arange(RET_HEADS, dtype=f32)
    return {
        "x_prompt": nrm(ks[0], (BATCH, SEQ, D_MODEL), 1.0),
        "x_sample": nrm(ks[1], (DEC_BATCH, DEC_SEQ, D_MODEL), 1.0),
        "state_hgrn_fwd": nrm(ks[2], (DEC_BATCH, DEPTH, HG_HEADS, HG_DK, HG_DV), 0.5),
        "state_hgrn_bwd": nrm(ks[3], (DEC_BATCH, DEPTH, HG_HEADS, HG_DK, HG_DV), 0.5),
        "state_ret_fwd": nrm(ks[4], (DEC_BATCH, DEPTH, RET_HEADS, RET_DK, RET_DV), 0.5),
        "state_ret_bwd": nrm(ks[5], (DEC_BATCH, DEPTH, RET_HEADS, RET_DK, RET_DV), 0.5),
        "c": nrm(ks[6], (DEC_BATCH, D_MODEL), 1.0),
        "c_ctx": nrm(ks[7], (D_MODEL,), 1.0),
        "ada_w": nrm(ks[8], (DEPTH, D_MODEL, N_MOD * D_MODEL), D_MODEL ** -0.5),
        "ada_b": nrm(ks[9], (DEPTH, N_MOD * D_MODEL), 0.02),
        "norm1_g": 1.0 + nrm(ks[10], (DEPTH, D_MODEL), 0.02),
        "norm2_g": 1.0 + nrm(ks[11], (DEPTH, D_MODEL), 0.02),
        "final_norm_g": 1.0 + nrm(ks[12], (D_MODEL,), 0.02),
        "w_in": nrm(ks[13], (DEPTH, D_MODEL, IN_COLS), D_MODEL ** -0.5),
        "hg_lb_fwd": nrm(ks[14], (DEPTH + 1, HG_KW), 0.1),
        "hg_lb_bwd": nrm(ks[15], (DEPTH + 1, HG_KW), 0.1),
        "hg_norm_g": 1.0 + nrm(ks[16], (DEPTH, HG_DV), 0.02),
        "ret_log2_fwd": log2_base + nrm(ks[17], (DEPTH, RET_HEADS), 0.1),
        "ret_log2_bwd": log2_base + nrm(ks[18], (DEPTH, RET_HEADS), 0.1),
        "ret_norm_g": 1.0 + nrm(ks[19], (DEPTH, RET_DV), 0.02),
        "w_proj_hgrn": nrm(ks[20], (DEPTH, HG_W, D_MODEL), HG_W ** -0.5),
        "w_proj_ret": nrm(ks[21], (DEPTH, RET_W, D_MODEL), RET_W ** -0.5),
        "w_out": nrm(ks[22], (DEPTH, D_MODEL, D_MODEL), D_MODEL ** -0.5),
        "router_w": nrm(ks[23], (DEPTH, D_MODEL, N_EXPERTS), D_MODEL ** -0.5),
        "router_b": nrm(ks[24], (DEPTH, N_EXPERTS), 0.01),
        "moe_w_gu": nrm(ks[25], (DEPTH, N_EXPERTS, D_MODEL, 2 * D_FF), D_MODEL ** -0.5),
        "moe_b_gu": nrm(ks[26], (DEPTH, N_EXPERTS, 2 * D_FF), 0.02),
        "moe_w_dn": nrm(ks[27], (DEPTH, N_EXPERTS, D_FF, D_MODEL), D_FF ** -0.5),
        "moe_b_dn": nrm(ks[28], (DEPTH, N_EXPERTS, D_MODEL), 0.02),
    }


def reference(x_prompt, x_sample, state_hgrn_fwd, state_hgrn_bwd, state_ret_fwd, state_ret_bwd,
              c, c_ctx, ada_w, ada_b, norm1_g, norm2_g, final_norm_g, w_in, hg_lb_fwd, hg_lb_bwd,
              hg_norm_g, ret_log2_fwd, ret_log2_bwd, ret_norm_g, w_proj_hgrn, w_proj_ret, w_out,
              router_w, router_b, moe_w_gu, moe_b_gu, moe_w_dn, moe_b_dn):
    f32 = jnp.float32
    lb_fwd_all = jnp.cumsum(jax.nn.softmax(hg_lb_fwd.astype(f32), axis=0), axis=0)
    lb_bwd_all = jnp.cumsum(jax.nn.softmax(hg_lb_bwd.astype(f32), axis=0), axis=0)
    b_ctx = x_prompt.shape[0]
    zeros_h = jnp.zeros((b_ctx, HG_HEADS, HG_DK, HG_DV), f32)
    zeros_r = jnp.zeros((b_ctx, RET_HEADS, RET_DK, RET_DV), f32)
    rope = latent_rope_tables(x_sample.shape[1])
    cond_ctx = c_ctx[None, :]
    h_ctx, h_lat = x_prompt, x_sample
    new_hf, new_hb, new_rf, new_rb = [], [], [], []
    for l in range(DEPTH):
        lw = (ada_w[l], ada_b[l], norm1_g[l], norm2_g[l], w_in[l], lb_fwd_all[l], lb_bwd_all[l],
              hg_norm_g[l], ret_log2_fwd[l], ret_log2_bwd[l], ret_norm_g[l], w_proj_hgrn[l],
              w_proj_ret[l], w_out[l], router_w[l], router_b[l], moe_w_gu[l], moe_b_gu[l],
              moe_w_dn[l], moe_b_dn[l])
        h_ctx, f_hf, f_hb, f_rf, f_rb = trunk_layer(h_ctx, cond_ctx, zeros_h, zeros_h, zeros_r,
                                                    zeros_r, None, *lw)
        new_hf.append(f_hf)
        new_hb.append(f_hb)
        new_rf.append(f_rf)
        new_rb.append(f_rb)
        h_lat, _, _, _, _ = trunk_layer(h_lat, c, state_hgrn_fwd[:, l], state_hgrn_bwd[:, l],
                                        state_ret_fwd[:, l], state_ret_bwd[:, l], rope, *lw)
    y_prompt = rmsnorm(h_ctx, final_norm_g).astype(x_prompt.dtype)
    y_sample = rmsnorm(h_lat, final_norm_g).astype(x_sample.dtype)
    new_hgrn_fwd = jnp.stack(new_hf, axis=1)
    new_hgrn_bwd = jnp.stack(new_hb, axis=1)
    new_ret_fwd = jnp.stack(new_rf, axis=1)
    new_ret_bwd = jnp.stack(new_rb, axis=1)
    return (y_prompt, y_sample, new_hgrn_fwd, new_hgrn_bwd, new_ret_fwd, new_ret_bwd)
```

```python
import math
from contextlib import ExitStack
import numpy as np
import ml_dtypes
import concourse.bass as bass
import concourse.mybir as mybir
from concourse.bass_utils import run_bass_kernel_spmd

F32 = mybir.dt.float32
BF16 = mybir.dt.bfloat16
I32 = mybir.dt.int32
AF = mybir.ActivationFunctionType
ALU = mybir.AluOpType
AX = mybir.AxisListType
EPS = 1e-6
CHUNK = 32
GRID_W = 64
ROPE_PAIRS = 32
TOPK = 4

FULL_CFG = dict(D=2048, H=8, TP=256, NP=2, TS=1024, E=32, NCORES=8)


class Sched:
    CE = ("pe", "act", "dve", "pool")

    def __init__(self, nc, nring=8):
        self.nc = nc
        self.q = {k: [] for k in ("pe", "act", "dve", "pool", "sp")}
        self.psem = {k: nc.alloc_semaphore(name="ps_" + k) for k in self.CE}
        self.pcnt = {k: 0 for k in self.CE}
        self.waited = {}
        self.ring = {}
        for qn in ("sp", "pool"):
            self.ring[qn] = dict(sems=[nc.alloc_semaphore(name="d_%s_%d" % (qn, i)) for i in range(nring)],
                                 vals=[0] * nring, nxt=0)
        self.lw = {}
        self.rd = {}
        self.ninst = 0

    def _wait(self, engn, ev):
        if ev is None:
            return
        sem, val = ev
        key = (engn, id(sem))
        if self.waited.get(key, 0) >= val:
            return
        self.waited[key] = val
        self.q[engn].append(lambda e, sem=sem, val=val: e.wait_ge(sem, val))
        self.ninst += 1

    def _deps(self, engn, reads, writes):
        for t in reads:
            self._wait(engn, self.lw.get(t))
        for t in writes:
            self._wait(engn, self.lw.get(t))
            for ev in self.rd.get(t, {}).values():
                self._wait(engn, ev)

    def _commit(self, ev, reads, writes):
        sem, val = ev
        for t in writes:
            self.lw[t] = ev
            self.rd[t] = {}
        for t in reads:
            d = self.rd.setdefault(t, {})
            old = d.get(id(sem))
            if old is None or old[1] < val:
                d[id(sem)] = ev

    @staticmethod
    def _excl(reads, writes):
        pr = [t for t in reads if t.startswith("ps")]
        if not pr:
            return list(reads), list(writes)
        return [t for t in reads if not t.startswith("ps")], list(writes) + [t for t in pr if t not in writes]

    def op(self, engn, fn, reads=(), writes=(), inc=True):
        reads, writes = self._excl(reads, writes)
        self._deps(engn, reads, writes)
        self.ninst += 1
        if inc:
            self.pcnt[engn] += 1
            sem = self.psem[engn]
            val = self.pcnt[engn]
            self.q[engn].append(lambda e, fn=fn, sem=sem: fn(e).then_inc(sem, 1))
            ev = (sem, val)
            self._commit(ev, reads, writes)
            return ev
        self.q[engn].append(lambda e, fn=fn: fn(e))
        return None

    def dma(self, qn, fn, reads=(), writes=()):
        r = self.ring[qn]
        i = r["nxt"]
        r["nxt"] = (i + 1) % len(r["sems"])
        sem = r["sems"][i]
        if r["vals"][i] > 0:
            self._wait(qn, (sem, r["vals"][i]))
        self._deps(qn, reads, writes)
        r["vals"][i] += 16
        val = r["vals"][i]
        self.q[qn].append(lambda e, fn=fn, sem=sem: fn(e).then_inc(sem, 16))
        self.ninst += 1
        ev = (sem, val)
        self._commit(ev, reads, writes)
        return ev

    def barrier(self):
        evs = [(self.psem[k], self.pcnt[k]) for k in self.CE if self.pcnt[k] > 0]
        for r in self.ring.values():
            evs += [(sem, v) for sem, v in zip(r["sems"], r["vals"]) if v > 0]
        for qn in self.q:
            for ev in evs:
                self._wait(qn, ev)

    def finish(self):
        self.barrier()
        with self.nc.Block() as block:
            @block.tensor
            def _(e):
                for t in self.q["pe"]:
                    t(e)

            @block.scalar
            def _(e):
                for t in self.q["act"]:
                    t(e)

            @block.vector
            def _(e):
                for t in self.q["dve"]:
                    t(e)

            @block.gpsimd
            def _(e):
                for t in self.q["pool"]:
                    t(e)

            @block.sync
            def _(e):
                for t in self.q["sp"]:
                    t(e)


class _Stop(Exception):
    pass


class Arena:
    def __init__(self, nc):
        self.nc = nc
        self.base = (nc.sbuf_base + 31) // 32 * 32
        self.top = nc.sbuf_top // 32 * 32
        self.cur = self.base
        self.n = 0
        self.peak = self.cur

    def alloc(self, name, shape, dtype):
        sz = {F32: 4, BF16: 2, I32: 4}[dtype]
        nbytes = int(np.prod(shape[1:])) * sz
        nbytes = (nbytes + 31) // 32 * 32
        assert self.cur + nbytes <= self.top, "SBUF overflow at %s: need %d have %d" % (name, nbytes, self.top - self.cur)
        self.n += 1
        t = self.nc.alloc_sbuf_tensor_at("%s_%d" % (name, self.n), list(shape), dtype, offset=self.cur)
        self.cur += nbytes
        self.peak = max(self.peak, self.cur)
        return t

    def mark(self):
        return self.cur

    def release(self, m):
        self.cur = m


def TS(out, in0, s1, s2, op0, op1=None):
    if op1 is None:
        return lambda e: e.tensor_scalar(out=out, in0=in0, scalar1=s1, scalar2=None, op0=op0)
    return lambda e: e.tensor_scalar(out=out, in0=in0, scalar1=s1, scalar2=s2, op0=op0, op1=op1)


def TT(out, a, b, op):
    return lambda e: e.tensor_tensor(out=out, in0=a, in1=b, op=op)


def STT(out, in0, sc, in1, op0, op1):
    return lambda e: e.scalar_tensor_tensor(out=out, in0=in0, scalar=sc, in1=in1, op0=op0, op1=op1)


def ACT(out, in_, func, bias=None, scale=None, accum=None):
    kw = {}
    if bias is not None:
        kw["bias"] = bias
    if scale is not None:
        kw["scale"] = scale
    if accum is not None:
        kw["accum_out"] = accum
    return lambda e: e.activation(out=out, in_=in_, func=func, **kw)


def CP(out, in_):
    return lambda e: e.tensor_copy(out=out, in_=in_)


def const_layout(cfg):
    T, TS_, E, KT, NB = cfg["T"], cfg["TS"], cfg["E"], cfg["KT"], cfg["NB"]
    NT = T // 128
    names = [("ident", 128), ("mf", 128), ("mb", 128), ("slf", 128), ("slb", 128), ("i2", 128),
             ("dpos", 128), ("dneg", 128), ("iota1", 128), ("iotar", 128), ("ones", 128), ("psw", 128),
             ("c127", 1), ("cp", 1), ("cmask", 4), ("reset", cfg["CH"]), ("iotae", E),
             ("tokid", NT), ("iotaw", KT), ("bbase", NB), ("onesrow", max(E, 8))]
    off = {}
    c = 0
    for n, w in names:
        off[n] = (c, w)
        c += w
    return off, c


def make_consts(cfg):
    off, ncol = const_layout(cfg)
    T, TS_, E, KT, NB = cfg["T"], cfg["TS"], cfg["E"], cfg["KT"], cfg["NB"]
    NT = T // 128
    C = np.zeros((128, ncol), np.float32)

    def put(n, a):
        o, w = off[n]
        C[:, o:o + w] = a

    s = np.arange(128)[:, None]
    t = np.arange(128)[None, :]
    same = (s // CHUNK) == (t // CHUNK)
    put("ident", (s == t))
    put("mf", same & (s <= t))
    put("mb", same & (s >= t))
    put("slf", s < t)
    put("slb", s > t)
    put("i2", 2.0 * (s == t))
    put("dpos", np.maximum(t - s, 0))
    put("dneg", np.maximum(s - t, 0))
    put("iota1", np.broadcast_to(t + 1, (128, 128)))
    put("iotar", np.broadcast_to(128 - t, (128, 128)))
    put("ones", 1.0)
    d = np.arange(128)
    partner = np.where((d % 64) < 32, d + 32, d - 32)
    psw = np.zeros((128, 128), np.float32)
    psw[partner, d] = 1.0
    put("psw", psw)
    put("c127", 127 - s)
    put("cp", s)
    put("cmask", (s // CHUNK) == np.arange(4)[None, :])
    put("reset", np.broadcast_to((np.arange(cfg["CH"]) % CHUNK != 0).astype(np.float32), (128, cfg["CH"])))
    tok = np.arange(TS_)
    rows = (tok // GRID_W).astype(np.float32)
    cols = (tok % GRID_W).astype(np.float32)
    inv = (10000.0 ** (-np.arange(ROPE_PAIRS, dtype=np.float32) / ROPE_PAIRS)).astype(np.float32)
    pos = np.where((d[:, None] // 64) == 0, rows[None, :], cols[None, :]).astype(np.float32)
    ang = (pos * inv[d % 32][:, None]).astype(np.float32)
    sign = np.where((d % 64) < 32, -1.0, 1.0)[:, None]
    rope = np.concatenate([np.cos(ang), np.sin(ang) * sign], axis=1).astype(np.float32)
    put("iotae", np.broadcast_to(np.arange(E), (128, E)))
    put("tokid", np.arange(NT)[None, :] * 128 + s)
    put("iotaw", np.arange(KT)[None, :] * 128 + s)
    put("bbase", np.broadcast_to(np.arange(NB) * 128, (128, NB)))
    put("onesrow", 1.0)
    return C, rope


def derive(cfg):
    cfg = dict(cfg)
    D, H = cfg["D"], cfg["H"]
    cfg["KT"] = D // 128
    cfg["T"] = cfg["NP"] * cfg["TP"] + cfg["TS"]
    cfg["NT"] = cfg["T"] // 128
    cfg["CH"] = min(512, cfg["T"])
    assert cfg["T"] % cfg["CH"] == 0
    cfg["NB"] = cfg["T"] * TOPK // 128 + cfg["E"]
    cfg["HGC"] = 640
    cfg["RTC"] = 768
    cfg["MG0"] = H * 640 + H * 768
    cfg["INC"] = cfg["MG0"] + 2 * D
    return cfg


def w_in_perm_index(cfg):
    D, H = cfg["D"], cfg["H"]
    HK = H * 128
    RW = H * 256
    o_hq, o_zf, o_zb, o_hi, o_hg = 0, HK, 2 * HK, 3 * HK, 4 * HK
    o_rq = 5 * HK
    o_rk = o_rq + HK
    o_rv = o_rk + HK
    o_rg = o_rv + RW
    o_ma = o_rg + RW
    o_mb = o_ma + D
    idx = []
    for h in range(H):
        r = np.arange(128)
        idx += [o_hq + h * 128 + r, o_zf + h * 128 + r, o_zb + h * 128 + r, o_hg + h * 128 + r, o_hi + h * 128 + r]
    for h in range(H):
        r = np.arange(128)
        r2 = np.arange(256)
        idx += [o_rq + h * 128 + r, o_rk + h * 128 + r, o_rg + h * 256 + r2, o_rv + h * 256 + r2]
    idx += [o_ma + np.arange(D), o_mb + np.arange(D)]
    return np.concatenate(idx)


def build(cfg, debug=False):
    cfg = derive(cfg)
    D, H, KT, T, NT, E, NB, CH = cfg["D"], cfg["H"], cfg["KT"], cfg["T"], cfg["NT"], cfg["E"], cfg["NB"], cfg["CH"]
    TP, NP, TS_ = cfg["TP"], cfg["NP"], cfg["TS"]
    DFF = D
    NCHK = T // CH
    TPC = CH // 128
    NCK = T // CHUNK
    R = NB * 128
    GW = min(512, D)
    NG = D // GW
    JT = GW // 128
    coff, ncst = const_layout(cfg)

    nc = bass.Bass("TRN2", target_bir_lowering=False)
    s = Sched(nc)
    A = Arena(nc)
    dbg_outs = {}

    def din(name, shape, dt=F32):
        return nc.dram_tensor(name, list(shape), dt, kind="ExternalInput")

    def dscr(name, shape, dt=F32):
        return nc.dram_tensor(name, list(shape), dt, kind="Internal")

    def dout(name, shape, dt=F32):
        return nc.dram_tensor(name, list(shape), dt, kind="ExternalOutput")

    x_d = din("x", [T, D])
    shf_d, shb_d = din("shf", [H, 128, 128]), din("shb", [H, 128, 128])
    srf_d, srb_d = din("srf", [H, 128, 256]), din("srb", [H, 128, 256])
    cT_d = din("cT", [128, KT, 2])
    adaw_d = din("ada_w", [D, 6 * D])
    adab_d = din("ada_b2", [2, 6 * D])
    n1g_d, n2g_d, nfg_d = din("n1g", [1, D]), din("n2g", [1, D]), din("nfg", [1, D])
    win_d = din("w_in", [D, cfg["INC"]])
    lbf_d, lbb_d = din("lbf", [128, 2, H]), din("lbb", [128, 2, H])
    hgng_d = din("hgng", [128, 1])
    retg_d = din("retg", [128, 2])
    rl2f_d, rl2b_d = din("rl2f", [1, H]), din("rl2b", [1, H])
    wpa_d, wpb_d, wout_d = din("w_pa", [H * 128, D]), din("w_pb", [H * 256, D]), din("w_out", [D, D])
    rw_d, rb_d = din("rw", [D, E]), din("rb", [1, E])
    wgu_d, bgu_d = din("w_gu", [E * D, 2 * DFF]), din("b_gu", [E, 2 * DFF])
    wdn_d, bdn_d = din("w_dn", [E * DFF, D]), din("b_dn", [E, D])
    cst_d = din("cst", [128, ncst])
    rope_d = din("rope", [128, 2 * TS_])

    y_o = dout("y", [T, D])
    hf_o, hb_o = dout("hf", [NP, H, 128, 128]), dout("hb", [NP, H, 128, 128])
    rf_o, rb_o = dout("rf", [NP, H, 128, 256]), dout("rb_o", [NP, H, 128, 256])

    MOD = dscr("MOD", [2, 6 * D])
    OAB = dscr("OAB", [3 * H, 128, T], BF16)
    X1 = dscr("X1", [T, D])
    XM2 = dscr("XM2", [T, D], BF16)
    ROWTOK = dscr("ROWTOK", [R, 128], I32)
    Y = dscr("Y", [R, D])

    def dump(name, ap, reads):
        if not debug:
            return
        shp = list(ap.shape)
        o = dout("dbg_" + name, shp, ap.dtype)
        dbg_outs[name] = shp
        s.dma("sp", lambda e: e.dma_start(out=o.ap(), in_=ap), reads=reads, writes=["dbg_" + name])

    psA = [nc.alloc_psum_tensor("psA%d" % i, [128, 512], F32) for i in range(4)]
    psS = [nc.alloc_psum_tensor("psS%d" % i, [128, 512], F32) for i in range(2)]
    psTbs = [nc.alloc_psum_tensor("psTb%d" % i, [128, 8, 128], BF16) for i in range(2)]

    cst = A.alloc("cst", [128, ncst], F32)

    def C(name):
        o, w = coff[name]
        return cst[:, o:o + w]

    identb = A.alloc("identb", [128, 128], BF16)
    onesb = A.alloc("onesb", [128, 128], BF16)
    slfb = A.alloc("slfb", [128, 128], BF16)
    small = A.alloc("small", [128, 64], F32)
    lb = A.alloc("lb", [128, 2, 2, H], F32)
    lg = A.alloc("lg", [128, 2, H], F32)
    hgng = A.alloc("hgng", [128, 1], F32)
    retg = A.alloc("retg", [128, 2], F32)
    oab_o = [A.alloc("oabo%d" % i, [128, CH], BF16) for i in range(2)]
    oab_ctr = [0]
    rw = A.alloc("rw", [128, KT, E], F32)
    rbrow = A.alloc("rbrow", [128, E], F32)
    Mf = A.alloc("Mf", [128, NT, E], F32)
    Mb = A.alloc("Mb", [128, NT, E], BF16)
    Wd = A.alloc("Wd", [128, NT, E], F32)
    lgt = A.alloc("lgt", [128, E], F32)
    pex = A.alloc("pex", [128, E], F32)
    top8 = A.alloc("top8", [128, 8], F32)
    lbraw = A.alloc("lbraw", [128, 2, 2, H], F32)
    mP = A.mark()
    WS = 768
    wslots = [A.alloc("wslot%d" % i, [128, max(KT, 2 * H), WS], BF16) for i in range(2)]
    wctr = [0]
    xmT = A.alloc("xmT", [128, KT, T], BF16)

    s.dma("sp", lambda e: e.dma_start(out=cst[:], in_=cst_d.ap()), writes=["cst"])
    s.op("dve", CP(identb[:], C("ident")), reads=["cst"], writes=["identb"])
    s.op("dve", CP(onesb[:], C("ones")), reads=["cst"], writes=["onesb"])
    s.op("dve", CP(slfb[:], C("slf")), reads=["cst"], writes=["slfb"])
    s.dma("sp", lambda e: e.dma_start(out=hgng[:], in_=hgng_d.ap()), writes=["hgng"])
    s.dma("sp", lambda e: e.dma_start(out=retg[:], in_=retg_d.ap()), writes=["retg"])

    def wload(src2d, kt_n, ncols):
        i = wctr[0] % 2
        wctr[0] += 1
        slot = wslots[i]
        tok = "w%d" % i
        s.dma("pool", lambda e: e.dma_start(out=slot[:, 0:kt_n, 0:ncols],
                                            in_=src2d.rearrange("(kt p) n -> p kt n", p=128)), writes=[tok])
        return slot, tok

    _bc = {}

    def BC(e, v):
        if v not in _bc:
            _bc[v] = e.to_reg(v)
        return _bc[v]

    def mms(items, reads, writes):
        n = len(items)
        for i, (o, l, r, st, sp_) in enumerate(items):
            s.op("pe", lambda e, o=o, l=l, r=r, st=st, sp_=sp_: e.matmul(o, lhsT=l, rhs=r, start=st, stop=sp_),
                 reads=reads, writes=writes, inc=(i == n - 1))

    def acc_mm(out, pairs, reads, writes):
        n = len(pairs)
        mms([(out, l, r, i == 0, i == n - 1) for i, (l, r) in enumerate(pairs)], reads, writes)

    def transposes(srcs, dst_fn, reads, dst_tok_fn, evac_eng="act", f32=False, extra=None):
        n = len(srcs)
        g = 0
        half = 0
        while g < n:
            m = min(4, n - g)
            if f32:
                pt, ptok = psS[1][:].rearrange("p (c v) -> p c v", v=128), "psS1"
                view = pt[:, 0:m, :]
            else:
                pt, ptok = psTbs[half], "psTb%d" % half
                view = pt[:, 0:m, :]
            for j in range(m):
                src = srcs[g + j]
                o = pt[:, j, :]
                idn = C("ident") if f32 else identb[:]
                s.op("pe", lambda e, o=o, src=src, idn=idn: e.transpose(out=o, in_=src, identity=idn),
                     reads=list(reads) + ["cst", "identb"], writes=[ptok], inc=(j == m - 1))
            dst = dst_fn(g, m)
            if evac_eng == "act":
                s.op("act", lambda e, dst=dst, view=view: e.copy(out=dst, in_=view), reads=[ptok], writes=dst_tok_fn(g, m))
            else:
                s.op("dve", CP(dst, view), reads=[ptok], writes=dst_tok_fn(g, m))
            if extra is not None:
                extra(pt, 0, g, m, ptok)
            g += m
            half ^= 1

    s.dma("sp", lambda e: e.dma_start(out=lbraw[:, 0], in_=lbf_d.ap()), writes=["lbraw0"])
    s.dma("sp", lambda e: e.dma_start(out=lbraw[:, 1], in_=lbb_d.ap()), writes=["lbraw1"])
    for d_ in range(2):
        s.op("dve", TT(lbraw[:, d_, 0, :], lbraw[:, d_, 0, :], lbraw[:, d_, 1, :], ALU.subtract),
             reads=["lbraw%d" % d_], writes=["lbraw%d" % d_])
        s.op("act", ACT(lb[:, d_, 0, :], lbraw[:, d_, 0, :], AF.Sigmoid), reads=["lbraw%d" % d_], writes=["lb%d" % d_])
        s.op("dve", TS(lb[:, d_, 1, :], lb[:, d_, 0, :], -1.0, 1.0, ALU.mult, ALU.add), reads=["lb%d" % d_], writes=["lb%d" % d_])
    for d_, src in enumerate((rl2f_d, rl2b_d)):
        s.dma("sp", lambda e, d_=d_, src=src: e.dma_start(out=lg[:, d_, :], in_=src.ap().partition_broadcast(128)),
              writes=["lg%d" % d_])
        s.op("act", ACT(lg[:, d_, :], lg[:, d_, :], AF.Exp, scale=math.log(2.0)), reads=["lg%d" % d_], writes=["lg%d" % d_])
        s.op("dve", TS(lg[:, d_, :], lg[:, d_, :], -1.0, 1.0, ALU.mult, ALU.add), reads=["lg%d" % d_], writes=["lg%d" % d_])
        s.op("act", ACT(lg[:, d_, :], lg[:, d_, :], AF.Ln), reads=["lg%d" % d_], writes=["lg%d" % d_])

    if cfg.get("STOP") == "0":
        s.finish(); nc._cfg = cfg; nc._ninst = s.ninst; nc._peak = 0; nc._dbg = dbg_outs
        return nc
    mA2 = A.mark()
    cT = A.alloc("cT", [128, KT, 2], F32)
    scb = A.alloc("scb", [128, KT, 2], BF16)
    s.dma("sp", lambda e: e.dma_start(out=cT[:], in_=cT_d.ap()), writes=["cT"])
    s.op("act", ACT(scb[:], cT[:], AF.Silu), reads=["cT"], writes=["scb"])
    modt = [A.alloc("modt%d" % i, [2, 512], F32) for i in range(2)]
    adab = [A.alloc("adab%d" % i, [2, 512], F32) for i in range(2)]
    NMC = (6 * D) // 512
    for j in range(NMC):
        slot, wt = wload(adaw_d.ap()[:, j * 512:(j + 1) * 512], KT, 512)
        ps = psA[j % 4]
        acc_mm(ps[0:2, :], [(scb[:, kt, :], slot[:, kt, 0:512]) for kt in range(KT)], ["scb", wt], ["psA%d" % (j % 4)])
        ab = adab[j % 2]
        mt = modt[j % 2]
        s.dma("sp", lambda e, ab=ab, j=j: e.dma_start(out=ab[:], in_=adab_d.ap()[:, j * 512:(j + 1) * 512]), writes=["adab%d" % (j % 2)])
        s.op("dve", TT(mt[:], ps[0:2, :], ab[:], ALU.add), reads=["psA%d" % (j % 4), "adab%d" % (j % 2)], writes=["modt%d" % (j % 2)])
        s.dma("sp", lambda e, mt=mt, j=j: e.dma_start(out=MOD.ap()[:, j * 512:(j + 1) * 512], in_=mt[:]),
              reads=["modt%d" % (j % 2)], writes=["MOD%d" % j])

    def modtoks(c0, c1):
        return ["MOD%d" % j for j in range(c0 // 512, (c1 - 1) // 512 + 1)]

    def load_row(dst, cond, which, tok):
        c0 = which * D
        s.dma("sp", lambda e: e.dma_start(out=dst, in_=MOD.ap()[cond:cond + 1, c0:c0 + D].partition_broadcast(128)),
              reads=modtoks(c0, c0 + D), writes=[tok])

    def load_vec_row(dst, src_d, tok):
        s.dma("sp", lambda e: e.dma_start(out=dst, in_=src_d.ap().partition_broadcast(128)), writes=[tok])

    tile_cond = [0] * (NP * TP // 128) + [1] * (TS_ // 128)
    segs = [(p * TP // 128, TP // 128, "p", p) for p in range(NP)] + [(NP * TP // 128, TS_ // 128, "s", 0)]

    rows_a = {}
    n1row = A.alloc("n1row", [128, D], F32)
    load_vec_row(n1row[:], n1g_d, "n1row")
    for c in range(2):
        a1 = A.alloc("a1row%d" % c, [128, D], F32)
        sh = A.alloc("sh1row%d" % c, [128, D], F32)
        load_row(a1[:], c, 1, "a1row%d" % c)
        load_row(sh[:], c, 0, "sh1row%d" % c)
        s.op("dve", STT(a1[:], a1[:], 1.0, n1row[:], ALU.add, ALU.mult), reads=["a1row%d" % c, "n1row"], writes=["a1row%d" % c])
        rows_a[c] = (a1, sh)
    xbuf = [A.alloc("xbuf%d" % i, [128, D], F32) for i in range(2)]
    t32 = A.alloc("t32", [128, D], F32)
    xmb = [A.alloc("xmb%d" % i, [128, D], BF16) for i in range(2)]
    junk = A.alloc("junk", [128, D], BF16)

    def rms_rstd(src, src_tok, col, n):
        sc = small[:, col:col + 1]
        tk = "small%d" % col
        s.op("dve", lambda e: e.memset(sc, 0.0), writes=[tk])
        s.op("act", ACT(junk[:, 0:n], src, AF.Square, accum=sc), reads=[src_tok, tk], writes=["junk", tk])
        s.op("dve", TS(sc, sc, 1.0 / n, EPS, ALU.mult, ALU.add), reads=[tk], writes=[tk])
        s.op("act", ACT(sc, sc, AF.Sqrt), reads=[tk], writes=[tk])
        s.op("dve", lambda e: e.reciprocal(out=sc, in_=sc), reads=[tk], writes=[tk])
        return sc, tk

    for i in range(NT):
        c = tile_cond[i]
        xt = xbuf[i % 2]
        xtok = "xbuf%d" % (i % 2)
        s.dma("sp", lambda e, xt=xt, i=i: e.dma_start(out=xt[:], in_=x_d.ap()[i * 128:(i + 1) * 128, :]), writes=[xtok])
        sc, tk = rms_rstd(xt[:], xtok, i % 2, D)
        a1, sh = rows_a[c]
        s.op("dve", STT(t32[:], xt[:], sc, a1[:], ALU.mult, ALU.mult), reads=[xtok, tk, "a1row%d" % c], writes=["t32"])
        xm = xmb[i % 2]
        s.op("dve", TT(xm[:], t32[:], sh[:], ALU.add), reads=["t32", "sh1row%d" % c], writes=["xmb%d" % (i % 2)])
        transposes([xm[:, kt * 128:(kt + 1) * 128] for kt in range(KT)],
                   lambda g, m, i=i: xmT[:, g:g + m, i * 128:(i + 1) * 128],
                   ["xmb%d" % (i % 2)], lambda g, m, i=i: ["xmT_%d" % i])
    xmT_toks = ["xmT_%d" % i for i in range(NT)]
    if debug:
        for kt in range(KT):
            dump("xmT%d" % kt, xmT[:, kt, :], xmT_toks)
    s.barrier()
    A.release(mA2)

    if cfg.get("STOP") == "A":
        s.finish(); nc._cfg = cfg; nc._ninst = s.ninst; nc._peak = 0; nc._dbg = dbg_outs
        return nc
    mB = A.mark()
    q32 = A.alloc("q32", [128, T], BF16)
    fdir = [A.alloc("f%d" % d_, [128, T], F32) for d_ in range(2)]
    s1 = A.alloc("s1", [128, T], F32)
    s2 = A.alloc("s2", [128, T], F32)
    gcol = A.alloc("gcol", [128, NCK], F32)
    sgate = A.alloc("sgate", [128, T], BF16)
    vTM = A.alloc("vTM", [128, NT, 128], BF16)
    qp = [A.alloc("qp%d" % d_, [128, T], BF16) for d_ in range(2)]
    kp = [A.alloc("kp%d" % d_, [128, T], BF16) for d_ in range(2)]
    kpTM = [A.alloc("kpTM%d" % d_, [128, NT, 128], BF16) for d_ in range(2)]
    vexp = A.alloc("vexp", [128, NT, 4, 128], BF16)
    Rpb = [A.alloc("Rpb%d" % d_, [128, NCK, 128], BF16) for d_ in range(2)]
    decay = [A.alloc("decay%d" % d_, [128, NCK], F32) for d_ in range(2)]
    R32 = [[A.alloc("R32_%d_%d" % (d_, sg), [128, 128], F32) for sg in range(len(segs))] for d_ in range(2)]
    Rp32 = A.alloc("Rp32", [128, 128], F32)
    msk = [A.alloc("msk%d" % i, [128, 128], BF16) for i in range(2)]
    sqb = A.alloc("sqb", [128, CH], BF16)
    Mdir = [C("mf"), C("mb")]

    SB = cfg.get("SB", 99)
    for h in range(H if SB == 99 else 1):
        slot, wt = wload(win_d.ap()[:, h * 640:(h + 1) * 640], KT, 640)
        for ch in range(NCHK):
            cs = slice(ch * CH, (ch + 1) * CH)
            for j in range(4):
                acc_mm(psA[j][:, 0:CH], [(slot[:, kt, j * 128:(j + 1) * 128], xmT[:, kt, cs]) for kt in range(KT)],
                       [wt] + xmT_toks, ["psA%d" % j])
            s.op("act", ACT(q32[:, cs], psA[0][:, 0:CH], AF.Silu), reads=["psA0"], writes=["q32"])
            for d_ in range(2):
                s.op("act", ACT(s1[:, cs], psA[1 + d_][:, 0:CH], AF.Sigmoid), reads=["psA%d" % (1 + d_)], writes=["s1"])
                s.op("dve", TS(fdir[d_][:, cs], s1[:, cs], lb[:, d_, 1, h:h + 1], lb[:, d_, 0, h:h + 1], ALU.mult, ALU.add),
                     reads=["s1", "lb%d" % d_], writes=["f%d" % d_])
            s.op("act", ACT(sgate[:, cs], psA[3][:, 0:CH], AF.Sigmoid), reads=["psA3"], writes=["sgate"])
        for i in range(NT):
            ps = psS[i % 2]
            acc_mm(ps[:, 0:128], [(xmT[:, kt, i * 128:(i + 1) * 128], slot[:, kt, 512:640]) for kt in range(KT)],
                   [wt] + xmT_toks, ["psS%d" % (i % 2)])
            s.op("act", lambda e, ps=ps, i=i: e.copy(out=vTM[:, i, :], in_=ps[:, 0:128]), reads=["psS%d" % (i % 2)], writes=["vTM"])
        cmo, _ = coff["cmask"]
        for c in range(4):
            s.op("dve", TS(vexp[:, :, c, :], vTM[:], cst[:, cmo + c:cmo + c + 1], None, ALU.mult), reads=["vTM", "cst"], writes=["vexp"])
        if SB == 1:
            break
        for d_ in range(2):
            f = fdir[d_]
            ftok = "f%d" % d_
            s.op("act", ACT(s1[:], f[:], AF.Ln), reads=[ftok], writes=["s1"])
            s.op("dve", TS(f[:], f[:], -1.0, 1.0, ALU.mult, ALU.add), reads=[ftok, "s1"], writes=[ftok])
            for ch in range(NCHK):
                cs = slice(ch * CH, (ch + 1) * CH)
                s.op("dve", lambda e, cs=cs: e.tensor_tensor_scan(out=s2[:, cs], data0=C("reset"), data1=s1[:, cs], initial=0.0,
                                                                  op0=ALU.mult, op1=ALU.add), reads=["s1", "cst"], writes=["s2"])
            gi3 = s2[:].rearrange("p (c k) -> p c k", k=CHUNK)
            s.op("act", ACT(decay[d_][:], gi3[:, :, CHUNK - 1], AF.Exp), reads=["s2"], writes=["decay%d" % d_])
            if d_ == 0:
                s.op("dve", CP(gcol[:], gi3[:, :, CHUNK - 1]), reads=["s2"], writes=["gcol"])
                s.op("dve", TT(gi3, gcol[:].unsqueeze(2).to_broadcast([128, NCK, CHUNK]), gi3, ALU.subtract),
                     reads=["s2", "gcol", "decay%d" % d_], writes=["s2"])
            else:
                s.op("dve", TT(s2[:], s2[:], s1[:], ALU.subtract), reads=["s2", "s1", "decay%d" % d_], writes=["s2"])
            s.op("act", ACT(s1[:], s2[:], AF.Exp), reads=["s2"], writes=["s1"])
            s.op("dve", TT(kp[d_][:], f[:], s1[:], ALU.mult), reads=[ftok, "s1"], writes=["kp%d" % d_])
            s.op("act", ACT(s1[:], s2[:], AF.Exp, scale=-1.0), reads=["s2", "kp%d" % d_], writes=["s1"])
            s.op("dve", TT(qp[d_][:], q32[:], s1[:], ALU.mult), reads=["q32", "s1"], writes=["qp%d" % d_])
            if SB == 2:
                continue
            transposes([kp[d_][:, i * 128:(i + 1) * 128] for i in range(NT)],
                       lambda g, m, d_=d_: kpTM[d_][:, g:g + m, :], ["kp%d" % d_], lambda g, m, d_=d_: ["kpTM%d" % d_])
            if SB == 3:
                continue
            for sgi, (t0, nt, kind, sidx) in enumerate(segs):
                Rr = R32[d_][sgi]
                rtok = "R32_%d_%d" % (d_, sgi)
                if kind == "p":
                    s.op("dve", lambda e, Rr=Rr: e.memset(Rr[:], 0.0), writes=[rtok])
                else:
                    src = (shf_d, shb_d)[d_]
                    s.dma("sp", lambda e, Rr=Rr, src=src, h=h: e.dma_start(out=Rr[:], in_=src.ap()[h]), writes=[rtok])
                tiles = list(range(t0, t0 + nt))
                if d_ == 1:
                    tiles = tiles[::-1]
                for i in tiles:
                    ps = psS[i % 2]
                    ptok = "psS%d" % (i % 2)
                    pv = ps[:].rearrange("p (c v) -> p c v", v=128)
                    mms([(ps[:, 0:512], kpTM[d_][:, i, :], vexp[:, i].rearrange("p c v -> p (c v)"), True, True)], ["kpTM%d" % d_, "vexp"], [ptok])
                    cl = list(range(4))
                    if d_ == 1:
                        cl = cl[::-1]
                    for c in cl:
                        cg = i * 4 + c
                        s.op("dve", TS(Rp32[:], Rr[:], decay[d_][:, cg:cg + 1], None, ALU.mult),
                             reads=[rtok, "decay%d" % d_], writes=["Rp32"])
                        s.op("act", lambda e, d_=d_, cg=cg: e.copy(out=Rpb[d_][:, cg, :], in_=Rp32[:]),
                             reads=["Rp32"], writes=["Rpb%d" % d_])
                        s.op("dve", TT(Rr[:], Rp32[:], pv[:, c, :], ALU.add), reads=["Rp32", ptok], writes=[rtok])
                if kind == "p":
                    dst = (hf_o, hb_o)[d_]
                    s.dma("sp", lambda e, Rr=Rr, dst=dst, sidx=sidx, h=h: e.dma_start(out=dst.ap()[sidx, h], in_=Rr[:]),
                          reads=[rtok], writes=["hout%d_%d_%d" % (d_, sidx, h)])
        if SB in (2, 3, 4):
            break
        for ch in range(NCHK):
            its = []
            for tl in range(TPC):
                i = ch * TPC + tl
                tsl = slice(i * 128, (i + 1) * 128)
                reg = psA[0][:, tl * 128:(tl + 1) * 128]
                first = True
                for d_ in range(2):
                    ps = psS[d_]
                    s.op("pe", lambda e, ps=ps, d_=d_, tsl=tsl: e.matmul(ps[:, 0:128], lhsT=kp[d_][:, tsl], rhs=qp[d_][:, tsl], start=True, stop=True),
                         reads=["kp%d" % d_, "qp%d" % d_], writes=["psS%d" % d_])
                    mk = msk[d_]
                    s.op("dve", TT(mk[:], ps[:, 0:128], Mdir[d_], ALU.mult), reads=["psS%d" % d_, "cst"], writes=["msk%d" % d_])
                    items = [(reg, vTM[:, i, :], mk[:], first, (SB == 5 and d_ == 1))]
                    first = False
                    for c in range(4 if SB != 5 else 0):
                        cg = i * 4 + c
                        items.append((psA[0][:, tl * 128 + c * 32: tl * 128 + (c + 1) * 32], Rpb[d_][:, cg, :],
                                      qp[d_][:, i * 128 + c * 32:i * 128 + (c + 1) * 32], False, (d_ == 1 and c == 3)))
                    mms(items, ["vTM", "msk%d" % d_, "Rpb%d" % d_, "qp%d" % d_], ["psA0"])
            cs = slice(ch * CH, (ch + 1) * CH)
            if SB in (5, 6):
                continue
            s.op("dve", CP(s1[:, 0:CH], psA[0][:, 0:CH]), reads=["psA0"], writes=["s1"])
            s.op("act", ACT(sqb[:], psA[0][:, 0:CH], AF.Square), reads=["psA0"], writes=["sqb"])
            acc_mm(psA[1][:, 0:CH], [(onesb[:], sqb[:])], ["onesb", "sqb"], ["psA1"])
            s.op("dve", TS(s2[:, 0:CH], psA[1][:, 0:CH], 1.0 / 128, EPS, ALU.mult, ALU.add), reads=["psA1"], writes=["s2"])
            s.op("act", ACT(s2[:, 0:CH], s2[:, 0:CH], AF.Sqrt), reads=["s2"], writes=["s2"])
            s.op("dve", lambda e: e.reciprocal(out=s2[:, 0:CH], in_=s2[:, 0:CH]), reads=["s2"], writes=["s2"])
            if SB == 7:
                continue
            s.op("dve", STT(s1[:, 0:CH], s1[:, 0:CH], hgng[:, 0:1], s2[:, 0:CH], ALU.mult, ALU.mult), reads=["s1", "s2", "hgng"], writes=["s1"])
            ob = oab_o[oab_ctr[0] % 2]
            otok = "oabo%d" % (oab_ctr[0] % 2)
            oab_ctr[0] += 1
            s.op("dve", TT(ob[:], s1[:, 0:CH], sgate[:, cs], ALU.mult), reads=["s1", "sgate"], writes=[otok])
            if SB == 8:
                continue
            s.dma("sp", lambda e, ob=ob, h=h, cs=cs: e.dma_start(out=OAB.ap()[h, :, cs], in_=ob[:]), reads=[otok], writes=["OAB_%d_%d" % (h, ch)])
    s.barrier()
    A.release(mB)

    if cfg.get("STOP") == "B":
        s.finish(); nc._cfg = cfg; nc._ninst = s.ninst; nc._peak = 0; nc._dbg = dbg_outs
        return nc
    mC = A.mark()
    q32 = A.alloc("rq32", [128, T], F32)
    k32 = A.alloc("rk32", [128, T], F32)
    qb = A.alloc("rqb", [128, T], BF16)
    kb = A.alloc("rkb", [128, T], BF16)
    qin = [A.alloc("qin%d" % d_, [128, T], BF16) for d_ in range(2)]
    kend = [A.alloc("kend%d" % d_, [128, NT, 128], BF16) for d_ in range(2)]
    rvTM = A.alloc("rvTM", [128, NT, 256], BF16)
    Rsb = [A.alloc("Rsb%d" % d_, [128, NT, 256], BF16) for d_ in range(2)]
    rg = A.alloc("rg", [128, 2, T], BF16)
    RR = [[A.alloc("RR_%d_%d" % (d_, sg), [128, 256], F32) for sg in range(len(segs))] for d_ in range(2)]
    tA = A.alloc("tA", [128, 128], F32)
    tB = A.alloc("tB", [128, 128], F32)
    Ds = A.alloc("Ds", [128, 128], F32)
    rowt = [A.alloc("rowt%d" % d_, [128, 128], F32) for d_ in range(2)]
    colt = A.alloc("colt", [128, 4], F32)
    t1 = A.alloc("t1", [128, CH], F32)
    t2 = A.alloc("t2", [128, CH], F32)
    rmsk = [A.alloc("rmsk%d" % i, [128, 128], BF16) for i in range(2)]
    ro32 = [A.alloc("ro32_%d" % v, [128, CH], F32) for v in range(2)]
    rob = [A.alloc("rob_%d" % v, [128, CH], BF16) for v in range(2)]
    rsq = [A.alloc("rsq_%d" % v, [128, CH], BF16) for v in range(2)]
    mean = A.alloc("mean", [128, CH], F32)
    var = A.alloc("var", [128, CH], F32)
    ts0 = NP * TP
    ropet = A.alloc("ropet", [128, 2 * TS_], F32)
    s.dma("sp", lambda e: e.dma_start(out=ropet[:], in_=rope_d.ap()), writes=["ropet"])

    for h in range(H):
        c0 = H * 640 + h * 768
        slot, wt = wload(win_d.ap()[:, c0:c0 + 768], KT, 768)
        for ch in range(NCHK):
            cs = slice(ch * CH, (ch + 1) * CH)
            for j in range(4):
                acc_mm(psA[j][:, 0:CH], [(slot[:, kt, j * 128:(j + 1) * 128], xmT[:, kt, cs]) for kt in range(KT)],
                       [wt] + xmT_toks, ["psA%d" % j])
            s.op("act", lambda e, cs=cs: e.copy(out=q32[:, cs], in_=psA[0][:, 0:CH]), reads=["psA0"], writes=["rq32"])
            s.op("act", ACT(k32[:, cs], psA[1][:, 0:CH], AF.Copy, scale=128.0 ** -0.5), reads=["psA1"], writes=["rk32"])
            for v in range(2):
                s.op("act", ACT(rg[:, v, cs], psA[2 + v][:, 0:CH], AF.Silu), reads=["psA%d" % (2 + v)], writes=["rg"])
        for i in range(NT):
            ps = psS[i % 2]
            acc_mm(ps[:, 0:256], [(xmT[:, kt, i * 128:(i + 1) * 128], slot[:, kt, 512:768]) for kt in range(KT)],
                   [wt] + xmT_toks, ["psS%d" % (i % 2)])
            s.op("act", lambda e, ps=ps, i=i: e.copy(out=rvTM[:, i, :], in_=ps[:, 0:256]), reads=["psS%d" % (i % 2)], writes=["rvTM"])
        if ts0 > 0:
            s.op("dve", CP(qb[:, 0:ts0], q32[:, 0:ts0]), reads=["rq32"], writes=["rqb"])
            s.op("dve", CP(kb[:, 0:ts0], k32[:, 0:ts0]), reads=["rk32"], writes=["rkb"])
        RC = min(512, TS_)
        for rc in range(TS_ // RC):
            cs = slice(ts0 + rc * RC, ts0 + (rc + 1) * RC)
            cosv = ropet[:, rc * RC:(rc + 1) * RC]
            sinv = ropet[:, TS_ + rc * RC: TS_ + (rc + 1) * RC]
            for src, dstb, stok, dtok, pi in ((q32, qb, "rq32", "rqb", 2), (k32, kb, "rk32", "rkb", 3)):
                acc_mm(psA[pi][:, 0:RC], [(C("psw"), src[:, cs])], ["cst", stok], ["psA%d" % pi])
                s.op("dve", TT(t1[:, 0:RC], src[:, cs], cosv, ALU.mult), reads=[stok, "ropet"], writes=["t1"])
                s.op("dve", TT(t2[:, 0:RC], psA[pi][:, 0:RC], sinv, ALU.mult), reads=["psA%d" % pi, "ropet"], writes=["t2"])
                s.op("dve", TT(dstb[:, cs], t1[:, 0:RC], t2[:, 0:RC], ALU.add), reads=["t1", "t2"], writes=[dtok])
        lgf = lg[:, 0, h:h + 1]
        lgb = lg[:, 1, h:h + 1]
        s.op("act", ACT(tA[:], C("dpos"), AF.Exp, scale=lgf), reads=["cst", "lg0"], writes=["tA"])
        s.op("dve", TT(tA[:], tA[:], C("slf"), ALU.mult), reads=["tA", "cst"], writes=["tA"])
        s.op("act", ACT(tB[:], C("dneg"), AF.Exp, scale=lgb), reads=["cst", "lg1"], writes=["tB"])
        s.op("dve", TT(tB[:], tB[:], C("slb"), ALU.mult), reads=["tB", "cst"], writes=["tB"])
        s.op("dve", TT(Ds[:], tA[:], tB[:], ALU.add), reads=["tA", "tB"], writes=["Ds"])
        s.op("dve", TT(Ds[:], Ds[:], C("i2"), ALU.add), reads=["Ds", "cst"], writes=["Ds"])
        s.op("act", ACT(rowt[0][:], C("iota1"), AF.Exp, scale=lgf), reads=["cst", "lg0"], writes=["rowt0"])
        s.op("act", ACT(rowt[1][:], C("iotar"), AF.Exp, scale=lgb), reads=["cst", "lg1"], writes=["rowt1"])
        s.op("act", ACT(colt[:, 0:1], C("c127"), AF.Exp, scale=lgf), reads=["cst", "lg0"], writes=["colt"])
        s.op("act", ACT(colt[:, 1:2], C("cp"), AF.Exp, scale=lgb), reads=["cst", "lg1"], writes=["colt"])
        s.op("act", ACT(colt[:, 2:3], lgf, AF.Exp, scale=128.0), reads=["lg0"], writes=["colt"])
        s.op("act", ACT(colt[:, 3:4], lgb, AF.Exp, scale=128.0), reads=["lg1"], writes=["colt"])
        for i in range(NT):
            tsl = slice(i * 128, (i + 1) * 128)
            for d_ in range(2):
                s.op("dve", TT(qin[d_][:, tsl], qb[:, tsl], rowt[d_][:], ALU.mult), reads=["rqb", "rowt%d" % d_], writes=["qin%d" % d_])
        g = 0
        half = 0
        while g < NT:
            m = min(4, NT - g)
            ptok = "psTb%d" % half
            for j in range(m):
                o = psTbs[half][:, j, :]
                src = kb[:, (g + j) * 128:(g + j + 1) * 128]
                s.op("pe", lambda e, o=o, src=src: e.transpose(out=o, in_=src, identity=identb[:]),
                     reads=["rkb", "identb"], writes=[ptok], inc=(j == m - 1))
            view = psTbs[half][:, 0:m, :]
            for d_ in range(2):
                s.op("act", ACT(kend[d_][:, g:g + m, :], view, AF.Copy, scale=colt[:, d_:d_ + 1]), reads=[ptok, "colt"], writes=["kend%d" % d_])
            g += m
            half ^= 1
        for d_ in range(2):
            for sgi, (t0, nt, kind, sidx) in enumerate(segs):
                Rr = RR[d_][sgi]
                rtok = "RR_%d_%d" % (d_, sgi)
                if kind == "p":
                    s.op("dve", lambda e, Rr=Rr: e.memset(Rr[:], 0.0), writes=[rtok])
                else:
                    src = (srf_d, srb_d)[d_]
                    s.dma("sp", lambda e, Rr=Rr, src=src, h=h: e.dma_start(out=Rr[:], in_=src.ap()[h]), writes=[rtok])
                tiles = list(range(t0, t0 + nt))
                if d_ == 1:
                    tiles = tiles[::-1]
                for i in tiles:
                    ps = psS[i % 2]
                    ptok = "psS%d" % (i % 2)
                    acc_mm(ps[:, 0:256], [(kend[d_][:, i, :], rvTM[:, i, :])], ["kend%d" % d_, "rvTM"], [ptok])
                    s.op("act", lambda e, d_=d_, i=i, Rr=Rr: e.copy(out=Rsb[d_][:, i, :], in_=Rr[:]), reads=[rtok], writes=["Rsb%d" % d_])
                    s.op("dve", STT(Rr[:], Rr[:], colt[:, 2 + d_:3 + d_], ps[:, 0:256], ALU.mult, ALU.add),
                         reads=[rtok, "colt", ptok], writes=[rtok])
                if kind == "p":
                    dst = (rf_o, rb_o)[d_]
                    s.dma("sp", lambda e, Rr=Rr, dst=dst, sidx=sidx, h=h: e.dma_start(out=dst.ap()[sidx, h], in_=Rr[:]),
                          reads=[rtok], writes=["rout%d_%d_%d" % (d_, sidx, h)])
        for ch in range(NCHK):
            cs = slice(ch * CH, (ch + 1) * CH)
            for tl in range(TPC):
                i = ch * TPC + tl
                tsl = slice(i * 128, (i + 1) * 128)
                ps = psS[i % 2]
                s.op("pe", lambda e, ps=ps, tsl=tsl: e.matmul(ps[:, 0:128], lhsT=kb[:, tsl], rhs=qb[:, tsl], start=True, stop=True),
                     reads=["rkb", "rqb"], writes=["psS%d" % (i % 2)])
                mk = rmsk[i % 2]
                s.op("dve", TT(mk[:], ps[:, 0:128], Ds[:], ALU.mult), reads=["psS%d" % (i % 2), "Ds"], writes=["rmsk%d" % (i % 2)])
                for v in range(2):
                    reg = psA[v][:, tl * 128:(tl + 1) * 128]
                    vs = slice(v * 128, (v + 1) * 128)
                    mms([(reg, rvTM[:, i, vs], mk[:], True, False),
                         (reg, Rsb[0][:, i, vs], qin[0][:, tsl], False, False),
                         (reg, Rsb[1][:, i, vs], qin[1][:, tsl], False, True)],
                        ["rvTM", "rmsk%d" % (i % 2), "Rsb0", "Rsb1", "qin0", "qin1"], ["psA%d" % v])
            for v in range(2):
                s.op("dve", CP(ro32[v][:], psA[v][:, 0:CH]), reads=["psA%d" % v], writes=["ro32_%d" % v])
                s.op("act", lambda e, v=v: e.copy(out=rob[v][:], in_=psA[v][:, 0:CH]), reads=["psA%d" % v], writes=["rob%d" % v])
                s.op("act", ACT(rsq[v][:], psA[v][:, 0:CH], AF.Square), reads=["psA%d" % v], writes=["rsq%d" % v])
            acc_mm(psA[2][:, 0:CH], [(onesb[:], rob[0][:]), (onesb[:], rob[1][:])], ["onesb", "rob0", "rob1"], ["psA2"])
            acc_mm(psA[3][:, 0:CH], [(onesb[:], rsq[0][:]), (onesb[:], rsq[1][:])], ["onesb", "rsq0", "rsq1"], ["psA3"])
            s.op("dve", TS(mean[:], psA[2][:, 0:CH], 1.0 / 256, None, ALU.mult), reads=["psA2"], writes=["mean"])
            s.op("dve", TT(var[:], mean[:], mean[:], ALU.mult), reads=["mean"], writes=["var"])
            s.op("dve", STT(var[:], psA[3][:, 0:CH], 1.0 / 256, var[:], ALU.mult, ALU.subtract), reads=["psA3", "var"], writes=["var"])
            s.op("dve", TS(var[:], var[:], EPS, None, ALU.add), reads=["var"], writes=["var"])
            s.op("act", ACT(var[:], var[:], AF.Sqrt), reads=["var"], writes=["var"])
            s.op("dve", lambda e: e.reciprocal(out=var[:], in_=var[:]), reads=["var"], writes=["var"])
            for v in range(2):
                s.op("dve", TT(ro32[v][:], ro32[v][:], mean[:], ALU.subtract), reads=["ro32_%d" % v, "mean"], writes=["ro32_%d" % v])
                s.op("dve", STT(ro32[v][:], ro32[v][:], retg[:, v:v + 1], var[:], ALU.mult, ALU.mult),
                     reads=["ro32_%d" % v, "retg", "var"], writes=["ro32_%d" % v])
                ob = oab_o[oab_ctr[0] % 2]
                otok = "oabo%d" % (oab_ctr[0] % 2)
                oab_ctr[0] += 1
                s.op("dve", TT(ob[:], ro32[v][:], rg[:, v, cs], ALU.mult), reads=["ro32_%d" % v, "rg"], writes=[otok])
                ci = H + 2 * h + v
                s.dma("sp", lambda e, ob=ob, ci=ci, cs=cs: e.dma_start(out=OAB.ap()[ci, :, cs], in_=ob[:]), reads=[otok], writes=["OAB_%d_%d" % (ci, ch)])
    s.barrier()
    A.release(mC)

    if cfg.get("STOP") == "C":
        s.finish(); nc._cfg = cfg; nc._ninst = s.ninst; nc._peak = 0; nc._dbg = dbg_outs
        return nc
    mD = A.mark()
    s.dma("sp", lambda e: e.dma_start(out=rw[:], in_=rw_d.ap().rearrange("(kt p) n -> p kt n", p=128)), writes=["rw"])
    load_vec_row(rbrow[:], rb_d, "rbrow")
    mD1 = A.mark()
    for ch in range(NCHK):
        cs = slice(ch * CH, (ch + 1) * CH)
        conds = sorted(set(tile_cond[ch * TPC:(ch + 1) * TPC]))
        A.release(mD1)
        oab = A.alloc("oab", [128, 3 * H, CH], BF16)
        s.dma("sp", lambda e, cs=cs: e.dma_start(out=oab[:], in_=OAB.ap()[:, :, cs].rearrange("c p t -> p c t")),
              reads=["OAB_%d_%d" % (ci, ch) for ci in range(3 * H)], writes=["oab"])
        merged = A.alloc("merged", [128, KT, CH], BF16)
        mD2 = A.mark()
        sga = A.alloc("sga", [128, JT, CH], F32)
        sgb = A.alloc("sgb", [128, JT, CH], F32)
        m1 = A.alloc("m1", [128, JT, CH], F32)
        for g in range(NG):
            slot, wt = wload(win_d.ap()[:, cfg["MG0"] + g * GW: cfg["MG0"] + (g + 1) * GW], KT, GW)
            for j in range(JT):
                acc_mm(psA[j][:, 0:CH], [(slot[:, kt, j * 128:(j + 1) * 128], xmT[:, kt, cs]) for kt in range(KT)], [wt] + xmT_toks, ["psA%d" % j])
                s.op("act", ACT(sga[:, j, :], psA[j][:, 0:CH], AF.Sigmoid), reads=["psA%d" % j], writes=["sga"])
            slot, wt = wload(win_d.ap()[:, cfg["MG0"] + D + g * GW: cfg["MG0"] + D + (g + 1) * GW], KT, GW)
            for j in range(JT):
                acc_mm(psA[j][:, 0:CH], [(slot[:, kt, j * 128:(j + 1) * 128], xmT[:, kt, cs]) for kt in range(KT)], [wt] + xmT_toks, ["psA%d" % j])
                s.op("act", ACT(sgb[:, j, :], psA[j][:, 0:CH], AF.Sigmoid), reads=["psA%d" % j], writes=["sgb"])
            slot, wt = wload(wpa_d.ap()[:, g * GW:(g + 1) * GW], H, GW)
            for j in range(JT):
                acc_mm(psA[j][:, 0:CH], [(slot[:, c, j * 128:(j + 1) * 128], oab[:, c, :]) for c in range(H)], [wt, "oab"], ["psA%d" % j])
                s.op("dve", TT(m1[:, j, :], sga[:, j, :], psA[j][:, 0:CH], ALU.mult), reads=["sga", "psA%d" % j], writes=["m1"])
            slot, wt = wload(wpb_d.ap()[:, g * GW:(g + 1) * GW], 2 * H, GW)
            for j in range(JT):
                acc_mm(psA[j][:, 0:CH], [(slot[:, c, j * 128:(j + 1) * 128], oab[:, H + c, :]) for c in range(2 * H)], [wt, "oab"], ["psA%d" % j])
                s.op("dve", TT(sgb[:, j, :], sgb[:, j, :], psA[j][:, 0:CH], ALU.mult), reads=["sgb", "psA%d" % j], writes=["sgb"])
                s.op("dve", TT(merged[:, g * JT + j, :], m1[:, j, :], sgb[:, j, :], ALU.add), reads=["m1", "sgb"], writes=["merged"])
        if debug:
            for kt in range(KT):
                dump("merged%d_%d" % (ch, kt), merged[:, kt, :], ["merged"])
        s.barrier()
        A.release(mD2)
        g1row = {}
        for c in conds:
            g1row[c] = A.alloc("g1row%d" % c, [128, D], F32)
            load_row(g1row[c][:], c, 2, "g1row%d" % c)
        xp = [A.alloc("xp%d" % i, [128, GW], F32) for i in range(2)]
        tp = A.alloc("tp", [128, GW], F32)
        xpc = 0
        for g in range(NG):
            slot, wt = wload(wout_d.ap()[:, g * GW:(g + 1) * GW], KT, GW)
            for tl in range(TPC):
                i = ch * TPC + tl
                c = tile_cond[i]
                ps = psA[tl % 4]
                acc_mm(ps[:, 0:GW], [(merged[:, kt, tl * 128:(tl + 1) * 128], slot[:, kt, 0:GW]) for kt in range(KT)], ["merged", wt], ["psA%d" % (tl % 4)])
                xq = xp[xpc % 2]
                xtok = "xp%d" % (xpc % 2)
                xpc += 1
                s.dma("sp", lambda e, xq=xq, i=i, g=g: e.dma_start(out=xq[:], in_=x_d.ap()[i * 128:(i + 1) * 128, g * GW:(g + 1) * GW]), writes=[xtok])
                s.op("dve", TT(tp[:], ps[:, 0:GW], g1row[c][:, g * GW:(g + 1) * GW], ALU.mult), reads=["psA%d" % (tl % 4), "g1row%d" % c], writes=["tp"])
                s.op("dve", TT(xq[:], tp[:], xq[:], ALU.add), reads=["tp", xtok], writes=[xtok])
                s.dma("sp", lambda e, xq=xq, i=i, g=g: e.dma_start(out=X1.ap()[i * 128:(i + 1) * 128, g * GW:(g + 1) * GW], in_=xq[:]),
                      reads=[xtok], writes=["X1_%d_%d" % (i, g)])
        s.barrier()
        A.release(mD1)
        rows2 = {}
        n2row = A.alloc("n2row", [128, D], F32)
        load_vec_row(n2row[:], n2g_d, "n2row")
        for c in conds:
            a2 = A.alloc("a2row%d" % c, [128, D], F32)
            sh2 = A.alloc("sh2row%d" % c, [128, D], F32)
            load_row(a2[:], c, 4, "a2row%d" % c)
            load_row(sh2[:], c, 3, "sh2row%d" % c)
            s.op("dve", STT(a2[:], a2[:], 1.0, n2row[:], ALU.add, ALU.mult), reads=["a2row%d" % c, "n2row"], writes=["a2row%d" % c])
            rows2[c] = (a2, sh2)
        x1t = [A.alloc("x1t%d" % i, [128, D], F32) for i in range(2)]
        xm2 = A.alloc("xm2", [128, D], F32)
        xm2b = [A.alloc("xm2b%d" % i, [128, D], BF16) for i in range(2)]
        xm2T = A.alloc("xm2T", [128, KT, 128], F32)
        junk = A.alloc("junk2", [128, D], BF16)
        for tl in range(TPC):
            i = ch * TPC + tl
            c = tile_cond[i]
            xt = x1t[i % 2]
            xtok = "x1t%d" % (i % 2)
            s.dma("sp", lambda e, xt=xt, i=i: e.dma_start(out=xt[:], in_=X1.ap()[i * 128:(i + 1) * 128, :]),
                  reads=["X1_%d_%d" % (i, g) for g in range(NG)], writes=[xtok])
            sc, tk = rms_rstd(xt[:], xtok, 2 + i % 2, D)
            a2, sh2 = rows2[c]
            s.op("dve", STT(xm2[:], xt[:], sc, a2[:], ALU.mult, ALU.mult), reads=[xtok, tk, "a2row%d" % c], writes=["xm2"])
            s.op("dve", TT(xm2[:], xm2[:], sh2[:], ALU.add), reads=["xm2", "sh2row%d" % c], writes=["xm2"])
            xb_ = xm2b[i % 2]
            s.op("act", lambda e, xb_=xb_: e.copy(out=xb_[:], in_=xm2[:]), reads=["xm2"], writes=["xm2b%d" % (i % 2)])
            s.dma("sp", lambda e, xb_=xb_, i=i: e.dma_start(out=XM2.ap()[i * 128:(i + 1) * 128, :], in_=xb_[:]),
                  reads=["xm2b%d" % (i % 2)], writes=["XM2_%d" % i])
            transposes([xm2[:, kt * 128:(kt + 1) * 128] for kt in range(KT)], lambda g, m: xm2T[:, g:g + m, :], ["xm2"],
                       lambda g, m: ["xm2T"], evac_eng="dve", f32=True)
            acc_mm(psS[0][:, 0:E], [(xm2T[:, kt, :], rw[:, kt, :]) for kt in range(KT)], ["xm2T", "rw"], ["psS0"])
            s.op("dve", TT(lgt[:], psS[0][:, 0:E], rbrow[:], ALU.add), reads=["psS0", "rbrow"], writes=["lgt"])
            if debug:
                dump("logits%d" % i, lgt[:], ["lgt"])
            s.op("dve", lambda e: e.max(out=top8[:], in_=lgt[:]), reads=["lgt"], writes=["top8"])
            s.op("dve", TS(Mf[:, i, :], lgt[:], top8[:, 3:4], None, ALU.is_ge), reads=["lgt", "top8"], writes=["Mf"])
            s.op("dve", TS(top8[:, 7:8], top8[:, 0:1], -1.0, None, ALU.mult), reads=["top8"], writes=["top8"])
            s.op("act", ACT(pex[:], lgt[:], AF.Exp, bias=top8[:, 7:8]), reads=["lgt", "top8"], writes=["pex"])
            s.op("dve", TT(pex[:], pex[:], Mf[:, i, :], ALU.mult), reads=["pex", "Mf"], writes=["pex"])
            s.op("dve", lambda e: e.reduce_sum(out=top8[:, 6:7], in_=pex[:], axis=AX.X), reads=["pex"], writes=["top8"])
            s.op("dve", lambda e: e.reciprocal(out=top8[:, 6:7], in_=top8[:, 6:7]), reads=["top8"], writes=["top8"])
            s.op("dve", TS(Wd[:, i, :], pex[:], top8[:, 6:7], None, ALU.mult), reads=["pex", "top8"], writes=["Wd"])
            s.op("dve", CP(Mb[:, i, :], Mf[:, i, :]), reads=["Mf"], writes=["Mb"])
        s.barrier()
    A.release(mP)

    if cfg.get("STOP") == "D":
        s.finish(); nc._cfg = cfg; nc._ninst = s.ninst; nc._peak = 0; nc._dbg = dbg_outs
        return nc
    destk = A.alloc("destk", [128, NT, TOPK], F32)
    destki = A.alloc("destki", [128, NT, TOPK], I32)
    wk = A.alloc("wk", [128, NT, TOPK], F32)
    bei = A.alloc("bei", [128, NB], I32)
    idxw = A.alloc("idxw", [128, NB, KT], I32)
    NCG_ = max(1, (2 * DFF) // cfg.get("CWCAP", 2048))
    idxg = A.alloc("idxg", [128, NCG_, NB, KT], I32)
    beig = A.alloc("beig", [128, NCG_, NB], I32)
    mE2 = A.mark()
    rank = A.alloc("rank", [128, NT, E], F32)
    dest = A.alloc("dest", [128, NT, E], F32)
    csel = A.alloc("csel", [128, NT, E], F32)
    oh = A.alloc("oh", [128, NT, E], F32)
    tmpE = A.alloc("tmpE", [128, NT, E], F32)
    cnt = A.alloc("cnt", [128, E], F32)
    cmpc = A.alloc("cmpc", [128, E, NT], F32)
    padf = A.alloc("padf", [128, E], F32)
    pend = A.alloc("pend", [128, E], F32)
    pstart = A.alloc("pstart", [128, E], F32)
    tokidi = A.alloc("tokidi", [128, NT], I32)
    be = A.alloc("be", [128, NB], F32)
    cmpb = A.alloc("cmpb", [128, NB, E], F32)
    idxwf = A.alloc("idxwf", [128, NB, KT], F32)
    rtinit = A.alloc("rtinit", [128, 128], I32)
    tokrep = A.alloc("tokrep", [128, NT, 128], I32)

    for i in range(NT):
        pairs = [(onesb[:], Mb[:, j, :]) for j in range(i)] + [(slfb[:], Mb[:, i, :])]
        acc_mm(psS[i % 2][:, 0:E], pairs, ["onesb", "slfb", "Mb"], ["psS%d" % (i % 2)])
        s.op("dve", CP(rank[:, i, :], psS[i % 2][:, 0:E]), reads=["psS%d" % (i % 2)], writes=["rank"])
    acc_mm(psS[0][:, 0:E], [(onesb[:], Mb[:, j, :]) for j in range(NT)], ["onesb", "Mb"], ["psS0"])
    s.op("dve", CP(cnt[:], psS[0][:, 0:E]), reads=["psS0"], writes=["cnt"])
    bbo, _ = coff["bbase"]
    s.op("dve", TT(cmpc[:], cnt[:].unsqueeze(2).to_broadcast([128, E, NT]), cst[:, bbo:bbo + NT].unsqueeze(1).to_broadcast([128, E, NT]), ALU.is_gt),
         reads=["cnt", "cst"], writes=["cmpc"])
    s.op("dve", lambda e: e.tensor_reduce(out=padf[:], in_=cmpc[:], axis=AX.X, op=ALU.add), reads=["cmpc"], writes=["padf"])
    s.op("dve", TS(padf[:], padf[:], 128.0, None, ALU.mult), reads=["padf"], writes=["padf"])
    oro, _ = coff["onesrow"]
    s.op("dve", lambda e: e.tensor_tensor_scan(out=pend[:], data0=cst[:, oro:oro + E], data1=padf[:], initial=0.0, op0=ALU.mult, op1=ALU.add),
         reads=["padf", "cst"], writes=["pend"])
    s.op("dve", TT(pstart[:], pend[:], padf[:], ALU.subtract), reads=["pend", "padf"], writes=["pstart"])
    for i in range(NT):
        s.op("dve", TT(dest[:, i, :], rank[:, i, :], pstart[:], ALU.add), reads=["rank", "pstart"], writes=["dest"])
        s.op("dve", lambda e, i=i: e.tensor_tensor_scan(out=csel[:, i, :], data0=cst[:, oro:oro + E], data1=Mf[:, i, :], initial=0.0,
                                                          op0=ALU.mult, op1=ALU.add), reads=["Mf", "cst"], writes=["csel"])
    for k in range(TOPK):
        s.op("dve", STT(oh[:], csel[:], float(k + 1), Mf[:], ALU.is_equal, ALU.mult), reads=["csel", "Mf"], writes=["oh"])
        s.op("dve", TT(tmpE[:], oh[:], dest[:], ALU.mult), reads=["oh", "dest"], writes=["tmpE"])
        s.op("dve", lambda e, k=k: e.tensor_reduce(out=destk[:, :, k], in_=tmpE[:], axis=AX.X, op=ALU.add), reads=["tmpE"], writes=["destk"])
        s.op("dve", TT(tmpE[:], oh[:], Wd[:], ALU.mult), reads=["oh", "Wd", "destk"], writes=["tmpE"])
        s.op("dve", lambda e, k=k: e.tensor_reduce(out=wk[:, :, k], in_=tmpE[:], axis=AX.X, op=ALU.add), reads=["tmpE"], writes=["wk"])
    s.op("dve", CP(destki[:], destk[:]), reads=["destk"], writes=["destki"])
    s.op("dve", CP(tokidi[:], C("tokid")), reads=["cst"], writes=["tokidi"])
    bbo, _ = coff["bbase"]
    bb = cst[:, bbo:bbo + NB]
    s.op("dve", TT(cmpb[:], pend[:].unsqueeze(1).to_broadcast([128, NB, E]), bb.unsqueeze(2).to_broadcast([128, NB, E]), ALU.is_le),
         reads=["pend", "cst"], writes=["cmpb"])
    s.op("dve", lambda e: e.tensor_reduce(out=be[:], in_=cmpb[:], axis=AX.X, op=ALU.add), reads=["cmpb"], writes=["be"])
    s.op("dve", TS(be[:], be[:], float(E - 1), None, ALU.min), reads=["be"], writes=["be"])
    s.op("dve", CP(bei[:], be[:]), reads=["be"], writes=["bei"])
    for cg in range(NCG_):
        s.op("dve", TS(cmpb[:, :, 0], be[:], float(NCG_), float(cg), ALU.mult, ALU.add), reads=["be", "bei"], writes=["cmpb"])
        s.op("dve", CP(beig[:, cg, :], cmpb[:, :, 0]), reads=["cmpb"], writes=["bei"])
    iwo, _ = coff["iotaw"]
    s.op("dve", TS(be[:], be[:], float(D), None, ALU.mult), reads=["be", "bei"], writes=["be"])
    s.op("dve", TT(idxwf[:], be[:].unsqueeze(2).to_broadcast([128, NB, KT]), cst[:, iwo:iwo + KT].unsqueeze(1).to_broadcast([128, NB, KT]), ALU.add),
         reads=["be", "cst"], writes=["idxwf"])
    s.op("dve", CP(idxw[:], idxwf[:]), reads=["idxwf"], writes=["idxw"])
    for cg in range(NCG_):
        s.op("dve", TS(cmpb[:, :, 0:KT], idxwf[:], float(NCG_), float(cg), ALU.mult, ALU.add), reads=["idxwf", "idxw"], writes=["cmpb"])
        s.op("dve", CP(idxg[:, cg], cmpb[:, :, 0:KT]), reads=["cmpb"], writes=["idxw"])
    s.op("dve", lambda e: e.memset(rtinit[:], T), writes=["rtinit"])
    for b in range(NB):
        s.dma("sp", lambda e, b=b: e.dma_start(out=ROWTOK.ap()[b * 128:(b + 1) * 128, :], in_=rtinit[:]), reads=["rtinit"], writes=["ROWTOK%d" % b])
    for i in range(NT):
        s.op("dve", CP(tokrep[:, i, :], tokidi[:, i:i + 1].to_broadcast([128, 128])), reads=["tokidi"], writes=["tokrep"])
    rt_toks = []
    for i in range(NT):
        for k in range(TOPK):
            tk = "RT_%d_%d" % (i, k)
            rt_toks.append(tk)
            s.dma("pool", lambda e, i=i, k=k: e.indirect_dma_start(
                out=ROWTOK.ap(), out_offset=bass.IndirectOffsetOnAxis(ap=destki[:, i, k:k + 1], axis=0),
                in_=tokrep[:, i, :], in_offset=None, bounds_check=BC(e, R - 1), oob_is_err=False),
                reads=["ROWTOK%d" % b for b in range(NB)] + ["destki", "tokrep"], writes=[tk])
    if debug:
        dump("destk", destk[:].rearrange("p t k -> p (t k)"), ["destk"])
        dump("wk", wk[:].rearrange("p t k -> p (t k)"), ["wk"])
        dump("be", bei[:], ["bei"])
    s.barrier()

    if cfg.get("STOP") == "E":
        s.finish(); nc._cfg = cfg; nc._ninst = s.ninst; nc._peak = 0; nc._dbg = dbg_outs
        return nc
    A.release(mE2)
    mF = A.mark()
    CWG = min(cfg.get("CWCAP", 2048), 2 * DFF)
    PWG = min(512, CWG)
    NCG = (2 * DFF) // CWG
    KH = max(1, KT // 2)
    NKH = KT // KH
    FT = DFF // 128
    mslots = [A.alloc("mslot%d" % i, [128, KH, 2048], BF16) for i in range(2)]
    mctr = [0]
    rtb = [A.alloc("rtb%d" % i, [128, 16], I32) for i in range(2)]
    xg = [A.alloc("xg%d" % i, [128, D], BF16) for i in range(2)]
    xgT = A.alloc("xgT", [128, KT, 128], BF16)
    bgu = [A.alloc("bgu%d" % i, [128, 2 * DFF], BF16) for i in range(2)]
    bdn = [A.alloc("bdn%d" % i, [128, D], BF16) for i in range(2)]
    hb = [A.alloc("hb%d" % i, [128, 512], F32) for i in range(2)]
    gt = A.alloc("gt", [128, 256], F32)
    ut = A.alloc("ut", [128, 256], F32)
    sgt = A.alloc("sgt", [128, 256], F32)
    actb = A.alloc("actb", [128, DFF], BF16)
    actT = A.alloc("actT", [128, FT, 128], BF16)
    yb = [A.alloc("yb%d" % i, [128, D], F32) for i in range(2)]
    for i in range(2):
        s.op("dve", lambda e, i=i: e.memset(xg[i][:], 0.0), writes=["xg%d" % i])
    wgu2 = wgu_d.ap().rearrange("r (g c) -> (r g) c", c=CWG)
    bgu2 = bgu_d.ap().rearrange("e (g c) -> (e g) c", c=CWG)
    wdn2 = wdn_d.ap()

    def mload(src2d, idx_fn, nrows, kh, ncols):
        i = mctr[0] % 2
        mctr[0] += 1
        slot = mslots[i]
        tok = "mslot%d" % i
        for kl in range(KH):
            kt = kh * KH + kl
            s.dma("pool", lambda e, slot=slot, kl=kl, kt=kt: e.indirect_dma_start(
                out=slot[:, kl, 0:ncols], out_offset=None, in_=src2d,
                in_offset=bass.IndirectOffsetOnAxis(ap=idx_fn(kt), axis=0),
                bounds_check=BC(e, nrows - 1), oob_is_err=False), reads=["idxw"], writes=[tok + "_%d" % kl])
        return slot, [tok + "_%d" % kl for kl in range(KH)]

    for b in range(NB):
        p2 = b % 2
        s.dma("sp", lambda e, b=b, p2=p2: e.dma_start(out=rtb[p2][:], in_=ROWTOK.ap()[b * 128:(b + 1) * 128, 0:16]),
              reads=rt_toks + ["ROWTOK%d" % b], writes=["rtb%d" % p2])
        s.dma("pool", lambda e, p2=p2: e.indirect_dma_start(
            out=xg[p2][:], out_offset=None, in_=XM2.ap(), in_offset=bass.IndirectOffsetOnAxis(ap=rtb[p2][:, 0:1], axis=0),
            bounds_check=BC(e, T - 1), oob_is_err=False), reads=["rtb%d" % p2] + ["XM2_%d" % i for i in range(NT)], writes=["xg%d" % p2])
        for cg in range(NCG):
            s.dma("pool", lambda e, p2=p2, b=b, cg=cg: e.indirect_dma_start(
                out=bgu[p2][:, cg * CWG:(cg + 1) * CWG], out_offset=None, in_=bgu2,
                in_offset=bass.IndirectOffsetOnAxis(ap=beig[:, cg, b:b + 1], axis=0), bounds_check=BC(e, E * NCG - 1), oob_is_err=False),
                reads=["bei"], writes=["bgu%d_%d" % (p2, cg)])
        s.dma("pool", lambda e, p2=p2, b=b: e.indirect_dma_start(
            out=bdn[p2][:], out_offset=None, in_=bdn_d.ap(), in_offset=bass.IndirectOffsetOnAxis(ap=bei[:, b:b + 1], axis=0),
            bounds_check=BC(e, E - 1), oob_is_err=False), reads=["bei"], writes=["bdn%d" % p2])
        bgu_toks = ["bgu%d_%d" % (p2, cg) for cg in range(NCG)]
        transposes([xg[p2][:, kt * 128:(kt + 1) * 128] for kt in range(KT)], lambda g, m: xgT[:, g:g + m, :], ["xg%d" % p2],
                   lambda g, m: ["xgT"])
        for cg in range(NCG):
            npc = CWG // PWG
            for kh in range(NKH):
                slot, stoks = mload(wgu2, (lambda kt, b=b, cg=cg: idxg[:, cg, b, kt:kt + 1]), E * D * NCG, kh, CWG)
                for pc in range(npc):
                    items = [(psA[pc][:, 0:PWG], xgT[:, kh * KH + kl, :], slot[:, kl, pc * PWG:(pc + 1) * PWG],
                              (kh == 0 and kl == 0), (kh == NKH - 1 and kl == KH - 1)) for kl in range(KH)]
                    mms(items, ["xgT"] + stoks, ["psA%d" % pc])
            for pc in range(npc):
                col0 = cg * CWG + pc * PWG
                hbb = hb[pc % 2]
                htok = "hb%d" % (pc % 2)
                HW_ = PWG // 2
                s.op("dve", TT(hbb[:, 0:PWG], psA[pc][:, 0:PWG], bgu[p2][:, col0:col0 + PWG], ALU.add), reads=["psA%d" % pc] + bgu_toks, writes=[htok])
                hv = hbb[:, 0:PWG].rearrange("p (f two) -> p f two", two=2)
                s.op("dve", TS(gt[:, 0:HW_], hv[:, :, 0], 7.0, None, ALU.min), reads=[htok], writes=["gt"])
                s.op("dve", TS(ut[:, 0:HW_], hv[:, :, 1], -7.0, 7.0, ALU.max, ALU.min), reads=[htok], writes=["ut"])
                s.op("act", ACT(sgt[:, 0:HW_], gt[:, 0:HW_], AF.Sigmoid, scale=1.702), reads=["gt"], writes=["sgt"])
                s.op("dve", TT(gt[:, 0:HW_], gt[:, 0:HW_], sgt[:, 0:HW_], ALU.mult), reads=["gt", "sgt"], writes=["gt"])
                f0 = col0 // 2
                s.op("dve", STT(actb[:, f0:f0 + HW_], ut[:, 0:HW_], 1.0, gt[:, 0:HW_], ALU.add, ALU.mult), reads=["ut", "gt"], writes=["actb"])
        transposes([actb[:, ft * 128:(ft + 1) * 128] for ft in range(FT)], lambda g, m: actT[:, g:g + m, :], ["actb"], lambda g, m: ["actT"])
        npc = (D + 511) // 512
        pw = min(512, D)
        for kh in range(NKH):
            slot, stoks = mload(wdn2, (lambda kt, b=b: idxw[:, b, kt:kt + 1]), E * DFF, kh, D)
            for pc in range(npc):
                items = [(psA[pc][:, 0:pw], actT[:, kh * KH + kl, :], slot[:, kl, pc * pw:(pc + 1) * pw],
                          (kh == 0 and kl == 0), (kh == NKH - 1 and kl == KH - 1)) for kl in range(KH)]
                mms(items, ["actT"] + stoks, ["psA%d" % pc])
        for pc in range(npc):
            s.op("dve", TT(yb[p2][:, pc * pw:(pc + 1) * pw], psA[pc][:, 0:pw], bdn[p2][:, pc * pw:(pc + 1) * pw], ALU.add),
                 reads=["psA%d" % pc, "bdn%d" % p2], writes=["yb%d" % p2])
        s.dma("sp", lambda e, p2=p2, b=b: e.dma_start(out=Y.ap()[b * 128:(b + 1) * 128, :], in_=yb[p2][:]), reads=["yb%d" % p2], writes=["Y_%d" % b])
    s.barrier()
    A.release(mF)

    if cfg.get("STOP") == "F":
        s.finish(); nc._cfg = cfg; nc._ninst = s.ninst; nc._peak = 0; nc._dbg = dbg_outs
        return nc
    y_toks = ["Y_%d" % b for b in range(NB)]
    yk = [A.alloc("yk%d" % k, [128, D], F32) for k in range(TOPK)]
    acc = A.alloc("acc", [128, D], F32)
    x1g = A.alloc("x1g", [128, D], F32)
    g2row = {}
    for c in range(2):
        g2row[c] = A.alloc("g2row%d" % c, [128, D], F32)
        load_row(g2row[c][:], c, 5, "g2row%d" % c)
    nfrow = A.alloc("nfrow", [128, D], F32)
    load_vec_row(nfrow[:], nfg_d, "nfrow")
    junk = A.alloc("junk3", [128, D], BF16)
    yout = [A.alloc("yout%d" % i, [128, D], F32) for i in range(2)]
    for k in range(TOPK):
        s.op("dve", lambda e, k=k: e.memset(yk[k][:], 0.0), writes=["yk%d" % k])
    for i in range(NT):
        c = tile_cond[i]
        for k in range(TOPK):
            s.dma("pool", lambda e, i=i, k=k: e.indirect_dma_start(
                out=yk[k][:], out_offset=None, in_=Y.ap(), in_offset=bass.IndirectOffsetOnAxis(ap=destki[:, i, k:k + 1], axis=0),
                bounds_check=BC(e, R - 1), oob_is_err=False), reads=y_toks + ["destki"], writes=["yk%d" % k])
        s.dma("sp", lambda e, i=i: e.dma_start(out=x1g[:], in_=X1.ap()[i * 128:(i + 1) * 128, :]), writes=["x1g"])
        s.op("dve", TS(acc[:], yk[0][:], wk[:, i, 0:1], None, ALU.mult), reads=["yk0", "wk"], writes=["acc"])
        for k in range(1, TOPK):
            s.op("dve", STT(acc[:], yk[k][:], wk[:, i, k:k + 1], acc[:], ALU.mult, ALU.add), reads=["yk%d" % k, "wk", "acc"], writes=["acc"])
        s.op("dve", TT(acc[:], acc[:], g2row[c][:], ALU.mult), reads=["acc", "g2row%d" % c], writes=["acc"])
        s.op("dve", TT(acc[:], acc[:], x1g[:], ALU.add), reads=["acc", "x1g"], writes=["acc"])
        sc, tk = rms_rstd(acc[:], "acc", 4 + i % 2, D)
        yo = yout[i % 2]
        s.op("dve", STT(yo[:], acc[:], sc, nfrow[:], ALU.mult, ALU.mult), reads=["acc", tk, "nfrow"], writes=["yout%d" % (i % 2)])
        s.dma("sp", lambda e, yo=yo, i=i: e.dma_start(out=y_o.ap()[i * 128:(i + 1) * 128, :], in_=yo[:]), reads=["yout%d" % (i % 2)], writes=["y_%d" % i])
    s.finish()
    nc._cfg = cfg
    nc._ninst = s.ninst
    nc._peak = A.peak - A.base
    nc._dbg = dbg_outs
    return nc


def prepare_core_inputs(inp, cfg, core):
    cfg = derive(cfg)
    D, H, KT, NP = cfg["D"], cfg["H"], cfg["KT"], cfg["NP"]
    f = np.float32
    xp = np.asarray(inp["x_prompt"], f)
    xs = np.asarray(inp["x_sample"], f)
    x = np.concatenate([xp[core * NP + p] for p in range(NP)] + [xs[core]], axis=0)
    c_ctx = np.asarray(inp["c_ctx"], f)
    c = np.asarray(inp["c"], f)[core]
    cT = np.stack([c_ctx.reshape(KT, 128).T, c.reshape(KT, 128).T], axis=-1)

    def fm(v, n):
        return np.ascontiguousarray(np.asarray(v, f).reshape(n, 128).T)

    lbf = np.stack([fm(inp["hg_lb_fwd"][0], H), fm(inp["hg_lb_fwd"][1], H)], axis=1)
    lbb = np.stack([fm(inp["hg_lb_bwd"][0], H), fm(inp["hg_lb_bwd"][1], H)], axis=1)
    m = {
        "x": np.ascontiguousarray(x),
        "shf": np.ascontiguousarray(np.asarray(inp["state_hgrn_fwd"], f)[core, 0]),
        "shb": np.ascontiguousarray(np.asarray(inp["state_hgrn_bwd"], f)[core, 0]),
        "srf": np.ascontiguousarray(np.asarray(inp["state_ret_fwd"], f)[core, 0]),
        "srb": np.ascontiguousarray(np.asarray(inp["state_ret_bwd"], f)[core, 0]),
        "cT": np.ascontiguousarray(cT),
        "lbf": np.ascontiguousarray(lbf), "lbb": np.ascontiguousarray(lbb),
    }
    return m


def prepare_shared_inputs(inp, cfg):
    cfg = derive(cfg)
    D, H, KT, E = cfg["D"], cfg["H"], cfg["KT"], cfg["E"]
    f = np.float32
    perm = w_in_perm_index(cfg)
    sh = {
        "ada_w": np.ascontiguousarray(np.asarray(inp["ada_w"], f)[0]),
        "ada_b2": np.ascontiguousarray(np.broadcast_to(np.asarray(inp["ada_b"], f)[0][None, :], (2, 6 * D))),
        "n1g": np.asarray(inp["norm1_g"], f)[0][None, :].copy(),
        "n2g": np.asarray(inp["norm2_g"], f)[0][None, :].copy(),
        "nfg": np.asarray(inp["final_norm_g"], f)[None, :].copy(),
        "w_in": np.ascontiguousarray(np.asarray(inp["w_in"], f)[0][:, perm]),
        "hgng": np.asarray(inp["hg_norm_g"], f)[0].reshape(128, 1).copy(),
        "retg": np.ascontiguousarray(np.asarray(inp["ret_norm_g"], f)[0].reshape(2, 128).T),
        "rl2f": np.asarray(inp["ret_log2_fwd"], f)[0][None, :].copy(),
        "rl2b": np.asarray(inp["ret_log2_bwd"], f)[0][None, :].copy(),
        "w_pa": np.ascontiguousarray(np.asarray(inp["w_proj_hgrn"], f)[0]),
        "w_pb": np.ascontiguousarray(np.asarray(inp["w_proj_ret"], f)[0]),
        "w_out": np.ascontiguousarray(np.asarray(inp["w_out"], f)[0]),
        "rw": np.ascontiguousarray(np.asarray(inp["router_w"], f)[0]),
        "rb": np.asarray(inp["router_b"], f)[0][None, :].copy(),
        "w_gu": np.asarray(inp["moe_w_gu"], f)[0].reshape(E * D, 2 * D),
        "b_gu": np.ascontiguousarray(np.asarray(inp["moe_b_gu"], f)[0]),
        "w_dn": np.asarray(inp["moe_w_dn"], f)[0].reshape(E * D, D),
        "b_dn": np.ascontiguousarray(np.asarray(inp["moe_b_dn"], f)[0]),
        "cst": make_consts(cfg)[0],
        "rope": make_consts(cfg)[1],
    }
    return sh


def run(inp, cfg, runner=None, debug=False):
    cfgd = derive(cfg)
    ncores = cfgd["NCORES"]
    nc = build(cfg, debug=debug)
    shared = prepare_shared_inputs(inp, cfg)
    in_maps = []
    for core in range(ncores):
        m = dict(shared)
        m.update(prepare_core_inputs(inp, cfg, core))
        in_maps.append(m)
    if runner is None:
        res = run_bass_kernel_spmd(nc, in_maps, core_ids=list(range(ncores))).results
    else:
        res = runner(nc, in_maps)
    NP, TP, TS_, D, H = cfgd["NP"], cfgd["TP"], cfgd["TS"], cfgd["D"], cfgd["H"]
    yp = np.stack([res[c]["y"][p * TP:(p + 1) * TP] for c in range(ncores) for p in range(NP)], axis=0)
    ys = np.stack([res[c]["y"][NP * TP:] for c in range(ncores)], axis=0)
    hf = np.concatenate([res[c]["hf"] for c in range(ncores)], axis=0)[:, None]
    hb = np.concatenate([res[c]["hb"] for c in range(ncores)], axis=0)[:, None]
    rf = np.concatenate([res[c]["rf"] for c in range(ncores)], axis=0)[:, None]
    rb = np.concatenate([res[c]["rb_o"] for c in range(ncores)], axis=0)[:, None]
    outs = tuple(np.ascontiguousarray(a.astype(np.float32)) for a in (yp, ys, hf, hb, rf, rb))
    return outs, res, nc


def kernel(**inputs):
    outs, _, _ = run(inputs, FULL_CFG)
    return outs
```

```python
import math
from contextlib import ExitStack
import numpy as np
import ml_dtypes
import concourse.bass as bass
import concourse.mybir as mybir
from concourse.bass_utils import run_bass_kernel_spmd

F32 = mybir.dt.float32
BF16 = mybir.dt.bfloat16
I32 = mybir.dt.int32
AF = mybir.ActivationFunctionType
ALU = mybir.AluOpType
AX = mybir.AxisListType
EPS = 1e-6
CHUNK = 32
GRID_W = 64
ROPE_PAIRS = 32
TOPK = 4

FULL_CFG = dict(D=2048, H=8, TP=256, NP=2, TS=1024, E=32, NCORES=8)


class Sched:
    CE = ("pe", "act", "dve", "pool")

    def __init__(self, nc, nring=8):
        self.nc = nc
        self.q = {k: [] for k in ("pe", "act", "dve", "pool", "sp")}
        self.psem = {k: nc.alloc_semaphore(name="ps_" + k) for k in self.CE}
        self.pcnt = {k: 0 for k in self.CE}
        self.waited = {}
        self.ring = {}
        for qn in ("sp", "pool"):
            self.ring[qn] = dict(sems=[nc.alloc_semaphore(name="d_%s_%d" % (qn, i)) for i in range(nring)],
                                 vals=[0] * nring, nxt=0)
        self.lw = {}
        self.rd = {}
        self.ninst = 0

    def _wait(self, engn, ev):
        if ev is None:
            return
        sem, val = ev
        key = (engn, id(sem))
        if self.waited.get(key, 0) >= val:
            return
        self.waited[key] = val
        self.q[engn].append(lambda e, sem=sem, val=val: e.wait_ge(sem, val))
        self.ninst += 1

    def _deps(self, engn, reads, writes):
        for t in reads:
            self._wait(engn, self.lw.get(t))
        for t in writes:
            self._wait(engn, self.lw.get(t))
            for ev in self.rd.get(t, {}).values():
                self._wait(engn, ev)

    def _commit(self, ev, reads, writes):
        sem, val = ev
        for t in writes:
            self.lw[t] = ev
            self.rd[t] = {}
        for t in reads:
            d = self.rd.setdefault(t, {})
            old = d.get(id(sem))
            if old is None or old[1] < val:
                d[id(sem)] = ev

    @staticmethod
    def _excl(reads, writes):
        pr = [t for t in reads if t.startswith("ps")]
        if not pr:
            return list(reads), list(writes)
        return [t for t in reads if not t.startswith("ps")], list(writes) + [t for t in pr if t not in writes]

    def op(self, engn, fn, reads=(), writes=(), inc=True):
        reads, writes = self._excl(reads, writes)
        self._deps(engn, reads, writes)
        self.ninst += 1
        if inc:
            self.pcnt[engn] += 1
            sem = self.psem[engn]
            val = self.pcnt[engn]
            self.q[engn].append(lambda e, fn=fn, sem=sem: fn(e).then_inc(sem, 1))
            ev = (sem, val)
            self._commit(ev, reads, writes)
            return ev
        self.q[engn].append(lambda e, fn=fn: fn(e))
        return None

    def dma(self, qn, fn, reads=(), writes=()):
        r = self.ring[qn]
        i = r["nxt"]
        r["nxt"] = (i + 1) % len(r["sems"])
        sem = r["sems"][i]
        if r["vals"][i] > 0:
            self._wait(qn, (sem, r["vals"][i]))
        self._deps(qn, reads, writes)
        r["vals"][i] += 16
        val = r["vals"][i]
        self.q[qn].append(lambda e, fn=fn, sem=sem: fn(e).then_inc(sem, 16))
        self.ninst += 1
        ev = (sem, val)
        self._commit(ev, reads, writes)
        return ev

    def barrier(self):
        evs = [(self.psem[k], self.pcnt[k]) for k in self.CE if self.pcnt[k] > 0]
        for r in self.ring.values():
            evs += [(sem, v) for sem, v in zip(r["sems"], r["vals"]) if v > 0]
        for qn in self.q:
            for ev in evs:
                self._wait(qn, ev)

    def finish(self):
        self.barrier()
        with self.nc.Block() as block:
            @block.tensor
            def _(e):
                for t in self.q["pe"]:
                    t(e)

            @block.scalar
            def _(e):
                for t in self.q["act"]:
                    t(e)

            @block.vector
            def _(e):
                for t in self.q["dve"]:
                    t(e)

            @block.gpsimd
            def _(e):
                for t in self.q["pool"]:
                    t(e)

            @block.sync
            def _(e):
                for t in self.q["sp"]:
                    t(e)


class _Stop(Exception):
    pass


class Arena:
    def __init__(self, nc):
        self.nc = nc
        self.base = (nc.sbuf_base + 31) // 32 * 32
        self.top = nc.sbuf_top // 32 * 32
        self.cur = self.base
        self.n = 0
        self.peak = self.cur

    def alloc(self, name, shape, dtype):
        sz = {F32: 4, BF16: 2, I32: 4}[dtype]
        nbytes = int(np.prod(shape[1:])) * sz
        nbytes = (nbytes + 31) // 32 * 32
        assert self.cur + nbytes <= self.top, "SBUF overflow at %s: need %d have %d" % (name, nbytes, self.top - self.cur)
        self.n += 1
        t = self.nc.alloc_sbuf_tensor_at("%s_%d" % (name, self.n), list(shape), dtype, offset=self.cur)
        self.cur += nbytes
        self.peak = max(self.peak, self.cur)
        return t

    def mark(self):
        return self.cur

    def release(self, m):
        self.cur = m


def TS(out, in0, s1, s2, op0, op1=None):
    if op1 is None:
        return lambda e: e.tensor_scalar(out=out, in0=in0, scalar1=s1, scalar2=None, op0=op0)
    return lambda e: e.tensor_scalar(out=out, in0=in0, scalar1=s1, scalar2=s2, op0=op0, op1=op1)


def TT(out, a, b, op):
    return lambda e: e.tensor_tensor(out=out, in0=a, in1=b, op=op)


def STT(out, in0, sc, in1, op0, op1):
    return lambda e: e.scalar_tensor_tensor(out=out, in0=in0, scalar=sc, in1=in1, op0=op0, op1=op1)


def ACT(out, in_, func, bias=None, scale=None, accum=None):
    kw = {}
    if bias is not None:
        kw["bias"] = bias
    if scale is not None:
        kw["scale"] = scale
    if accum is not None:
        kw["accum_out"] = accum
    return lambda e: e.activation(out=out, in_=in_, func=func, **kw)


def CP(out, in_):
    return lambda e: e.tensor_copy(out=out, in_=in_)


def const_layout(cfg):
    T, TS_, E, KT, NB = cfg["T"], cfg["TS"], cfg["E"], cfg["KT"], cfg["NB"]
    NT = T // 128
    names = [("ident", 128), ("mf", 128), ("mb", 128), ("slf", 128), ("slb", 128), ("i2", 128),
             ("dpos", 128), ("dneg", 128), ("iota1", 128), ("iotar", 128), ("ones", 128), ("psw", 128),
             ("c127", 1), ("cp", 1), ("cmask", 4), ("reset", cfg["CH"]), ("iotae", E),
             ("tokid", NT), ("iotaw", KT), ("bbase", NB), ("pbase", cfg["NPAIR"]), ("onesrow", max(E, 8))]
    off = {}
    c = 0
    for n, w in names:
        off[n] = (c, w)
        c += w
    return off, c


def make_consts(cfg):
    off, ncol = const_layout(cfg)
    T, TS_, E, KT, NB = cfg["T"], cfg["TS"], cfg["E"], cfg["KT"], cfg["NB"]
    NT = T // 128
    C = np.zeros((128, ncol), np.float32)

    def put(n, a):
        o, w = off[n]
        C[:, o:o + w] = a

    s = np.arange(128)[:, None]
    t = np.arange(128)[None, :]
    same = (s // CHUNK) == (t // CHUNK)
    put("ident", (s == t))
    put("mf", same & (s <= t))
    put("mb", same & (s >= t))
    put("slf", s < t)
    put("slb", s > t)
    put("i2", 2.0 * (s == t))
    put("dpos", np.maximum(t - s, 0))
    put("dneg", np.maximum(s - t, 0))
    put("iota1", np.broadcast_to(t + 1, (128, 128)))
    put("iotar", np.broadcast_to(128 - t, (128, 128)))
    put("ones", 1.0)
    d = np.arange(128)
    partner = np.where((d % 64) < 32, d + 32, d - 32)
    psw = np.zeros((128, 128), np.float32)
    psw[partner, d] = 1.0
    put("psw", psw)
    put("c127", 127 - s)
    put("cp", s)
    put("cmask", (s // CHUNK) == np.arange(4)[None, :])
    put("reset", np.broadcast_to((np.arange(cfg["CH"]) % CHUNK != 0).astype(np.float32), (128, cfg["CH"])))
    tok = np.arange(TS_)
    rows = (tok // GRID_W).astype(np.float32)
    cols = (tok % GRID_W).astype(np.float32)
    inv = (10000.0 ** (-np.arange(ROPE_PAIRS, dtype=np.float32) / ROPE_PAIRS)).astype(np.float32)
    pos = np.where((d[:, None] // 64) == 0, rows[None, :], cols[None, :]).astype(np.float32)
    ang = (pos * inv[d % 32][:, None]).astype(np.float32)
    sign = np.where((d % 64) < 32, -1.0, 1.0)[:, None]
    rope = np.concatenate([np.cos(ang), np.sin(ang) * sign], axis=1).astype(np.float32)
    put("iotae", np.broadcast_to(np.arange(E), (128, E)))
    put("tokid", np.arange(NT)[None, :] * 128 + s)
    put("iotaw", np.arange(KT)[None, :] * 128 + s)
    put("bbase", np.broadcast_to(np.arange(NB) * 128, (128, NB)))
    put("pbase", np.broadcast_to(np.arange(cfg["NPAIR"]) * 256, (128, cfg["NPAIR"])))
    put("onesrow", 1.0)
    return C, rope


def derive(cfg):
    cfg = dict(cfg)
    D, H = cfg["D"], cfg["H"]
    cfg["KT"] = D // 128
    cfg["T"] = cfg["NP"] * cfg["TP"] + cfg["TS"]
    cfg["NT"] = cfg["T"] // 128
    cfg["CH"] = min(512, cfg["T"])
    assert cfg["T"] % cfg["CH"] == 0
    cfg["NPAIR"] = cfg["T"] * TOPK // 256 + cfg["E"]
    cfg["NB"] = 2 * cfg["NPAIR"]
    cap = cfg.get("CWCAP", 1024)
    cfg["CWG"] = min(cap, 2 * cfg["D"])
    cfg["CWD"] = min(cap, cfg["D"])
    cfg["NCJG"] = 2 * cfg["D"] // cfg["CWG"]
    cfg["NCJD"] = cfg["D"] // cfg["CWD"]
    cfg["HGC"] = 640
    cfg["RTC"] = 768
    cfg["MG0"] = H * 640 + H * 768
    cfg["INC"] = cfg["MG0"] + 2 * D
    return cfg


def w_in_perm_index(cfg):
    D, H = cfg["D"], cfg["H"]
    HK = H * 128
    RW = H * 256
    o_hq, o_zf, o_zb, o_hi, o_hg = 0, HK, 2 * HK, 3 * HK, 4 * HK
    o_rq = 5 * HK
    o_rk = o_rq + HK
    o_rv = o_rk + HK
    o_rg = o_rv + RW
    o_ma = o_rg + RW
    o_mb = o_ma + D
    idx = []
    for h in range(H):
        r = np.arange(128)
        idx += [o_hq + h * 128 + r, o_zf + h * 128 + r, o_zb + h * 128 + r, o_hg + h * 128 + r, o_hi + h * 128 + r]
    for h in range(H):
        r = np.arange(128)
        r2 = np.arange(256)
        idx += [o_rq + h * 128 + r, o_rk + h * 128 + r, o_rg + h * 256 + r2, o_rv + h * 256 + r2]
    idx += [o_ma + np.arange(D), o_mb + np.arange(D)]
    return np.concatenate(idx)


def build(cfg, debug=False):
    cfg = derive(cfg)
    D, H, KT, T, NT, E, NB, CH = cfg["D"], cfg["H"], cfg["KT"], cfg["T"], cfg["NT"], cfg["E"], cfg["NB"], cfg["CH"]
    TP, NP, TS_ = cfg["TP"], cfg["NP"], cfg["TS"]
    DFF = D
    NCHK = T // CH
    TPC = CH // 128
    NCK = T // CHUNK
    R = NB * 128
    GW = min(512, D)
    NG = D // GW
    JT = GW // 128
    coff, ncst = const_layout(cfg)

    nc = bass.Bass("TRN2", target_bir_lowering=False)
    s = Sched(nc)
    A = Arena(nc)
    dbg_outs = {}

    def din(name, shape, dt=F32):
        return nc.dram_tensor(name, list(shape), dt, kind="ExternalInput")

    def dscr(name, shape, dt=F32):
        return nc.dram_tensor(name, list(shape), dt, kind="Internal")

    def dout(name, shape, dt=F32):
        return nc.dram_tensor(name, list(shape), dt, kind="ExternalOutput")

    x_d = din("x", [T, D])
    shf_d, shb_d = din("shf", [H, 128, 128]), din("shb", [H, 128, 128])
    srf_d, srb_d = din("srf", [H, 128, 256]), din("srb", [H, 128, 256])
    cT_d = din("cT", [128, KT, 2])
    adaw_d = din("ada_w", [D, 6 * D])
    adab_d = din("ada_b2", [2, 6 * D])
    n1g_d, n2g_d, nfg_d = din("n1g", [1, D]), din("n2g", [1, D]), din("nfg", [1, D])
    win_d = din("w_in", [D, cfg["INC"]])
    lbf_d, lbb_d = din("lbf", [128, 2, H]), din("lbb", [128, 2, H])
    hgng_d = din("hgng", [128, 1])
    retg_d = din("retg", [128, 2])
    rl2f_d, rl2b_d = din("rl2f", [1, H]), din("rl2b", [1, H])
    wpa_d, wpb_d, wout_d = din("w_pa", [H * 128, D]), din("w_pb", [H * 256, D]), din("w_out", [D, D])
    rw_d, rb_d = din("rw", [D, E]), din("rb", [1, E])
    CWG, CWD, NCJG, NCJD, NPAIR = cfg["CWG"], cfg["CWD"], cfg["NCJG"], cfg["NCJD"], cfg["NPAIR"]
    wgu_d, bgu_d = din("w_gu", [E * NCJG * 128, KT * CWG]), din("b_gu", [E * NCJG, CWG])
    wdn_d, bdn_d = din("w_dn", [E * NCJD * 128, KT * CWD]), din("b_dn", [E * NCJD, CWD])
    cst_d = din("cst", [128, ncst])
    rope_d = din("rope", [128, 2 * TS_])

    y_o = dout("y", [T, D])
    hf_o, hb_o = dout("hf", [NP, H, 128, 128]), dout("hb", [NP, H, 128, 128])
    rf_o, rb_o = dout("rf", [NP, H, 128, 256]), dout("rb_o", [NP, H, 128, 256])

    MOD = dscr("MOD", [2, 6 * D])
    OAB = dscr("OAB", [3 * H, 128, T], BF16)
    X1 = dscr("X1", [T, D])
    XM2 = dscr("XM2", [T, D], BF16)
    ROWTOK = dscr("ROWTOK", [R, 128], I32)
    Y = dscr("Y", [R, D])

    def dump(name, ap, reads):
        if not debug:
            return
        shp = list(ap.shape)
        o = dout("dbg_" + name, shp, ap.dtype)
        dbg_outs[name] = shp
        s.dma("sp", lambda e: e.dma_start(out=o.ap(), in_=ap), reads=reads, writes=["dbg_" + name])

    psA = [nc.alloc_psum_tensor("psA%d" % i, [128, 512], F32) for i in range(4)]
    psS = [nc.alloc_psum_tensor("psS%d" % i, [128, 512], F32) for i in range(2)]
    psTbs = [nc.alloc_psum_tensor("psTb%d" % i, [128, 8, 128], BF16) for i in range(2)]

    cst = A.alloc("cst", [128, ncst], F32)

    def C(name):
        o, w = coff[name]
        return cst[:, o:o + w]

    identb = A.alloc("identb", [128, 128], BF16)
    onesb = A.alloc("onesb", [128, 128], BF16)
    slfb = A.alloc("slfb", [128, 128], BF16)
    small = A.alloc("small", [128, 64], F32)
    lb = A.alloc("lb", [128, 2, 2, H], F32)
    lg = A.alloc("lg", [128, 2, H], F32)
    hgng = A.alloc("hgng", [128, 1], F32)
    retg = A.alloc("retg", [128, 2], F32)
    oab_o = [A.alloc("oabo%d" % i, [128, CH], BF16) for i in range(2)]
    oab_ctr = [0]
    rw = A.alloc("rw", [128, KT, E], F32)
    rbrow = A.alloc("rbrow", [128, E], F32)
    Mf = A.alloc("Mf", [128, NT, E], F32)
    Mb = A.alloc("Mb", [128, NT, E], BF16)
    Wd = A.alloc("Wd", [128, NT, E], F32)
    lgt = A.alloc("lgt", [128, E], F32)
    pex = A.alloc("pex", [128, E], F32)
    top8 = A.alloc("top8", [128, 8], F32)
    lbraw = A.alloc("lbraw", [128, 2, 2, H], F32)
    mP = A.mark()
    WS = 768
    wslots = [A.alloc("wslot%d" % i, [128, max(KT, 2 * H), WS], BF16) for i in range(2)]
    wctr = [0]
    xmT = A.alloc("xmT", [128, KT, T], BF16)

    s.dma("sp", lambda e: e.dma_start(out=cst[:], in_=cst_d.ap()), writes=["cst"])
    s.op("dve", CP(identb[:], C("ident")), reads=["cst"], writes=["identb"])
    s.op("dve", CP(onesb[:], C("ones")), reads=["cst"], writes=["onesb"])
    s.op("dve", CP(slfb[:], C("slf")), reads=["cst"], writes=["slfb"])
    s.dma("sp", lambda e: e.dma_start(out=hgng[:], in_=hgng_d.ap()), writes=["hgng"])
    s.dma("sp", lambda e: e.dma_start(out=retg[:], in_=retg_d.ap()), writes=["retg"])

    def wload(src2d, kt_n, ncols):
        i = wctr[0] % 2
        wctr[0] += 1
        slot = wslots[i]
        tok = "w%d" % i
        s.dma("pool", lambda e: e.dma_start(out=slot[:, 0:kt_n, 0:ncols],
                                            in_=src2d.rearrange("(kt p) n -> p kt n", p=128)), writes=[tok])
        return slot, tok

    _bc = {}

    def BC(e, v):
        if v not in _bc:
            _bc[v] = e.to_reg(v)
        return _bc[v]

    def mms(items, reads, writes):
        n = len(items)
        for i, (o, l, r, st, sp_) in enumerate(items):
            s.op("pe", lambda e, o=o, l=l, r=r, st=st, sp_=sp_: e.matmul(o, lhsT=l, rhs=r, start=st, stop=sp_),
                 reads=reads, writes=writes, inc=(i == n - 1))

    def acc_mm(out, pairs, reads, writes):
        n = len(pairs)
        mms([(out, l, r, i == 0, i == n - 1) for i, (l, r) in enumerate(pairs)], reads, writes)

    def transposes(srcs, dst_fn, reads, dst_tok_fn, evac_eng="act", f32=False, extra=None):
        n = len(srcs)
        g = 0
        half = 0
        while g < n:
            m = min(4, n - g)
            if f32:
                pt, ptok = psS[1][:].rearrange("p (c v) -> p c v", v=128), "psS1"
                view = pt[:, 0:m, :]
            else:
                pt, ptok = psTbs[half], "psTb%d" % half
                view = pt[:, 0:m, :]
            for j in range(m):
                src = srcs[g + j]
                o = pt[:, j, :]
                idn = C("ident") if f32 else identb[:]
                s.op("pe", lambda e, o=o, src=src, idn=idn: e.transpose(out=o, in_=src, identity=idn),
                     reads=list(reads) + ["cst", "identb"], writes=[ptok], inc=(j == m - 1))
            dst = dst_fn(g, m)
            if evac_eng == "act":
                s.op("act", lambda e, dst=dst, view=view: e.copy(out=dst, in_=view), reads=[ptok], writes=dst_tok_fn(g, m))
            else:
                s.op("dve", CP(dst, view), reads=[ptok], writes=dst_tok_fn(g, m))
            if extra is not None:
                extra(pt, 0, g, m, ptok)
            g += m
            half ^= 1

    s.dma("sp", lambda e: e.dma_start(out=lbraw[:, 0], in_=lbf_d.ap()), writes=["lbraw0"])
    s.dma("sp", lambda e: e.dma_start(out=lbraw[:, 1], in_=lbb_d.ap()), writes=["lbraw1"])
    for d_ in range(2):
        s.op("dve", TT(lbraw[:, d_, 0, :], lbraw[:, d_, 0, :], lbraw[:, d_, 1, :], ALU.subtract),
             reads=["lbraw%d" % d_], writes=["lbraw%d" % d_])
        s.op("act", ACT(lb[:, d_, 0, :], lbraw[:, d_, 0, :], AF.Sigmoid), reads=["lbraw%d" % d_], writes=["lb%d" % d_])
        s.op("dve", TS(lb[:, d_, 1, :], lb[:, d_, 0, :], -1.0, 1.0, ALU.mult, ALU.add), reads=["lb%d" % d_], writes=["lb%d" % d_])
    for d_, src in enumerate((rl2f_d, rl2b_d)):
        s.dma("sp", lambda e, d_=d_, src=src: e.dma_start(out=lg[:, d_, :], in_=src.ap().partition_broadcast(128)),
              writes=["lg%d" % d_])
        s.op("act", ACT(lg[:, d_, :], lg[:, d_, :], AF.Exp, scale=math.log(2.0)), reads=["lg%d" % d_], writes=["lg%d" % d_])
        s.op("dve", TS(lg[:, d_, :], lg[:, d_, :], -1.0, 1.0, ALU.mult, ALU.add), reads=["lg%d" % d_], writes=["lg%d" % d_])
        s.op("act", ACT(lg[:, d_, :], lg[:, d_, :], AF.Ln), reads=["lg%d" % d_], writes=["lg%d" % d_])

    if cfg.get("STOP") == "0":
        s.finish(); nc._cfg = cfg; nc._ninst = s.ninst; nc._peak = 0; nc._dbg = dbg_outs
        return nc
    mA2 = A.mark()
    cT = A.alloc("cT", [128, KT, 2], F32)
    scb = A.alloc("scb", [128, KT, 2], BF16)
    s.dma("sp", lambda e: e.dma_start(out=cT[:], in_=cT_d.ap()), writes=["cT"])
    s.op("act", ACT(scb[:], cT[:], AF.Silu), reads=["cT"], writes=["scb"])
    modt = [A.alloc("modt%d" % i, [2, 512], F32) for i in range(2)]
    adab = [A.alloc("adab%d" % i, [2, 512], F32) for i in range(2)]
    NMC = (6 * D) // 512
    for j in range(NMC):
        slot, wt = wload(adaw_d.ap()[:, j * 512:(j + 1) * 512], KT, 512)
        ps = psA[j % 4]
        acc_mm(ps[0:2, :], [(scb[:, kt, :], slot[:, kt, 0:512]) for kt in range(KT)], ["scb", wt], ["psA%d" % (j % 4)])
        ab = adab[j % 2]
        mt = modt[j % 2]
        s.dma("sp", lambda e, ab=ab, j=j: e.dma_start(out=ab[:], in_=adab_d.ap()[:, j * 512:(j + 1) * 512]), writes=["adab%d" % (j % 2)])
        s.op("dve", TT(mt[:], ps[0:2, :], ab[:], ALU.add), reads=["psA%d" % (j % 4), "adab%d" % (j % 2)], writes=["modt%d" % (j % 2)])
        s.dma("sp", lambda e, mt=mt, j=j: e.dma_start(out=MOD.ap()[:, j * 512:(j + 1) * 512], in_=mt[:]),
              reads=["modt%d" % (j % 2)], writes=["MOD%d" % j])

    def modtoks(c0, c1):
        return ["MOD%d" % j for j in range(c0 // 512, (c1 - 1) // 512 + 1)]

    def load_row(dst, cond, which, tok):
        c0 = which * D
        s.dma("sp", lambda e: e.dma_start(out=dst, in_=MOD.ap()[cond:cond + 1, c0:c0 + D].partition_broadcast(128)),
              reads=modtoks(c0, c0 + D), writes=[tok])

    def load_vec_row(dst, src_d, tok):
        s.dma("sp", lambda e: e.dma_start(out=dst, in_=src_d.ap().partition_broadcast(128)), writes=[tok])

    tile_cond = [0] * (NP * TP // 128) + [1] * (TS_ // 128)
    segs = [(p * TP // 128, TP // 128, "p", p) for p in range(NP)] + [(NP * TP // 128, TS_ // 128, "s", 0)]

    rows_a = {}
    n1row = A.alloc("n1row", [128, D], F32)
    load_vec_row(n1row[:], n1g_d, "n1row")
    for c in range(2):
        a1 = A.alloc("a1row%d" % c, [128, D], F32)
        sh = A.alloc("sh1row%d" % c, [128, D], F32)
        load_row(a1[:], c, 1, "a1row%d" % c)
        load_row(sh[:], c, 0, "sh1row%d" % c)
        s.op("dve", STT(a1[:], a1[:], 1.0, n1row[:], ALU.add, ALU.mult), reads=["a1row%d" % c, "n1row"], writes=["a1row%d" % c])
        rows_a[c] = (a1, sh)
    xbuf = [A.alloc("xbuf%d" % i, [128, D], F32) for i in range(2)]
    t32 = A.alloc("t32", [128, D], F32)
    xmb = [A.alloc("xmb%d" % i, [128, D], BF16) for i in range(2)]
    junk = A.alloc("junk", [128, D], BF16)

    def rms_rstd(src, src_tok, col, n):
        sc = small[:, col:col + 1]
        tk = "small%d" % col
        s.op("dve", lambda e: e.memset(sc, 0.0), writes=[tk])
        s.op("act", ACT(junk[:, 0:n], src, AF.Square, accum=sc), reads=[src_tok, tk], writes=["junk", tk])
        s.op("dve", TS(sc, sc, 1.0 / n, EPS, ALU.mult, ALU.add), reads=[tk], writes=[tk])
        s.op("act", ACT(sc, sc, AF.Sqrt), reads=[tk], writes=[tk])
        s.op("dve", lambda e: e.reciprocal(out=sc, in_=sc), reads=[tk], writes=[tk])
        return sc, tk

    for i in range(NT):
        c = tile_cond[i]
        xt = xbuf[i % 2]
        xtok = "xbuf%d" % (i % 2)
        s.dma("sp", lambda e, xt=xt, i=i: e.dma_start(out=xt[:], in_=x_d.ap()[i * 128:(i + 1) * 128, :]), writes=[xtok])
        sc, tk = rms_rstd(xt[:], xtok, i % 2, D)
        a1, sh = rows_a[c]
        s.op("dve", STT(t32[:], xt[:], sc, a1[:], ALU.mult, ALU.mult), reads=[xtok, tk, "a1row%d" % c], writes=["t32"])
        xm = xmb[i % 2]
        s.op("dve", TT(xm[:], t32[:], sh[:], ALU.add), reads=["t32", "sh1row%d" % c], writes=["xmb%d" % (i % 2)])
        transposes([xm[:, kt * 128:(kt + 1) * 128] for kt in range(KT)],
                   lambda g, m, i=i: xmT[:, g:g + m, i * 128:(i + 1) * 128],
                   ["xmb%d" % (i % 2)], lambda g, m, i=i: ["xmT_%d" % i])
    xmT_toks = ["xmT_%d" % i for i in range(NT)]
    if debug:
        for kt in range(KT):
            dump("xmT%d" % kt, xmT[:, kt, :], xmT_toks)
    s.barrier()
    A.release(mA2)

    if cfg.get("STOP") == "A":
        s.finish(); nc._cfg = cfg; nc._ninst = s.ninst; nc._peak = 0; nc._dbg = dbg_outs
        return nc
    mB = A.mark()
    q32 = A.alloc("q32", [128, T], BF16)
    fdir = [A.alloc("f%d" % d_, [128, T], F32) for d_ in range(2)]
    s1 = A.alloc("s1", [128, T], F32)
    s2 = A.alloc("s2", [128, T], F32)
    gcol = A.alloc("gcol", [128, NCK], F32)
    sgate = A.alloc("sgate", [128, T], BF16)
    vTM = A.alloc("vTM", [128, NT, 128], BF16)
    qp = [A.alloc("qp%d" % d_, [128, T], BF16) for d_ in range(2)]
    kp = [A.alloc("kp%d" % d_, [128, T], BF16) for d_ in range(2)]
    kpTM = [A.alloc("kpTM%d" % d_, [128, NT, 128], BF16) for d_ in range(2)]
    vexp = A.alloc("vexp", [128, NT, 4, 128], BF16)
    Rpb = [A.alloc("Rpb%d" % d_, [128, NCK, 128], BF16) for d_ in range(2)]
    decay = [A.alloc("decay%d" % d_, [128, NCK], F32) for d_ in range(2)]
    R32 = [[A.alloc("R32_%d_%d" % (d_, sg), [128, 128], F32) for sg in range(len(segs))] for d_ in range(2)]
    Rp32 = A.alloc("Rp32", [128, 128], F32)
    msk = [A.alloc("msk%d" % i, [128, 128], BF16) for i in range(2)]
    sqb = A.alloc("sqb", [128, CH], BF16)
    Mdir = [C("mf"), C("mb")]

    SB = cfg.get("SB", 99)
    for h in range(H if SB == 99 else 1):
        slot, wt = wload(win_d.ap()[:, h * 640:(h + 1) * 640], KT, 640)
        for ch in range(NCHK):
            cs = slice(ch * CH, (ch + 1) * CH)
            for j in range(4):
                acc_mm(psA[j][:, 0:CH], [(slot[:, kt, j * 128:(j + 1) * 128], xmT[:, kt, cs]) for kt in range(KT)],
                       [wt] + xmT_toks, ["psA%d" % j])
            s.op("act", ACT(q32[:, cs], psA[0][:, 0:CH], AF.Silu), reads=["psA0"], writes=["q32"])
            for d_ in range(2):
                s.op("act", ACT(s1[:, cs], psA[1 + d_][:, 0:CH], AF.Sigmoid), reads=["psA%d" % (1 + d_)], writes=["s1"])
                s.op("dve", TS(fdir[d_][:, cs], s1[:, cs], lb[:, d_, 1, h:h + 1], lb[:, d_, 0, h:h + 1], ALU.mult, ALU.add),
                     reads=["s1", "lb%d" % d_], writes=["f%d" % d_])
            s.op("act", ACT(sgate[:, cs], psA[3][:, 0:CH], AF.Sigmoid), reads=["psA3"], writes=["sgate"])
        for i in range(NT):
            ps = psS[i % 2]
            acc_mm(ps[:, 0:128], [(xmT[:, kt, i * 128:(i + 1) * 128], slot[:, kt, 512:640]) for kt in range(KT)],
                   [wt] + xmT_toks, ["psS%d" % (i % 2)])
            s.op("act", lambda e, ps=ps, i=i: e.copy(out=vTM[:, i, :], in_=ps[:, 0:128]), reads=["psS%d" % (i % 2)], writes=["vTM"])
        cmo, _ = coff["cmask"]
        for c in range(4):
            s.op("dve", TS(vexp[:, :, c, :], vTM[:], cst[:, cmo + c:cmo + c + 1], None, ALU.mult), reads=["vTM", "cst"], writes=["vexp"])
        if SB == 1:
            break
        for d_ in range(2):
            f = fdir[d_]
            ftok = "f%d" % d_
            s.op("act", ACT(s1[:], f[:], AF.Ln), reads=[ftok], writes=["s1"])
            s.op("dve", TS(f[:], f[:], -1.0, 1.0, ALU.mult, ALU.add), reads=[ftok, "s1"], writes=[ftok])
            for ch in range(NCHK):
                cs = slice(ch * CH, (ch + 1) * CH)
                s.op("dve", lambda e, cs=cs: e.tensor_tensor_scan(out=s2[:, cs], data0=C("reset"), data1=s1[:, cs], initial=0.0,
                                                                  op0=ALU.mult, op1=ALU.add), reads=["s1", "cst"], writes=["s2"])
            gi3 = s2[:].rearrange("p (c k) -> p c k", k=CHUNK)
            s.op("act", ACT(decay[d_][:], gi3[:, :, CHUNK - 1], AF.Exp), reads=["s2"], writes=["decay%d" % d_])
            if d_ == 0:
                s.op("dve", CP(gcol[:], gi3[:, :, CHUNK - 1]), reads=["s2"], writes=["gcol"])
                s.op("dve", TT(gi3, gcol[:].unsqueeze(2).to_broadcast([128, NCK, CHUNK]), gi3, ALU.subtract),
                     reads=["s2", "gcol", "decay%d" % d_], writes=["s2"])
            else:
                s.op("dve", TT(s2[:], s2[:], s1[:], ALU.subtract), reads=["s2", "s1", "decay%d" % d_], writes=["s2"])
            s.op("act", ACT(s1[:], s2[:], AF.Exp), reads=["s2"], writes=["s1"])
            s.op("dve", TT(kp[d_][:], f[:], s1[:], ALU.mult), reads=[ftok, "s1"], writes=["kp%d" % d_])
            s.op("act", ACT(s1[:], s2[:], AF.Exp, scale=-1.0), reads=["s2", "kp%d" % d_], writes=["s1"])
            s.op("dve", TT(qp[d_][:], q32[:], s1[:], ALU.mult), reads=["q32", "s1"], writes=["qp%d" % d_])
            if SB == 2:
                continue
            transposes([kp[d_][:, i * 128:(i + 1) * 128] for i in range(NT)],
                       lambda g, m, d_=d_: kpTM[d_][:, g:g + m, :], ["kp%d" % d_], lambda g, m, d_=d_: ["kpTM%d" % d_])
            if SB == 3:
                continue
            for sgi, (t0, nt, kind, sidx) in enumerate(segs):
                Rr = R32[d_][sgi]
                rtok = "R32_%d_%d" % (d_, sgi)
                if kind == "p":
                    s.op("dve", lambda e, Rr=Rr: e.memset(Rr[:], 0.0), writes=[rtok])
                else:
                    src = (shf_d, shb_d)[d_]
                    s.dma("sp", lambda e, Rr=Rr, src=src, h=h: e.dma_start(out=Rr[:], in_=src.ap()[h]), writes=[rtok])
                tiles = list(range(t0, t0 + nt))
                if d_ == 1:
                    tiles = tiles[::-1]
                for i in tiles:
                    ps = psS[i % 2]
                    ptok = "psS%d" % (i % 2)
                    pv = ps[:].rearrange("p (c v) -> p c v", v=128)
                    mms([(ps[:, 0:512], kpTM[d_][:, i, :], vexp[:, i].rearrange("p c v -> p (c v)"), True, True)], ["kpTM%d" % d_, "vexp"], [ptok])
                    cl = list(range(4))
                    if d_ == 1:
                        cl = cl[::-1]
                    for c in cl:
                        cg = i * 4 + c
                        s.op("dve", TS(Rp32[:], Rr[:], decay[d_][:, cg:cg + 1], None, ALU.mult),
                             reads=[rtok, "decay%d" % d_], writes=["Rp32"])
                        s.op("act", lambda e, d_=d_, cg=cg: e.copy(out=Rpb[d_][:, cg, :], in_=Rp32[:]),
                             reads=["Rp32"], writes=["Rpb%d" % d_])
                        s.op("dve", TT(Rr[:], Rp32[:], pv[:, c, :], ALU.add), reads=["Rp32", ptok], writes=[rtok])
                if kind == "p":
                    dst = (hf_o, hb_o)[d_]
                    s.dma("sp", lambda e, Rr=Rr, dst=dst, sidx=sidx, h=h: e.dma_start(out=dst.ap()[sidx, h], in_=Rr[:]),
                          reads=[rtok], writes=["hout%d_%d_%d" % (d_, sidx, h)])
        if SB in (2, 3, 4):
            break
        for ch in range(NCHK):
            its = []
            for tl in range(TPC):
                i = ch * TPC + tl
                tsl = slice(i * 128, (i + 1) * 128)
                reg = psA[0][:, tl * 128:(tl + 1) * 128]
                first = True
                for d_ in range(2):
                    ps = psS[d_]
                    s.op("pe", lambda e, ps=ps, d_=d_, tsl=tsl: e.matmul(ps[:, 0:128], lhsT=kp[d_][:, tsl], rhs=qp[d_][:, tsl], start=True, stop=True),
                         reads=["kp%d" % d_, "qp%d" % d_], writes=["psS%d" % d_])
                    mk = msk[d_]
                    s.op("dve", TT(mk[:], ps[:, 0:128], Mdir[d_], ALU.mult), reads=["psS%d" % d_, "cst"], writes=["msk%d" % d_])
                    items = [(reg, vTM[:, i, :], mk[:], first, (SB == 5 and d_ == 1))]
                    first = False
                    for c in range(4 if SB != 5 else 0):
                        cg = i * 4 + c
                        items.append((psA[0][:, tl * 128 + c * 32: tl * 128 + (c + 1) * 32], Rpb[d_][:, cg, :],
                                      qp[d_][:, i * 128 + c * 32:i * 128 + (c + 1) * 32], False, (d_ == 1 and c == 3)))
                    mms(items, ["vTM", "msk%d" % d_, "Rpb%d" % d_, "qp%d" % d_], ["psA0"])
            cs = slice(ch * CH, (ch + 1) * CH)
            if SB in (5, 6):
                continue
            s.op("dve", CP(s1[:, 0:CH], psA[0][:, 0:CH]), reads=["psA0"], writes=["s1"])
            s.op("act", ACT(sqb[:], psA[0][:, 0:CH], AF.Square), reads=["psA0"], writes=["sqb"])
            acc_mm(psA[1][:, 0:CH], [(onesb[:], sqb[:])], ["onesb", "sqb"], ["psA1"])
            s.op("dve", TS(s2[:, 0:CH], psA[1][:, 0:CH], 1.0 / 128, EPS, ALU.mult, ALU.add), reads=["psA1"], writes=["s2"])
            s.op("act", ACT(s2[:, 0:CH], s2[:, 0:CH], AF.Sqrt), reads=["s2"], writes=["s2"])
            s.op("dve", lambda e: e.reciprocal(out=s2[:, 0:CH], in_=s2[:, 0:CH]), reads=["s2"], writes=["s2"])
            if SB == 7:
                continue
            s.op("dve", STT(s1[:, 0:CH], s1[:, 0:CH], hgng[:, 0:1], s2[:, 0:CH], ALU.mult, ALU.mult), reads=["s1", "s2", "hgng"], writes=["s1"])
            ob = oab_o[oab_ctr[0] % 2]
            otok = "oabo%d" % (oab_ctr[0] % 2)
            oab_ctr[0] += 1
            s.op("dve", TT(ob[:], s1[:, 0:CH], sgate[:, cs], ALU.mult), reads=["s1", "sgate"], writes=[otok])
            if SB == 8:
                continue
            s.dma("sp", lambda e, ob=ob, h=h, cs=cs: e.dma_start(out=OAB.ap()[h, :, cs], in_=ob[:]), reads=[otok], writes=["OAB_%d_%d" % (h, ch)])
    s.barrier()
    A.release(mB)

    if cfg.get("STOP") == "B":
        s.finish(); nc._cfg = cfg; nc._ninst = s.ninst; nc._peak = 0; nc._dbg = dbg_outs
        return nc
    mC = A.mark()
    q32 = A.alloc("rq32", [128, T], F32)
    k32 = A.alloc("rk32", [128, T], F32)
    qb = A.alloc("rqb", [128, T], BF16)
    kb = A.alloc("rkb", [128, T], BF16)
    qin = [A.alloc("qin%d" % d_, [128, T], BF16) for d_ in range(2)]
    kend = [A.alloc("kend%d" % d_, [128, NT, 128], BF16) for d_ in range(2)]
    rvTM = A.alloc("rvTM", [128, NT, 256], BF16)
    Rsb = [A.alloc("Rsb%d" % d_, [128, NT, 256], BF16) for d_ in range(2)]
    rg = A.alloc("rg", [128, 2, T], BF16)
    RR = [[A.alloc("RR_%d_%d" % (d_, sg), [128, 256], F32) for sg in range(len(segs))] for d_ in range(2)]
    tA = A.alloc("tA", [128, 128], F32)
    tB = A.alloc("tB", [128, 128], F32)
    Ds = A.alloc("Ds", [128, 128], F32)
    rowt = [A.alloc("rowt%d" % d_, [128, 128], F32) for d_ in range(2)]
    colt = A.alloc("colt", [128, 4], F32)
    t1 = A.alloc("t1", [128, CH], F32)
    t2 = A.alloc("t2", [128, CH], F32)
    rmsk = [A.alloc("rmsk%d" % i, [128, 128], BF16) for i in range(2)]
    ro32 = [A.alloc("ro32_%d" % v, [128, CH], F32) for v in range(2)]
    rob = [A.alloc("rob_%d" % v, [128, CH], BF16) for v in range(2)]
    rsq = [A.alloc("rsq_%d" % v, [128, CH], BF16) for v in range(2)]
    mean = A.alloc("mean", [128, CH], F32)
    var = A.alloc("var", [128, CH], F32)
    ts0 = NP * TP
    ropet = A.alloc("ropet", [128, 2 * TS_], F32)
    s.dma("sp", lambda e: e.dma_start(out=ropet[:], in_=rope_d.ap()), writes=["ropet"])

    for h in range(H):
        c0 = H * 640 + h * 768
        slot, wt = wload(win_d.ap()[:, c0:c0 + 768], KT, 768)
        for ch in range(NCHK):
            cs = slice(ch * CH, (ch + 1) * CH)
            for j in range(4):
                acc_mm(psA[j][:, 0:CH], [(slot[:, kt, j * 128:(j + 1) * 128], xmT[:, kt, cs]) for kt in range(KT)],
                       [wt] + xmT_toks, ["psA%d" % j])
            s.op("act", lambda e, cs=cs: e.copy(out=q32[:, cs], in_=psA[0][:, 0:CH]), reads=["psA0"], writes=["rq32"])
            s.op("act", ACT(k32[:, cs], psA[1][:, 0:CH], AF.Copy, scale=128.0 ** -0.5), reads=["psA1"], writes=["rk32"])
            for v in range(2):
                s.op("act", ACT(rg[:, v, cs], psA[2 + v][:, 0:CH], AF.Silu), reads=["psA%d" % (2 + v)], writes=["rg"])
        for i in range(NT):
            ps = psS[i % 2]
            acc_mm(ps[:, 0:256], [(xmT[:, kt, i * 128:(i + 1) * 128], slot[:, kt, 512:768]) for kt in range(KT)],
                   [wt] + xmT_toks, ["psS%d" % (i % 2)])
            s.op("act", lambda e, ps=ps, i=i: e.copy(out=rvTM[:, i, :], in_=ps[:, 0:256]), reads=["psS%d" % (i % 2)], writes=["rvTM"])
        if ts0 > 0:
            s.op("dve", CP(qb[:, 0:ts0], q32[:, 0:ts0]), reads=["rq32"], writes=["rqb"])
            s.op("dve", CP(kb[:, 0:ts0], k32[:, 0:ts0]), reads=["rk32"], writes=["rkb"])
        RC = min(512, TS_)
        for rc in range(TS_ // RC):
            cs = slice(ts0 + rc * RC, ts0 + (rc + 1) * RC)
            cosv = ropet[:, rc * RC:(rc + 1) * RC]
            sinv = ropet[:, TS_ + rc * RC: TS_ + (rc + 1) * RC]
            for src, dstb, stok, dtok, pi in ((q32, qb, "rq32", "rqb", 2), (k32, kb, "rk32", "rkb", 3)):
                acc_mm(psA[pi][:, 0:RC], [(C("psw"), src[:, cs])], ["cst", stok], ["psA%d" % pi])
                s.op("dve", TT(t1[:, 0:RC], src[:, cs], cosv, ALU.mult), reads=[stok, "ropet"], writes=["t1"])
                s.op("dve", TT(t2[:, 0:RC], psA[pi][:, 0:RC], sinv, ALU.mult), reads=["psA%d" % pi, "ropet"], writes=["t2"])
                s.op("dve", TT(dstb[:, cs], t1[:, 0:RC], t2[:, 0:RC], ALU.add), reads=["t1", "t2"], writes=[dtok])
        lgf = lg[:, 0, h:h + 1]
        lgb = lg[:, 1, h:h + 1]
        s.op("act", ACT(tA[:], C("dpos"), AF.Exp, scale=lgf), reads=["cst", "lg0"], writes=["tA"])
        s.op("dve", TT(tA[:], tA[:], C("slf"), ALU.mult), reads=["tA", "cst"], writes=["tA"])
        s.op("act", ACT(tB[:], C("dneg"), AF.Exp, scale=lgb), reads=["cst", "lg1"], writes=["tB"])
        s.op("dve", TT(tB[:], tB[:], C("slb"), ALU.mult), reads=["tB", "cst"], writes=["tB"])
        s.op("dve", TT(Ds[:], tA[:], tB[:], ALU.add), reads=["tA", "tB"], writes=["Ds"])
        s.op("dve", TT(Ds[:], Ds[:], C("i2"), ALU.add), reads=["Ds", "cst"], writes=["Ds"])
        s.op("act", ACT(rowt[0][:], C("iota1"), AF.Exp, scale=lgf), reads=["cst", "lg0"], writes=["rowt0"])
        s.op("act", ACT(rowt[1][:], C("iotar"), AF.Exp, scale=lgb), reads=["cst", "lg1"], writes=["rowt1"])
        s.op("act", ACT(colt[:, 0:1], C("c127"), AF.Exp, scale=lgf), reads=["cst", "lg0"], writes=["colt"])
        s.op("act", ACT(colt[:, 1:2], C("cp"), AF.Exp, scale=lgb), reads=["cst", "lg1"], writes=["colt"])
        s.op("act", ACT(colt[:, 2:3], lgf, AF.Exp, scale=128.0), reads=["lg0"], writes=["colt"])
        s.op("act", ACT(colt[:, 3:4], lgb, AF.Exp, scale=128.0), reads=["lg1"], writes=["colt"])
        for i in range(NT):
            tsl = slice(i * 128, (i + 1) * 128)
            for d_ in range(2):
                s.op("dve", TT(qin[d_][:, tsl], qb[:, tsl], rowt[d_][:], ALU.mult), reads=["rqb", "rowt%d" % d_], writes=["qin%d" % d_])
        g = 0
        half = 0
        while g < NT:
            m = min(4, NT - g)
            ptok = "psTb%d" % half
            for j in range(m):
                o = psTbs[half][:, j, :]
                src = kb[:, (g + j) * 128:(g + j + 1) * 128]
                s.op("pe", lambda e, o=o, src=src: e.transpose(out=o, in_=src, identity=identb[:]),
                     reads=["rkb", "identb"], writes=[ptok], inc=(j == m - 1))
            view = psTbs[half][:, 0:m, :]
            for d_ in range(2):
                s.op("act", ACT(kend[d_][:, g:g + m, :], view, AF.Copy, scale=colt[:, d_:d_ + 1]), reads=[ptok, "colt"], writes=["kend%d" % d_])
            g += m
            half ^= 1
        for d_ in range(2):
            for sgi, (t0, nt, kind, sidx) in enumerate(segs):
                Rr = RR[d_][sgi]
                rtok = "RR_%d_%d" % (d_, sgi)
                if kind == "p":
                    s.op("dve", lambda e, Rr=Rr: e.memset(Rr[:], 0.0), writes=[rtok])
                else:
                    src = (srf_d, srb_d)[d_]
                    s.dma("sp", lambda e, Rr=Rr, src=src, h=h: e.dma_start(out=Rr[:], in_=src.ap()[h]), writes=[rtok])
                tiles = list(range(t0, t0 + nt))
                if d_ == 1:
                    tiles = tiles[::-1]
                for i in tiles:
                    ps = psS[i % 2]
                    ptok = "psS%d" % (i % 2)
                    acc_mm(ps[:, 0:256], [(kend[d_][:, i, :], rvTM[:, i, :])], ["kend%d" % d_, "rvTM"], [ptok])
                    s.op("act", lambda e, d_=d_, i=i, Rr=Rr: e.copy(out=Rsb[d_][:, i, :], in_=Rr[:]), reads=[rtok], writes=["Rsb%d" % d_])
                    s.op("dve", STT(Rr[:], Rr[:], colt[:, 2 + d_:3 + d_], ps[:, 0:256], ALU.mult, ALU.add),
                         reads=[rtok, "colt", ptok], writes=[rtok])
                if kind == "p":
                    dst = (rf_o, rb_o)[d_]
                    s.dma("sp", lambda e, Rr=Rr, dst=dst, sidx=sidx, h=h: e.dma_start(out=dst.ap()[sidx, h], in_=Rr[:]),
                          reads=[rtok], writes=["rout%d_%d_%d" % (d_, sidx, h)])
        for ch in range(NCHK):
            cs = slice(ch * CH, (ch + 1) * CH)
            for tl in range(TPC):
                i = ch * TPC + tl
                tsl = slice(i * 128, (i + 1) * 128)
                ps = psS[i % 2]
                s.op("pe", lambda e, ps=ps, tsl=tsl: e.matmul(ps[:, 0:128], lhsT=kb[:, tsl], rhs=qb[:, tsl], start=True, stop=True),
                     reads=["rkb", "rqb"], writes=["psS%d" % (i % 2)])
                mk = rmsk[i % 2]
                s.op("dve", TT(mk[:], ps[:, 0:128], Ds[:], ALU.mult), reads=["psS%d" % (i % 2), "Ds"], writes=["rmsk%d" % (i % 2)])
                for v in range(2):
                    reg = psA[v][:, tl * 128:(tl + 1) * 128]
                    vs = slice(v * 128, (v + 1) * 128)
                    mms([(reg, rvTM[:, i, vs], mk[:], True, False),
                         (reg, Rsb[0][:, i, vs], qin[0][:, tsl], False, False),
                         (reg, Rsb[1][:, i, vs], qin[1][:, tsl], False, True)],
                        ["rvTM", "rmsk%d" % (i % 2), "Rsb0", "Rsb1", "qin0", "qin1"], ["psA%d" % v])
            for v in range(2):
                s.op("dve", CP(ro32[v][:], psA[v][:, 0:CH]), reads=["psA%d" % v], writes=["ro32_%d" % v])
                s.op("act", lambda e, v=v: e.copy(out=rob[v][:], in_=psA[v][:, 0:CH]), reads=["psA%d" % v], writes=["rob%d" % v])
                s.op("act", ACT(rsq[v][:], psA[v][:, 0:CH], AF.Square), reads=["psA%d" % v], writes=["rsq%d" % v])
            acc_mm(psA[2][:, 0:CH], [(onesb[:], rob[0][:]), (onesb[:], rob[1][:])], ["onesb", "rob0", "rob1"], ["psA2"])
            acc_mm(psA[3][:, 0:CH], [(onesb[:], rsq[0][:]), (onesb[:], rsq[1][:])], ["onesb", "rsq0", "rsq1"], ["psA3"])
            s.op("dve", TS(mean[:], psA[2][:, 0:CH], 1.0 / 256, None, ALU.mult), reads=["psA2"], writes=["mean"])
            s.op("dve", TT(var[:], mean[:], mean[:], ALU.mult), reads=["mean"], writes=["var"])
            s.op("dve", STT(var[:], psA[3][:, 0:CH], 1.0 / 256, var[:], ALU.mult, ALU.subtract), reads=["psA3", "var"], writes=["var"])
            s.op("dve", TS(var[:], var[:], EPS, None, ALU.add), reads=["var"], writes=["var"])
            s.op("act", ACT(var[:], var[:], AF.Sqrt), reads=["var"], writes=["var"])
            s.op("dve", lambda e: e.reciprocal(out=var[:], in_=var[:]), reads=["var"], writes=["var"])
            for v in range(2):
                s.op("dve", TT(ro32[v][:], ro32[v][:], mean[:], ALU.subtract), reads=["ro32_%d" % v, "mean"], writes=["ro32_%d" % v])
                s.op("dve", STT(ro32[v][:], ro32[v][:], retg[:, v:v + 1], var[:], ALU.mult, ALU.mult),
                     reads=["ro32_%d" % v, "retg", "var"], writes=["ro32_%d" % v])
                ob = oab_o[oab_ctr[0] % 2]
                otok = "oabo%d" % (oab_ctr[0] % 2)
                oab_ctr[0] += 1
                s.op("dve", TT(ob[:], ro32[v][:], rg[:, v, cs], ALU.mult), reads=["ro32_%d" % v, "rg"], writes=[otok])
                ci = H + 2 * h + v
                s.dma("sp", lambda e, ob=ob, ci=ci, cs=cs: e.dma_start(out=OAB.ap()[ci, :, cs], in_=ob[:]), reads=[otok], writes=["OAB_%d_%d" % (ci, ch)])
    s.barrier()
    A.release(mC)

    if cfg.get("STOP") == "C":
        s.finish(); nc._cfg = cfg; nc._ninst = s.ninst; nc._peak = 0; nc._dbg = dbg_outs
        return nc
    mD = A.mark()
    s.dma("sp", lambda e: e.dma_start(out=rw[:], in_=rw_d.ap().rearrange("(kt p) n -> p kt n", p=128)), writes=["rw"])
    load_vec_row(rbrow[:], rb_d, "rbrow")
    mD1 = A.mark()
    for ch in range(NCHK):
        cs = slice(ch * CH, (ch + 1) * CH)
        conds = sorted(set(tile_cond[ch * TPC:(ch + 1) * TPC]))
        A.release(mD1)
        oab = A.alloc("oab", [128, 3 * H, CH], BF16)
        s.dma("sp", lambda e, cs=cs: e.dma_start(out=oab[:], in_=OAB.ap()[:, :, cs].rearrange("c p t -> p c t")),
              reads=["OAB_%d_%d" % (ci, ch) for ci in range(3 * H)], writes=["oab"])
        merged = A.alloc("merged", [128, KT, CH], BF16)
        mD2 = A.mark()
        sga = A.alloc("sga", [128, JT, CH], F32)
        sgb = A.alloc("sgb", [128, JT, CH], F32)
        m1 = A.alloc("m1", [128, JT, CH], F32)
        for g in range(NG):
            slot, wt = wload(win_d.ap()[:, cfg["MG0"] + g * GW: cfg["MG0"] + (g + 1) * GW], KT, GW)
            for j in range(JT):
                acc_mm(psA[j][:, 0:CH], [(slot[:, kt, j * 128:(j + 1) * 128], xmT[:, kt, cs]) for kt in range(KT)], [wt] + xmT_toks, ["psA%d" % j])
                s.op("act", ACT(sga[:, j, :], psA[j][:, 0:CH], AF.Sigmoid), reads=["psA%d" % j], writes=["sga"])
            slot, wt = wload(win_d.ap()[:, cfg["MG0"] + D + g * GW: cfg["MG0"] + D + (g + 1) * GW], KT, GW)
            for j in range(JT):
                acc_mm(psA[j][:, 0:CH], [(slot[:, kt, j * 128:(j + 1) * 128], xmT[:, kt, cs]) for kt in range(KT)], [wt] + xmT_toks, ["psA%d" % j])
                s.op("act", ACT(sgb[:, j, :], psA[j][:, 0:CH], AF.Sigmoid), reads=["psA%d" % j], writes=["sgb"])
            slot, wt = wload(wpa_d.ap()[:, g * GW:(g + 1) * GW], H, GW)
            for j in range(JT):
                acc_mm(psA[j][:, 0:CH], [(slot[:, c, j * 128:(j + 1) * 128], oab[:, c, :]) for c in range(H)], [wt, "oab"], ["psA%d" % j])
                s.op("dve", TT(m1[:, j, :], sga[:, j, :], psA[j][:, 0:CH], ALU.mult), reads=["sga", "psA%d" % j], writes=["m1"])
            slot, wt = wload(wpb_d.ap()[:, g * GW:(g + 1) * GW], 2 * H, GW)
            for j in range(JT):
                acc_mm(psA[j][:, 0:CH], [(slot[:, c, j * 128:(j + 1) * 128], oab[:, H + c, :]) for c in range(2 * H)], [wt, "oab"], ["psA%d" % j])
                s.op("dve", TT(sgb[:, j, :], sgb[:, j, :], psA[j][:, 0:CH], ALU.mult), reads=["sgb", "psA%d" % j], writes=["sgb"])
                s.op("dve", TT(merged[:, g * JT + j, :], m1[:, j, :], sgb[:, j, :], ALU.add), reads=["m1", "sgb"], writes=["merged"])
        if debug:
            for kt in range(KT):
                dump("merged%d_%d" % (ch, kt), merged[:, kt, :], ["merged"])
        s.barrier()
        A.release(mD2)
        g1row = {}
        for c in conds:
            g1row[c] = A.alloc("g1row%d" % c, [128, D], F32)
            load_row(g1row[c][:], c, 2, "g1row%d" % c)
        xp = [A.alloc("xp%d" % i, [128, GW], F32) for i in range(2)]
        tp = A.alloc("tp", [128, GW], F32)
        xpc = 0
        for g in range(NG):
            slot, wt = wload(wout_d.ap()[:, g * GW:(g + 1) * GW], KT, GW)
            for tl in range(TPC):
                i = ch * TPC + tl
                c = tile_cond[i]
                ps = psA[tl % 4]
                acc_mm(ps[:, 0:GW], [(merged[:, kt, tl * 128:(tl + 1) * 128], slot[:, kt, 0:GW]) for kt in range(KT)], ["merged", wt], ["psA%d" % (tl % 4)])
                xq = xp[xpc % 2]
                xtok = "xp%d" % (xpc % 2)
                xpc += 1
                s.dma("sp", lambda e, xq=xq, i=i, g=g: e.dma_start(out=xq[:], in_=x_d.ap()[i * 128:(i + 1) * 128, g * GW:(g + 1) * GW]), writes=[xtok])
                s.op("dve", TT(tp[:], ps[:, 0:GW], g1row[c][:, g * GW:(g + 1) * GW], ALU.mult), reads=["psA%d" % (tl % 4), "g1row%d" % c], writes=["tp"])
                s.op("dve", TT(xq[:], tp[:], xq[:], ALU.add), reads=["tp", xtok], writes=[xtok])
                s.dma("sp", lambda e, xq=xq, i=i, g=g: e.dma_start(out=X1.ap()[i * 128:(i + 1) * 128, g * GW:(g + 1) * GW], in_=xq[:]),
                      reads=[xtok], writes=["X1_%d_%d" % (i, g)])
        s.barrier()
        A.release(mD1)
        rows2 = {}
        n2row = A.alloc("n2row", [128, D], F32)
        load_vec_row(n2row[:], n2g_d, "n2row")
        for c in conds:
            a2 = A.alloc("a2row%d" % c, [128, D], F32)
            sh2 = A.alloc("sh2row%d" % c, [128, D], F32)
            load_row(a2[:], c, 4, "a2row%d" % c)
            load_row(sh2[:], c, 3, "sh2row%d" % c)
            s.op("dve", STT(a2[:], a2[:], 1.0, n2row[:], ALU.add, ALU.mult), reads=["a2row%d" % c, "n2row"], writes=["a2row%d" % c])
            rows2[c] = (a2, sh2)
        x1t = [A.alloc("x1t%d" % i, [128, D], F32) for i in range(2)]
        xm2 = A.alloc("xm2", [128, D], F32)
        xm2b = [A.alloc("xm2b%d" % i, [128, D], BF16) for i in range(2)]
        xm2T = A.alloc("xm2T", [128, KT, 128], F32)
        junk = A.alloc("junk2", [128, D], BF16)
        for tl in range(TPC):
            i = ch * TPC + tl
            c = tile_cond[i]
            xt = x1t[i % 2]
            xtok = "x1t%d" % (i % 2)
            s.dma("sp", lambda e, xt=xt, i=i: e.dma_start(out=xt[:], in_=X1.ap()[i * 128:(i + 1) * 128, :]),
                  reads=["X1_%d_%d" % (i, g) for g in range(NG)], writes=[xtok])
            sc, tk = rms_rstd(xt[:], xtok, 2 + i % 2, D)
            a2, sh2 = rows2[c]
            s.op("dve", STT(xm2[:], xt[:], sc, a2[:], ALU.mult, ALU.mult), reads=[xtok, tk, "a2row%d" % c], writes=["xm2"])
            s.op("dve", TT(xm2[:], xm2[:], sh2[:], ALU.add), reads=["xm2", "sh2row%d" % c], writes=["xm2"])
            xb_ = xm2b[i % 2]
            s.op("act", lambda e, xb_=xb_: e.copy(out=xb_[:], in_=xm2[:]), reads=["xm2"], writes=["xm2b%d" % (i % 2)])
            s.dma("sp", lambda e, xb_=xb_, i=i: e.dma_start(out=XM2.ap()[i * 128:(i + 1) * 128, :], in_=xb_[:]),
                  reads=["xm2b%d" % (i % 2)], writes=["XM2_%d" % i])
            transposes([xm2[:, kt * 128:(kt + 1) * 128] for kt in range(KT)], lambda g, m: xm2T[:, g:g + m, :], ["xm2"],
                       lambda g, m: ["xm2T"], evac_eng="dve", f32=True)
            acc_mm(psS[0][:, 0:E], [(xm2T[:, kt, :], rw[:, kt, :]) for kt in range(KT)], ["xm2T", "rw"], ["psS0"])
            s.op("dve", TT(lgt[:], psS[0][:, 0:E], rbrow[:], ALU.add), reads=["psS0", "rbrow"], writes=["lgt"])
            if debug:
                dump("logits%d" % i, lgt[:], ["lgt"])
            s.op("dve", lambda e: e.max(out=top8[:], in_=lgt[:]), reads=["lgt"], writes=["top8"])
            s.op("dve", TS(Mf[:, i, :], lgt[:], top8[:, 3:4], None, ALU.is_ge), reads=["lgt", "top8"], writes=["Mf"])
            s.op("dve", TS(top8[:, 7:8], top8[:, 0:1], -1.0, None, ALU.mult), reads=["top8"], writes=["top8"])
            s.op("act", ACT(pex[:], lgt[:], AF.Exp, bias=top8[:, 7:8]), reads=["lgt", "top8"], writes=["pex"])
            s.op("dve", TT(pex[:], pex[:], Mf[:, i, :], ALU.mult), reads=["pex", "Mf"], writes=["pex"])
            s.op("dve", lambda e: e.reduce_sum(out=top8[:, 6:7], in_=pex[:], axis=AX.X), reads=["pex"], writes=["top8"])
            s.op("dve", lambda e: e.reciprocal(out=top8[:, 6:7], in_=top8[:, 6:7]), reads=["top8"], writes=["top8"])
            s.op("dve", TS(Wd[:, i, :], pex[:], top8[:, 6:7], None, ALU.mult), reads=["pex", "top8"], writes=["Wd"])
            s.op("dve", CP(Mb[:, i, :], Mf[:, i, :]), reads=["Mf"], writes=["Mb"])
        s.barrier()
    A.release(mP)

    if cfg.get("STOP") == "D":
        s.finish(); nc._cfg = cfg; nc._ninst = s.ninst; nc._peak = 0; nc._dbg = dbg_outs
        return nc
    destk = A.alloc("destk", [128, NT, TOPK], F32)
    destki = A.alloc("destki", [128, NT, TOPK], I32)
    wk = A.alloc("wk", [128, NT, TOPK], F32)
    SEGCAP = cfg.get("SEGCAP", 2048)
    SEGG, SEGD = min(SEGCAP, KT * CWG), min(SEGCAP, KT * CWD)
    AG, AD = KT * CWG // SEGG, KT * CWD // SEGD
    idxg = A.alloc("idxg", [128, NCJG, AG, NPAIR], I32)
    idxd = A.alloc("idxd", [128, NCJD, AD, NPAIR], I32)
    beig = A.alloc("beig", [128, NCJG, NPAIR], I32)
    beid = A.alloc("beid", [128, NCJD, NPAIR], I32)
    mE2 = A.mark()
    rank = A.alloc("rank", [128, NT, E], F32)
    dest = A.alloc("dest", [128, NT, E], F32)
    csel = A.alloc("csel", [128, NT, E], F32)
    oh = A.alloc("oh", [128, NT, E], F32)
    tmpE = A.alloc("tmpE", [128, NT, E], F32)
    cnt = A.alloc("cnt", [128, E], F32)
    NT2 = NT // 2
    cmpc = A.alloc("cmpc", [128, E, NT2], F32)
    padf = A.alloc("padf", [128, E], F32)
    pend = A.alloc("pend", [128, E], F32)
    pstart = A.alloc("pstart", [128, E], F32)
    tokidi = A.alloc("tokidi", [128, NT], I32)
    pe_ = A.alloc("pe", [128, NPAIR], F32)
    tpe = A.alloc("tpe", [128, NPAIR], F32)
    cmpb = A.alloc("cmpb", [128, NPAIR, E], F32)
    rtinit = A.alloc("rtinit", [128, 128], I32)
    tokrep = A.alloc("tokrep", [128, NT, 128], I32)

    for i in range(NT):
        pairs = [(onesb[:], Mb[:, j, :]) for j in range(i)] + [(slfb[:], Mb[:, i, :])]
        acc_mm(psS[i % 2][:, 0:E], pairs, ["onesb", "slfb", "Mb"], ["psS%d" % (i % 2)])
        s.op("dve", CP(rank[:, i, :], psS[i % 2][:, 0:E]), reads=["psS%d" % (i % 2)], writes=["rank"])
    acc_mm(psS[0][:, 0:E], [(onesb[:], Mb[:, j, :]) for j in range(NT)], ["onesb", "Mb"], ["psS0"])
    s.op("dve", CP(cnt[:], psS[0][:, 0:E]), reads=["psS0"], writes=["cnt"])
    pbo, _ = coff["pbase"]
    s.op("dve", TT(cmpc[:], cnt[:].unsqueeze(2).to_broadcast([128, E, NT2]), cst[:, pbo:pbo + NT2].unsqueeze(1).to_broadcast([128, E, NT2]), ALU.is_gt),
         reads=["cnt", "cst"], writes=["cmpc"])
    s.op("dve", lambda e: e.tensor_reduce(out=padf[:], in_=cmpc[:], axis=AX.X, op=ALU.add), reads=["cmpc"], writes=["padf"])
    s.op("dve", TS(padf[:], padf[:], 256.0, None, ALU.mult), reads=["padf"], writes=["padf"])
    oro, _ = coff["onesrow"]
    s.op("dve", lambda e: e.tensor_tensor_scan(out=pend[:], data0=cst[:, oro:oro + E], data1=padf[:], initial=0.0, op0=ALU.mult, op1=ALU.add),
         reads=["padf", "cst"], writes=["pend"])
    s.op("dve", TT(pstart[:], pend[:], padf[:], ALU.subtract), reads=["pend", "padf"], writes=["pstart"])
    for i in range(NT):
        s.op("dve", TT(dest[:, i, :], rank[:, i, :], pstart[:], ALU.add), reads=["rank", "pstart"], writes=["dest"])
        s.op("dve", lambda e, i=i: e.tensor_tensor_scan(out=csel[:, i, :], data0=cst[:, oro:oro + E], data1=Mf[:, i, :], initial=0.0,
                                                          op0=ALU.mult, op1=ALU.add), reads=["Mf", "cst"], writes=["csel"])
    for k in range(TOPK):
        s.op("dve", STT(oh[:], csel[:], float(k + 1), Mf[:], ALU.is_equal, ALU.mult), reads=["csel", "Mf"], writes=["oh"])
        s.op("dve", TT(tmpE[:], oh[:], dest[:], ALU.mult), reads=["oh", "dest"], writes=["tmpE"])
        s.op("dve", lambda e, k=k: e.tensor_reduce(out=destk[:, :, k], in_=tmpE[:], axis=AX.X, op=ALU.add), reads=["tmpE"], writes=["destk"])
        s.op("dve", TT(tmpE[:], oh[:], Wd[:], ALU.mult), reads=["oh", "Wd", "destk"], writes=["tmpE"])
        s.op("dve", lambda e, k=k: e.tensor_reduce(out=wk[:, :, k], in_=tmpE[:], axis=AX.X, op=ALU.add), reads=["tmpE"], writes=["wk"])
    s.op("dve", CP(destki[:], destk[:]), reads=["destk"], writes=["destki"])
    s.op("dve", CP(tokidi[:], C("tokid")), reads=["cst"], writes=["tokidi"])
    s.op("dve", TT(cmpb[:], pend[:].unsqueeze(1).to_broadcast([128, NPAIR, E]), cst[:, pbo:pbo + NPAIR].unsqueeze(2).to_broadcast([128, NPAIR, E]), ALU.is_le),
         reads=["pend", "cst"], writes=["cmpb"])
    s.op("dve", lambda e: e.tensor_reduce(out=pe_[:], in_=cmpb[:], axis=AX.X, op=ALU.add), reads=["cmpb"], writes=["pe"])
    cpo, _ = coff["cp"]
    tpe2 = A.alloc("tpe2", [128, NPAIR], F32)
    for (ncj, na, idx_t, be_t) in ((NCJG, AG, idxg, beig), (NCJD, AD, idxd, beid)):
        for cj in range(ncj):
            s.op("dve", TS(tpe[:], pe_[:], float(ncj), float(cj), ALU.mult, ALU.add), reads=["pe", "idxtabs"], writes=["tpe"])
            s.op("dve", CP(be_t[:, cj, :], tpe[:]), reads=["tpe"], writes=["idxtabs"])
            s.op("dve", TS(tpe[:], tpe[:], 128.0, cst[:, cpo:cpo + 1], ALU.mult, ALU.add), reads=["tpe", "cst", "idxtabs"], writes=["tpe"])
            for a_ in range(na):
                s.op("dve", TS(tpe2[:], tpe[:], float(na), float(a_), ALU.mult, ALU.add), reads=["tpe", "idxtabs"], writes=["tpe2"])
                s.op("dve", CP(idx_t[:, cj, a_, :], tpe2[:]), reads=["tpe2"], writes=["idxtabs"])
    s.op("dve", lambda e: e.memset(rtinit[:], T), writes=["rtinit"])
    for b in range(NB):
        s.dma("sp", lambda e, b=b: e.dma_start(out=ROWTOK.ap()[b * 128:(b + 1) * 128, :], in_=rtinit[:]), reads=["rtinit"], writes=["ROWTOK%d" % b])
    for i in range(NT):
        s.op("dve", CP(tokrep[:, i, :], tokidi[:, i:i + 1].to_broadcast([128, 128])), reads=["tokidi"], writes=["tokrep"])
    rt_toks = []
    for i in range(NT):
        for k in range(TOPK):
            tk = "RT_%d_%d" % (i, k)
            rt_toks.append(tk)
            s.dma("pool", lambda e, i=i, k=k: e.indirect_dma_start(
                out=ROWTOK.ap(), out_offset=bass.IndirectOffsetOnAxis(ap=destki[:, i, k:k + 1], axis=0),
                in_=tokrep[:, i, :], in_offset=None, bounds_check=BC(e, R - 1), oob_is_err=False),
                reads=["ROWTOK%d" % b for b in range(NB)] + ["destki", "tokrep"], writes=[tk])
    if debug:
        dump("destk", destk[:].rearrange("p t k -> p (t k)"), ["destk"])
        dump("wk", wk[:].rearrange("p t k -> p (t k)"), ["wk"])
    s.barrier()
    if cfg.get("STOP") == "E":
        s.finish(); nc._cfg = cfg; nc._ninst = s.ninst; nc._peak = 0; nc._dbg = dbg_outs
        return nc
    A.release(mE2)
    mF = A.mark()
    FT = DFF // 128
    CWM = max(CWG, CWD)
    NSLOT = 3
    mslots = [A.alloc("mslot%d" % i, [128, KT * CWM], BF16) for i in range(NSLOT)]
    mctr = [0]
    rtb = [A.alloc("rtb%d" % i, [128, 16], I32) for i in range(2)]
    xg = [A.alloc("xg%d" % i, [128, D], BF16) for i in range(2)]
    xgT = [A.alloc("xgT%d" % i, [128, KT, 128], BF16) for i in range(2)]
    bch = [A.alloc("bch%d" % i, [128, CWM], BF16) for i in range(2)]
    bctr = [0]
    PWG = min(512, CWG)
    PWD = min(512, CWD)
    hb = [A.alloc("hb%d" % i, [128, PWG], F32) for i in range(2)]
    gt = A.alloc("gt", [128, PWG // 2], F32)
    ut = A.alloc("ut", [128, PWG // 2], F32)
    sgt = A.alloc("sgt", [128, PWG // 2], F32)
    actb = [A.alloc("actb%d" % i, [128, DFF], BF16) for i in range(2)]
    actT = [A.alloc("actT%d" % i, [128, FT, 128], BF16) for i in range(2)]
    yp = [A.alloc("yp%d" % i, [128, CWD], F32) for i in range(2)]
    ypc = [0]
    for i in range(2):
        s.op("dve", lambda e, i=i: e.memset(xg[i][:], 0.0), writes=["xg%d" % i])
    for i in range(NSLOT):
        s.op("dve", lambda e, i=i: e.memset(mslots[i][:], 0.0), writes=["mslot%d_%d" % (i, a_) for a_ in range(max(AG, AD))])

    def mload(src_d, idx_tab, cj, g, nrows, ncols, seg, na):
        i = mctr[0] % NSLOT
        mctr[0] += 1
        slot = mslots[i]
        tok = "mslot%d" % i
        n = KT * ncols
        srcv = src_d.ap().rearrange("r (a s) -> (r a) s", s=seg)
        toks = []
        for a_ in range(na):
            s.dma("pool", lambda e, a_=a_: e.indirect_dma_start(
                out=slot[:, a_ * seg:(a_ + 1) * seg], out_offset=None, in_=srcv,
                in_offset=bass.IndirectOffsetOnAxis(ap=idx_tab[:, cj, a_, g:g + 1], axis=0),
                bounds_check=BC(e, nrows * na - 1), oob_is_err=False), reads=["idxtabs"], writes=[tok + "_%d" % a_])
            toks.append(tok + "_%d" % a_)
        return slot[:, 0:n].rearrange("p (k c) -> p k c", c=ncols), toks

    def bload(src_d, idx_ap, nrows, ncols):
        i = bctr[0] % 2
        bctr[0] += 1
        bt = bch[i]
        tok = "bch%d" % i
        s.dma("pool", lambda e: e.indirect_dma_start(
            out=bt[:, 0:ncols], out_offset=None, in_=src_d.ap(), in_offset=bass.IndirectOffsetOnAxis(ap=idx_ap, axis=0),
            bounds_check=BC(e, nrows - 1), oob_is_err=False), reads=["idxtabs"], writes=[tok])
        return bt, tok

    for g in range(NPAIR):
        for hf in range(2):
            b = 2 * g + hf
            s.dma("sp", lambda e, b=b, hf=hf: e.dma_start(out=rtb[hf][:], in_=ROWTOK.ap()[b * 128:(b + 1) * 128, 0:16]),
                  reads=rt_toks + ["ROWTOK%d" % b], writes=["rtb%d" % hf])
            s.dma("pool", lambda e, hf=hf: e.indirect_dma_start(
                out=xg[hf][:], out_offset=None, in_=XM2.ap(), in_offset=bass.IndirectOffsetOnAxis(ap=rtb[hf][:, 0:1], axis=0),
                bounds_check=BC(e, T - 1), oob_is_err=False), reads=["rtb%d" % hf] + ["XM2_%d" % i for i in range(NT)], writes=["xg%d" % hf])
            transposes([xg[hf][:, kt * 128:(kt + 1) * 128] for kt in range(KT)], lambda g_, m, hf=hf: xgT[hf][:, g_:g_ + m, :], ["xg%d" % hf],
                       lambda g_, m, hf=hf: ["xgT%d" % hf])
        for cj in range(NCJG):
            slot, stoks = mload(wgu_d, idxg, cj, g, E * NCJG * 128, CWG, SEGG, AG)
            bt, btok = bload(bgu_d, beig[:, cj, g:g + 1], E * NCJG, CWG)
            npc = CWG // PWG
            for hf in range(2):
                for pc in range(npc):
                    bank = (hf * npc + pc) % 4
                    acc_mm(psA[bank][:, 0:PWG], [(xgT[hf][:, kt, :], slot[:, kt, pc * PWG:(pc + 1) * PWG]) for kt in range(KT)],
                           ["xgT%d" % hf] + stoks, ["psA%d" % bank])
                    hbb = hb[pc % 2]
                    htok = "hb%d" % (pc % 2)
                    HW_ = PWG // 2
                    s.op("dve", TT(hbb[:], psA[bank][:, 0:PWG], bt[:, pc * PWG:(pc + 1) * PWG], ALU.add), reads=["psA%d" % bank, btok], writes=[htok])
                    hv = hbb[:].rearrange("p (f two) -> p f two", two=2)
                    s.op("dve", TS(gt[:], hv[:, :, 0], 7.0, None, ALU.min), reads=[htok], writes=["gt"])
                    s.op("dve", TS(ut[:], hv[:, :, 1], -7.0, 7.0, ALU.max, ALU.min), reads=[htok], writes=["ut"])
                    s.op("act", ACT(sgt[:], gt[:], AF.Sigmoid, scale=1.702), reads=["gt"], writes=["sgt"])
                    s.op("dve", TT(gt[:], gt[:], sgt[:], ALU.mult), reads=["gt", "sgt"], writes=["gt"])
                    f0 = (cj * CWG + pc * PWG) // 2
                    s.op("dve", STT(actb[hf][:, f0:f0 + HW_], ut[:], 1.0, gt[:], ALU.add, ALU.mult), reads=["ut", "gt"], writes=["actb%d" % hf])
        for hf in range(2):
            transposes([actb[hf][:, ft * 128:(ft + 1) * 128] for ft in range(FT)], lambda g_, m, hf=hf: actT[hf][:, g_:g_ + m, :], ["actb%d" % hf],
                       lambda g_, m, hf=hf: ["actT%d" % hf])
        for cj in range(NCJD):
            slot, stoks = mload(wdn_d, idxd, cj, g, E * NCJD * 128, CWD, SEGD, AD)
            bt, btok = bload(bdn_d, beid[:, cj, g:g + 1], E * NCJD, CWD)
            npc = CWD // PWD
            for hf in range(2):
                b = 2 * g + hf
                ypb = yp[ypc[0] % 2]
                ytok = "yp%d" % (ypc[0] % 2)
                ypc[0] += 1
                for pc in range(npc):
                    bank = (hf * npc + pc) % 4
                    acc_mm(psA[bank][:, 0:PWD], [(actT[hf][:, kt, :], slot[:, kt, pc * PWD:(pc + 1) * PWD]) for kt in range(KT)],
                           ["actT%d" % hf] + stoks, ["psA%d" % bank])
                    s.op("dve", TT(ypb[:, pc * PWD:(pc + 1) * PWD], psA[bank][:, 0:PWD], bt[:, pc * PWD:(pc + 1) * PWD], ALU.add),
                         reads=["psA%d" % bank, btok], writes=[ytok])
                s.dma("sp", lambda e, ypb=ypb, b=b, cj=cj: e.dma_start(out=Y.ap()[b * 128:(b + 1) * 128, cj * CWD:(cj + 1) * CWD], in_=ypb[:]),
                      reads=[ytok], writes=["Y_%d_%d" % (b, cj)])
    s.barrier()
    A.release(mF)

    if cfg.get("STOP") == "F":
        s.finish(); nc._cfg = cfg; nc._ninst = s.ninst; nc._peak = 0; nc._dbg = dbg_outs
        return nc
    y_toks = ["Y_%d_%d" % (b, cj) for b in range(NB) for cj in range(NCJD)]
    yk = [A.alloc("yk%d" % k, [128, D], F32) for k in range(TOPK)]
    acc = A.alloc("acc", [128, D], F32)
    x1g = A.alloc("x1g", [128, D], F32)
    g2row = {}
    for c in range(2):
        g2row[c] = A.alloc("g2row%d" % c, [128, D], F32)
        load_row(g2row[c][:], c, 5, "g2row%d" % c)
    nfrow = A.alloc("nfrow", [128, D], F32)
    load_vec_row(nfrow[:], nfg_d, "nfrow")
    junk = A.alloc("junk3", [128, D], BF16)
    yout = [A.alloc("yout%d" % i, [128, D], F32) for i in range(2)]
    for k in range(TOPK):
        s.op("dve", lambda e, k=k: e.memset(yk[k][:], 0.0), writes=["yk%d" % k])
    for i in range(NT):
        c = tile_cond[i]
        for k in range(TOPK):
            s.dma("pool", lambda e, i=i, k=k: e.indirect_dma_start(
                out=yk[k][:], out_offset=None, in_=Y.ap(), in_offset=bass.IndirectOffsetOnAxis(ap=destki[:, i, k:k + 1], axis=0),
                bounds_check=BC(e, R - 1), oob_is_err=False), reads=y_toks + ["destki"], writes=["yk%d" % k])
        s.dma("sp", lambda e, i=i: e.dma_start(out=x1g[:], in_=X1.ap()[i * 128:(i + 1) * 128, :]), writes=["x1g"])
        s.op("dve", TS(acc[:], yk[0][:], wk[:, i, 0:1], None, ALU.mult), reads=["yk0", "wk"], writes=["acc"])
        for k in range(1, TOPK):
            s.op("dve", STT(acc[:], yk[k][:], wk[:, i, k:k + 1], acc[:], ALU.mult, ALU.add), reads=["yk%d" % k, "wk", "acc"], writes=["acc"])
        s.op("dve", TT(acc[:], acc[:], g2row[c][:], ALU.mult), reads=["acc", "g2row%d" % c], writes=["acc"])
        s.op("dve", TT(acc[:], acc[:], x1g[:], ALU.add), reads=["acc", "x1g"], writes=["acc"])
        sc, tk = rms_rstd(acc[:], "acc", 4 + i % 2, D)
        yo = yout[i % 2]
        s.op("dve", STT(yo[:], acc[:], sc, nfrow[:], ALU.mult, ALU.mult), reads=["acc", tk, "nfrow"], writes=["yout%d" % (i % 2)])
        s.dma("sp", lambda e, yo=yo, i=i: e.dma_start(out=y_o.ap()[i * 128:(i + 1) * 128, :], in_=yo[:]), reads=["yout%d" % (i % 2)], writes=["y_%d" % i])
    s.finish()
    nc._cfg = cfg
    nc._ninst = s.ninst
    nc._peak = A.peak - A.base
    nc._dbg = dbg_outs
    return nc


def prepare_core_inputs(inp, cfg, core):
    cfg = derive(cfg)
    D, H, KT, NP = cfg["D"], cfg["H"], cfg["KT"], cfg["NP"]
    f = np.float32
    xp = np.asarray(inp["x_prompt"], f)
    xs = np.asarray(inp["x_sample"], f)
    x = np.concatenate([xp[core * NP + p] for p in range(NP)] + [xs[core]], axis=0)
    c_ctx = np.asarray(inp["c_ctx"], f)
    c = np.asarray(inp["c"], f)[core]
    cT = np.stack([c_ctx.reshape(KT, 128).T, c.reshape(KT, 128).T], axis=-1)

    def fm(v, n):
        return np.ascontiguousarray(np.asarray(v, f).reshape(n, 128).T)

    lbf = np.stack([fm(inp["hg_lb_fwd"][0], H), fm(inp["hg_lb_fwd"][1], H)], axis=1)
    lbb = np.stack([fm(inp["hg_lb_bwd"][0], H), fm(inp["hg_lb_bwd"][1], H)], axis=1)
    m = {
        "x": np.ascontiguousarray(x),
        "shf": np.ascontiguousarray(np.asarray(inp["state_hgrn_fwd"], f)[core, 0]),
        "shb": np.ascontiguousarray(np.asarray(inp["state_hgrn_bwd"], f)[core, 0]),
        "srf": np.ascontiguousarray(np.asarray(inp["state_ret_fwd"], f)[core, 0]),
        "srb": np.ascontiguousarray(np.asarray(inp["state_ret_bwd"], f)[core, 0]),
        "cT": np.ascontiguousarray(cT),
        "lbf": np.ascontiguousarray(lbf), "lbb": np.ascontiguousarray(lbb),
    }
    return m


def prepare_shared_inputs(inp, cfg):
    cfg = derive(cfg)
    D, H, KT, E = cfg["D"], cfg["H"], cfg["KT"], cfg["E"]
    f = np.float32
    perm = w_in_perm_index(cfg)
    sh = {
        "ada_w": np.ascontiguousarray(np.asarray(inp["ada_w"], f)[0]),
        "ada_b2": np.ascontiguousarray(np.broadcast_to(np.asarray(inp["ada_b"], f)[0][None, :], (2, 6 * D))),
        "n1g": np.asarray(inp["norm1_g"], f)[0][None, :].copy(),
        "n2g": np.asarray(inp["norm2_g"], f)[0][None, :].copy(),
        "nfg": np.asarray(inp["final_norm_g"], f)[None, :].copy(),
        "w_in": np.ascontiguousarray(np.asarray(inp["w_in"], f)[0][:, perm]),
        "hgng": np.asarray(inp["hg_norm_g"], f)[0].reshape(128, 1).copy(),
        "retg": np.ascontiguousarray(np.asarray(inp["ret_norm_g"], f)[0].reshape(2, 128).T),
        "rl2f": np.asarray(inp["ret_log2_fwd"], f)[0][None, :].copy(),
        "rl2b": np.asarray(inp["ret_log2_bwd"], f)[0][None, :].copy(),
        "w_pa": np.ascontiguousarray(np.asarray(inp["w_proj_hgrn"], f)[0]),
        "w_pb": np.ascontiguousarray(np.asarray(inp["w_proj_ret"], f)[0]),
        "w_out": np.ascontiguousarray(np.asarray(inp["w_out"], f)[0]),
        "rw": np.ascontiguousarray(np.asarray(inp["router_w"], f)[0]),
        "rb": np.asarray(inp["router_b"], f)[0][None, :].copy(),
        "w_gu": np.ascontiguousarray(np.asarray(inp["moe_w_gu"], f)[0].reshape(E, KT, 128, cfg["NCJG"], cfg["CWG"]).transpose(0, 3, 2, 1, 4)).reshape(E * cfg["NCJG"] * 128, KT * cfg["CWG"]),
        "b_gu": np.ascontiguousarray(np.asarray(inp["moe_b_gu"], f)[0]).reshape(E * cfg["NCJG"], cfg["CWG"]),
        "w_dn": np.ascontiguousarray(np.asarray(inp["moe_w_dn"], f)[0].reshape(E, KT, 128, cfg["NCJD"], cfg["CWD"]).transpose(0, 3, 2, 1, 4)).reshape(E * cfg["NCJD"] * 128, KT * cfg["CWD"]),
        "b_dn": np.ascontiguousarray(np.asarray(inp["moe_b_dn"], f)[0]).reshape(E * cfg["NCJD"], cfg["CWD"]),
        "cst": make_consts(cfg)[0],
        "rope": make_consts(cfg)[1],
    }
    return sh


def run(inp, cfg, runner=None, debug=False):
    cfgd = derive(cfg)
    ncores = cfgd["NCORES"]
    nc = build(cfg, debug=debug)
    shared = prepare_shared_inputs(inp, cfg)
    in_maps = []
    for core in range(ncores):
        m = dict(shared)
        m.update(prepare_core_inputs(inp, cfg, core))
        in_maps.append(m)
    if runner is None:
        res = run_bass_kernel_spmd(nc, in_maps, core_ids=list(range(ncores))).results
    else:
        res = runner(nc, in_maps)
    NP, TP, TS_, D, H = cfgd["NP"], cfgd["TP"], cfgd["TS"], cfgd["D"], cfgd["H"]
    yp = np.stack([res[c]["y"][p * TP:(p + 1) * TP] for c in range(ncores) for p in range(NP)], axis=0)
    ys = np.stack([res[c]["y"][NP * TP:] for c in range(ncores)], axis=0)
    hf = np.concatenate([res[c]["hf"] for c in range(ncores)], axis=0)[:, None]
    hb = np.concatenate([res[c]["hb"] for c in range(ncores)], axis=0)[:, None]
    rf = np.concatenate([res[c]["rf"] for c in range(ncores)], axis=0)[:, None]
    rb = np.concatenate([res[c]["rb_o"] for c in range(ncores)], axis=0)[:, None]
    outs = tuple(np.ascontiguousarray(a.astype(np.float32)) for a in (yp, ys, hf, hb, rf, rb))
    return outs, res, nc


def kernel(**inputs):
    outs, _, _ = run(inputs, FULL_CFG)
    return outs
```

```python
import math
from contextlib import ExitStack
import numpy as np
import ml_dtypes
import concourse.bass as bass
import concourse.mybir as mybir
from concourse.bass_utils import run_bass_kernel_spmd

F32 = mybir.dt.float32
BF16 = mybir.dt.bfloat16
I32 = mybir.dt.int32
AF = mybir.ActivationFunctionType
ALU = mybir.AluOpType
AX = mybir.AxisListType
EPS = 1e-6
CHUNK = 32
GRID_W = 64
ROPE_PAIRS = 32
TOPK = 4

FULL_CFG = dict(D=2048, H=8, TP=256, NP=2, TS=1024, E=32, NCORES=8)


class Sched:
    CE = ("pe", "act", "dve", "pool")

    def __init__(self, nc, nring=8):
        self.nc = nc
        self.q = {k: [] for k in ("pe", "act", "dve", "pool", "sp")}
        self.psem = {k: nc.alloc_semaphore(name="ps_" + k) for k in self.CE}
        self.pcnt = {k: 0 for k in self.CE}
        self.waited = {}
        self.ring = {}
        for qn in ("sp", "pool"):
            self.ring[qn] = dict(sems=[nc.alloc_semaphore(name="d_%s_%d" % (qn, i)) for i in range(nring)],
                                 vals=[0] * nring, nxt=0)
        self.lw = {}
        self.rd = {}
        self.ninst = 0
        self.dummy = None
        self.dummy_ctr = 0

    def _wait(self, engn, ev):
        if ev is None:
            return
        sem, val = ev
        key = (engn, id(sem))
        if self.waited.get(key, 0) >= val:
            return
        self.waited[key] = val
        self.q[engn].append(lambda e, sem=sem, val=val: e.wait_ge(sem, val))
        self.ninst += 1

    def _deps(self, engn, reads, writes):
        for t in reads:
            self._wait(engn, self.lw.get(t))
        for t in writes:
            self._wait(engn, self.lw.get(t))
            for ev in self.rd.get(t, {}).values():
                self._wait(engn, ev)

    def _commit(self, ev, reads, writes):
        sem, val = ev
        for t in writes:
            self.lw[t] = ev
            self.rd[t] = {}
        for t in reads:
            d = self.rd.setdefault(t, {})
            old = d.get(id(sem))
            if old is None or old[1] < val:
                d[id(sem)] = ev

    @staticmethod
    def _excl(reads, writes):
        pr = [t for t in reads if t.startswith("ps")]
        if not pr:
            return list(reads), list(writes)
        return [t for t in reads if not t.startswith("ps")], list(writes) + [t for t in pr if t not in writes]

    def op(self, engn, fn, reads=(), writes=(), inc=True):
        reads, writes = self._excl(reads, writes)
        self._deps(engn, reads, writes)
        self.ninst += 1
        if inc:
            self.pcnt[engn] += 1
            sem = self.psem[engn]
            val = self.pcnt[engn]
            self.q[engn].append(lambda e, fn=fn, sem=sem: fn(e).then_inc(sem, 1))
            ev = (sem, val)
            self._commit(ev, reads, writes)
            return ev
        self.q[engn].append(lambda e, fn=fn: fn(e))
        return None

    def dma(self, qn, fn, reads=(), writes=()):
        r = self.ring[qn]
        i = r["nxt"]
        r["nxt"] = (i + 1) % len(r["sems"])
        sem = r["sems"][i]
        if r["vals"][i] > 0:
            self._wait(qn, (sem, r["vals"][i]))
        self._deps(qn, reads, writes)
        r["vals"][i] += 16
        val = r["vals"][i]
        self.q[qn].append(lambda e, fn=fn, sem=sem: fn(e).then_inc(sem, 16))
        self.ninst += 1
        ev = (sem, val)
        self._commit(ev, reads, writes)
        return ev

    def group_begin(self, flag_ap):
        self.waited.clear()
        self._grp = dict(flag=flag_ap, pcnt=dict(self.pcnt), rings={qn: list(r["vals"]) for qn, r in self.ring.items()},
                         qlen={qn: len(q) for qn, q in self.q.items()})
        for qn in self.q:
            self.q[qn].append(("IF", flag_ap))

    def group_end(self, else_dmas=None):
        g = self._grp
        else_dmas = else_dmas or {}
        for qn in self.q:
            comp = []
            if qn in self.CE and self.pcnt[qn] > g["pcnt"][qn]:
                comp.append(("drain_inc", self.psem[qn], self.pcnt[qn] - g["pcnt"][qn]))
            if qn in self.ring:
                r = self.ring[qn]
                for sem, v0, v1 in zip(r["sems"], g["rings"][qn], r["vals"]):
                    d = v1 - v0
                    first = True
                    while d > 0:
                        k = min(32, d)
                        comp.append(("wait_inc", sem, v0 if first else 0, k))
                        first = False
                        d -= k
            self.q[qn].append(("ELSE_END", comp, list(else_dmas.get(qn, []))))
        self.waited.clear()
        self._grp = None

    def _replay(self, qn, e):
        reg = None
        ctx = None
        for t in self.q[qn]:
            if isinstance(t, tuple):
                if t[0] == "IF":
                    if reg is None:
                        reg = e.alloc_register("flag_" + qn)
                    e.reg_load(reg, t[1])
                    ctx = e.If_eq(reg, 1)
                    ctx.__enter__()
                else:
                    ctx.__exit__(None, None, None)
                    ctx = None
                    if t[1]:
                        c2 = e.Else()
                        c2.__enter__()
                        useful = list(t[2])
                        for c in t[1]:
                            if c[0] == "drain_inc":
                                e.drain().then_inc(c[1], c[2])
                            else:
                                if c[2] > 0:
                                    e.wait_ge(c[1], c[2])
                                if useful:
                                    o_, i_ = useful.pop(0)
                                    e.dma_start(out=o_, in_=i_).then_inc(c[1], c[3])
                                else:
                                    k = self.dummy_ctr
                                    self.dummy_ctr += 1
                                    e.dma_start(out=self.dummy[1][k:k + 1, :], in_=self.dummy[0]).then_inc(c[1], c[3])
                        assert not useful, "not enough compensation DMAs for the else-branch fills"
                        c2.__exit__(None, None, None)
            else:
                t(e)

    def barrier(self):
        evs = [(self.psem[k], self.pcnt[k]) for k in self.CE if self.pcnt[k] > 0]
        for r in self.ring.values():
            evs += [(sem, v) for sem, v in zip(r["sems"], r["vals"]) if v > 0]
        for qn in self.q:
            for ev in evs:
                self._wait(qn, ev)

    def finish(self):
        self.barrier()
        with self.nc.Block() as block:
            @block.tensor
            def _(e):
                self._replay("pe", e)

            @block.scalar
            def _(e):
                self._replay("act", e)

            @block.vector
            def _(e):
                self._replay("dve", e)

            @block.gpsimd
            def _(e):
                self._replay("pool", e)

            @block.sync
            def _(e):
                self._replay("sp", e)


class _Stop(Exception):
    pass


class Arena:
    def __init__(self, nc):
        self.nc = nc
        self.base = (nc.sbuf_base + 31) // 32 * 32
        self.top = nc.sbuf_top // 32 * 32
        self.cur = self.base
        self.n = 0
        self.peak = self.cur

    def alloc(self, name, shape, dtype):
        sz = {F32: 4, BF16: 2, I32: 4}[dtype]
        nbytes = int(np.prod(shape[1:])) * sz
        nbytes = (nbytes + 31) // 32 * 32
        assert self.cur + nbytes <= self.top, "SBUF overflow at %s: need %d have %d" % (name, nbytes, self.top - self.cur)
        self.n += 1
        t = self.nc.alloc_sbuf_tensor_at("%s_%d" % (name, self.n), list(shape), dtype, offset=self.cur)
        self.cur += nbytes
        self.peak = max(self.peak, self.cur)
        return t

    def mark(self):
        return self.cur

    def release(self, m):
        self.cur = m


def TS(out, in0, s1, s2, op0, op1=None):
    if op1 is None:
        return lambda e: e.tensor_scalar(out=out, in0=in0, scalar1=s1, scalar2=None, op0=op0)
    return lambda e: e.tensor_scalar(out=out, in0=in0, scalar1=s1, scalar2=s2, op0=op0, op1=op1)


def TT(out, a, b, op):
    return lambda e: e.tensor_tensor(out=out, in0=a, in1=b, op=op)


def STT(out, in0, sc, in1, op0, op1):
    return lambda e: e.scalar_tensor_tensor(out=out, in0=in0, scalar=sc, in1=in1, op0=op0, op1=op1)


def ACT(out, in_, func, bias=None, scale=None, accum=None):
    kw = {}
    if bias is not None:
        kw["bias"] = bias
    if scale is not None:
        kw["scale"] = scale
    if accum is not None:
        kw["accum_out"] = accum
    return lambda e: e.activation(out=out, in_=in_, func=func, **kw)


def CP(out, in_):
    return lambda e: e.tensor_copy(out=out, in_=in_)


def const_layout(cfg):
    T, TS_, E, KT, NB = cfg["T"], cfg["TS"], cfg["E"], cfg["KT"], cfg["NB"]
    NT = T // 128
    names = [("ident", 128), ("mf", 128), ("mb", 128), ("slf", 128), ("slb", 128), ("i2", 128),
             ("dpos", 128), ("dneg", 128), ("iota1", 128), ("iotar", 128), ("ones", 128), ("psw", 128),
             ("c127", 1), ("cp", 1), ("cmask", 4), ("reset", cfg["CH"]), ("iotae", E),
             ("tokid", NT), ("iotaw", KT), ("bbase", NB), ("pbase", cfg["NPAIR"]), ("onesrow", max(E, 8))]
    off = {}
    c = 0
    for n, w in names:
        off[n] = (c, w)
        c += w
    return off, c


def make_consts(cfg):
    off, ncol = const_layout(cfg)
    T, TS_, E, KT, NB = cfg["T"], cfg["TS"], cfg["E"], cfg["KT"], cfg["NB"]
    NT = T // 128
    C = np.zeros((128, ncol), np.float32)

    def put(n, a):
        o, w = off[n]
        C[:, o:o + w] = a

    s = np.arange(128)[:, None]
    t = np.arange(128)[None, :]
    same = (s // CHUNK) == (t // CHUNK)
    put("ident", (s == t))
    put("mf", same & (s <= t))
    put("mb", same & (s >= t))
    put("slf", s < t)
    put("slb", s > t)
    put("i2", 2.0 * (s == t))
    put("dpos", np.maximum(t - s, 0))
    put("dneg", np.maximum(s - t, 0))
    put("iota1", np.broadcast_to(t + 1, (128, 128)))
    put("iotar", np.broadcast_to(128 - t, (128, 128)))
    put("ones", 1.0)
    d = np.arange(128)
    partner = np.where((d % 64) < 32, d + 32, d - 32)
    psw = np.zeros((128, 128), np.float32)
    psw[partner, d] = 1.0
    put("psw", psw)
    put("c127", 127 - s)
    put("cp", s)
    put("cmask", (s // CHUNK) == np.arange(4)[None, :])
    put("reset", np.broadcast_to((np.arange(cfg["CH"]) % CHUNK != 0).astype(np.float32), (128, cfg["CH"])))
    tok = np.arange(TS_)
    rows = (tok // GRID_W).astype(np.float32)
    cols = (tok % GRID_W).astype(np.float32)
    inv = (10000.0 ** (-np.arange(ROPE_PAIRS, dtype=np.float32) / ROPE_PAIRS)).astype(np.float32)
    pos = np.where((d[:, None] // 64) == 0, rows[None, :], cols[None, :]).astype(np.float32)
    ang = (pos * inv[d % 32][:, None]).astype(np.float32)
    sign = np.where((d % 64) < 32, -1.0, 1.0)[:, None]
    rope = np.concatenate([np.cos(ang), np.sin(ang) * sign], axis=1).astype(np.float32)
    put("iotae", np.broadcast_to(np.arange(E), (128, E)))
    put("tokid", np.arange(NT)[None, :] * 128 + s)
    put("iotaw", np.arange(KT)[None, :] * 128 + s)
    put("bbase", np.broadcast_to(np.arange(NB) * 128, (128, NB)))
    put("pbase", np.broadcast_to(np.arange(cfg["NPAIR"]) * 256, (128, cfg["NPAIR"])))
    put("onesrow", 1.0)
    return C, rope


def derive(cfg):
    cfg = dict(cfg)
    D, H = cfg["D"], cfg["H"]
    cfg["KT"] = D // 128
    cfg["T"] = cfg["NP"] * cfg["TP"] + cfg["TS"]
    cfg["NT"] = cfg["T"] // 128
    cfg["CH"] = min(512, cfg["T"])
    assert cfg["T"] % cfg["CH"] == 0
    cfg["NPAIR"] = cfg["T"] * TOPK // 256 + cfg["E"]
    cfg["NB"] = 2 * cfg["NPAIR"]
    cap = cfg.get("CWCAP", 1024)
    cfg["CWG"] = min(cap, 2 * cfg["D"])
    cfg["CWD"] = min(cap, cfg["D"])
    cfg["NCJG"] = 2 * cfg["D"] // cfg["CWG"]
    cfg["NCJD"] = cfg["D"] // cfg["CWD"]
    cfg["HGC"] = 640
    cfg["RTC"] = 768
    cfg["MG0"] = H * 640 + H * 768
    cfg["INC"] = cfg["MG0"] + 2 * D
    return cfg


def w_in_perm_index(cfg):
    D, H = cfg["D"], cfg["H"]
    HK = H * 128
    RW = H * 256
    o_hq, o_zf, o_zb, o_hi, o_hg = 0, HK, 2 * HK, 3 * HK, 4 * HK
    o_rq = 5 * HK
    o_rk = o_rq + HK
    o_rv = o_rk + HK
    o_rg = o_rv + RW
    o_ma = o_rg + RW
    o_mb = o_ma + D
    idx = []
    for h in range(H):
        r = np.arange(128)
        idx += [o_hq + h * 128 + r, o_zf + h * 128 + r, o_zb + h * 128 + r, o_hg + h * 128 + r, o_hi + h * 128 + r]
    for h in range(H):
        r = np.arange(128)
        r2 = np.arange(256)
        idx += [o_rq + h * 128 + r, o_rk + h * 128 + r, o_rg + h * 256 + r2, o_rv + h * 256 + r2]
    idx += [o_ma + np.arange(D), o_mb + np.arange(D)]
    return np.concatenate(idx)


def build(cfg, debug=False):
    cfg = derive(cfg)
    D, H, KT, T, NT, E, NB, CH = cfg["D"], cfg["H"], cfg["KT"], cfg["T"], cfg["NT"], cfg["E"], cfg["NB"], cfg["CH"]
    TP, NP, TS_ = cfg["TP"], cfg["NP"], cfg["TS"]
    DFF = D
    NCHK = T // CH
    TPC = CH // 128
    NCK = T // CHUNK
    R = NB * 128
    GW = min(512, D)
    NG = D // GW
    JT = GW // 128
    coff, ncst = const_layout(cfg)

    nc = bass.Bass("TRN2", target_bir_lowering=False)
    s = Sched(nc)
    A = Arena(nc)
    dbg_outs = {}

    def din(name, shape, dt=F32):
        return nc.dram_tensor(name, list(shape), dt, kind="ExternalInput")

    def dscr(name, shape, dt=F32):
        return nc.dram_tensor(name, list(shape), dt, kind="Internal")

    def dout(name, shape, dt=F32):
        return nc.dram_tensor(name, list(shape), dt, kind="ExternalOutput")

    x_d = din("x", [T, D])
    shf_d, shb_d = din("shf", [H, 128, 128]), din("shb", [H, 128, 128])
    srf_d, srb_d = din("srf", [H, 128, 256]), din("srb", [H, 128, 256])
    cT_d = din("cT", [128, KT, 2])
    adaw_d = din("ada_w", [D, 6 * D])
    adab_d = din("ada_b2", [2, 6 * D])
    n1g_d, n2g_d, nfg_d = din("n1g", [1, D]), din("n2g", [1, D]), din("nfg", [1, D])
    win_d = din("w_in", [D, cfg["INC"]])
    lbf_d, lbb_d = din("lbf", [128, 2, H]), din("lbb", [128, 2, H])
    hgng_d = din("hgng", [128, 1])
    retg_d = din("retg", [128, 2])
    rl2f_d, rl2b_d = din("rl2f", [1, H]), din("rl2b", [1, H])
    wpa_d, wpb_d, wout_d = din("w_pa", [H * 128, D]), din("w_pb", [H * 256, D]), din("w_out", [D, D])
    rw_d, rb_d = din("rw", [D, E]), din("rb", [1, E])
    CWG, CWD, NCJG, NCJD, NPAIR = cfg["CWG"], cfg["CWD"], cfg["NCJG"], cfg["NCJD"], cfg["NPAIR"]
    wgu_d, bgu_d = din("w_gu", [E * NCJG * 128, KT * CWG]), din("b_gu", [E * NCJG, CWG])
    wdn_d, bdn_d = din("w_dn", [E * NCJD * 128, KT * CWD]), din("b_dn", [E * NCJD, CWD])
    cst_d = din("cst", [128, ncst])
    rope_d = din("rope", [128, 2 * TS_])

    y_o = dout("y", [T, D])
    hf_o, hb_o = dout("hf", [NP, H, 128, 128]), dout("hb", [NP, H, 128, 128])
    rf_o, rb_o = dout("rf", [NP, H, 128, 256]), dout("rb_o", [NP, H, 128, 256])

    MOD = dscr("MOD", [2, 6 * D])
    OAB = dscr("OAB", [3 * H, 128, T], BF16)
    X1 = dscr("X1", [T, D])
    XM2 = dscr("XM2", [T, D], BF16)
    ROWTOK = dscr("ROWTOK", [R, 128], I32)
    Y = dscr("Y", [R, D])

    def dump(name, ap, reads):
        if not debug:
            return
        shp = list(ap.shape)
        o = dout("dbg_" + name, shp, ap.dtype)
        dbg_outs[name] = shp
        s.dma("sp", lambda e: e.dma_start(out=o.ap(), in_=ap), reads=reads, writes=["dbg_" + name])

    psA = [nc.alloc_psum_tensor("psA%d" % i, [128, 512], F32) for i in range(4)]
    psS = [nc.alloc_psum_tensor("psS%d" % i, [128, 512], F32) for i in range(2)]
    psTbs = [nc.alloc_psum_tensor("psTb%d" % i, [128, 8, 128], BF16) for i in range(2)]

    cst = A.alloc("cst", [128, ncst], F32)
    dmy = A.alloc("dmy", [1, 32], F32)

    def C(name):
        o, w = coff[name]
        return cst[:, o:o + w]

    identb = A.alloc("identb", [128, 128], BF16)
    onesb = A.alloc("onesb", [128, 128], BF16)
    slfb = A.alloc("slfb", [128, 128], BF16)
    small = A.alloc("small", [128, 64], F32)
    lb = A.alloc("lb", [128, 2, 2, H], F32)
    lg = A.alloc("lg", [128, 2, H], F32)
    hgng = A.alloc("hgng", [128, 1], F32)
    retg = A.alloc("retg", [128, 2], F32)
    oab_o = [A.alloc("oabo%d" % i, [128, CH], BF16) for i in range(2)]
    oab_ctr = [0]
    rw = A.alloc("rw", [128, KT, E], F32)
    rbrow = A.alloc("rbrow", [128, E], F32)
    Mf = A.alloc("Mf", [128, NT, E], F32)
    Mb = A.alloc("Mb", [128, NT, E], BF16)
    Wd = A.alloc("Wd", [128, NT, E], F32)
    lgt = A.alloc("lgt", [128, E], F32)
    pex = A.alloc("pex", [128, E], F32)
    top8 = A.alloc("top8", [128, 8], F32)
    lbraw = A.alloc("lbraw", [128, 2, 2, H], F32)
    mP = A.mark()
    WS = 768
    wslots = [A.alloc("wslot%d" % i, [128, max(KT, 2 * H), WS], BF16) for i in range(2)]
    wctr = [0]
    xmT = A.alloc("xmT", [128, KT, T], BF16)

    s.op("dve", lambda e: e.memset(dmy[:], 0.0), writes=["dmy"])
    DUMMY = dscr("DUMMY", [4096, 16])
    s.dummy = (dmy[0:1, 0:16], DUMMY.ap())
    s.dma("sp", lambda e: e.dma_start(out=cst[:], in_=cst_d.ap()), writes=["cst"])
    s.op("dve", CP(identb[:], C("ident")), reads=["cst"], writes=["identb"])
    s.op("dve", CP(onesb[:], C("ones")), reads=["cst"], writes=["onesb"])
    s.op("dve", CP(slfb[:], C("slf")), reads=["cst"], writes=["slfb"])
    s.dma("sp", lambda e: e.dma_start(out=hgng[:], in_=hgng_d.ap()), writes=["hgng"])
    s.dma("sp", lambda e: e.dma_start(out=retg[:], in_=retg_d.ap()), writes=["retg"])

    def wload(src2d, kt_n, ncols):
        i = wctr[0] % 2
        wctr[0] += 1
        slot = wslots[i]
        tok = "w%d" % i
        s.dma("pool", lambda e: e.dma_start(out=slot[:, 0:kt_n, 0:ncols],
                                            in_=src2d.rearrange("(kt p) n -> p kt n", p=128)), writes=[tok])
        return slot, tok

    _bc = {}

    def BC(e, v):
        if v not in _bc:
            _bc[v] = e.to_reg(v)
        return _bc[v]

    def mms(items, reads, writes):
        n = len(items)
        for i, (o, l, r, st, sp_) in enumerate(items):
            s.op("pe", lambda e, o=o, l=l, r=r, st=st, sp_=sp_: e.matmul(o, lhsT=l, rhs=r, start=st, stop=sp_),
                 reads=reads, writes=writes, inc=(i == n - 1))

    def acc_mm(out, pairs, reads, writes):
        n = len(pairs)
        mms([(out, l, r, i == 0, i == n - 1) for i, (l, r) in enumerate(pairs)], reads, writes)

    def transposes(srcs, dst_fn, reads, dst_tok_fn, evac_eng="act", f32=False, extra=None):
        n = len(srcs)
        g = 0
        half = 0
        while g < n:
            m = min(4, n - g)
            if f32:
                pt, ptok = psS[1][:].rearrange("p (c v) -> p c v", v=128), "psS1"
                view = pt[:, 0:m, :]
            else:
                pt, ptok = psTbs[half], "psTb%d" % half
                view = pt[:, 0:m, :]
            for j in range(m):
                src = srcs[g + j]
                o = pt[:, j, :]
                idn = C("ident") if f32 else identb[:]
                s.op("pe", lambda e, o=o, src=src, idn=idn: e.transpose(out=o, in_=src, identity=idn),
                     reads=list(reads) + ["cst", "identb"], writes=[ptok], inc=(j == m - 1))
            dst = dst_fn(g, m)
            if evac_eng == "act":
                s.op("act", lambda e, dst=dst, view=view: e.copy(out=dst, in_=view), reads=[ptok], writes=dst_tok_fn(g, m))
            else:
                s.op("dve", CP(dst, view), reads=[ptok], writes=dst_tok_fn(g, m))
            if extra is not None:
                extra(pt, 0, g, m, ptok)
            g += m
            half ^= 1

    s.dma("sp", lambda e: e.dma_start(out=lbraw[:, 0], in_=lbf_d.ap()), writes=["lbraw0"])
    s.dma("sp", lambda e: e.dma_start(out=lbraw[:, 1], in_=lbb_d.ap()), writes=["lbraw1"])
    for d_ in range(2):
        s.op("dve", TT(lbraw[:, d_, 0, :], lbraw[:, d_, 0, :], lbraw[:, d_, 1, :], ALU.subtract),
             reads=["lbraw%d" % d_], writes=["lbraw%d" % d_])
        s.op("act", ACT(lb[:, d_, 0, :], lbraw[:, d_, 0, :], AF.Sigmoid), reads=["lbraw%d" % d_], writes=["lb%d" % d_])
        s.op("dve", TS(lb[:, d_, 1, :], lb[:, d_, 0, :], -1.0, 1.0, ALU.mult, ALU.add), reads=["lb%d" % d_], writes=["lb%d" % d_])
    for d_, src in enumerate((rl2f_d, rl2b_d)):
        s.dma("sp", lambda e, d_=d_, src=src: e.dma_start(out=lg[:, d_, :], in_=src.ap().partition_broadcast(128)),
              writes=["lg%d" % d_])
        s.op("act", ACT(lg[:, d_, :], lg[:, d_, :], AF.Exp, scale=math.log(2.0)), reads=["lg%d" % d_], writes=["lg%d" % d_])
        s.op("dve", TS(lg[:, d_, :], lg[:, d_, :], -1.0, 1.0, ALU.mult, ALU.add), reads=["lg%d" % d_], writes=["lg%d" % d_])
        s.op("act", ACT(lg[:, d_, :], lg[:, d_, :], AF.Ln), reads=["lg%d" % d_], writes=["lg%d" % d_])

    if cfg.get("STOP") == "0":
        s.finish(); nc._cfg = cfg; nc._ninst = s.ninst; nc._peak = 0; nc._dbg = dbg_outs
        return nc
    mA2 = A.mark()
    cT = A.alloc("cT", [128, KT, 2], F32)
    scb = A.alloc("scb", [128, KT, 2], BF16)
    s.dma("sp", lambda e: e.dma_start(out=cT[:], in_=cT_d.ap()), writes=["cT"])
    s.op("act", ACT(scb[:], cT[:], AF.Silu), reads=["cT"], writes=["scb"])
    modt = [A.alloc("modt%d" % i, [2, 512], F32) for i in range(2)]
    adab = [A.alloc("adab%d" % i, [2, 512], F32) for i in range(2)]
    NMC = (6 * D) // 512
    for j in range(NMC):
        slot, wt = wload(adaw_d.ap()[:, j * 512:(j + 1) * 512], KT, 512)
        ps = psA[j % 4]
        acc_mm(ps[0:2, :], [(scb[:, kt, :], slot[:, kt, 0:512]) for kt in range(KT)], ["scb", wt], ["psA%d" % (j % 4)])
        ab = adab[j % 2]
        mt = modt[j % 2]
        s.dma("sp", lambda e, ab=ab, j=j: e.dma_start(out=ab[:], in_=adab_d.ap()[:, j * 512:(j + 1) * 512]), writes=["adab%d" % (j % 2)])
        s.op("dve", TT(mt[:], ps[0:2, :], ab[:], ALU.add), reads=["psA%d" % (j % 4), "adab%d" % (j % 2)], writes=["modt%d" % (j % 2)])
        s.dma("sp", lambda e, mt=mt, j=j: e.dma_start(out=MOD.ap()[:, j * 512:(j + 1) * 512], in_=mt[:]),
              reads=["modt%d" % (j % 2)], writes=["MOD%d" % j])

    def modtoks(c0, c1):
        return ["MOD%d" % j for j in range(c0 // 512, (c1 - 1) // 512 + 1)]

    def load_row(dst, cond, which, tok):
        c0 = which * D
        s.dma("sp", lambda e: e.dma_start(out=dst, in_=MOD.ap()[cond:cond + 1, c0:c0 + D].partition_broadcast(128)),
              reads=modtoks(c0, c0 + D), writes=[tok])

    def load_vec_row(dst, src_d, tok):
        s.dma("sp", lambda e: e.dma_start(out=dst, in_=src_d.ap().partition_broadcast(128)), writes=[tok])

    tile_cond = [0] * (NP * TP // 128) + [1] * (TS_ // 128)
    segs = [(p * TP // 128, TP // 128, "p", p) for p in range(NP)] + [(NP * TP // 128, TS_ // 128, "s", 0)]

    rows_a = {}
    n1row = A.alloc("n1row", [128, D], F32)
    load_vec_row(n1row[:], n1g_d, "n1row")
    for c in range(2):
        a1 = A.alloc("a1row%d" % c, [128, D], F32)
        sh = A.alloc("sh1row%d" % c, [128, D], F32)
        load_row(a1[:], c, 1, "a1row%d" % c)
        load_row(sh[:], c, 0, "sh1row%d" % c)
        s.op("dve", STT(a1[:], a1[:], 1.0, n1row[:], ALU.add, ALU.mult), reads=["a1row%d" % c, "n1row"], writes=["a1row%d" % c])
        rows_a[c] = (a1, sh)
    xbuf = [A.alloc("xbuf%d" % i, [128, D], F32) for i in range(2)]
    t32 = A.alloc("t32", [128, D], F32)
    xmb = [A.alloc("xmb%d" % i, [128, D], BF16) for i in range(2)]
    junk = A.alloc("junk", [128, D], BF16)

    def rms_rstd(src, src_tok, col, n):
        sc = small[:, col:col + 1]
        tk = "small%d" % col
        s.op("dve", lambda e: e.memset(sc, 0.0), writes=[tk])
        s.op("act", ACT(junk[:, 0:n], src, AF.Square, accum=sc), reads=[src_tok, tk], writes=["junk", tk])
        s.op("dve", TS(sc, sc, 1.0 / n, EPS, ALU.mult, ALU.add), reads=[tk], writes=[tk])
        s.op("act", ACT(sc, sc, AF.Sqrt), reads=[tk], writes=[tk])
        s.op("dve", lambda e: e.reciprocal(out=sc, in_=sc), reads=[tk], writes=[tk])
        return sc, tk

    for i in range(NT):
        c = tile_cond[i]
        xt = xbuf[i % 2]
        xtok = "xbuf%d" % (i % 2)
        s.dma("sp", lambda e, xt=xt, i=i: e.dma_start(out=xt[:], in_=x_d.ap()[i * 128:(i + 1) * 128, :]), writes=[xtok])
        sc, tk = rms_rstd(xt[:], xtok, i % 2, D)
        a1, sh = rows_a[c]
        s.op("dve", STT(t32[:], xt[:], sc, a1[:], ALU.mult, ALU.mult), reads=[xtok, tk, "a1row%d" % c], writes=["t32"])
        xm = xmb[i % 2]
        s.op("dve", TT(xm[:], t32[:], sh[:], ALU.add), reads=["t32", "sh1row%d" % c], writes=["xmb%d" % (i % 2)])
        transposes([xm[:, kt * 128:(kt + 1) * 128] for kt in range(KT)],
                   lambda g, m, i=i: xmT[:, g:g + m, i * 128:(i + 1) * 128],
                   ["xmb%d" % (i % 2)], lambda g, m, i=i: ["xmT_%d" % i])
    xmT_toks = ["xmT_%d" % i for i in range(NT)]
    if debug:
        for kt in range(KT):
            dump("xmT%d" % kt, xmT[:, kt, :], xmT_toks)
    s.barrier()
    A.release(mA2)

    if cfg.get("STOP") == "A":
        s.finish(); nc._cfg = cfg; nc._ninst = s.ninst; nc._peak = 0; nc._dbg = dbg_outs
        return nc
    mB = A.mark()
    q32 = A.alloc("q32", [128, T], BF16)
    fdir = [A.alloc("f%d" % d_, [128, T], F32) for d_ in range(2)]
    s1 = A.alloc("s1", [128, T], F32)
    s2 = A.alloc("s2", [128, T], F32)
    gcol = A.alloc("gcol", [128, NCK], F32)
    sgate = A.alloc("sgate", [128, T], BF16)
    vTM = A.alloc("vTM", [128, NT, 128], BF16)
    qp = [A.alloc("qp%d" % d_, [128, T], BF16) for d_ in range(2)]
    kp = [A.alloc("kp%d" % d_, [128, T], BF16) for d_ in range(2)]
    kpTM = [A.alloc("kpTM%d" % d_, [128, NT, 128], BF16) for d_ in range(2)]
    vexp = A.alloc("vexp", [128, NT, 4, 128], BF16)
    Rpb = [A.alloc("Rpb%d" % d_, [128, NCK, 128], BF16) for d_ in range(2)]
    decay = [A.alloc("decay%d" % d_, [128, NCK], F32) for d_ in range(2)]
    R32 = [[A.alloc("R32_%d_%d" % (d_, sg), [128, 128], F32) for sg in range(len(segs))] for d_ in range(2)]
    msk = [A.alloc("msk%d" % i, [128, 128], BF16) for i in range(2)]
    sqb = A.alloc("sqb", [128, CH], BF16)
    Mdir = [C("mf"), C("mb")]

    SB = cfg.get("SB", 99)
    for h in range(H if SB == 99 else 1):
        slot, wt = wload(win_d.ap()[:, h * 640:(h + 1) * 640], KT, 640)
        for ch in range(NCHK):
            cs = slice(ch * CH, (ch + 1) * CH)
            for j in range(4):
                acc_mm(psA[j][:, 0:CH], [(slot[:, kt, j * 128:(j + 1) * 128], xmT[:, kt, cs]) for kt in range(KT)],
                       [wt] + xmT_toks, ["psA%d" % j])
            s.op("act", ACT(q32[:, cs], psA[0][:, 0:CH], AF.Silu), reads=["psA0"], writes=["q32"])
            for d_ in range(2):
                s.op("act", ACT(s1[:, cs], psA[1 + d_][:, 0:CH], AF.Sigmoid), reads=["psA%d" % (1 + d_)], writes=["s1"])
                s.op("dve", TS(fdir[d_][:, cs], s1[:, cs], lb[:, d_, 1, h:h + 1], lb[:, d_, 0, h:h + 1], ALU.mult, ALU.add),
                     reads=["s1", "lb%d" % d_], writes=["f%d" % d_])
            s.op("act", ACT(sgate[:, cs], psA[3][:, 0:CH], AF.Sigmoid), reads=["psA3"], writes=["sgate"])
        for i in range(NT):
            ps = psS[i % 2]
            acc_mm(ps[:, 0:128], [(xmT[:, kt, i * 128:(i + 1) * 128], slot[:, kt, 512:640]) for kt in range(KT)],
                   [wt] + xmT_toks, ["psS%d" % (i % 2)])
            s.op("act", lambda e, ps=ps, i=i: e.copy(out=vTM[:, i, :], in_=ps[:, 0:128]), reads=["psS%d" % (i % 2)], writes=["vTM"])
        cmo, _ = coff["cmask"]
        for c in range(4):
            s.op("dve", TS(vexp[:, :, c, :], vTM[:], cst[:, cmo + c:cmo + c + 1], None, ALU.mult), reads=["vTM", "cst"], writes=["vexp"])
        if SB == 1:
            break
        for d_ in range(2):
            f = fdir[d_]
            ftok = "f%d" % d_
            s.op("act", ACT(s1[:], f[:], AF.Ln), reads=[ftok], writes=["s1"])
            s.op("dve", TS(f[:], f[:], -1.0, 1.0, ALU.mult, ALU.add), reads=[ftok, "s1"], writes=[ftok])
            for ch in range(NCHK):
                cs = slice(ch * CH, (ch + 1) * CH)
                s.op("dve", lambda e, cs=cs: e.tensor_tensor_scan(out=s2[:, cs], data0=C("reset"), data1=s1[:, cs], initial=0.0,
                                                                  op0=ALU.mult, op1=ALU.add), reads=["s1", "cst"], writes=["s2"])
            gi3 = s2[:].rearrange("p (c k) -> p c k", k=CHUNK)
            s.op("act", ACT(decay[d_][:], gi3[:, :, CHUNK - 1], AF.Exp), reads=["s2"], writes=["decay%d" % d_])
            if d_ == 0:
                s.op("dve", CP(gcol[:], gi3[:, :, CHUNK - 1]), reads=["s2"], writes=["gcol"])
                s.op("dve", TT(gi3, gcol[:].unsqueeze(2).to_broadcast([128, NCK, CHUNK]), gi3, ALU.subtract),
                     reads=["s2", "gcol", "decay%d" % d_], writes=["s2"])
            else:
                s.op("dve", TT(s2[:], s2[:], s1[:], ALU.subtract), reads=["s2", "s1", "decay%d" % d_], writes=["s2"])
            s.op("act", ACT(s1[:], s2[:], AF.Exp), reads=["s2"], writes=["s1"])
            s.op("dve", TT(kp[d_][:], f[:], s1[:], ALU.mult), reads=[ftok, "s1"], writes=["kp%d" % d_])
            s.op("act", ACT(s1[:], s2[:], AF.Exp, scale=-1.0), reads=["s2", "kp%d" % d_], writes=["s1"])
            s.op("dve", TT(qp[d_][:], q32[:], s1[:], ALU.mult), reads=["q32", "s1"], writes=["qp%d" % d_])
            if SB == 2:
                continue
            transposes([kp[d_][:, i * 128:(i + 1) * 128] for i in range(NT)],
                       lambda g, m, d_=d_: kpTM[d_][:, g:g + m, :], ["kp%d" % d_], lambda g, m, d_=d_: ["kpTM%d" % d_])
        banks = [(psS[0], "psS0"), (psS[1], "psS1"), (psA[1], "psA1"), (psA[2], "psA2"), (psA[3], "psA3"), (psA[0], "psA0")]
        chains = []
        for d_ in range(2):
            for sgi, (t0, nt, kind, sidx) in enumerate(segs):
                Rr = R32[d_][sgi]
                rtok = "R32_%d_%d" % (d_, sgi)
                if kind == "p":
                    s.op("dve", lambda e, Rr=Rr: e.memset(Rr[:], 0.0), writes=[rtok])
                else:
                    src = (shf_d, shb_d)[d_]
                    s.dma("sp", lambda e, Rr=Rr, src=src, h=h: e.dma_start(out=Rr[:], in_=src.ap()[h]), writes=[rtok])
                tiles = list(range(t0, t0 + nt))
                if d_ == 1:
                    tiles = tiles[::-1]
                chains.append(dict(d=d_, Rr=Rr, rtok=rtok, tiles=tiles, kind=kind, sidx=sidx))
        for ci, cn in enumerate(chains):
            cn["bank"], cn["btok"] = banks[ci % len(banks)]
        for r in range(max(len(cn["tiles"]) for cn in chains)):
            act_ch = [cn for cn in chains if r < len(cn["tiles"])]
            for cn in act_ch:
                i = cn["tiles"][r]
                d_ = cn["d"]
                mms([(cn["bank"][:, 0:512], kpTM[d_][:, i, :], vexp[:, i].rearrange("p c v -> p (c v)"), True, True)],
                    ["kpTM%d" % d_, "vexp"], [cn["btok"]])
            for st in range(4):
                for cn in act_ch:
                    i = cn["tiles"][r]
                    d_ = cn["d"]
                    c = st if d_ == 0 else 3 - st
                    cg = i * 4 + c
                    Rr, rtok = cn["Rr"], cn["rtok"]
                    pv = cn["bank"][:].rearrange("p (c v) -> p c v", v=128)
                    s.op("act", ACT(Rpb[d_][:, cg, :], Rr[:], AF.Copy, scale=decay[d_][:, cg:cg + 1]),
                         reads=[rtok, "decay%d" % d_], writes=["Rpb%d_%d" % (d_, i)])
                    s.op("dve", STT(Rr[:], Rr[:], decay[d_][:, cg:cg + 1], pv[:, c, :], ALU.mult, ALU.add),
                         reads=[rtok, "decay%d" % d_, cn["btok"]], writes=[rtok])
        for cn in chains:
            if cn["kind"] == "p":
                dst = (hf_o, hb_o)[cn["d"]]
                s.dma("sp", lambda e, Rr=cn["Rr"], dst=dst, sidx=cn["sidx"], h=h: e.dma_start(out=dst.ap()[sidx, h], in_=Rr[:]),
                      reads=[cn["rtok"]], writes=["hout%d_%d_%d" % (cn["d"], cn["sidx"], h)])
        if SB in (2, 3, 4):
            break
        for ch in range(NCHK):
            its = []
            for tl in range(TPC):
                i = ch * TPC + tl
                tsl = slice(i * 128, (i + 1) * 128)
                reg = psA[0][:, tl * 128:(tl + 1) * 128]
                first = True
                for d_ in range(2):
                    ps = psS[d_]
                    s.op("pe", lambda e, ps=ps, d_=d_, tsl=tsl: e.matmul(ps[:, 0:128], lhsT=kp[d_][:, tsl], rhs=qp[d_][:, tsl], start=True, stop=True),
                         reads=["kp%d" % d_, "qp%d" % d_], writes=["psS%d" % d_])
                    mk = msk[d_]
                    s.op("dve", TT(mk[:], ps[:, 0:128], Mdir[d_], ALU.mult), reads=["psS%d" % d_, "cst"], writes=["msk%d" % d_])
                    items = [(reg, vTM[:, i, :], mk[:], first, (SB == 5 and d_ == 1))]
                    first = False
                    for c in range(4 if SB != 5 else 0):
                        cg = i * 4 + c
                        items.append((psA[0][:, tl * 128 + c * 32: tl * 128 + (c + 1) * 32], Rpb[d_][:, cg, :],
                                      qp[d_][:, i * 128 + c * 32:i * 128 + (c + 1) * 32], False, (d_ == 1 and c == 3)))
                    mms(items, ["vTM", "msk%d" % d_, "Rpb%d_%d" % (d_, i), "qp%d" % d_], ["psA0"])
            cs = slice(ch * CH, (ch + 1) * CH)
            if SB in (5, 6):
                continue
            s.op("dve", CP(s1[:, 0:CH], psA[0][:, 0:CH]), reads=["psA0"], writes=["s1"])
            s.op("act", ACT(sqb[:], psA[0][:, 0:CH], AF.Square), reads=["psA0"], writes=["sqb"])
            acc_mm(psA[1][:, 0:CH], [(onesb[:], sqb[:])], ["onesb", "sqb"], ["psA1"])
            s.op("dve", TS(s2[:, 0:CH], psA[1][:, 0:CH], 1.0 / 128, EPS, ALU.mult, ALU.add), reads=["psA1"], writes=["s2"])
            s.op("act", ACT(s2[:, 0:CH], s2[:, 0:CH], AF.Sqrt), reads=["s2"], writes=["s2"])
            s.op("dve", lambda e: e.reciprocal(out=s2[:, 0:CH], in_=s2[:, 0:CH]), reads=["s2"], writes=["s2"])
            if SB == 7:
                continue
            s.op("dve", STT(s1[:, 0:CH], s1[:, 0:CH], hgng[:, 0:1], s2[:, 0:CH], ALU.mult, ALU.mult), reads=["s1", "s2", "hgng"], writes=["s1"])
            ob = oab_o[oab_ctr[0] % 2]
            otok = "oabo%d" % (oab_ctr[0] % 2)
            oab_ctr[0] += 1
            s.op("dve", TT(ob[:], s1[:, 0:CH], sgate[:, cs], ALU.mult), reads=["s1", "sgate"], writes=[otok])
            if SB == 8:
                continue
            s.dma("sp", lambda e, ob=ob, h=h, cs=cs: e.dma_start(out=OAB.ap()[h, :, cs], in_=ob[:]), reads=[otok], writes=["OAB_%d_%d" % (h, ch)])
    s.barrier()
    A.release(mB)

    if cfg.get("STOP") == "B":
        s.finish(); nc._cfg = cfg; nc._ninst = s.ninst; nc._peak = 0; nc._dbg = dbg_outs
        return nc
    mC = A.mark()
    q32 = A.alloc("rq32", [128, T], F32)
    k32 = A.alloc("rk32", [128, T], F32)
    qb = A.alloc("rqb", [128, T], BF16)
    kb = A.alloc("rkb", [128, T], BF16)
    qin = [A.alloc("qin%d" % d_, [128, T], BF16) for d_ in range(2)]
    kend = [A.alloc("kend%d" % d_, [128, NT, 128], BF16) for d_ in range(2)]
    rvTM = A.alloc("rvTM", [128, NT, 256], BF16)
    Rsb = [A.alloc("Rsb%d" % d_, [128, NT, 256], BF16) for d_ in range(2)]
    rg = A.alloc("rg", [128, 2, T], BF16)
    RR = [[A.alloc("RR_%d_%d" % (d_, sg), [128, 256], F32) for sg in range(len(segs))] for d_ in range(2)]
    tA = A.alloc("tA", [128, 128], F32)
    tB = A.alloc("tB", [128, 128], F32)
    Ds = A.alloc("Ds", [128, 128], F32)
    rowt = [A.alloc("rowt%d" % d_, [128, 128], F32) for d_ in range(2)]
    colt = A.alloc("colt", [128, 4], F32)
    t1 = A.alloc("t1", [128, CH], F32)
    t2 = A.alloc("t2", [128, CH], F32)
    rmsk = [A.alloc("rmsk%d" % i, [128, 128], BF16) for i in range(2)]
    ro32 = [A.alloc("ro32_%d" % v, [128, CH], F32) for v in range(2)]
    rob = [A.alloc("rob_%d" % v, [128, CH], BF16) for v in range(2)]
    rsq = [A.alloc("rsq_%d" % v, [128, CH], BF16) for v in range(2)]
    mean = A.alloc("mean", [128, CH], F32)
    var = A.alloc("var", [128, CH], F32)
    ts0 = NP * TP
    ropet = A.alloc("ropet", [128, 2 * TS_], F32)
    s.dma("sp", lambda e: e.dma_start(out=ropet[:], in_=rope_d.ap()), writes=["ropet"])

    for h in range(H):
        c0 = H * 640 + h * 768
        slot, wt = wload(win_d.ap()[:, c0:c0 + 768], KT, 768)
        for ch in range(NCHK):
            cs = slice(ch * CH, (ch + 1) * CH)
            for j in range(4):
                acc_mm(psA[j][:, 0:CH], [(slot[:, kt, j * 128:(j + 1) * 128], xmT[:, kt, cs]) for kt in range(KT)],
                       [wt] + xmT_toks, ["psA%d" % j])
            s.op("act", lambda e, cs=cs: e.copy(out=q32[:, cs], in_=psA[0][:, 0:CH]), reads=["psA0"], writes=["rq32"])
            s.op("act", ACT(k32[:, cs], psA[1][:, 0:CH], AF.Copy, scale=128.0 ** -0.5), reads=["psA1"], writes=["rk32"])
            for v in range(2):
                s.op("act", ACT(rg[:, v, cs], psA[2 + v][:, 0:CH], AF.Silu), reads=["psA%d" % (2 + v)], writes=["rg"])
        for i in range(NT):
            ps = psS[i % 2]
            acc_mm(ps[:, 0:256], [(xmT[:, kt, i * 128:(i + 1) * 128], slot[:, kt, 512:768]) for kt in range(KT)],
                   [wt] + xmT_toks, ["psS%d" % (i % 2)])
            s.op("act", lambda e, ps=ps, i=i: e.copy(out=rvTM[:, i, :], in_=ps[:, 0:256]), reads=["psS%d" % (i % 2)], writes=["rvTM"])
        if ts0 > 0:
            s.op("dve", CP(qb[:, 0:ts0], q32[:, 0:ts0]), reads=["rq32"], writes=["rqb"])
            s.op("dve", CP(kb[:, 0:ts0], k32[:, 0:ts0]), reads=["rk32"], writes=["rkb"])
        RC = min(512, TS_)
        for rc in range(TS_ // RC):
            cs = slice(ts0 + rc * RC, ts0 + (rc + 1) * RC)
            cosv = ropet[:, rc * RC:(rc + 1) * RC]
            sinv = ropet[:, TS_ + rc * RC: TS_ + (rc + 1) * RC]
            for src, dstb, stok, dtok, pi in ((q32, qb, "rq32", "rqb", 2), (k32, kb, "rk32", "rkb", 3)):
                acc_mm(psA[pi][:, 0:RC], [(C("psw"), src[:, cs])], ["cst", stok], ["psA%d" % pi])
                s.op("dve", TT(t1[:, 0:RC], src[:, cs], cosv, ALU.mult), reads=[stok, "ropet"], writes=["t1"])
                s.op("dve", TT(t2[:, 0:RC], psA[pi][:, 0:RC], sinv, ALU.mult), reads=["psA%d" % pi, "ropet"], writes=["t2"])
                s.op("dve", TT(dstb[:, cs], t1[:, 0:RC], t2[:, 0:RC], ALU.add), reads=["t1", "t2"], writes=[dtok])
        lgf = lg[:, 0, h:h + 1]
        lgb = lg[:, 1, h:h + 1]
        s.op("act", ACT(tA[:], C("dpos"), AF.Exp, scale=lgf), reads=["cst", "lg0"], writes=["tA"])
        s.op("dve", TT(tA[:], tA[:], C("slf"), ALU.mult), reads=["tA", "cst"], writes=["tA"])
        s.op("act", ACT(tB[:], C("dneg"), AF.Exp, scale=lgb), reads=["cst", "lg1"], writes=["tB"])
        s.op("dve", TT(tB[:], tB[:], C("slb"), ALU.mult), reads=["tB", "cst"], writes=["tB"])
        s.op("dve", TT(Ds[:], tA[:], tB[:], ALU.add), reads=["tA", "tB"], writes=["Ds"])
        s.op("dve", TT(Ds[:], Ds[:], C("i2"), ALU.add), reads=["Ds", "cst"], writes=["Ds"])
        s.op("act", ACT(rowt[0][:], C("iota1"), AF.Exp, scale=lgf), reads=["cst", "lg0"], writes=["rowt0"])
        s.op("act", ACT(rowt[1][:], C("iotar"), AF.Exp, scale=lgb), reads=["cst", "lg1"], writes=["rowt1"])
        s.op("act", ACT(colt[:, 0:1], C("c127"), AF.Exp, scale=lgf), reads=["cst", "lg0"], writes=["colt"])
        s.op("act", ACT(colt[:, 1:2], C("cp"), AF.Exp, scale=lgb), reads=["cst", "lg1"], writes=["colt"])
        s.op("act", ACT(colt[:, 2:3], lgf, AF.Exp, scale=128.0), reads=["lg0"], writes=["colt"])
        s.op("act", ACT(colt[:, 3:4], lgb, AF.Exp, scale=128.0), reads=["lg1"], writes=["colt"])
        for i in range(NT):
            tsl = slice(i * 128, (i + 1) * 128)
            for d_ in range(2):
                s.op("dve", TT(qin[d_][:, tsl], qb[:, tsl], rowt[d_][:], ALU.mult), reads=["rqb", "rowt%d" % d_], writes=["qin%d" % d_])
        g = 0
        half = 0
        while g < NT:
            m = min(4, NT - g)
            ptok = "psTb%d" % half
            for j in range(m):
                o = psTbs[half][:, j, :]
                src = kb[:, (g + j) * 128:(g + j + 1) * 128]
                s.op("pe", lambda e, o=o, src=src: e.transpose(out=o, in_=src, identity=identb[:]),
                     reads=["rkb", "identb"], writes=[ptok], inc=(j == m - 1))
            view = psTbs[half][:, 0:m, :]
            for d_ in range(2):
                s.op("act", ACT(kend[d_][:, g:g + m, :], view, AF.Copy, scale=colt[:, d_:d_ + 1]), reads=[ptok, "colt"], writes=["kend%d" % d_])
            g += m
            half ^= 1
        banks = [(psS[0], "psS0"), (psS[1], "psS1"), (psA[1], "psA1"), (psA[2], "psA2"), (psA[3], "psA3"), (psA[0], "psA0")]
        chains = []
        for d_ in range(2):
            for sgi, (t0, nt, kind, sidx) in enumerate(segs):
                Rr = RR[d_][sgi]
                rtok = "RR_%d_%d" % (d_, sgi)
                if kind == "p":
                    s.op("dve", lambda e, Rr=Rr: e.memset(Rr[:], 0.0), writes=[rtok])
                else:
                    src = (srf_d, srb_d)[d_]
                    s.dma("sp", lambda e, Rr=Rr, src=src, h=h: e.dma_start(out=Rr[:], in_=src.ap()[h]), writes=[rtok])
                tiles = list(range(t0, t0 + nt))
                if d_ == 1:
                    tiles = tiles[::-1]
                chains.append(dict(d=d_, Rr=Rr, rtok=rtok, tiles=tiles, kind=kind, sidx=sidx))
        for ci, cn in enumerate(chains):
            cn["bank"], cn["btok"] = banks[ci % len(banks)]
        for r in range(max(len(cn["tiles"]) for cn in chains)):
            act_ch = [cn for cn in chains if r < len(cn["tiles"])]
            for cn in act_ch:
                i = cn["tiles"][r]
                d_ = cn["d"]
                acc_mm(cn["bank"][:, 0:256], [(kend[d_][:, i, :], rvTM[:, i, :])], ["kend%d" % d_, "rvTM"], [cn["btok"]])
            for cn in act_ch:
                i = cn["tiles"][r]
                d_ = cn["d"]
                Rr, rtok = cn["Rr"], cn["rtok"]
                s.op("act", lambda e, d_=d_, i=i, Rr=Rr: e.copy(out=Rsb[d_][:, i, :], in_=Rr[:]), reads=[rtok], writes=["Rsb%d_%d" % (d_, i)])
                s.op("dve", STT(Rr[:], Rr[:], colt[:, 2 + d_:3 + d_], cn["bank"][:, 0:256], ALU.mult, ALU.add),
                     reads=[rtok, "colt", cn["btok"]], writes=[rtok])
        for cn in chains:
            if cn["kind"] == "p":
                dst = (rf_o, rb_o)[cn["d"]]
                s.dma("sp", lambda e, Rr=cn["Rr"], dst=dst, sidx=cn["sidx"], h=h: e.dma_start(out=dst.ap()[sidx, h], in_=Rr[:]),
                      reads=[cn["rtok"]], writes=["rout%d_%d_%d" % (cn["d"], cn["sidx"], h)])
        for ch in range(NCHK):
            cs = slice(ch * CH, (ch + 1) * CH)
            for tl in range(TPC):
                i = ch * TPC + tl
                tsl = slice(i * 128, (i + 1) * 128)
                ps = psS[i % 2]
                s.op("pe", lambda e, ps=ps, tsl=tsl: e.matmul(ps[:, 0:128], lhsT=kb[:, tsl], rhs=qb[:, tsl], start=True, stop=True),
                     reads=["rkb", "rqb"], writes=["psS%d" % (i % 2)])
                mk = rmsk[i % 2]
                s.op("dve", TT(mk[:], ps[:, 0:128], Ds[:], ALU.mult), reads=["psS%d" % (i % 2), "Ds"], writes=["rmsk%d" % (i % 2)])
                for v in range(2):
                    reg = psA[v][:, tl * 128:(tl + 1) * 128]
                    vs = slice(v * 128, (v + 1) * 128)
                    mms([(reg, rvTM[:, i, vs], mk[:], True, False),
                         (reg, Rsb[0][:, i, vs], qin[0][:, tsl], False, False),
                         (reg, Rsb[1][:, i, vs], qin[1][:, tsl], False, True)],
                        ["rvTM", "rmsk%d" % (i % 2), "Rsb0_%d" % i, "Rsb1_%d" % i, "qin0", "qin1"], ["psA%d" % v])
            for v in range(2):
                s.op("dve", CP(ro32[v][:], psA[v][:, 0:CH]), reads=["psA%d" % v], writes=["ro32_%d" % v])
                s.op("act", lambda e, v=v: e.copy(out=rob[v][:], in_=psA[v][:, 0:CH]), reads=["psA%d" % v], writes=["rob%d" % v])
                s.op("act", ACT(rsq[v][:], psA[v][:, 0:CH], AF.Square), reads=["psA%d" % v], writes=["rsq%d" % v])
            acc_mm(psA[2][:, 0:CH], [(onesb[:], rob[0][:]), (onesb[:], rob[1][:])], ["onesb", "rob0", "rob1"], ["psA2"])
            acc_mm(psA[3][:, 0:CH], [(onesb[:], rsq[0][:]), (onesb[:], rsq[1][:])], ["onesb", "rsq0", "rsq1"], ["psA3"])
            s.op("dve", TS(mean[:], psA[2][:, 0:CH], 1.0 / 256, None, ALU.mult), reads=["psA2"], writes=["mean"])
            s.op("dve", TT(var[:], mean[:], mean[:], ALU.mult), reads=["mean"], writes=["var"])
            s.op("dve", STT(var[:], psA[3][:, 0:CH], 1.0 / 256, var[:], ALU.mult, ALU.subtract), reads=["psA3", "var"], writes=["var"])
            s.op("dve", TS(var[:], var[:], EPS, None, ALU.add), reads=["var"], writes=["var"])
            s.op("act", ACT(var[:], var[:], AF.Sqrt), reads=["var"], writes=["var"])
            s.op("dve", lambda e: e.reciprocal(out=var[:], in_=var[:]), reads=["var"], writes=["var"])
            for v in range(2):
                s.op("dve", TT(ro32[v][:], ro32[v][:], mean[:], ALU.subtract), reads=["ro32_%d" % v, "mean"], writes=["ro32_%d" % v])
                s.op("dve", STT(ro32[v][:], ro32[v][:], retg[:, v:v + 1], var[:], ALU.mult, ALU.mult),
                     reads=["ro32_%d" % v, "retg", "var"], writes=["ro32_%d" % v])
                ob = oab_o[oab_ctr[0] % 2]
                otok = "oabo%d" % (oab_ctr[0] % 2)
                oab_ctr[0] += 1
                s.op("dve", TT(ob[:], ro32[v][:], rg[:, v, cs], ALU.mult), reads=["ro32_%d" % v, "rg"], writes=[otok])
                ci = H + 2 * h + v
                s.dma("sp", lambda e, ob=ob, ci=ci, cs=cs: e.dma_start(out=OAB.ap()[ci, :, cs], in_=ob[:]), reads=[otok], writes=["OAB_%d_%d" % (ci, ch)])
    s.barrier()
    A.release(mC)

    if cfg.get("STOP") == "C":
        s.finish(); nc._cfg = cfg; nc._ninst = s.ninst; nc._peak = 0; nc._dbg = dbg_outs
        return nc
    mD = A.mark()
    s.dma("sp", lambda e: e.dma_start(out=rw[:], in_=rw_d.ap().rearrange("(kt p) n -> p kt n", p=128)), writes=["rw"])
    load_vec_row(rbrow[:], rb_d, "rbrow")
    mD1 = A.mark()
    for ch in range(NCHK):
        cs = slice(ch * CH, (ch + 1) * CH)
        conds = sorted(set(tile_cond[ch * TPC:(ch + 1) * TPC]))
        A.release(mD1)
        oab = A.alloc("oab", [128, 3 * H, CH], BF16)
        s.dma("sp", lambda e, cs=cs: e.dma_start(out=oab[:], in_=OAB.ap()[:, :, cs].rearrange("c p t -> p c t")),
              reads=["OAB_%d_%d" % (ci, ch) for ci in range(3 * H)], writes=["oab"])
        merged = A.alloc("merged", [128, KT, CH], BF16)
        mD2 = A.mark()
        sga = A.alloc("sga", [128, JT, CH], F32)
        sgb = A.alloc("sgb", [128, JT, CH], F32)
        m1 = A.alloc("m1", [128, JT, CH], F32)
        for g in range(NG):
            slot, wt = wload(win_d.ap()[:, cfg["MG0"] + g * GW: cfg["MG0"] + (g + 1) * GW], KT, GW)
            for j in range(JT):
                acc_mm(psA[j][:, 0:CH], [(slot[:, kt, j * 128:(j + 1) * 128], xmT[:, kt, cs]) for kt in range(KT)], [wt] + xmT_toks, ["psA%d" % j])
                s.op("act", ACT(sga[:, j, :], psA[j][:, 0:CH], AF.Sigmoid), reads=["psA%d" % j], writes=["sga"])
            slot, wt = wload(win_d.ap()[:, cfg["MG0"] + D + g * GW: cfg["MG0"] + D + (g + 1) * GW], KT, GW)
            for j in range(JT):
                acc_mm(psA[j][:, 0:CH], [(slot[:, kt, j * 128:(j + 1) * 128], xmT[:, kt, cs]) for kt in range(KT)], [wt] + xmT_toks, ["psA%d" % j])
                s.op("act", ACT(sgb[:, j, :], psA[j][:, 0:CH], AF.Sigmoid), reads=["psA%d" % j], writes=["sgb"])
            slot, wt = wload(wpa_d.ap()[:, g * GW:(g + 1) * GW], H, GW)
            for j in range(JT):
                acc_mm(psA[j][:, 0:CH], [(slot[:, c, j * 128:(j + 1) * 128], oab[:, c, :]) for c in range(H)], [wt, "oab"], ["psA%d" % j])
                s.op("dve", TT(m1[:, j, :], sga[:, j, :], psA[j][:, 0:CH], ALU.mult), reads=["sga", "psA%d" % j], writes=["m1"])
            slot, wt = wload(wpb_d.ap()[:, g * GW:(g + 1) * GW], 2 * H, GW)
            for j in range(JT):
                acc_mm(psA[j][:, 0:CH], [(slot[:, c, j * 128:(j + 1) * 128], oab[:, H + c, :]) for c in range(2 * H)], [wt, "oab"], ["psA%d" % j])
                s.op("dve", TT(sgb[:, j, :], sgb[:, j, :], psA[j][:, 0:CH], ALU.mult), reads=["sgb", "psA%d" % j], writes=["sgb"])
                s.op("dve", TT(merged[:, g * JT + j, :], m1[:, j, :], sgb[:, j, :], ALU.add), reads=["m1", "sgb"], writes=["merged"])
        if debug:
            for kt in range(KT):
                dump("merged%d_%d" % (ch, kt), merged[:, kt, :], ["merged"])
        s.barrier()
        A.release(mD2)
        g1row = {}
        for c in conds:
            g1row[c] = A.alloc("g1row%d" % c, [128, D], F32)
            load_row(g1row[c][:], c, 2, "g1row%d" % c)
        xp = [A.alloc("xp%d" % i, [128, GW], F32) for i in range(2)]
        tp = A.alloc("tp", [128, GW], F32)
        xpc = 0
        for g in range(NG):
            slot, wt = wload(wout_d.ap()[:, g * GW:(g + 1) * GW], KT, GW)
            for tl in range(TPC):
                i = ch * TPC + tl
                c = tile_cond[i]
                ps = psA[tl % 4]
                acc_mm(ps[:, 0:GW], [(merged[:, kt, tl * 128:(tl + 1) * 128], slot[:, kt, 0:GW]) for kt in range(KT)], ["merged", wt], ["psA%d" % (tl % 4)])
                xq = xp[xpc % 2]
                xtok = "xp%d" % (xpc % 2)
                xpc += 1
                s.dma("sp", lambda e, xq=xq, i=i, g=g: e.dma_start(out=xq[:], in_=x_d.ap()[i * 128:(i + 1) * 128, g * GW:(g + 1) * GW]), writes=[xtok])
                s.op("dve", TT(tp[:], ps[:, 0:GW], g1row[c][:, g * GW:(g + 1) * GW], ALU.mult), reads=["psA%d" % (tl % 4), "g1row%d" % c], writes=["tp"])
                s.op("dve", TT(xq[:], tp[:], xq[:], ALU.add), reads=["tp", xtok], writes=[xtok])
                s.dma("sp", lambda e, xq=xq, i=i, g=g: e.dma_start(out=X1.ap()[i * 128:(i + 1) * 128, g * GW:(g + 1) * GW], in_=xq[:]),
                      reads=[xtok], writes=["X1_%d_%d" % (i, g)])
        s.barrier()
        A.release(mD1)
        rows2 = {}
        n2row = A.alloc("n2row", [128, D], F32)
        load_vec_row(n2row[:], n2g_d, "n2row")
        for c in conds:
            a2 = A.alloc("a2row%d" % c, [128, D], F32)
            sh2 = A.alloc("sh2row%d" % c, [128, D], F32)
            load_row(a2[:], c, 4, "a2row%d" % c)
            load_row(sh2[:], c, 3, "sh2row%d" % c)
            s.op("dve", STT(a2[:], a2[:], 1.0, n2row[:], ALU.add, ALU.mult), reads=["a2row%d" % c, "n2row"], writes=["a2row%d" % c])
            rows2[c] = (a2, sh2)
        x1t = [A.alloc("x1t%d" % i, [128, D], F32) for i in range(2)]
        xm2 = A.alloc("xm2", [128, D], F32)
        xm2b = [A.alloc("xm2b%d" % i, [128, D], BF16) for i in range(2)]
        xm2T = A.alloc("xm2T", [128, KT, 128], F32)
        junk = A.alloc("junk2", [128, D], BF16)
        for tl in range(TPC):
            i = ch * TPC + tl
            c = tile_cond[i]
            xt = x1t[i % 2]
            xtok = "x1t%d" % (i % 2)
            s.dma("sp", lambda e, xt=xt, i=i: e.dma_start(out=xt[:], in_=X1.ap()[i * 128:(i + 1) * 128, :]),
                  reads=["X1_%d_%d" % (i, g) for g in range(NG)], writes=[xtok])
            sc, tk = rms_rstd(xt[:], xtok, 2 + i % 2, D)
            a2, sh2 = rows2[c]
            s.op("dve", STT(xm2[:], xt[:], sc, a2[:], ALU.mult, ALU.mult), reads=[xtok, tk, "a2row%d" % c], writes=["xm2"])
            s.op("dve", TT(xm2[:], xm2[:], sh2[:], ALU.add), reads=["xm2", "sh2row%d" % c], writes=["xm2"])
            xb_ = xm2b[i % 2]
            s.op("act", lambda e, xb_=xb_: e.copy(out=xb_[:], in_=xm2[:]), reads=["xm2"], writes=["xm2b%d" % (i % 2)])
            s.dma("sp", lambda e, xb_=xb_, i=i: e.dma_start(out=XM2.ap()[i * 128:(i + 1) * 128, :], in_=xb_[:]),
                  reads=["xm2b%d" % (i % 2)], writes=["XM2_%d" % i])
            transposes([xm2[:, kt * 128:(kt + 1) * 128] for kt in range(KT)], lambda g, m: xm2T[:, g:g + m, :], ["xm2"],
                       lambda g, m: ["xm2T"], evac_eng="dve", f32=True)
            acc_mm(psS[0][:, 0:E], [(xm2T[:, kt, :], rw[:, kt, :]) for kt in range(KT)], ["xm2T", "rw"], ["psS0"])
            s.op("dve", TT(lgt[:], psS[0][:, 0:E], rbrow[:], ALU.add), reads=["psS0", "rbrow"], writes=["lgt"])
            if debug:
                dump("logits%d" % i, lgt[:], ["lgt"])
            s.op("dve", lambda e: e.max(out=top8[:], in_=lgt[:]), reads=["lgt"], writes=["top8"])
            s.op("dve", TS(Mf[:, i, :], lgt[:], top8[:, 3:4], None, ALU.is_ge), reads=["lgt", "top8"], writes=["Mf"])
            s.op("dve", TS(top8[:, 7:8], top8[:, 0:1], -1.0, None, ALU.mult), reads=["top8"], writes=["top8"])
            s.op("act", ACT(pex[:], lgt[:], AF.Exp, bias=top8[:, 7:8]), reads=["lgt", "top8"], writes=["pex"])
            s.op("dve", TT(pex[:], pex[:], Mf[:, i, :], ALU.mult), reads=["pex", "Mf"], writes=["pex"])
            s.op("dve", lambda e: e.reduce_sum(out=top8[:, 6:7], in_=pex[:], axis=AX.X), reads=["pex"], writes=["top8"])
            s.op("dve", lambda e: e.reciprocal(out=top8[:, 6:7], in_=top8[:, 6:7]), reads=["top8"], writes=["top8"])
            s.op("dve", TS(Wd[:, i, :], pex[:], top8[:, 6:7], None, ALU.mult), reads=["pex", "top8"], writes=["Wd"])
            s.op("dve", CP(Mb[:, i, :], Mf[:, i, :]), reads=["Mf"], writes=["Mb"])
        s.barrier()
    A.release(mP)

    if cfg.get("STOP") == "D":
        s.finish(); nc._cfg = cfg; nc._ninst = s.ninst; nc._peak = 0; nc._dbg = dbg_outs
        return nc
    destk = A.alloc("destk", [128, NT, TOPK], F32)
    destki = A.alloc("destki", [128, NT, TOPK], I32)
    wk = A.alloc("wk", [128, NT, TOPK], F32)
    SEGCAP = cfg.get("SEGCAP", 2048)
    SEGG, SEGD = min(SEGCAP, KT * CWG), min(SEGCAP, KT * CWD)
    AG, AD = KT * CWG // SEGG, KT * CWD // SEGD
    idxg = A.alloc("idxg", [128, NCJG, AG, NPAIR], I32)
    idxd = A.alloc("idxd", [128, NCJD, AD, NPAIR], I32)
    beig = A.alloc("beig", [128, NCJG, NPAIR], I32)
    beid = A.alloc("beid", [128, NCJD, NPAIR], I32)
    usedi = A.alloc("usedi", [128, NPAIR], I32)
    mE2 = A.mark()
    rank = A.alloc("rank", [128, NT, E], F32)
    dest = A.alloc("dest", [128, NT, E], F32)
    csel = A.alloc("csel", [128, NT, E], F32)
    oh = A.alloc("oh", [128, NT, E], F32)
    tmpE = A.alloc("tmpE", [128, NT, E], F32)
    cnt = A.alloc("cnt", [128, E], F32)
    NT2 = NT // 2
    cmpc = A.alloc("cmpc", [128, E, NT2], F32)
    padf = A.alloc("padf", [128, E], F32)
    pend = A.alloc("pend", [128, E], F32)
    pstart = A.alloc("pstart", [128, E], F32)
    tokidi = A.alloc("tokidi", [128, NT], I32)
    pe_ = A.alloc("pe", [128, NPAIR], F32)
    tpe = A.alloc("tpe", [128, NPAIR], F32)
    cmpb = A.alloc("cmpb", [128, NPAIR, E], F32)
    rtinit = A.alloc("rtinit", [128, 128], I32)
    tokrep = A.alloc("tokrep", [128, NT, 128], I32)

    for i in range(NT):
        pairs = [(onesb[:], Mb[:, j, :]) for j in range(i)] + [(slfb[:], Mb[:, i, :])]
        acc_mm(psS[i % 2][:, 0:E], pairs, ["onesb", "slfb", "Mb"], ["psS%d" % (i % 2)])
        s.op("dve", CP(rank[:, i, :], psS[i % 2][:, 0:E]), reads=["psS%d" % (i % 2)], writes=["rank"])
    acc_mm(psS[0][:, 0:E], [(onesb[:], Mb[:, j, :]) for j in range(NT)], ["onesb", "Mb"], ["psS0"])
    s.op("dve", CP(cnt[:], psS[0][:, 0:E]), reads=["psS0"], writes=["cnt"])
    pbo, _ = coff["pbase"]
    s.op("dve", TT(cmpc[:], cnt[:].unsqueeze(2).to_broadcast([128, E, NT2]), cst[:, pbo:pbo + NT2].unsqueeze(1).to_broadcast([128, E, NT2]), ALU.is_gt),
         reads=["cnt", "cst"], writes=["cmpc"])
    s.op("dve", lambda e: e.tensor_reduce(out=padf[:], in_=cmpc[:], axis=AX.X, op=ALU.add), reads=["cmpc"], writes=["padf"])
    s.op("dve", TS(padf[:], padf[:], 256.0, None, ALU.mult), reads=["padf"], writes=["padf"])
    oro, _ = coff["onesrow"]
    s.op("dve", lambda e: e.tensor_tensor_scan(out=pend[:], data0=cst[:, oro:oro + E], data1=padf[:], initial=0.0, op0=ALU.mult, op1=ALU.add),
         reads=["padf", "cst"], writes=["pend"])
    s.op("dve", TT(pstart[:], pend[:], padf[:], ALU.subtract), reads=["pend", "padf"], writes=["pstart"])
    for i in range(NT):
        s.op("dve", TT(dest[:, i, :], rank[:, i, :], pstart[:], ALU.add), reads=["rank", "pstart"], writes=["dest"])
        s.op("dve", lambda e, i=i: e.tensor_tensor_scan(out=csel[:, i, :], data0=cst[:, oro:oro + E], data1=Mf[:, i, :], initial=0.0,
                                                          op0=ALU.mult, op1=ALU.add), reads=["Mf", "cst"], writes=["csel"])
    for k in range(TOPK):
        s.op("dve", STT(oh[:], csel[:], float(k + 1), Mf[:], ALU.is_equal, ALU.mult), reads=["csel", "Mf"], writes=["oh"])
        s.op("dve", TT(tmpE[:], oh[:], dest[:], ALU.mult), reads=["oh", "dest"], writes=["tmpE"])
        s.op("dve", lambda e, k=k: e.tensor_reduce(out=destk[:, :, k], in_=tmpE[:], axis=AX.X, op=ALU.add), reads=["tmpE"], writes=["destk"])
        s.op("dve", TT(tmpE[:], oh[:], Wd[:], ALU.mult), reads=["oh", "Wd", "destk"], writes=["tmpE"])
        s.op("dve", lambda e, k=k: e.tensor_reduce(out=wk[:, :, k], in_=tmpE[:], axis=AX.X, op=ALU.add), reads=["tmpE"], writes=["wk"])
    s.op("dve", CP(destki[:], destk[:]), reads=["destk"], writes=["destki"])
    s.op("dve", CP(tokidi[:], C("tokid")), reads=["cst"], writes=["tokidi"])
    s.op("dve", TT(cmpb[:], pend[:].unsqueeze(1).to_broadcast([128, NPAIR, E]), cst[:, pbo:pbo + NPAIR].unsqueeze(2).to_broadcast([128, NPAIR, E]), ALU.is_le),
         reads=["pend", "cst"], writes=["cmpb"])
    s.op("dve", lambda e: e.tensor_reduce(out=pe_[:], in_=cmpb[:], axis=AX.X, op=ALU.add), reads=["cmpb"], writes=["pe"])
    cpo, _ = coff["cp"]
    tpe2 = A.alloc("tpe2", [128, NPAIR], F32)
    s.op("dve", TS(tpe2[:], pe_[:], float(E), None, ALU.is_lt), reads=["pe"], writes=["tpe2"])
    s.op("dve", CP(usedi[:], tpe2[:]), reads=["tpe2"], writes=["usedi"])
    for (ncj, na, idx_t, be_t) in ((NCJG, AG, idxg, beig), (NCJD, AD, idxd, beid)):
        for cj in range(ncj):
            s.op("dve", TS(tpe[:], pe_[:], float(ncj), float(cj), ALU.mult, ALU.add), reads=["pe", "idxtabs"], writes=["tpe"])
            s.op("dve", CP(be_t[:, cj, :], tpe[:]), reads=["tpe"], writes=["idxtabs"])
            s.op("dve", TS(tpe[:], tpe[:], 128.0, cst[:, cpo:cpo + 1], ALU.mult, ALU.add), reads=["tpe", "cst", "idxtabs"], writes=["tpe"])
            for a_ in range(na):
                s.op("dve", TS(tpe2[:], tpe[:], float(na), float(a_), ALU.mult, ALU.add), reads=["tpe", "idxtabs"], writes=["tpe2"])
                s.op("dve", CP(idx_t[:, cj, a_, :], tpe2[:]), reads=["tpe2"], writes=["idxtabs"])
    s.op("dve", lambda e: e.memset(rtinit[:], T), writes=["rtinit"])
    for b in range(NB):
        s.dma("sp", lambda e, b=b: e.dma_start(out=ROWTOK.ap()[b * 128:(b + 1) * 128, :], in_=rtinit[:]), reads=["rtinit"], writes=["ROWTOK%d" % b])
    for i in range(NT):
        s.op("dve", CP(tokrep[:, i, :], tokidi[:, i:i + 1].to_broadcast([128, 128])), reads=["tokidi"], writes=["tokrep"])
    rt_toks = []
    for i in range(NT):
        for k in range(TOPK):
            tk = "RT_%d_%d" % (i, k)
            rt_toks.append(tk)
            s.dma("pool", lambda e, i=i, k=k: e.indirect_dma_start(
                out=ROWTOK.ap(), out_offset=bass.IndirectOffsetOnAxis(ap=destki[:, i, k:k + 1], axis=0),
                in_=tokrep[:, i, :], in_offset=None, bounds_check=BC(e, R - 1), oob_is_err=False),
                reads=["ROWTOK%d" % b for b in range(NB)] + ["destki", "tokrep"], writes=[tk])
    if debug:
        dump("destk", destk[:].rearrange("p t k -> p (t k)"), ["destk"])
        dump("wk", wk[:].rearrange("p t k -> p (t k)"), ["wk"])
    s.barrier()
    if cfg.get("STOP") == "E":
        s.finish(); nc._cfg = cfg; nc._ninst = s.ninst; nc._peak = 0; nc._dbg = dbg_outs
        return nc
    A.release(mE2)
    mF = A.mark()
    FT = DFF // 128
    CWM = max(CWG, CWD)
    NSLOT = 3
    mslots = [A.alloc("mslot%d" % i, [128, KT * CWM], BF16) for i in range(NSLOT)]
    mctr = [0]
    rtb = [A.alloc("rtb%d" % i, [128, 16], I32) for i in range(2)]
    xg = [A.alloc("xg%d" % i, [128, D], BF16) for i in range(2)]
    xgT = [A.alloc("xgT%d" % i, [128, KT, 128], BF16) for i in range(2)]
    bch = [A.alloc("bch%d" % i, [128, CWM], BF16) for i in range(2)]
    bctr = [0]
    PWG = min(512, CWG)
    PWD = min(512, CWD)
    hb = [A.alloc("hb%d" % i, [128, PWG], F32) for i in range(2)]
    gt = A.alloc("gt", [128, PWG // 2], F32)
    ut = A.alloc("ut", [128, PWG // 2], F32)
    sgt = A.alloc("sgt", [128, PWG // 2], F32)
    actb = [A.alloc("actb%d" % i, [128, DFF], BF16) for i in range(2)]
    actT = [A.alloc("actT%d" % i, [128, FT, 128], BF16) for i in range(2)]
    yp = [A.alloc("yp%d" % i, [128, CWD], F32) for i in range(2)]
    ypc = [0]
    zt = A.alloc("zt", [128, CWD], F32)
    s.op("dve", lambda e: e.memset(zt[:], 0.0), writes=["zt"])
    for i in range(2):
        s.op("dve", lambda e, i=i: e.memset(xg[i][:], 0.0), writes=["xg%d" % i])
    for i in range(NSLOT):
        s.op("dve", lambda e, i=i: e.memset(mslots[i][:], 0.0), writes=["mslot%d_%d" % (i, a_) for a_ in range(max(AG, AD))])

    def mload(src_d, idx_tab, cj, g, nrows, ncols, seg, na):
        i = mctr[0] % NSLOT
        mctr[0] += 1
        slot = mslots[i]
        tok = "mslot%d" % i
        n = KT * ncols
        srcv = src_d.ap().rearrange("r (a s) -> (r a) s", s=seg)
        toks = []
        for a_ in range(na):
            s.dma("pool", lambda e, a_=a_: e.indirect_dma_start(
                out=slot[:, a_ * seg:(a_ + 1) * seg], out_offset=None, in_=srcv,
                in_offset=bass.IndirectOffsetOnAxis(ap=idx_tab[:, cj, a_, g:g + 1], axis=0),
                bounds_check=BC(e, nrows * na - 1), oob_is_err=False), reads=["idxtabs"], writes=[tok + "_%d" % a_])
            toks.append(tok + "_%d" % a_)
        return slot[:, 0:n].rearrange("p (k c) -> p k c", c=ncols), toks

    def bload(src_d, idx_ap, nrows, ncols):
        i = bctr[0] % 2
        bctr[0] += 1
        bt = bch[i]
        tok = "bch%d" % i
        s.dma("pool", lambda e: e.indirect_dma_start(
            out=bt[:, 0:ncols], out_offset=None, in_=src_d.ap(), in_offset=bass.IndirectOffsetOnAxis(ap=idx_ap, axis=0),
            bounds_check=BC(e, nrows - 1), oob_is_err=False), reads=["idxtabs"], writes=[tok])
        return bt, tok

    USE_CF = cfg.get("CF", True)
    for g in range(NPAIR):
        if USE_CF and g >= cfg.get("CF_MIN", T * TOPK // 256):
            s.group_begin(usedi[0:1, g:g + 1])
        for hf in range(2):
            b = 2 * g + hf
            s.dma("sp", lambda e, b=b, hf=hf: e.dma_start(out=rtb[hf][:], in_=ROWTOK.ap()[b * 128:(b + 1) * 128, 0:16]),
                  reads=rt_toks + ["ROWTOK%d" % b], writes=["rtb%d" % hf])
            s.dma("pool", lambda e, hf=hf: e.indirect_dma_start(
                out=xg[hf][:], out_offset=None, in_=XM2.ap(), in_offset=bass.IndirectOffsetOnAxis(ap=rtb[hf][:, 0:1], axis=0),
                bounds_check=BC(e, T - 1), oob_is_err=False), reads=["rtb%d" % hf] + ["XM2_%d" % i for i in range(NT)], writes=["xg%d" % hf])
            transposes([xg[hf][:, kt * 128:(kt + 1) * 128] for kt in range(KT)], lambda g_, m, hf=hf: xgT[hf][:, g_:g_ + m, :], ["xg%d" % hf],
                       lambda g_, m, hf=hf: ["xgT%d" % hf])
        for cj in range(NCJG):
            slot, stoks = mload(wgu_d, idxg, cj, g, E * NCJG * 128, CWG, SEGG, AG)
            bt, btok = bload(bgu_d, beig[:, cj, g:g + 1], E * NCJG, CWG)
            npc = CWG // PWG
            for hf in range(2):
                for pc in range(npc):
                    bank = (hf * npc + pc) % 4
                    acc_mm(psA[bank][:, 0:PWG], [(xgT[hf][:, kt, :], slot[:, kt, pc * PWG:(pc + 1) * PWG]) for kt in range(KT)],
                           ["xgT%d" % hf] + stoks, ["psA%d" % bank])
                    hbb = hb[pc % 2]
                    htok = "hb%d" % (pc % 2)
                    HW_ = PWG // 2
                    s.op("dve", TT(hbb[:], psA[bank][:, 0:PWG], bt[:, pc * PWG:(pc + 1) * PWG], ALU.add), reads=["psA%d" % bank, btok], writes=[htok])
                    hv = hbb[:].rearrange("p (f two) -> p f two", two=2)
                    s.op("dve", TS(gt[:], hv[:, :, 0], 7.0, None, ALU.min), reads=[htok], writes=["gt"])
                    s.op("dve", TS(ut[:], hv[:, :, 1], -7.0, 7.0, ALU.max, ALU.min), reads=[htok], writes=["ut"])
                    s.op("act", ACT(sgt[:], gt[:], AF.Sigmoid, scale=1.702), reads=["gt"], writes=["sgt"])
                    s.op("dve", TT(gt[:], gt[:], sgt[:], ALU.mult), reads=["gt", "sgt"], writes=["gt"])
                    f0 = (cj * CWG + pc * PWG) // 2
                    s.op("dve", STT(actb[hf][:, f0:f0 + HW_], ut[:], 1.0, gt[:], ALU.add, ALU.mult), reads=["ut", "gt"], writes=["actb%d" % hf])
        for hf in range(2):
            transposes([actb[hf][:, ft * 128:(ft + 1) * 128] for ft in range(FT)], lambda g_, m, hf=hf: actT[hf][:, g_:g_ + m, :], ["actb%d" % hf],
                       lambda g_, m, hf=hf: ["actT%d" % hf])
        for cj in range(NCJD):
            slot, stoks = mload(wdn_d, idxd, cj, g, E * NCJD * 128, CWD, SEGD, AD)
            bt, btok = bload(bdn_d, beid[:, cj, g:g + 1], E * NCJD, CWD)
            npc = CWD // PWD
            for hf in range(2):
                b = 2 * g + hf
                ypb = yp[ypc[0] % 2]
                ytok = "yp%d" % (ypc[0] % 2)
                ypc[0] += 1
                for pc in range(npc):
                    bank = (hf * npc + pc) % 4
                    acc_mm(psA[bank][:, 0:PWD], [(actT[hf][:, kt, :], slot[:, kt, pc * PWD:(pc + 1) * PWD]) for kt in range(KT)],
                           ["actT%d" % hf] + stoks, ["psA%d" % bank])
                    s.op("dve", TT(ypb[:, pc * PWD:(pc + 1) * PWD], psA[bank][:, 0:PWD], bt[:, pc * PWD:(pc + 1) * PWD], ALU.add),
                         reads=["psA%d" % bank, btok], writes=[ytok])
                s.dma("sp", lambda e, ypb=ypb, b=b, cj=cj: e.dma_start(out=Y.ap()[b * 128:(b + 1) * 128, cj * CWD:(cj + 1) * CWD], in_=ypb[:]),
                      reads=[ytok], writes=["Y_%d_%d" % (b, cj)])
        if USE_CF and g >= cfg.get("CF_MIN", T * TOPK // 256):
            s.group_end(else_dmas={"sp": [(Y.ap()[(2 * g + hf) * 128:(2 * g + hf + 1) * 128, cj * CWD:(cj + 1) * CWD], zt[:])
                                          for hf in range(2) for cj in range(NCJD)]})
    s.barrier()
    A.release(mF)

    if cfg.get("STOP") == "F":
        s.finish(); nc._cfg = cfg; nc._ninst = s.ninst; nc._peak = 0; nc._dbg = dbg_outs
        return nc
    y_toks = ["Y_%d_%d" % (b, cj) for b in range(NB) for cj in range(NCJD)]
    yk = [A.alloc("yk%d" % k, [128, D], F32) for k in range(TOPK)]
    acc = A.alloc("acc", [128, D], F32)
    x1g = A.alloc("x1g", [128, D], F32)
    g2row = {}
    for c in range(2):
        g2row[c] = A.alloc("g2row%d" % c, [128, D], F32)
        load_row(g2row[c][:], c, 5, "g2row%d" % c)
    nfrow = A.alloc("nfrow", [128, D], F32)
    load_vec_row(nfrow[:], nfg_d, "nfrow")
    junk = A.alloc("junk3", [128, D], BF16)
    yout = [A.alloc("yout%d" % i, [128, D], F32) for i in range(2)]
    for k in range(TOPK):
        s.op("dve", lambda e, k=k: e.memset(yk[k][:], 0.0), writes=["yk%d" % k])
    for i in range(NT):
        c = tile_cond[i]
        for k in range(TOPK):
            s.dma("pool", lambda e, i=i, k=k: e.indirect_dma_start(
                out=yk[k][:], out_offset=None, in_=Y.ap(), in_offset=bass.IndirectOffsetOnAxis(ap=destki[:, i, k:k + 1], axis=0),
                bounds_check=BC(e, R - 1), oob_is_err=False), reads=y_toks + ["destki"], writes=["yk%d" % k])
        s.dma("sp", lambda e, i=i: e.dma_start(out=x1g[:], in_=X1.ap()[i * 128:(i + 1) * 128, :]), writes=["x1g"])
        s.op("dve", TS(acc[:], yk[0][:], wk[:, i, 0:1], None, ALU.mult), reads=["yk0", "wk"], writes=["acc"])
        for k in range(1, TOPK):
            s.op("dve", STT(acc[:], yk[k][:], wk[:, i, k:k + 1], acc[:], ALU.mult, ALU.add), reads=["yk%d" % k, "wk", "acc"], writes=["acc"])
        s.op("dve", TT(acc[:], acc[:], g2row[c][:], ALU.mult), reads=["acc", "g2row%d" % c], writes=["acc"])
        s.op("dve", TT(acc[:], acc[:], x1g[:], ALU.add), reads=["acc", "x1g"], writes=["acc"])
        sc, tk = rms_rstd(acc[:], "acc", 4 + i % 2, D)
        yo = yout[i % 2]
        s.op("dve", STT(yo[:], acc[:], sc, nfrow[:], ALU.mult, ALU.mult), reads=["acc", tk, "nfrow"], writes=["yout%d" % (i % 2)])
        s.dma("sp", lambda e, yo=yo, i=i: e.dma_start(out=y_o.ap()[i * 128:(i + 1) * 128, :], in_=yo[:]), reads=["yout%d" % (i % 2)], writes=["y_%d" % i])
    s.finish()
    nc._cfg = cfg
    nc._ninst = s.ninst
    nc._peak = A.peak - A.base
    nc._dbg = dbg_outs
    return nc


def prepare_core_inputs(inp, cfg, core):
    cfg = derive(cfg)
    D, H, KT, NP = cfg["D"], cfg["H"], cfg["KT"], cfg["NP"]
    f = np.float32
    xp = np.asarray(inp["x_prompt"], f)
    xs = np.asarray(inp["x_sample"], f)
    x = np.concatenate([xp[core * NP + p] for p in range(NP)] + [xs[core]], axis=0)
    c_ctx = np.asarray(inp["c_ctx"], f)
    c = np.asarray(inp["c"], f)[core]
    cT = np.stack([c_ctx.reshape(KT, 128).T, c.reshape(KT, 128).T], axis=-1)

    def fm(v, n):
        return np.ascontiguousarray(np.asarray(v, f).reshape(n, 128).T)

    lbf = np.stack([fm(inp["hg_lb_fwd"][0], H), fm(inp["hg_lb_fwd"][1], H)], axis=1)
    lbb = np.stack([fm(inp["hg_lb_bwd"][0], H), fm(inp["hg_lb_bwd"][1], H)], axis=1)
    m = {
        "x": np.ascontiguousarray(x),
        "shf": np.ascontiguousarray(np.asarray(inp["state_hgrn_fwd"], f)[core, 0]),
        "shb": np.ascontiguousarray(np.asarray(inp["state_hgrn_bwd"], f)[core, 0]),
        "srf": np.ascontiguousarray(np.asarray(inp["state_ret_fwd"], f)[core, 0]),
        "srb": np.ascontiguousarray(np.asarray(inp["state_ret_bwd"], f)[core, 0]),
        "cT": np.ascontiguousarray(cT),
        "lbf": np.ascontiguousarray(lbf), "lbb": np.ascontiguousarray(lbb),
    }
    return m


def prepare_shared_inputs(inp, cfg):
    cfg = derive(cfg)
    D, H, KT, E = cfg["D"], cfg["H"], cfg["KT"], cfg["E"]
    f = np.float32
    perm = w_in_perm_index(cfg)
    sh = {
        "ada_w": np.ascontiguousarray(np.asarray(inp["ada_w"], f)[0]),
        "ada_b2": np.ascontiguousarray(np.broadcast_to(np.asarray(inp["ada_b"], f)[0][None, :], (2, 6 * D))),
        "n1g": np.asarray(inp["norm1_g"], f)[0][None, :].copy(),
        "n2g": np.asarray(inp["norm2_g"], f)[0][None, :].copy(),
        "nfg": np.asarray(inp["final_norm_g"], f)[None, :].copy(),
        "w_in": np.ascontiguousarray(np.asarray(inp["w_in"], f)[0][:, perm]),
        "hgng": np.asarray(inp["hg_norm_g"], f)[0].reshape(128, 1).copy(),
        "retg": np.ascontiguousarray(np.asarray(inp["ret_norm_g"], f)[0].reshape(2, 128).T),
        "rl2f": np.asarray(inp["ret_log2_fwd"], f)[0][None, :].copy(),
        "rl2b": np.asarray(inp["ret_log2_bwd"], f)[0][None, :].copy(),
        "w_pa": np.ascontiguousarray(np.asarray(inp["w_proj_hgrn"], f)[0]),
        "w_pb": np.ascontiguousarray(np.asarray(inp["w_proj_ret"], f)[0]),
        "w_out": np.ascontiguousarray(np.asarray(inp["w_out"], f)[0]),
        "rw": np.ascontiguousarray(np.asarray(inp["router_w"], f)[0]),
        "rb": np.asarray(inp["router_b"], f)[0][None, :].copy(),
        "w_gu": np.ascontiguousarray(np.asarray(inp["moe_w_gu"], f)[0].reshape(E, KT, 128, cfg["NCJG"], cfg["CWG"]).transpose(0, 3, 2, 1, 4)).reshape(E * cfg["NCJG"] * 128, KT * cfg["CWG"]),
        "b_gu": np.ascontiguousarray(np.asarray(inp["moe_b_gu"], f)[0]).reshape(E * cfg["NCJG"], cfg["CWG"]),
        "w_dn": np.ascontiguousarray(np.asarray(inp["moe_w_dn"], f)[0].reshape(E, KT, 128, cfg["NCJD"], cfg["CWD"]).transpose(0, 3, 2, 1, 4)).reshape(E * cfg["NCJD"] * 128, KT * cfg["CWD"]),
        "b_dn": np.ascontiguousarray(np.asarray(inp["moe_b_dn"], f)[0]).reshape(E * cfg["NCJD"], cfg["CWD"]),
        "cst": make_consts(cfg)[0],
        "rope": make_consts(cfg)[1],
    }
    return sh


def run(inp, cfg, runner=None, debug=False):
    cfgd = derive(cfg)
    ncores = cfgd["NCORES"]
    nc = build(cfg, debug=debug)
    shared = prepare_shared_inputs(inp, cfg)
    in_maps = []
    for core in range(ncores):
        m = dict(shared)
        m.update(prepare_core_inputs(inp, cfg, core))
        in_maps.append(m)
    if runner is None:
        res = run_bass_kernel_spmd(nc, in_maps, core_ids=list(range(ncores))).results
    else:
        res = runner(nc, in_maps)
    NP, TP, TS_, D, H = cfgd["NP"], cfgd["TP"], cfgd["TS"], cfgd["D"], cfgd["H"]
    yp = np.stack([res[c]["y"][p * TP:(p + 1) * TP] for c in range(ncores) for p in range(NP)], axis=0)
    ys = np.stack([res[c]["y"][NP * TP:] for c in range(ncores)], axis=0)
    hf = np.concatenate([res[c]["hf"] for c in range(ncores)], axis=0)[:, None]
    hb = np.concatenate([res[c]["hb"] for c in range(ncores)], axis=0)[:, None]
    rf = np.concatenate([res[c]["rf"] for c in range(ncores)], axis=0)[:, None]
    rb = np.concatenate([res[c]["rb_o"] for c in range(ncores)], axis=0)[:, None]
    outs = tuple(np.ascontiguousarray(a.astype(np.float32)) for a in (yp, ys, hf, hb, rf, rb))
    return outs, res, nc


def kernel(**inputs):
    outs, _, _ = run(inputs, FULL_CFG)
    return outs
```

```python
import math
from contextlib import ExitStack
import numpy as np
import ml_dtypes
import concourse.bass as bass
import concourse.mybir as mybir
from concourse.bass_utils import run_bass_kernel_spmd

F32 = mybir.dt.float32
BF16 = mybir.dt.bfloat16
I32 = mybir.dt.int32
AF = mybir.ActivationFunctionType
ALU = mybir.AluOpType
AX = mybir.AxisListType
EPS = 1e-6
CHUNK = 32
GRID_W = 64
ROPE_PAIRS = 32
TOPK = 4

FULL_CFG = dict(D=2048, H=8, TP=256, NP=2, TS=1024, E=32, NCORES=8)


class Sched:
    CE = ("pe", "act", "dve", "pool")

    def __init__(self, nc, nring=8):
        self.nc = nc
        self.q = {k: [] for k in ("pe", "act", "dve", "pool", "sp")}
        self.psem = {k: nc.alloc_semaphore(name="ps_" + k) for k in self.CE}
        self.pcnt = {k: 0 for k in self.CE}
        self.waited = {}
        self.ring = {}
        for qn in ("sp", "pool"):
            self.ring[qn] = dict(sems=[nc.alloc_semaphore(name="d_%s_%d" % (qn, i)) for i in range(nring)],
                                 vals=[0] * nring, nxt=0)
        self.lw = {}
        self.rd = {}
        self.ninst = 0
        self.dummy = None
        self.dummy_ctr = 0

    def _wait(self, engn, ev):
        if ev is None:
            return
        sem, val = ev
        key = (engn, id(sem))
        if self.waited.get(key, 0) >= val:
            return
        self.waited[key] = val
        self.q[engn].append(lambda e, sem=sem, val=val: e.wait_ge(sem, val))
        self.ninst += 1
        for g in getattr(self, "_gstack", []):
            if val <= g["snap"].get(id(sem), 0):
                d = g["waits"].setdefault(engn, {})
                if d.get(id(sem), (None, 0))[1] < val:
                    d[id(sem)] = (sem, val)

    def _deps(self, engn, reads, writes):
        for t in reads:
            self._wait(engn, self.lw.get(t))
        for t in writes:
            self._wait(engn, self.lw.get(t))
            for ev in self.rd.get(t, {}).values():
                self._wait(engn, ev)

    def _commit(self, ev, reads, writes):
        sem, val = ev
        for t in writes:
            self.lw[t] = ev
            self.rd[t] = {}
        for t in reads:
            d = self.rd.setdefault(t, {})
            old = d.get(id(sem))
            if old is None or old[1] < val:
                d[id(sem)] = ev

    @staticmethod
    def _excl(reads, writes):
        pr = [t for t in reads if t.startswith("ps")]
        if not pr:
            return list(reads), list(writes)
        return [t for t in reads if not t.startswith("ps")], list(writes) + [t for t in pr if t not in writes]

    def op(self, engn, fn, reads=(), writes=(), inc=True):
        reads, writes = self._excl(reads, writes)
        self._deps(engn, reads, writes)
        self.ninst += 1
        if inc:
            self.pcnt[engn] += 1
            sem = self.psem[engn]
            val = self.pcnt[engn]
            self.q[engn].append(lambda e, fn=fn, sem=sem: fn(e).then_inc(sem, 1))
            ev = (sem, val)
            self._commit(ev, reads, writes)
            return ev
        self.q[engn].append(lambda e, fn=fn: fn(e))
        return None

    def dma(self, qn, fn, reads=(), writes=()):
        r = self.ring[qn]
        i = r["nxt"]
        r["nxt"] = (i + 1) % len(r["sems"])
        sem = r["sems"][i]
        if r["vals"][i] > 0:
            self._wait(qn, (sem, r["vals"][i]))
        self._deps(qn, reads, writes)
        r["vals"][i] += 16
        val = r["vals"][i]
        self.q[qn].append(lambda e, fn=fn, sem=sem: fn(e).then_inc(sem, 16))
        self.ninst += 1
        ev = (sem, val)
        self._commit(ev, reads, writes)
        return ev

    def group_begin(self, flag_ap):
        self.waited.clear()
        if not hasattr(self, "_gstack"):
            self._gstack = []
        snap = {id(self.psem[k]): self.pcnt[k] for k in self.CE}
        for r in self.ring.values():
            for sem, v in zip(r["sems"], r["vals"]):
                snap[id(sem)] = v
        self._gstack.append(dict(flag=flag_ap, pcnt=dict(self.pcnt), rings={qn: list(r["vals"]) for qn, r in self.ring.items()},
                                 snap=snap, waits={}))
        for qn in self.q:
            self.q[qn].append(("IF", flag_ap))

    def group_end(self, else_dmas=None):
        g = self._gstack.pop()
        else_dmas = else_dmas or {}
        for qn in self.q:
            comp = []
            if qn in self.CE and self.pcnt[qn] > g["pcnt"][qn]:
                comp.append(("drain_inc", self.psem[qn], self.pcnt[qn] - g["pcnt"][qn]))
            if qn in self.ring:
                r = self.ring[qn]
                for sem, v0, v1 in zip(r["sems"], g["rings"][qn], r["vals"]):
                    d = v1 - v0
                    first = True
                    while d > 0:
                        k = min(16, d)
                        comp.append(("wait_inc", sem, v0 if first else 0, k))
                        first = False
                        d -= k
            self.q[qn].append(("ELSE_END", comp, list(else_dmas.get(qn, [])), list(g["waits"].get(qn, {}).values())))
        self.waited.clear()

    def _replay(self, qn, e):
        regs = []
        stack = []
        for t in self.q[qn]:
            if isinstance(t, tuple):
                if t[0] == "IF":
                    lvl = len(stack)
                    while len(regs) <= lvl:
                        regs.append(e.alloc_register("flag_%s_%d" % (qn, len(regs))))
                    e.reg_load(regs[lvl], t[1])
                    ctx = e.If_eq(regs[lvl], 1)
                    ctx.__enter__()
                    stack.append(ctx)
                else:
                    ctx = stack.pop()
                    ctx.__exit__(None, None, None)
                    if t[1] or t[3]:
                        c2 = e.Else()
                        c2.__enter__()
                        for (wsem, wval) in t[3]:
                            e.wait_ge(wsem, wval)
                        useful = list(t[2])
                        for c in t[1]:
                            if c[0] == "drain_inc":
                                e.drain().then_inc(c[1], c[2])
                            else:
                                if c[2] > 0:
                                    e.wait_ge(c[1], c[2])
                                if useful:
                                    o_, i_ = useful.pop(0)
                                    e.dma_start(out=o_, in_=i_).then_inc(c[1], c[3])
                                else:
                                    k = self.dummy_ctr
                                    self.dummy_ctr += 1
                                    e.dma_start(out=self.dummy[1][k:k + 1, :], in_=self.dummy[0]).then_inc(c[1], c[3])
                        c2.__exit__(None, None, None)
            else:
                t(e)

    def barrier(self):
        evs = [(self.psem[k], self.pcnt[k]) for k in self.CE if self.pcnt[k] > 0]
        for r in self.ring.values():
            evs += [(sem, v) for sem, v in zip(r["sems"], r["vals"]) if v > 0]
        for qn in self.q:
            for ev in evs:
                self._wait(qn, ev)

    def finish(self):
        self.barrier()
        with self.nc.Block() as block:
            @block.tensor
            def _(e):
                self._replay("pe", e)

            @block.scalar
            def _(e):
                self._replay("act", e)

            @block.vector
            def _(e):
                self._replay("dve", e)

            @block.gpsimd
            def _(e):
                self._replay("pool", e)

            @block.sync
            def _(e):
                self._replay("sp", e)


class _Stop(Exception):
    pass


class Arena:
    def __init__(self, nc):
        self.nc = nc
        self.base = (nc.sbuf_base + 31) // 32 * 32
        self.top = nc.sbuf_top // 32 * 32
        self.cur = self.base
        self.n = 0
        self.peak = self.cur

    def alloc(self, name, shape, dtype):
        sz = {F32: 4, BF16: 2, I32: 4}[dtype]
        nbytes = int(np.prod(shape[1:])) * sz
        nbytes = (nbytes + 31) // 32 * 32
        assert self.cur + nbytes <= self.top, "SBUF overflow at %s: need %d have %d" % (name, nbytes, self.top - self.cur)
        self.n += 1
        t = self.nc.alloc_sbuf_tensor_at("%s_%d" % (name, self.n), list(shape), dtype, offset=self.cur)
        self.cur += nbytes
        self.peak = max(self.peak, self.cur)
        return t

    def mark(self):
        return self.cur

    def release(self, m):
        self.cur = m


def TS(out, in0, s1, s2, op0, op1=None):
    if op1 is None:
        return lambda e: e.tensor_scalar(out=out, in0=in0, scalar1=s1, scalar2=None, op0=op0)
    return lambda e: e.tensor_scalar(out=out, in0=in0, scalar1=s1, scalar2=s2, op0=op0, op1=op1)


def TT(out, a, b, op):
    return lambda e: e.tensor_tensor(out=out, in0=a, in1=b, op=op)


def STT(out, in0, sc, in1, op0, op1):
    return lambda e: e.scalar_tensor_tensor(out=out, in0=in0, scalar=sc, in1=in1, op0=op0, op1=op1)


def ACT(out, in_, func, bias=None, scale=None, accum=None):
    kw = {}
    if bias is not None:
        kw["bias"] = bias
    if scale is not None:
        kw["scale"] = scale
    if accum is not None:
        kw["accum_out"] = accum
    return lambda e: e.activation(out=out, in_=in_, func=func, **kw)


def CP(out, in_):
    return lambda e: e.tensor_copy(out=out, in_=in_)


def const_layout(cfg):
    T, TS_, E, KT, NB = cfg["T"], cfg["TS"], cfg["E"], cfg["KT"], cfg["NB"]
    NT = T // 128
    names = [("ident", 128), ("mf", 128), ("mb", 128), ("slf", 128), ("slb", 128), ("i2", 128),
             ("dpos", 128), ("dneg", 128), ("iota1", 128), ("iotar", 128), ("ones", 128), ("psw", 128),
             ("c127", 1), ("cp", 1), ("cmask", 4), ("reset", cfg["CH"]), ("iotae", E),
             ("tokid", NT), ("iotaw", KT), ("bbase", NB), ("pbase", cfg["NPAIR"]), ("onesrow", max(E, 8))]
    off = {}
    c = 0
    for n, w in names:
        off[n] = (c, w)
        c += w
    return off, c


def make_consts(cfg):
    off, ncol = const_layout(cfg)
    T, TS_, E, KT, NB = cfg["T"], cfg["TS"], cfg["E"], cfg["KT"], cfg["NB"]
    NT = T // 128
    C = np.zeros((128, ncol), np.float32)

    def put(n, a):
        o, w = off[n]
        C[:, o:o + w] = a

    s = np.arange(128)[:, None]
    t = np.arange(128)[None, :]
    same = (s // CHUNK) == (t // CHUNK)
    put("ident", (s == t))
    put("mf", same & (s <= t))
    put("mb", same & (s >= t))
    put("slf", s < t)
    put("slb", s > t)
    put("i2", 2.0 * (s == t))
    put("dpos", np.maximum(t - s, 0))
    put("dneg", np.maximum(s - t, 0))
    put("iota1", np.broadcast_to(t + 1, (128, 128)))
    put("iotar", np.broadcast_to(128 - t, (128, 128)))
    put("ones", 1.0)
    d = np.arange(128)
    partner = np.where((d % 64) < 32, d + 32, d - 32)
    psw = np.zeros((128, 128), np.float32)
    psw[partner, d] = 1.0
    put("psw", psw)
    put("c127", 127 - s)
    put("cp", s)
    put("cmask", (s // CHUNK) == np.arange(4)[None, :])
    put("reset", np.broadcast_to((np.arange(cfg["CH"]) % CHUNK != 0).astype(np.float32), (128, cfg["CH"])))
    tok = np.arange(TS_)
    rows = (tok // GRID_W).astype(np.float32)
    cols = (tok % GRID_W).astype(np.float32)
    inv = (10000.0 ** (-np.arange(ROPE_PAIRS, dtype=np.float32) / ROPE_PAIRS)).astype(np.float32)
    pos = np.where((d[:, None] // 64) == 0, rows[None, :], cols[None, :]).astype(np.float32)
    ang = (pos * inv[d % 32][:, None]).astype(np.float32)
    sign = np.where((d % 64) < 32, -1.0, 1.0)[:, None]
    rope = np.concatenate([np.cos(ang), np.sin(ang) * sign], axis=1).astype(np.float32)
    put("iotae", np.broadcast_to(np.arange(E), (128, E)))
    put("tokid", np.arange(NT)[None, :] * 128 + s)
    put("iotaw", np.arange(KT)[None, :] * 128 + s)
    put("bbase", np.broadcast_to(np.arange(NB) * 128, (128, NB)))
    put("pbase", np.broadcast_to(np.arange(cfg["NPAIR"]) * 128 * cfg["GH"], (128, cfg["NPAIR"])))
    put("onesrow", 1.0)
    return C, rope


def derive(cfg):
    cfg = dict(cfg)
    D, H = cfg["D"], cfg["H"]
    cfg["KT"] = D // 128
    cfg["T"] = cfg["NP"] * cfg["TP"] + cfg["TS"]
    cfg["NT"] = cfg["T"] // 128
    cfg["CH"] = min(512, cfg["T"])
    assert cfg["T"] % cfg["CH"] == 0
    cfg["GH"] = cfg.get("GH", 4)
    cfg["NPAIR"] = cfg["T"] * TOPK // (128 * cfg["GH"]) + cfg["E"]
    cfg["NB"] = cfg["GH"] * cfg["NPAIR"]
    cap = cfg.get("CWCAP", 1024)
    cfg["CWG"] = min(cap, 2 * cfg["D"])
    cfg["CWD"] = min(cap, cfg["D"])
    cfg["NCJG"] = 2 * cfg["D"] // cfg["CWG"]
    cfg["NCJD"] = cfg["D"] // cfg["CWD"]
    cfg["HGC"] = 640
    cfg["RTC"] = 768
    cfg["MG0"] = H * 640 + H * 768
    cfg["INC"] = cfg["MG0"] + 2 * D
    return cfg


def w_in_perm_index(cfg):
    D, H = cfg["D"], cfg["H"]
    HK = H * 128
    RW = H * 256
    o_hq, o_zf, o_zb, o_hi, o_hg = 0, HK, 2 * HK, 3 * HK, 4 * HK
    o_rq = 5 * HK
    o_rk = o_rq + HK
    o_rv = o_rk + HK
    o_rg = o_rv + RW
    o_ma = o_rg + RW
    o_mb = o_ma + D
    idx = []
    for h in range(H):
        r = np.arange(128)
        idx += [o_hq + h * 128 + r, o_zf + h * 128 + r, o_zb + h * 128 + r, o_hg + h * 128 + r, o_hi + h * 128 + r]
    for h in range(H):
        r = np.arange(128)
        r2 = np.arange(256)
        idx += [o_rq + h * 128 + r, o_rk + h * 128 + r, o_rg + h * 256 + r2, o_rv + h * 256 + r2]
    idx += [o_ma + np.arange(D), o_mb + np.arange(D)]
    return np.concatenate(idx)


def build(cfg, debug=False):
    cfg = derive(cfg)
    D, H, KT, T, NT, E, NB, CH = cfg["D"], cfg["H"], cfg["KT"], cfg["T"], cfg["NT"], cfg["E"], cfg["NB"], cfg["CH"]
    TP, NP, TS_ = cfg["TP"], cfg["NP"], cfg["TS"]
    DFF = D
    NCHK = T // CH
    TPC = CH // 128
    NCK = T // CHUNK
    R = NB * 128
    GW = min(512, D)
    NG = D // GW
    JT = GW // 128
    coff, ncst = const_layout(cfg)

    nc = bass.Bass("TRN2", target_bir_lowering=False)
    s = Sched(nc)
    A = Arena(nc)
    dbg_outs = {}

    def din(name, shape, dt=F32):
        return nc.dram_tensor(name, list(shape), dt, kind="ExternalInput")

    def dscr(name, shape, dt=F32):
        return nc.dram_tensor(name, list(shape), dt, kind="Internal")

    def dout(name, shape, dt=F32):
        return nc.dram_tensor(name, list(shape), dt, kind="ExternalOutput")

    x_d = din("x", [T, D])
    shf_d, shb_d = din("shf", [H, 128, 128]), din("shb", [H, 128, 128])
    srf_d, srb_d = din("srf", [H, 128, 256]), din("srb", [H, 128, 256])
    cT_d = din("cT", [128, KT, 2])
    adaw_d = din("ada_w", [D, 6 * D])
    adab_d = din("ada_b2", [2, 6 * D])
    n1g_d, n2g_d, nfg_d = din("n1g", [1, D]), din("n2g", [1, D]), din("nfg", [1, D])
    win_d = din("w_in", [D, cfg["INC"]])
    lbf_d, lbb_d = din("lbf", [128, 2, H]), din("lbb", [128, 2, H])
    hgng_d = din("hgng", [128, 1])
    retg_d = din("retg", [128, 2])
    rl2f_d, rl2b_d = din("rl2f", [1, H]), din("rl2b", [1, H])
    wpa_d, wpb_d, wout_d = din("w_pa", [H * 128, D]), din("w_pb", [H * 256, D]), din("w_out", [D, D])
    rw_d, rb_d = din("rw", [D, E]), din("rb", [1, E])
    CWG, CWD, NCJG, NCJD, NPAIR = cfg["CWG"], cfg["CWD"], cfg["NCJG"], cfg["NCJD"], cfg["NPAIR"]
    wgu_d, bgu_d = din("w_gu", [E * NCJG * 128, KT * CWG]), din("b_gu", [E * NCJG, CWG])
    wdn_d, bdn_d = din("w_dn", [E * NCJD * 128, KT * CWD]), din("b_dn", [E * NCJD, CWD])
    cst_d = din("cst", [128, ncst])
    rope_d = din("rope", [128, 2 * TS_])

    y_o = dout("y", [T, D])
    hf_o, hb_o = dout("hf", [NP, H, 128, 128]), dout("hb", [NP, H, 128, 128])
    rf_o, rb_o = dout("rf", [NP, H, 128, 256]), dout("rb_o", [NP, H, 128, 256])

    MOD = dscr("MOD", [2, 6 * D])
    OAB = dscr("OAB", [3 * H, 128, T], BF16)
    X1 = dscr("X1", [T, D])
    XM2 = dscr("XM2", [T, D], BF16)
    ROWTOK = dscr("ROWTOK", [R, 128], I32)
    Y = dscr("Y", [R, D])

    def dump(name, ap, reads):
        if not debug:
            return
        shp = list(ap.shape)
        o = dout("dbg_" + name, shp, ap.dtype)
        dbg_outs[name] = shp
        s.dma("sp", lambda e: e.dma_start(out=o.ap(), in_=ap), reads=reads, writes=["dbg_" + name])

    psA = [nc.alloc_psum_tensor("psA%d" % i, [128, 512], F32) for i in range(4)]
    psS = [nc.alloc_psum_tensor("psS%d" % i, [128, 512], F32) for i in range(2)]
    psTbs = [nc.alloc_psum_tensor("psTb%d" % i, [128, 8, 128], BF16) for i in range(2)]

    cst = A.alloc("cst", [128, ncst], F32)
    dmy = A.alloc("dmy", [1, 32], F32)

    def C(name):
        o, w = coff[name]
        return cst[:, o:o + w]

    identb = A.alloc("identb", [128, 128], BF16)
    onesb = A.alloc("onesb", [128, 128], BF16)
    slfb = A.alloc("slfb", [128, 128], BF16)
    small = A.alloc("small", [128, 64], F32)
    lb = A.alloc("lb", [128, 2, 2, H], F32)
    lg = A.alloc("lg", [128, 2, H], F32)
    hgng = A.alloc("hgng", [128, 1], F32)
    retg = A.alloc("retg", [128, 2], F32)
    oab_o = [A.alloc("oabo%d" % i, [128, CH], BF16) for i in range(2)]
    oab_ctr = [0]
    rw = A.alloc("rw", [128, KT, E], F32)
    rbrow = A.alloc("rbrow", [128, E], F32)
    Mf = A.alloc("Mf", [128, NT, E], F32)
    Mb = A.alloc("Mb", [128, NT, E], BF16)
    Wd = A.alloc("Wd", [128, NT, E], F32)
    lgt = A.alloc("lgt", [128, E], F32)
    pex = A.alloc("pex", [128, E], F32)
    top8 = A.alloc("top8", [128, 8], F32)
    lbraw = A.alloc("lbraw", [128, 2, 2, H], F32)
    scb = A.alloc("scb", [128, KT, 2], BF16)
    mP = A.mark()
    WS = 768
    wslots = [A.alloc("wslot%d" % i, [128, max(KT, 2 * H), WS], BF16) for i in range(2)]
    wctr = [0]
    xmT = A.alloc("xmT", [128, KT, T], BF16)

    s.op("dve", lambda e: e.memset(dmy[:], 0.0), writes=["dmy"])
    DUMMY = dscr("DUMMY", [4096, 16])
    s.dummy = (dmy[0:1, 0:16], DUMMY.ap())
    s.dma("sp", lambda e: e.dma_start(out=cst[:], in_=cst_d.ap()), writes=["cst"])
    s.op("dve", CP(identb[:], C("ident")), reads=["cst"], writes=["identb"])
    s.op("dve", CP(onesb[:], C("ones")), reads=["cst"], writes=["onesb"])
    s.op("dve", CP(slfb[:], C("slf")), reads=["cst"], writes=["slfb"])
    s.dma("sp", lambda e: e.dma_start(out=hgng[:], in_=hgng_d.ap()), writes=["hgng"])
    s.dma("sp", lambda e: e.dma_start(out=retg[:], in_=retg_d.ap()), writes=["retg"])

    def wload(src2d, kt_n, ncols):
        i = wctr[0] % 2
        wctr[0] += 1
        slot = wslots[i]
        tok = "w%d" % i
        s.dma("pool", lambda e: e.dma_start(out=slot[:, 0:kt_n, 0:ncols],
                                            in_=src2d.rearrange("(kt p) n -> p kt n", p=128)), writes=[tok])
        return slot, tok

    _bc = {}

    def BC(e, v):
        if v not in _bc:
            _bc[v] = e.to_reg(v)
        return _bc[v]

    def mms(items, reads, writes):
        n = len(items)
        for i, (o, l, r, st, sp_) in enumerate(items):
            s.op("pe", lambda e, o=o, l=l, r=r, st=st, sp_=sp_: e.matmul(o, lhsT=l, rhs=r, start=st, stop=sp_),
                 reads=reads, writes=writes, inc=(i == n - 1))

    def acc_mm(out, pairs, reads, writes):
        n = len(pairs)
        mms([(out, l, r, i == 0, i == n - 1) for i, (l, r) in enumerate(pairs)], reads, writes)

    def transposes(srcs, dst_fn, reads, dst_tok_fn, evac_eng="act", f32=False, extra=None):
        n = len(srcs)
        g = 0
        half = 0
        while g < n:
            m = min(4, n - g)
            if f32:
                pt, ptok = psS[1][:].rearrange("p (c v) -> p c v", v=128), "psS1"
                view = pt[:, 0:m, :]
            else:
                pt, ptok = psTbs[half], "psTb%d" % half
                view = pt[:, 0:m, :]
            for j in range(m):
                src = srcs[g + j]
                o = pt[:, j, :]
                idn = C("ident") if f32 else identb[:]
                s.op("pe", lambda e, o=o, src=src, idn=idn: e.transpose(out=o, in_=src, identity=idn),
                     reads=list(reads) + ["cst", "identb"], writes=[ptok], inc=(j == m - 1))
            dst = dst_fn(g, m)
            if evac_eng == "act":
                s.op("act", lambda e, dst=dst, view=view: e.copy(out=dst, in_=view), reads=[ptok], writes=dst_tok_fn(g, m))
            else:
                s.op("dve", CP(dst, view), reads=[ptok], writes=dst_tok_fn(g, m))
            if extra is not None:
                extra(pt, 0, g, m, ptok)
            g += m
            half ^= 1

    s.dma("sp", lambda e: e.dma_start(out=lbraw[:, 0], in_=lbf_d.ap()), writes=["lbraw0"])
    s.dma("sp", lambda e: e.dma_start(out=lbraw[:, 1], in_=lbb_d.ap()), writes=["lbraw1"])
    for d_ in range(2):
        s.op("dve", TT(lbraw[:, d_, 0, :], lbraw[:, d_, 0, :], lbraw[:, d_, 1, :], ALU.subtract),
             reads=["lbraw%d" % d_], writes=["lbraw%d" % d_])
        s.op("act", ACT(lb[:, d_, 0, :], lbraw[:, d_, 0, :], AF.Sigmoid), reads=["lbraw%d" % d_], writes=["lb%d" % d_])
        s.op("dve", TS(lb[:, d_, 1, :], lb[:, d_, 0, :], -1.0, 1.0, ALU.mult, ALU.add), reads=["lb%d" % d_], writes=["lb%d" % d_])
    for d_, src in enumerate((rl2f_d, rl2b_d)):
        s.dma("sp", lambda e, d_=d_, src=src: e.dma_start(out=lg[:, d_, :], in_=src.ap().partition_broadcast(128)),
              writes=["lg%d" % d_])
        s.op("act", ACT(lg[:, d_, :], lg[:, d_, :], AF.Exp, scale=math.log(2.0)), reads=["lg%d" % d_], writes=["lg%d" % d_])
        s.op("dve", TS(lg[:, d_, :], lg[:, d_, :], -1.0, 1.0, ALU.mult, ALU.add), reads=["lg%d" % d_], writes=["lg%d" % d_])
        s.op("act", ACT(lg[:, d_, :], lg[:, d_, :], AF.Ln), reads=["lg%d" % d_], writes=["lg%d" % d_])

    if cfg.get("STOP") == "0":
        s.finish(); nc._cfg = cfg; nc._ninst = s.ninst; nc._peak = 0; nc._dbg = dbg_outs
        return nc
    mA2 = A.mark()
    cT = A.alloc("cT", [128, KT, 2], F32)
    s.dma("sp", lambda e: e.dma_start(out=cT[:], in_=cT_d.ap()), writes=["cT"])
    s.op("act", ACT(scb[:], cT[:], AF.Silu), reads=["cT"], writes=["scb"])
    modt = [A.alloc("modt%d" % i, [2, 512], F32) for i in range(2)]
    adab = [A.alloc("adab%d" % i, [2, 512], F32) for i in range(2)]
    NMC = (6 * D) // 512
    def ada_chunk(j, scb_, modt_, adab_):
        slot, wt = wload(adaw_d.ap()[:, j * 512:(j + 1) * 512], KT, 512)
        ps = psA[j % 4]
        acc_mm(ps[0:2, :], [(scb_[:, kt, :], slot[:, kt, 0:512]) for kt in range(KT)], ["scb", wt], ["psA%d" % (j % 4)])
        jj = j % len(adab_)
        tag = "c" if len(adab_) == 1 else ""
        ab = adab_[jj]
        mt = modt_[jj]
        s.dma("sp", lambda e: e.dma_start(out=ab[:], in_=adab_d.ap()[:, j * 512:(j + 1) * 512]), writes=["adab%s%d" % (tag, jj)])
        s.op("dve", TT(mt[:], ps[0:2, :], ab[:], ALU.add), reads=["psA%d" % (j % 4), "adab%s%d" % (tag, jj)], writes=["modt%s%d" % (tag, jj)])
        s.dma("sp", lambda e: e.dma_start(out=MOD.ap()[:, j * 512:(j + 1) * 512], in_=mt[:]),
              reads=["modt%s%d" % (tag, jj)], writes=["MOD%d" % j])

    NMA = min(NMC, (2 * D + 511) // 512)
    for j in range(NMA):
        ada_chunk(j, scb, modt, adab)

    def modtoks(c0, c1):
        return ["MOD%d" % j for j in range(c0 // 512, (c1 - 1) // 512 + 1)]

    def load_row(dst, cond, which, tok):
        c0 = which * D
        s.dma("sp", lambda e: e.dma_start(out=dst, in_=MOD.ap()[cond:cond + 1, c0:c0 + D].partition_broadcast(128)),
              reads=modtoks(c0, c0 + D), writes=[tok])

    def load_vec_row(dst, src_d, tok):
        s.dma("sp", lambda e: e.dma_start(out=dst, in_=src_d.ap().partition_broadcast(128)), writes=[tok])

    tile_cond = [0] * (NP * TP // 128) + [1] * (TS_ // 128)
    segs = [(p * TP // 128, TP // 128, "p", p) for p in range(NP)] + [(NP * TP // 128, TS_ // 128, "s", 0)]

    rows_a = {}
    n1row = A.alloc("n1row", [128, D], F32)
    load_vec_row(n1row[:], n1g_d, "n1row")
    for c in range(2):
        a1 = A.alloc("a1row%d" % c, [128, D], F32)
        sh = A.alloc("sh1row%d" % c, [128, D], F32)
        load_row(a1[:], c, 1, "a1row%d" % c)
        load_row(sh[:], c, 0, "sh1row%d" % c)
        s.op("dve", STT(a1[:], a1[:], 1.0, n1row[:], ALU.add, ALU.mult), reads=["a1row%d" % c, "n1row"], writes=["a1row%d" % c])
        rows_a[c] = (a1, sh)
    xbuf = [A.alloc("xbuf%d" % i, [128, D], F32) for i in range(2)]
    t32 = A.alloc("t32", [128, D], F32)
    xmb = [A.alloc("xmb%d" % i, [128, D], BF16) for i in range(2)]
    junk = A.alloc("junk", [128, D], BF16)

    def rms_rstd(src, src_tok, col, n):
        sc = small[:, col:col + 1]
        tk = "small%d" % col
        s.op("dve", lambda e: e.memset(sc, 0.0), writes=[tk])
        s.op("act", ACT(junk[:, 0:n], src, AF.Square, accum=sc), reads=[src_tok, tk], writes=["junk", tk])
        s.op("dve", TS(sc, sc, 1.0 / n, EPS, ALU.mult, ALU.add), reads=[tk], writes=[tk])
        s.op("act", ACT(sc, sc, AF.Sqrt), reads=[tk], writes=[tk])
        s.op("dve", lambda e: e.reciprocal(out=sc, in_=sc), reads=[tk], writes=[tk])
        return sc, tk

    for i in range(NT):
        c = tile_cond[i]
        xt = xbuf[i % 2]
        xtok = "xbuf%d" % (i % 2)
        s.dma("sp", lambda e, xt=xt, i=i: e.dma_start(out=xt[:], in_=x_d.ap()[i * 128:(i + 1) * 128, :]), writes=[xtok])
        sc, tk = rms_rstd(xt[:], xtok, i % 2, D)
        a1, sh = rows_a[c]
        s.op("dve", STT(t32[:], xt[:], sc, a1[:], ALU.mult, ALU.mult), reads=[xtok, tk, "a1row%d" % c], writes=["t32"])
        xm = xmb[i % 2]
        s.op("dve", TT(xm[:], t32[:], sh[:], ALU.add), reads=["t32", "sh1row%d" % c], writes=["xmb%d" % (i % 2)])
        transposes([xm[:, kt * 128:(kt + 1) * 128] for kt in range(KT)],
                   lambda g, m, i=i: xmT[:, g:g + m, i * 128:(i + 1) * 128],
                   ["xmb%d" % (i % 2)], lambda g, m, i=i: ["xmT_%d" % i])
    xmT_toks = ["xmT_%d" % i for i in range(NT)]
    if debug:
        for kt in range(KT):
            dump("xmT%d" % kt, xmT[:, kt, :], xmT_toks)
    s.barrier()
    A.release(mA2)

    if cfg.get("STOP") == "A":
        s.finish(); nc._cfg = cfg; nc._ninst = s.ninst; nc._peak = 0; nc._dbg = dbg_outs
        return nc
    mB = A.mark()
    q32 = A.alloc("q32", [128, T], BF16)
    fdir = [A.alloc("f%d" % d_, [128, T], F32) for d_ in range(2)]
    s1 = A.alloc("s1", [128, T], F32)
    s2 = A.alloc("s2", [128, T], F32)
    gcol = A.alloc("gcol", [128, NCK], F32)
    sgate = A.alloc("sgate", [128, T], BF16)
    vTM = A.alloc("vTM", [128, NT, 128], BF16)
    qp = [A.alloc("qp%d" % d_, [128, T], BF16) for d_ in range(2)]
    kp = [A.alloc("kp%d" % d_, [128, T], BF16) for d_ in range(2)]
    kpTM = [A.alloc("kpTM%d" % d_, [128, NT, 128], BF16) for d_ in range(2)]
    vexp = A.alloc("vexp", [128, NT, 4, 128], BF16)
    Rpb = [A.alloc("Rpb%d" % d_, [128, NCK, 128], BF16) for d_ in range(2)]
    decay = [A.alloc("decay%d" % d_, [128, NCK], F32) for d_ in range(2)]
    R32 = [[A.alloc("R32_%d_%d" % (d_, sg), [128, 128], F32) for sg in range(len(segs))] for d_ in range(2)]
    msk = [A.alloc("msk%d" % i, [128, 128], BF16) for i in range(2)]
    sqb = A.alloc("sqb", [128, CH], BF16)
    Mdir = [C("mf"), C("mb")]

    SB = cfg.get("SB", 99)
    for h in range(H if SB == 99 else 1):
        slot, wt = wload(win_d.ap()[:, h * 640:(h + 1) * 640], KT, 640)
        for ch in range(NCHK):
            cs = slice(ch * CH, (ch + 1) * CH)
            for j in range(4):
                acc_mm(psA[j][:, 0:CH], [(slot[:, kt, j * 128:(j + 1) * 128], xmT[:, kt, cs]) for kt in range(KT)],
                       [wt] + xmT_toks, ["psA%d" % j])
            s.op("act", ACT(q32[:, cs], psA[0][:, 0:CH], AF.Silu), reads=["psA0"], writes=["q32"])
            for d_ in range(2):
                s.op("act", ACT(s1[:, cs], psA[1 + d_][:, 0:CH], AF.Sigmoid), reads=["psA%d" % (1 + d_)], writes=["s1"])
                s.op("dve", TS(fdir[d_][:, cs], s1[:, cs], lb[:, d_, 1, h:h + 1], lb[:, d_, 0, h:h + 1], ALU.mult, ALU.add),
                     reads=["s1", "lb%d" % d_], writes=["f%d" % d_])
            s.op("act", ACT(sgate[:, cs], psA[3][:, 0:CH], AF.Sigmoid), reads=["psA3"], writes=["sgate"])
        for i in range(NT):
            ps = psS[i % 2]
            acc_mm(ps[:, 0:128], [(xmT[:, kt, i * 128:(i + 1) * 128], slot[:, kt, 512:640]) for kt in range(KT)],
                   [wt] + xmT_toks, ["psS%d" % (i % 2)])
            s.op("act", lambda e, ps=ps, i=i: e.copy(out=vTM[:, i, :], in_=ps[:, 0:128]), reads=["psS%d" % (i % 2)], writes=["vTM"])
        cmo, _ = coff["cmask"]
        for c in range(4):
            s.op("dve", TS(vexp[:, :, c, :], vTM[:], cst[:, cmo + c:cmo + c + 1], None, ALU.mult), reads=["vTM", "cst"], writes=["vexp"])
        if SB == 1:
            break
        for d_ in range(2):
            f = fdir[d_]
            ftok = "f%d" % d_
            s.op("act", ACT(s1[:], f[:], AF.Ln), reads=[ftok], writes=["s1"])
            s.op("dve", TS(f[:], f[:], -1.0, 1.0, ALU.mult, ALU.add), reads=[ftok, "s1"], writes=[ftok])
            for ch in range(NCHK):
                cs = slice(ch * CH, (ch + 1) * CH)
                s.op("dve", lambda e, cs=cs: e.tensor_tensor_scan(out=s2[:, cs], data0=C("reset"), data1=s1[:, cs], initial=0.0,
                                                                  op0=ALU.mult, op1=ALU.add), reads=["s1", "cst"], writes=["s2"])
            gi3 = s2[:].rearrange("p (c k) -> p c k", k=CHUNK)
            s.op("act", ACT(decay[d_][:], gi3[:, :, CHUNK - 1], AF.Exp), reads=["s2"], writes=["decay%d" % d_])
            if d_ == 0:
                s.op("dve", CP(gcol[:], gi3[:, :, CHUNK - 1]), reads=["s2"], writes=["gcol"])
                s.op("dve", TT(gi3, gcol[:].unsqueeze(2).to_broadcast([128, NCK, CHUNK]), gi3, ALU.subtract),
                     reads=["s2", "gcol", "decay%d" % d_], writes=["s2"])
            else:
                s.op("dve", TT(s2[:], s2[:], s1[:], ALU.subtract), reads=["s2", "s1", "decay%d" % d_], writes=["s2"])
            s.op("act", ACT(s1[:], s2[:], AF.Exp), reads=["s2"], writes=["s1"])
            s.op("dve", TT(kp[d_][:], f[:], s1[:], ALU.mult), reads=[ftok, "s1"], writes=["kp%d" % d_])
            s.op("act", ACT(s1[:], s2[:], AF.Exp, scale=-1.0), reads=["s2", "kp%d" % d_], writes=["s1"])
            s.op("dve", TT(qp[d_][:], q32[:], s1[:], ALU.mult), reads=["q32", "s1"], writes=["qp%d" % d_])
            if SB == 2:
                continue
            transposes([kp[d_][:, i * 128:(i + 1) * 128] for i in range(NT)],
                       lambda g, m, d_=d_: kpTM[d_][:, g:g + m, :], ["kp%d" % d_], lambda g, m, d_=d_: ["kpTM%d" % d_])
        banks = [(psS[0], "psS0"), (psS[1], "psS1"), (psA[1], "psA1"), (psA[2], "psA2"), (psA[3], "psA3"), (psA[0], "psA0")]
        chains = []
        for d_ in range(2):
            for sgi, (t0, nt, kind, sidx) in enumerate(segs):
                Rr = R32[d_][sgi]
                rtok = "R32_%d_%d" % (d_, sgi)
                if kind == "p":
                    s.op("dve", lambda e, Rr=Rr: e.memset(Rr[:], 0.0), writes=[rtok])
                else:
                    src = (shf_d, shb_d)[d_]
                    s.dma("sp", lambda e, Rr=Rr, src=src, h=h: e.dma_start(out=Rr[:], in_=src.ap()[h]), writes=[rtok])
                tiles = list(range(t0, t0 + nt))
                if d_ == 1:
                    tiles = tiles[::-1]
                chains.append(dict(d=d_, Rr=Rr, rtok=rtok, tiles=tiles, kind=kind, sidx=sidx))
        for ci, cn in enumerate(chains):
            cn["bank"], cn["btok"] = banks[ci % len(banks)]
        for r in range(max(len(cn["tiles"]) for cn in chains)):
            act_ch = [cn for cn in chains if r < len(cn["tiles"])]
            for cn in act_ch:
                i = cn["tiles"][r]
                d_ = cn["d"]
                mms([(cn["bank"][:, 0:512], kpTM[d_][:, i, :], vexp[:, i].rearrange("p c v -> p (c v)"), True, True)],
                    ["kpTM%d" % d_, "vexp"], [cn["btok"]])
            for st in range(4):
                for cn in act_ch:
                    i = cn["tiles"][r]
                    d_ = cn["d"]
                    c = st if d_ == 0 else 3 - st
                    cg = i * 4 + c
                    Rr, rtok = cn["Rr"], cn["rtok"]
                    pv = cn["bank"][:].rearrange("p (c v) -> p c v", v=128)
                    s.op("act", ACT(Rpb[d_][:, cg, :], Rr[:], AF.Copy, scale=decay[d_][:, cg:cg + 1]),
                         reads=[rtok, "decay%d" % d_], writes=["Rpb%d_%d" % (d_, i)])
                    s.op("dve", STT(Rr[:], Rr[:], decay[d_][:, cg:cg + 1], pv[:, c, :], ALU.mult, ALU.add),
                         reads=[rtok, "decay%d" % d_, cn["btok"]], writes=[rtok])
        for cn in chains:
            if cn["kind"] == "p":
                dst = (hf_o, hb_o)[cn["d"]]
                s.dma("sp", lambda e, Rr=cn["Rr"], dst=dst, sidx=cn["sidx"], h=h: e.dma_start(out=dst.ap()[sidx, h], in_=Rr[:]),
                      reads=[cn["rtok"]], writes=["hout%d_%d_%d" % (cn["d"], cn["sidx"], h)])
        if SB in (2, 3, 4):
            break
        for ch in range(NCHK):
            its = []
            for tl in range(TPC):
                i = ch * TPC + tl
                tsl = slice(i * 128, (i + 1) * 128)
                reg = psA[0][:, tl * 128:(tl + 1) * 128]
                first = True
                for d_ in range(2):
                    ps = psS[d_]
                    s.op("pe", lambda e, ps=ps, d_=d_, tsl=tsl: e.matmul(ps[:, 0:128], lhsT=kp[d_][:, tsl], rhs=qp[d_][:, tsl], start=True, stop=True),
                         reads=["kp%d" % d_, "qp%d" % d_], writes=["psS%d" % d_])
                    mk = msk[d_]
                    s.op("dve", TT(mk[:], ps[:, 0:128], Mdir[d_], ALU.mult), reads=["psS%d" % d_, "cst"], writes=["msk%d" % d_])
                    items = [(reg, vTM[:, i, :], mk[:], first, (SB == 5 and d_ == 1))]
                    first = False
                    for c in range(4 if SB != 5 else 0):
                        cg = i * 4 + c
                        items.append((psA[0][:, tl * 128 + c * 32: tl * 128 + (c + 1) * 32], Rpb[d_][:, cg, :],
                                      qp[d_][:, i * 128 + c * 32:i * 128 + (c + 1) * 32], False, (d_ == 1 and c == 3)))
                    mms(items, ["vTM", "msk%d" % d_, "Rpb%d_%d" % (d_, i), "qp%d" % d_], ["psA0"])
            cs = slice(ch * CH, (ch + 1) * CH)
            if SB in (5, 6):
                continue
            s.op("dve", CP(s1[:, 0:CH], psA[0][:, 0:CH]), reads=["psA0"], writes=["s1"])
            s.op("act", ACT(sqb[:], psA[0][:, 0:CH], AF.Square), reads=["psA0"], writes=["sqb"])
            acc_mm(psA[1][:, 0:CH], [(onesb[:], sqb[:])], ["onesb", "sqb"], ["psA1"])
            s.op("dve", TS(s2[:, 0:CH], psA[1][:, 0:CH], 1.0 / 128, EPS, ALU.mult, ALU.add), reads=["psA1"], writes=["s2"])
            s.op("act", ACT(s2[:, 0:CH], s2[:, 0:CH], AF.Sqrt), reads=["s2"], writes=["s2"])
            s.op("dve", lambda e: e.reciprocal(out=s2[:, 0:CH], in_=s2[:, 0:CH]), reads=["s2"], writes=["s2"])
            if SB == 7:
                continue
            s.op("dve", STT(s1[:, 0:CH], s1[:, 0:CH], hgng[:, 0:1], s2[:, 0:CH], ALU.mult, ALU.mult), reads=["s1", "s2", "hgng"], writes=["s1"])
            ob = oab_o[oab_ctr[0] % 2]
            otok = "oabo%d" % (oab_ctr[0] % 2)
            oab_ctr[0] += 1
            s.op("dve", TT(ob[:], s1[:, 0:CH], sgate[:, cs], ALU.mult), reads=["s1", "sgate"], writes=[otok])
            if SB == 8:
                continue
            s.dma("sp", lambda e, ob=ob, h=h, cs=cs: e.dma_start(out=OAB.ap()[h, :, cs], in_=ob[:]), reads=[otok], writes=["OAB_%d_%d" % (h, ch)])
    s.barrier()
    A.release(mB)

    if cfg.get("STOP") == "B":
        s.finish(); nc._cfg = cfg; nc._ninst = s.ninst; nc._peak = 0; nc._dbg = dbg_outs
        return nc
    mC = A.mark()
    q32 = A.alloc("rq32", [128, T], F32)
    k32 = A.alloc("rk32", [128, T], F32)
    qb = A.alloc("rqb", [128, T], BF16)
    kb = A.alloc("rkb", [128, T], BF16)
    qin = [A.alloc("qin%d" % d_, [128, T], BF16) for d_ in range(2)]
    kend = [A.alloc("kend%d" % d_, [128, NT, 128], BF16) for d_ in range(2)]
    rvTM = A.alloc("rvTM", [128, NT, 256], BF16)
    Rsb = [A.alloc("Rsb%d" % d_, [128, NT, 256], BF16) for d_ in range(2)]
    rg = A.alloc("rg", [128, 2, T], BF16)
    RR = [[A.alloc("RR_%d_%d" % (d_, sg), [128, 256], F32) for sg in range(len(segs))] for d_ in range(2)]
    tA = A.alloc("tA", [128, 128], F32)
    tB = A.alloc("tB", [128, 128], F32)
    Ds = A.alloc("Ds", [128, 128], F32)
    rowt = [A.alloc("rowt%d" % d_, [128, 128], F32) for d_ in range(2)]
    colt = A.alloc("colt", [128, 4], F32)
    t1 = A.alloc("t1", [128, CH], F32)
    t2 = A.alloc("t2", [128, CH], F32)
    rmsk = [A.alloc("rmsk%d" % i, [128, 128], BF16) for i in range(2)]
    ro32 = [A.alloc("ro32_%d" % v, [128, CH], F32) for v in range(2)]
    rob = [A.alloc("rob_%d" % v, [128, CH], BF16) for v in range(2)]
    rsq = [A.alloc("rsq_%d" % v, [128, CH], BF16) for v in range(2)]
    mean = A.alloc("mean", [128, CH], F32)
    var = A.alloc("var", [128, CH], F32)
    ts0 = NP * TP
    modt_c = [A.alloc("modtc0", [2, 512], F32)]
    adab_c = [A.alloc("adabc0", [2, 512], F32)]
    ada_rest = list(range(NMA, NMC))
    ropet = A.alloc("ropet", [128, 2 * TS_], F32)
    s.dma("sp", lambda e: e.dma_start(out=ropet[:], in_=rope_d.ap()), writes=["ropet"])

    for h in range(H):
        c0 = H * 640 + h * 768
        slot, wt = wload(win_d.ap()[:, c0:c0 + 768], KT, 768)
        nrest = (len(ada_rest) + (H - h) - 1) // (H - h)
        ada_now, ada_rest = ada_rest[:nrest], ada_rest[nrest:]
        for ch in range(NCHK):
            cs = slice(ch * CH, (ch + 1) * CH)
            for j in range(4):
                acc_mm(psA[j][:, 0:CH], [(slot[:, kt, j * 128:(j + 1) * 128], xmT[:, kt, cs]) for kt in range(KT)],
                       [wt] + xmT_toks, ["psA%d" % j])
            s.op("act", lambda e, cs=cs: e.copy(out=q32[:, cs], in_=psA[0][:, 0:CH]), reads=["psA0"], writes=["rq32"])
            s.op("act", ACT(k32[:, cs], psA[1][:, 0:CH], AF.Copy, scale=128.0 ** -0.5), reads=["psA1"], writes=["rk32"])
            for v in range(2):
                s.op("act", ACT(rg[:, v, cs], psA[2 + v][:, 0:CH], AF.Silu), reads=["psA%d" % (2 + v)], writes=["rg"])
        for i in range(NT):
            ps = psS[i % 2]
            acc_mm(ps[:, 0:256], [(xmT[:, kt, i * 128:(i + 1) * 128], slot[:, kt, 512:768]) for kt in range(KT)],
                   [wt] + xmT_toks, ["psS%d" % (i % 2)])
            s.op("act", lambda e, ps=ps, i=i: e.copy(out=rvTM[:, i, :], in_=ps[:, 0:256]), reads=["psS%d" % (i % 2)], writes=["rvTM"])
        for j in ada_now:
            ada_chunk(j, scb, modt_c, adab_c)
        if ts0 > 0:
            s.op("dve", CP(qb[:, 0:ts0], q32[:, 0:ts0]), reads=["rq32"], writes=["rqb"])
            s.op("dve", CP(kb[:, 0:ts0], k32[:, 0:ts0]), reads=["rk32"], writes=["rkb"])
        RC = min(512, TS_)
        for rc in range(TS_ // RC):
            cs = slice(ts0 + rc * RC, ts0 + (rc + 1) * RC)
            cosv = ropet[:, rc * RC:(rc + 1) * RC]
            sinv = ropet[:, TS_ + rc * RC: TS_ + (rc + 1) * RC]
            for src, dstb, stok, dtok, pi in ((q32, qb, "rq32", "rqb", 2), (k32, kb, "rk32", "rkb", 3)):
                acc_mm(psA[pi][:, 0:RC], [(C("psw"), src[:, cs])], ["cst", stok], ["psA%d" % pi])
                s.op("dve", TT(t1[:, 0:RC], src[:, cs], cosv, ALU.mult), reads=[stok, "ropet"], writes=["t1"])
                s.op("dve", TT(t2[:, 0:RC], psA[pi][:, 0:RC], sinv, ALU.mult), reads=["psA%d" % pi, "ropet"], writes=["t2"])
                s.op("dve", TT(dstb[:, cs], t1[:, 0:RC], t2[:, 0:RC], ALU.add), reads=["t1", "t2"], writes=[dtok])
        lgf = lg[:, 0, h:h + 1]
        lgb = lg[:, 1, h:h + 1]
        s.op("act", ACT(tA[:], C("dpos"), AF.Exp, scale=lgf), reads=["cst", "lg0"], writes=["tA"])
        s.op("dve", TT(tA[:], tA[:], C("slf"), ALU.mult), reads=["tA", "cst"], writes=["tA"])
        s.op("act", ACT(tB[:], C("dneg"), AF.Exp, scale=lgb), reads=["cst", "lg1"], writes=["tB"])
        s.op("dve", TT(tB[:], tB[:], C("slb"), ALU.mult), reads=["tB", "cst"], writes=["tB"])
        s.op("dve", TT(Ds[:], tA[:], tB[:], ALU.add), reads=["tA", "tB"], writes=["Ds"])
        s.op("dve", TT(Ds[:], Ds[:], C("i2"), ALU.add), reads=["Ds", "cst"], writes=["Ds"])
        s.op("act", ACT(rowt[0][:], C("iota1"), AF.Exp, scale=lgf), reads=["cst", "lg0"], writes=["rowt0"])
        s.op("act", ACT(rowt[1][:], C("iotar"), AF.Exp, scale=lgb), reads=["cst", "lg1"], writes=["rowt1"])
        s.op("act", ACT(colt[:, 0:1], C("c127"), AF.Exp, scale=lgf), reads=["cst", "lg0"], writes=["colt"])
        s.op("act", ACT(colt[:, 1:2], C("cp"), AF.Exp, scale=lgb), reads=["cst", "lg1"], writes=["colt"])
        s.op("act", ACT(colt[:, 2:3], lgf, AF.Exp, scale=128.0), reads=["lg0"], writes=["colt"])
        s.op("act", ACT(colt[:, 3:4], lgb, AF.Exp, scale=128.0), reads=["lg1"], writes=["colt"])
        for i in range(NT):
            tsl = slice(i * 128, (i + 1) * 128)
            for d_ in range(2):
                s.op("dve", TT(qin[d_][:, tsl], qb[:, tsl], rowt[d_][:], ALU.mult), reads=["rqb", "rowt%d" % d_], writes=["qin%d" % d_])
        g = 0
        half = 0
        while g < NT:
            m = min(4, NT - g)
            ptok = "psTb%d" % half
            for j in range(m):
                o = psTbs[half][:, j, :]
                src = kb[:, (g + j) * 128:(g + j + 1) * 128]
                s.op("pe", lambda e, o=o, src=src: e.transpose(out=o, in_=src, identity=identb[:]),
                     reads=["rkb", "identb"], writes=[ptok], inc=(j == m - 1))
            view = psTbs[half][:, 0:m, :]
            for d_ in range(2):
                s.op("act", ACT(kend[d_][:, g:g + m, :], view, AF.Copy, scale=colt[:, d_:d_ + 1]), reads=[ptok, "colt"], writes=["kend%d" % d_])
            g += m
            half ^= 1
        banks = [(psS[0], "psS0"), (psS[1], "psS1"), (psA[1], "psA1"), (psA[2], "psA2"), (psA[3], "psA3"), (psA[0], "psA0")]
        chains = []
        for d_ in range(2):
            for sgi, (t0, nt, kind, sidx) in enumerate(segs):
                Rr = RR[d_][sgi]
                rtok = "RR_%d_%d" % (d_, sgi)
                if kind == "p":
                    s.op("dve", lambda e, Rr=Rr: e.memset(Rr[:], 0.0), writes=[rtok])
                else:
                    src = (srf_d, srb_d)[d_]
                    s.dma("sp", lambda e, Rr=Rr, src=src, h=h: e.dma_start(out=Rr[:], in_=src.ap()[h]), writes=[rtok])
                tiles = list(range(t0, t0 + nt))
                if d_ == 1:
                    tiles = tiles[::-1]
                chains.append(dict(d=d_, Rr=Rr, rtok=rtok, tiles=tiles, kind=kind, sidx=sidx))
        for ci, cn in enumerate(chains):
            cn["bank"], cn["btok"] = banks[ci % len(banks)]
        for r in range(max(len(cn["tiles"]) for cn in chains)):
            act_ch = [cn for cn in chains if r < len(cn["tiles"])]
            for cn in act_ch:
                i = cn["tiles"][r]
                d_ = cn["d"]
                acc_mm(cn["bank"][:, 0:256], [(kend[d_][:, i, :], rvTM[:, i, :])], ["kend%d" % d_, "rvTM"], [cn["btok"]])
            for cn in act_ch:
                i = cn["tiles"][r]
                d_ = cn["d"]
                Rr, rtok = cn["Rr"], cn["rtok"]
                s.op("act", lambda e, d_=d_, i=i, Rr=Rr: e.copy(out=Rsb[d_][:, i, :], in_=Rr[:]), reads=[rtok], writes=["Rsb%d_%d" % (d_, i)])
                s.op("dve", STT(Rr[:], Rr[:], colt[:, 2 + d_:3 + d_], cn["bank"][:, 0:256], ALU.mult, ALU.add),
                     reads=[rtok, "colt", cn["btok"]], writes=[rtok])
        for cn in chains:
            if cn["kind"] == "p":
                dst = (rf_o, rb_o)[cn["d"]]
                s.dma("sp", lambda e, Rr=cn["Rr"], dst=dst, sidx=cn["sidx"], h=h: e.dma_start(out=dst.ap()[sidx, h], in_=Rr[:]),
                      reads=[cn["rtok"]], writes=["rout%d_%d_%d" % (cn["d"], cn["sidx"], h)])
        for ch in range(NCHK):
            cs = slice(ch * CH, (ch + 1) * CH)
            for tl in range(TPC):
                i = ch * TPC + tl
                tsl = slice(i * 128, (i + 1) * 128)
                ps = psS[i % 2]
                s.op("pe", lambda e, ps=ps, tsl=tsl: e.matmul(ps[:, 0:128], lhsT=kb[:, tsl], rhs=qb[:, tsl], start=True, stop=True),
                     reads=["rkb", "rqb"], writes=["psS%d" % (i % 2)])
                mk = rmsk[i % 2]
                s.op("dve", TT(mk[:], ps[:, 0:128], Ds[:], ALU.mult), reads=["psS%d" % (i % 2), "Ds"], writes=["rmsk%d" % (i % 2)])
                for v in range(2):
                    reg = psA[v][:, tl * 128:(tl + 1) * 128]
                    vs = slice(v * 128, (v + 1) * 128)
                    mms([(reg, rvTM[:, i, vs], mk[:], True, False),
                         (reg, Rsb[0][:, i, vs], qin[0][:, tsl], False, False),
                         (reg, Rsb[1][:, i, vs], qin[1][:, tsl], False, True)],
                        ["rvTM", "rmsk%d" % (i % 2), "Rsb0_%d" % i, "Rsb1_%d" % i, "qin0", "qin1"], ["psA%d" % v])
            for v in range(2):
                s.op("dve", CP(ro32[v][:], psA[v][:, 0:CH]), reads=["psA%d" % v], writes=["ro32_%d" % v])
                s.op("act", lambda e, v=v: e.copy(out=rob[v][:], in_=psA[v][:, 0:CH]), reads=["psA%d" % v], writes=["rob%d" % v])
                s.op("act", ACT(rsq[v][:], psA[v][:, 0:CH], AF.Square), reads=["psA%d" % v], writes=["rsq%d" % v])
            acc_mm(psA[2][:, 0:CH], [(onesb[:], rob[0][:]), (onesb[:], rob[1][:])], ["onesb", "rob0", "rob1"], ["psA2"])
            acc_mm(psA[3][:, 0:CH], [(onesb[:], rsq[0][:]), (onesb[:], rsq[1][:])], ["onesb", "rsq0", "rsq1"], ["psA3"])
            s.op("dve", TS(mean[:], psA[2][:, 0:CH], 1.0 / 256, None, ALU.mult), reads=["psA2"], writes=["mean"])
            s.op("dve", TT(var[:], mean[:], mean[:], ALU.mult), reads=["mean"], writes=["var"])
            s.op("dve", STT(var[:], psA[3][:, 0:CH], 1.0 / 256, var[:], ALU.mult, ALU.subtract), reads=["psA3", "var"], writes=["var"])
            s.op("dve", TS(var[:], var[:], EPS, None, ALU.add), reads=["var"], writes=["var"])
            s.op("act", ACT(var[:], var[:], AF.Sqrt), reads=["var"], writes=["var"])
            s.op("dve", lambda e: e.reciprocal(out=var[:], in_=var[:]), reads=["var"], writes=["var"])
            for v in range(2):
                s.op("dve", TT(ro32[v][:], ro32[v][:], mean[:], ALU.subtract), reads=["ro32_%d" % v, "mean"], writes=["ro32_%d" % v])
                s.op("dve", STT(ro32[v][:], ro32[v][:], retg[:, v:v + 1], var[:], ALU.mult, ALU.mult),
                     reads=["ro32_%d" % v, "retg", "var"], writes=["ro32_%d" % v])
                ob = oab_o[oab_ctr[0] % 2]
                otok = "oabo%d" % (oab_ctr[0] % 2)
                oab_ctr[0] += 1
                s.op("dve", TT(ob[:], ro32[v][:], rg[:, v, cs], ALU.mult), reads=["ro32_%d" % v, "rg"], writes=[otok])
                ci = H + 2 * h + v
                s.dma("sp", lambda e, ob=ob, ci=ci, cs=cs: e.dma_start(out=OAB.ap()[ci, :, cs], in_=ob[:]), reads=[otok], writes=["OAB_%d_%d" % (ci, ch)])
    s.barrier()
    A.release(mC)

    if cfg.get("STOP") == "C":
        s.finish(); nc._cfg = cfg; nc._ninst = s.ninst; nc._peak = 0; nc._dbg = dbg_outs
        return nc
    mD = A.mark()
    s.dma("sp", lambda e: e.dma_start(out=rw[:], in_=rw_d.ap().rearrange("(kt p) n -> p kt n", p=128)), writes=["rw"])
    load_vec_row(rbrow[:], rb_d, "rbrow")
    mD1 = A.mark()
    for ch in range(NCHK):
        cs = slice(ch * CH, (ch + 1) * CH)
        conds = sorted(set(tile_cond[ch * TPC:(ch + 1) * TPC]))
        A.release(mD1)
        oab = A.alloc("oab", [128, 3 * H, CH], BF16)
        s.dma("sp", lambda e, cs=cs: e.dma_start(out=oab[:], in_=OAB.ap()[:, :, cs].rearrange("c p t -> p c t")),
              reads=["OAB_%d_%d" % (ci, ch) for ci in range(3 * H)], writes=["oab"])
        merged = A.alloc("merged", [128, KT, CH], BF16)
        mD2 = A.mark()
        sga = A.alloc("sga", [128, JT, CH], F32)
        sgb = A.alloc("sgb", [128, JT, CH], F32)
        m1 = A.alloc("m1", [128, JT, CH], F32)
        for g in range(NG):
            slot, wt = wload(win_d.ap()[:, cfg["MG0"] + g * GW: cfg["MG0"] + (g + 1) * GW], KT, GW)
            for j in range(JT):
                acc_mm(psA[j][:, 0:CH], [(slot[:, kt, j * 128:(j + 1) * 128], xmT[:, kt, cs]) for kt in range(KT)], [wt] + xmT_toks, ["psA%d" % j])
                s.op("act", ACT(sga[:, j, :], psA[j][:, 0:CH], AF.Sigmoid), reads=["psA%d" % j], writes=["sga"])
            slot, wt = wload(win_d.ap()[:, cfg["MG0"] + D + g * GW: cfg["MG0"] + D + (g + 1) * GW], KT, GW)
            for j in range(JT):
                acc_mm(psA[j][:, 0:CH], [(slot[:, kt, j * 128:(j + 1) * 128], xmT[:, kt, cs]) for kt in range(KT)], [wt] + xmT_toks, ["psA%d" % j])
                s.op("act", ACT(sgb[:, j, :], psA[j][:, 0:CH], AF.Sigmoid), reads=["psA%d" % j], writes=["sgb"])
            slot, wt = wload(wpa_d.ap()[:, g * GW:(g + 1) * GW], H, GW)
            for j in range(JT):
                acc_mm(psA[j][:, 0:CH], [(slot[:, c, j * 128:(j + 1) * 128], oab[:, c, :]) for c in range(H)], [wt, "oab"], ["psA%d" % j])
                s.op("dve", TT(m1[:, j, :], sga[:, j, :], psA[j][:, 0:CH], ALU.mult), reads=["sga", "psA%d" % j], writes=["m1"])
            slot, wt = wload(wpb_d.ap()[:, g * GW:(g + 1) * GW], 2 * H, GW)
            for j in range(JT):
                acc_mm(psA[j][:, 0:CH], [(slot[:, c, j * 128:(j + 1) * 128], oab[:, H + c, :]) for c in range(2 * H)], [wt, "oab"], ["psA%d" % j])
                s.op("dve", TT(sgb[:, j, :], sgb[:, j, :], psA[j][:, 0:CH], ALU.mult), reads=["sgb", "psA%d" % j], writes=["sgb"])
                s.op("dve", TT(merged[:, g * JT + j, :], m1[:, j, :], sgb[:, j, :], ALU.add), reads=["m1", "sgb"], writes=["merged"])
        if debug:
            for kt in range(KT):
                dump("merged%d_%d" % (ch, kt), merged[:, kt, :], ["merged"])
        s.barrier()
        A.release(mD2)
        g1row = {}
        for c in conds:
            g1row[c] = A.alloc("g1row%d" % c, [128, D], F32)
            load_row(g1row[c][:], c, 2, "g1row%d" % c)
        xp = [A.alloc("xp%d" % i, [128, GW], F32) for i in range(2)]
        tp = A.alloc("tp", [128, GW], F32)
        xpc = 0
        for g in range(NG):
            slot, wt = wload(wout_d.ap()[:, g * GW:(g + 1) * GW], KT, GW)
            for tl in range(TPC):
                i = ch * TPC + tl
                c = tile_cond[i]
                ps = psA[tl % 4]
                acc_mm(ps[:, 0:GW], [(merged[:, kt, tl * 128:(tl + 1) * 128], slot[:, kt, 0:GW]) for kt in range(KT)], ["merged", wt], ["psA%d" % (tl % 4)])
                xq = xp[xpc % 2]
                xtok = "xp%d" % (xpc % 2)
                xpc += 1
                s.dma("sp", lambda e, xq=xq, i=i, g=g: e.dma_start(out=xq[:], in_=x_d.ap()[i * 128:(i + 1) * 128, g * GW:(g + 1) * GW]), writes=[xtok])
                s.op("dve", TT(tp[:], ps[:, 0:GW], g1row[c][:, g * GW:(g + 1) * GW], ALU.mult), reads=["psA%d" % (tl % 4), "g1row%d" % c], writes=["tp"])
                s.op("dve", TT(xq[:], tp[:], xq[:], ALU.add), reads=["tp", xtok], writes=[xtok])
                s.dma("sp", lambda e, xq=xq, i=i, g=g: e.dma_start(out=X1.ap()[i * 128:(i + 1) * 128, g * GW:(g + 1) * GW], in_=xq[:]),
                      reads=[xtok], writes=["X1_%d_%d" % (i, g)])
        s.barrier()
        A.release(mD1)
        rows2 = {}
        n2row = A.alloc("n2row", [128, D], F32)
        load_vec_row(n2row[:], n2g_d, "n2row")
        for c in conds:
            a2 = A.alloc("a2row%d" % c, [128, D], F32)
            sh2 = A.alloc("sh2row%d" % c, [128, D], F32)
            load_row(a2[:], c, 4, "a2row%d" % c)
            load_row(sh2[:], c, 3, "sh2row%d" % c)
            s.op("dve", STT(a2[:], a2[:], 1.0, n2row[:], ALU.add, ALU.mult), reads=["a2row%d" % c, "n2row"], writes=["a2row%d" % c])
            rows2[c] = (a2, sh2)
        x1t = [A.alloc("x1t%d" % i, [128, D], F32) for i in range(2)]
        xm2 = A.alloc("xm2", [128, D], F32)
        xm2b = [A.alloc("xm2b%d" % i, [128, D], BF16) for i in range(2)]
        xm2T = A.alloc("xm2T", [128, KT, 128], F32)
        junk = A.alloc("junk2", [128, D], BF16)
        for tl in range(TPC):
            i = ch * TPC + tl
            c = tile_cond[i]
            xt = x1t[i % 2]
            xtok = "x1t%d" % (i % 2)
            s.dma("sp", lambda e, xt=xt, i=i: e.dma_start(out=xt[:], in_=X1.ap()[i * 128:(i + 1) * 128, :]),
                  reads=["X1_%d_%d" % (i, g) for g in range(NG)], writes=[xtok])
            sc, tk = rms_rstd(xt[:], xtok, 2 + i % 2, D)
            a2, sh2 = rows2[c]
            s.op("dve", STT(xm2[:], xt[:], sc, a2[:], ALU.mult, ALU.mult), reads=[xtok, tk, "a2row%d" % c], writes=["xm2"])
            s.op("dve", TT(xm2[:], xm2[:], sh2[:], ALU.add), reads=["xm2", "sh2row%d" % c], writes=["xm2"])
            xb_ = xm2b[i % 2]
            s.op("act", lambda e, xb_=xb_: e.copy(out=xb_[:], in_=xm2[:]), reads=["xm2"], writes=["xm2b%d" % (i % 2)])
            s.dma("sp", lambda e, xb_=xb_, i=i: e.dma_start(out=XM2.ap()[i * 128:(i + 1) * 128, :], in_=xb_[:]),
                  reads=["xm2b%d" % (i % 2)], writes=["XM2_%d" % i])
            transposes([xm2[:, kt * 128:(kt + 1) * 128] for kt in range(KT)], lambda g, m: xm2T[:, g:g + m, :], ["xm2"],
                       lambda g, m: ["xm2T"], evac_eng="dve", f32=True)
            acc_mm(psS[0][:, 0:E], [(xm2T[:, kt, :], rw[:, kt, :]) for kt in range(KT)], ["xm2T", "rw"], ["psS0"])
            s.op("dve", TT(lgt[:], psS[0][:, 0:E], rbrow[:], ALU.add), reads=["psS0", "rbrow"], writes=["lgt"])
            if debug:
                dump("logits%d" % i, lgt[:], ["lgt"])
            s.op("dve", lambda e: e.max(out=top8[:], in_=lgt[:]), reads=["lgt"], writes=["top8"])
            s.op("dve", TS(Mf[:, i, :], lgt[:], top8[:, 3:4], None, ALU.is_ge), reads=["lgt", "top8"], writes=["Mf"])
            s.op("dve", TS(top8[:, 7:8], top8[:, 0:1], -1.0, None, ALU.mult), reads=["top8"], writes=["top8"])
            s.op("act", ACT(pex[:], lgt[:], AF.Exp, bias=top8[:, 7:8]), reads=["lgt", "top8"], writes=["pex"])
            s.op("dve", TT(pex[:], pex[:], Mf[:, i, :], ALU.mult), reads=["pex", "Mf"], writes=["pex"])
            s.op("dve", lambda e: e.reduce_sum(out=top8[:, 6:7], in_=pex[:], axis=AX.X), reads=["pex"], writes=["top8"])
            s.op("dve", lambda e: e.reciprocal(out=top8[:, 6:7], in_=top8[:, 6:7]), reads=["top8"], writes=["top8"])
            s.op("dve", TS(Wd[:, i, :], pex[:], top8[:, 6:7], None, ALU.mult), reads=["pex", "top8"], writes=["Wd"])
            s.op("dve", CP(Mb[:, i, :], Mf[:, i, :]), reads=["Mf"], writes=["Mb"])
        s.barrier()
    A.release(mP)

    if cfg.get("STOP") == "D":
        s.finish(); nc._cfg = cfg; nc._ninst = s.ninst; nc._peak = 0; nc._dbg = dbg_outs
        return nc
    destk = A.alloc("destk", [128, NT, TOPK], F32)
    destki = A.alloc("destki", [128, NT, TOPK], I32)
    wk = A.alloc("wk", [128, NT, TOPK], F32)
    SEGCAP = cfg.get("SEGCAP", 2048)
    SEGG, SEGD = min(SEGCAP, KT * CWG), min(SEGCAP, KT * CWD)
    AG, AD = KT * CWG // SEGG, KT * CWD // SEGD
    idxg = A.alloc("idxg", [128, NCJG, AG, NPAIR], I32)
    idxd = A.alloc("idxd", [128, NCJD, AD, NPAIR], I32)
    beig = A.alloc("beig", [128, NCJG, NPAIR], I32)
    beid = A.alloc("beid", [128, NCJD, NPAIR], I32)
    usedi = A.alloc("usedi", [128, NPAIR], I32)
    usedh = A.alloc("usedh", [128, NB], I32)
    GH = cfg["GH"]
    GR = 128 * GH
    mE2 = A.mark()
    rank = A.alloc("rank", [128, NT, E], F32)
    dest = A.alloc("dest", [128, NT, E], F32)
    csel = A.alloc("csel", [128, NT, E], F32)
    oh = A.alloc("oh", [128, NT, E], F32)
    tmpE = A.alloc("tmpE", [128, NT, E], F32)
    cnt = A.alloc("cnt", [128, E], F32)
    NT2 = (NT + GH - 1) // GH
    cmpc = A.alloc("cmpc", [128, E, NT2], F32)
    hc1 = A.alloc("hc1", [128, NB, E], F32)
    hc2 = A.alloc("hc2", [128, NB, E], F32)
    endr = A.alloc("endr", [128, E], F32)
    uhf = A.alloc("uhf", [128, NB], F32)
    padf = A.alloc("padf", [128, E], F32)
    pend = A.alloc("pend", [128, E], F32)
    pstart = A.alloc("pstart", [128, E], F32)
    tokidi = A.alloc("tokidi", [128, NT], I32)
    pe_ = A.alloc("pe", [128, NPAIR], F32)
    tpe = A.alloc("tpe", [128, NPAIR], F32)
    cmpb = A.alloc("cmpb", [128, NPAIR, E], F32)
    rtinit = A.alloc("rtinit", [128, 128], I32)
    tokrep = A.alloc("tokrep", [128, NT, 128], I32)

    for i in range(NT):
        pairs = [(onesb[:], Mb[:, j, :]) for j in range(i)] + [(slfb[:], Mb[:, i, :])]
        acc_mm(psS[i % 2][:, 0:E], pairs, ["onesb", "slfb", "Mb"], ["psS%d" % (i % 2)])
        s.op("dve", CP(rank[:, i, :], psS[i % 2][:, 0:E]), reads=["psS%d" % (i % 2)], writes=["rank"])
    acc_mm(psS[0][:, 0:E], [(onesb[:], Mb[:, j, :]) for j in range(NT)], ["onesb", "Mb"], ["psS0"])
    s.op("dve", CP(cnt[:], psS[0][:, 0:E]), reads=["psS0"], writes=["cnt"])
    pbo, _ = coff["pbase"]
    s.op("dve", TT(cmpc[:], cnt[:].unsqueeze(2).to_broadcast([128, E, NT2]), cst[:, pbo:pbo + NT2].unsqueeze(1).to_broadcast([128, E, NT2]), ALU.is_gt),
         reads=["cnt", "cst"], writes=["cmpc"])
    s.op("dve", lambda e: e.tensor_reduce(out=padf[:], in_=cmpc[:], axis=AX.X, op=ALU.add), reads=["cmpc"], writes=["padf"])
    s.op("dve", TS(padf[:], padf[:], float(GR), None, ALU.mult), reads=["padf"], writes=["padf"])
    oro, _ = coff["onesrow"]
    s.op("dve", lambda e: e.tensor_tensor_scan(out=pend[:], data0=cst[:, oro:oro + E], data1=padf[:], initial=0.0, op0=ALU.mult, op1=ALU.add),
         reads=["padf", "cst"], writes=["pend"])
    s.op("dve", TT(pstart[:], pend[:], padf[:], ALU.subtract), reads=["pend", "padf"], writes=["pstart"])
    s.op("dve", TT(endr[:], pstart[:], cnt[:], ALU.add), reads=["pstart", "cnt"], writes=["endr"])
    bbo_, _ = coff["bbase"]
    bbv = cst[:, bbo_:bbo_ + NB].unsqueeze(2).to_broadcast([128, NB, E])
    s.op("dve", TT(hc1[:], pstart[:].unsqueeze(1).to_broadcast([128, NB, E]), bbv, ALU.is_le), reads=["pstart", "cst"], writes=["hc1"])
    s.op("dve", TT(hc2[:], endr[:].unsqueeze(1).to_broadcast([128, NB, E]), bbv, ALU.is_gt), reads=["endr", "cst"], writes=["hc2"])
    s.op("dve", TT(hc1[:], hc1[:], hc2[:], ALU.mult), reads=["hc1", "hc2"], writes=["hc1"])
    s.op("dve", lambda e: e.tensor_reduce(out=uhf[:], in_=hc1[:], axis=AX.X, op=ALU.add), reads=["hc1"], writes=["uhf"])
    s.op("dve", CP(usedh[:], uhf[:]), reads=["uhf"], writes=["usedi"])
    for i in range(NT):
        s.op("dve", TT(dest[:, i, :], rank[:, i, :], pstart[:], ALU.add), reads=["rank", "pstart"], writes=["dest"])
        s.op("dve", lambda e, i=i: e.tensor_tensor_scan(out=csel[:, i, :], data0=cst[:, oro:oro + E], data1=Mf[:, i, :], initial=0.0,
                                                          op0=ALU.mult, op1=ALU.add), reads=["Mf", "cst"], writes=["csel"])
    for k in range(TOPK):
        s.op("dve", STT(oh[:], csel[:], float(k + 1), Mf[:], ALU.is_equal, ALU.mult), reads=["csel", "Mf"], writes=["oh"])
        s.op("dve", TT(tmpE[:], oh[:], dest[:], ALU.mult), reads=["oh", "dest"], writes=["tmpE"])
        s.op("dve", lambda e, k=k: e.tensor_reduce(out=destk[:, :, k], in_=tmpE[:], axis=AX.X, op=ALU.add), reads=["tmpE"], writes=["destk"])
        s.op("dve", TT(tmpE[:], oh[:], Wd[:], ALU.mult), reads=["oh", "Wd", "destk"], writes=["tmpE"])
        s.op("dve", lambda e, k=k: e.tensor_reduce(out=wk[:, :, k], in_=tmpE[:], axis=AX.X, op=ALU.add), reads=["tmpE"], writes=["wk"])
    s.op("dve", CP(destki[:], destk[:]), reads=["destk"], writes=["destki"])
    s.op("dve", CP(tokidi[:], C("tokid")), reads=["cst"], writes=["tokidi"])
    s.op("dve", TT(cmpb[:], pend[:].unsqueeze(1).to_broadcast([128, NPAIR, E]), cst[:, pbo:pbo + NPAIR].unsqueeze(2).to_broadcast([128, NPAIR, E]), ALU.is_le),
         reads=["pend", "cst"], writes=["cmpb"])
    s.op("dve", lambda e: e.tensor_reduce(out=pe_[:], in_=cmpb[:], axis=AX.X, op=ALU.add), reads=["cmpb"], writes=["pe"])
    cpo, _ = coff["cp"]
    tpe2 = A.alloc("tpe2", [128, NPAIR], F32)
    s.op("dve", TS(tpe2[:], pe_[:], float(E), None, ALU.is_lt), reads=["pe"], writes=["tpe2"])
    s.op("dve", CP(usedi[:], tpe2[:]), reads=["tpe2"], writes=["usedi"])
    for (ncj, na, idx_t, be_t) in ((NCJG, AG, idxg, beig), (NCJD, AD, idxd, beid)):
        for cj in range(ncj):
            s.op("dve", TS(tpe[:], pe_[:], float(ncj), float(cj), ALU.mult, ALU.add), reads=["pe", "idxtabs"], writes=["tpe"])
            s.op("dve", CP(be_t[:, cj, :], tpe[:]), reads=["tpe"], writes=["idxtabs"])
            s.op("dve", TS(tpe[:], tpe[:], 128.0, cst[:, cpo:cpo + 1], ALU.mult, ALU.add), reads=["tpe", "cst", "idxtabs"], writes=["tpe"])
            for a_ in range(na):
                s.op("dve", TS(tpe2[:], tpe[:], float(na), float(a_), ALU.mult, ALU.add), reads=["tpe", "idxtabs"], writes=["tpe2"])
                s.op("dve", CP(idx_t[:, cj, a_, :], tpe2[:]), reads=["tpe2"], writes=["idxtabs"])
    s.op("dve", lambda e: e.memset(rtinit[:], T), writes=["rtinit"])
    for b in range(NB):
        s.dma("sp", lambda e, b=b: e.dma_start(out=ROWTOK.ap()[b * 128:(b + 1) * 128, :], in_=rtinit[:]), reads=["rtinit"], writes=["ROWTOK%d" % b])
    for i in range(NT):
        s.op("dve", CP(tokrep[:, i, :], tokidi[:, i:i + 1].to_broadcast([128, 128])), reads=["tokidi"], writes=["tokrep"])
    rt_toks = []
    for i in range(NT):
        for k in range(TOPK):
            tk = "RT_%d_%d" % (i, k)
            rt_toks.append(tk)
            s.dma("pool", lambda e, i=i, k=k: e.indirect_dma_start(
                out=ROWTOK.ap(), out_offset=bass.IndirectOffsetOnAxis(ap=destki[:, i, k:k + 1], axis=0),
                in_=tokrep[:, i, :], in_offset=None, bounds_check=BC(e, R - 1), oob_is_err=False),
                reads=["ROWTOK%d" % b for b in range(NB)] + ["destki", "tokrep"], writes=[tk])
    if debug:
        dump("destk", destk[:].rearrange("p t k -> p (t k)"), ["destk"])
        dump("wk", wk[:].rearrange("p t k -> p (t k)"), ["wk"])
    s.barrier()
    if cfg.get("STOP") == "E":
        s.finish(); nc._cfg = cfg; nc._ninst = s.ninst; nc._peak = 0; nc._dbg = dbg_outs
        return nc
    A.release(mE2)
    mF = A.mark()
    FT = DFF // 128
    CWM = max(CWG, CWD)
    NSLOT = 2
    mslots = [A.alloc("mslot%d" % i, [128, KT * CWM], BF16) for i in range(NSLOT)]
    mctr = [0]
    rtb = [A.alloc("rtb%d" % i, [128, 16], I32) for i in range(GH)]
    xg = [A.alloc("xg%d" % i, [128, D], BF16) for i in range(GH)]
    xgT = [A.alloc("xgT%d" % i, [128, KT, 128], BF16) for i in range(GH)]
    bch = [A.alloc("bch%d" % i, [128, CWM], BF16) for i in range(2)]
    bctr = [0]
    PWG = min(512, CWG)
    PWD = min(512, CWD)
    hb = [A.alloc("hb%d" % i, [128, PWG], F32) for i in range(2)]
    gt = A.alloc("gt", [128, PWG // 2], F32)
    ut = A.alloc("ut", [128, PWG // 2], F32)
    sgt = A.alloc("sgt", [128, PWG // 2], F32)
    actb = [A.alloc("actb%d" % i, [128, DFF], BF16) for i in range(GH)]
    actT = [A.alloc("actT%d" % i, [128, FT, 128], BF16) for i in range(GH)]
    yp = [A.alloc("yp%d" % i, [128, CWD], F32) for i in range(2)]
    ypc = [0]
    zt = A.alloc("zt", [128, CWD], F32)
    s.op("dve", lambda e: e.memset(zt[:], 0.0), writes=["zt"])
    for i in range(GH):
        s.op("dve", lambda e, i=i: e.memset(xg[i][:], 0.0), writes=["xg%d" % i])
    for i in range(NSLOT):
        s.op("dve", lambda e, i=i: e.memset(mslots[i][:], 0.0), writes=["mslot%d_%d" % (i, a_) for a_ in range(max(AG, AD))])

    def mload(src_d, idx_tab, cj, g, nrows, ncols, seg, na):
        i = mctr[0] % NSLOT
        mctr[0] += 1
        slot = mslots[i]
        tok = "mslot%d" % i
        n = KT * ncols
        srcv = src_d.ap().rearrange("r (a s) -> (r a) s", s=seg)
        toks = []
        for a_ in range(na):
            s.dma("pool", lambda e, a_=a_: e.indirect_dma_start(
                out=slot[:, a_ * seg:(a_ + 1) * seg], out_offset=None, in_=srcv,
                in_offset=bass.IndirectOffsetOnAxis(ap=idx_tab[:, cj, a_, g:g + 1], axis=0),
                bounds_check=BC(e, nrows * na - 1), oob_is_err=False), reads=["idxtabs"], writes=[tok + "_%d" % a_])
            toks.append(tok + "_%d" % a_)
        return slot[:, 0:n].rearrange("p (k c) -> p k c", c=ncols), toks

    def bload(src_d, idx_ap, nrows, ncols):
        i = bctr[0] % 2
        bctr[0] += 1
        bt = bch[i]
        tok = "bch%d" % i
        s.dma("pool", lambda e: e.indirect_dma_start(
            out=bt[:, 0:ncols], out_offset=None, in_=src_d.ap(), in_offset=bass.IndirectOffsetOnAxis(ap=idx_ap, axis=0),
            bounds_check=BC(e, nrows - 1), oob_is_err=False), reads=["idxtabs"], writes=[tok])
        return bt, tok

    USE_CF = cfg.get("CF", True)

    def hbeg(g, hf):
        if USE_CF:
            s.group_begin(usedh[0:1, g * GH + hf:g * GH + hf + 1])

    def hend(else_dmas=None):
        if USE_CF:
            s.group_end(else_dmas)

    for g in range(NPAIR):
        outer = USE_CF and g >= cfg.get("CF_MIN", T * TOPK // GR)
        if outer:
            s.group_begin(usedi[0:1, g:g + 1])
        for hf in range(GH):
            b = GH * g + hf
            hbeg(g, hf)
            s.dma("sp", lambda e, b=b, hf=hf: e.dma_start(out=rtb[hf][:], in_=ROWTOK.ap()[b * 128:(b + 1) * 128, 0:16]),
                  reads=rt_toks + ["ROWTOK%d" % b], writes=["rtb%d" % hf])
            s.dma("pool", lambda e, hf=hf: e.indirect_dma_start(
                out=xg[hf][:], out_offset=None, in_=XM2.ap(), in_offset=bass.IndirectOffsetOnAxis(ap=rtb[hf][:, 0:1], axis=0),
                bounds_check=BC(e, T - 1), oob_is_err=False), reads=["rtb%d" % hf] + ["XM2_%d" % i for i in range(NT)], writes=["xg%d" % hf])
            transposes([xg[hf][:, kt * 128:(kt + 1) * 128] for kt in range(KT)], lambda g_, m, hf=hf: xgT[hf][:, g_:g_ + m, :], ["xg%d" % hf],
                       lambda g_, m, hf=hf: ["xgT%d" % hf])
            hend()
        for cj in range(NCJG):
            slot, stoks = mload(wgu_d, idxg, cj, g, E * NCJG * 128, CWG, SEGG, AG)
            bt, btok = bload(bgu_d, beig[:, cj, g:g + 1], E * NCJG, CWG)
            npc = CWG // PWG
            for hf in range(GH):
                hbeg(g, hf)
                for pc in range(npc):
                    bank = (hf * npc + pc) % 4
                    acc_mm(psA[bank][:, 0:PWG], [(xgT[hf][:, kt, :], slot[:, kt, pc * PWG:(pc + 1) * PWG]) for kt in range(KT)],
                           ["xgT%d" % hf] + stoks, ["psA%d" % bank])
                    hbb = hb[pc % 2]
                    htok = "hb%d" % (pc % 2)
                    HW_ = PWG // 2
                    s.op("dve", TT(hbb[:], psA[bank][:, 0:PWG], bt[:, pc * PWG:(pc + 1) * PWG], ALU.add), reads=["psA%d" % bank, btok], writes=[htok])
                    hv = hbb[:].rearrange("p (f two) -> p f two", two=2)
                    s.op("dve", TS(gt[:], hv[:, :, 0], 7.0, None, ALU.min), reads=[htok], writes=["gt"])
                    s.op("dve", TS(ut[:], hv[:, :, 1], -7.0, 7.0, ALU.max, ALU.min), reads=[htok], writes=["ut"])
                    s.op("act", ACT(sgt[:], gt[:], AF.Sigmoid, scale=1.702), reads=["gt"], writes=["sgt"])
                    s.op("dve", TT(gt[:], gt[:], sgt[:], ALU.mult), reads=["gt", "sgt"], writes=["gt"])
                    f0 = (cj * CWG + pc * PWG) // 2
                    s.op("dve", STT(actb[hf][:, f0:f0 + HW_], ut[:], 1.0, gt[:], ALU.add, ALU.mult), reads=["ut", "gt"], writes=["actb%d" % hf])
                hend()
        for hf in range(GH):
            hbeg(g, hf)
            transposes([actb[hf][:, ft * 128:(ft + 1) * 128] for ft in range(FT)], lambda g_, m, hf=hf: actT[hf][:, g_:g_ + m, :], ["actb%d" % hf],
                       lambda g_, m, hf=hf: ["actT%d" % hf])
            hend()
        for cj in range(NCJD):
            slot, stoks = mload(wdn_d, idxd, cj, g, E * NCJD * 128, CWD, SEGD, AD)
            bt, btok = bload(bdn_d, beid[:, cj, g:g + 1], E * NCJD, CWD)
            npc = CWD // PWD
            for hf in range(GH):
                b = GH * g + hf
                hbeg(g, hf)
                ypb = yp[ypc[0] % 2]
                ytok = "yp%d" % (ypc[0] % 2)
                ypc[0] += 1
                for pc in range(npc):
                    bank = (hf * npc + pc) % 4
                    acc_mm(psA[bank][:, 0:PWD], [(actT[hf][:, kt, :], slot[:, kt, pc * PWD:(pc + 1) * PWD]) for kt in range(KT)],
                           ["actT%d" % hf] + stoks, ["psA%d" % bank])
                    s.op("dve", TT(ypb[:, pc * PWD:(pc + 1) * PWD], psA[bank][:, 0:PWD], bt[:, pc * PWD:(pc + 1) * PWD], ALU.add),
                         reads=["psA%d" % bank, btok], writes=[ytok])
                ydst = Y.ap()[b * 128:(b + 1) * 128, cj * CWD:(cj + 1) * CWD]
                s.dma("sp", lambda e, ypb=ypb, ydst=ydst: e.dma_start(out=ydst, in_=ypb[:]), reads=[ytok], writes=["Y_%d_%d" % (b, cj)])
                hend(else_dmas={"sp": [(ydst, zt[:])]})
        if outer:
            s.group_end(else_dmas={"sp": [(Y.ap()[(GH * g + hf) * 128:(GH * g + hf + 1) * 128, cj * CWD:(cj + 1) * CWD], zt[:])
                                          for hf in range(GH) for cj in range(NCJD)]})
    s.barrier()
    A.release(mF)

    if cfg.get("STOP") == "F":
        s.finish(); nc._cfg = cfg; nc._ninst = s.ninst; nc._peak = 0; nc._dbg = dbg_outs
        return nc
    y_toks = ["Y_%d_%d" % (b, cj) for b in range(NB) for cj in range(NCJD)]
    yk = [A.alloc("yk%d" % k, [128, D], F32) for k in range(2 * TOPK)]
    acc = A.alloc("acc", [128, D], F32)
    x1gs = [A.alloc("x1g%d" % i, [128, D], F32) for i in range(2)]
    g2row = {}
    for c in range(2):
        g2row[c] = A.alloc("g2row%d" % c, [128, D], F32)
        load_row(g2row[c][:], c, 5, "g2row%d" % c)
    nfrow = A.alloc("nfrow", [128, D], F32)
    load_vec_row(nfrow[:], nfg_d, "nfrow")
    junk = A.alloc("junk3", [128, D], BF16)
    yout = [A.alloc("yout%d" % i, [128, D], F32) for i in range(2)]
    for k in range(2 * TOPK):
        s.op("dve", lambda e, k=k: e.memset(yk[k][:], 0.0), writes=["yk%d" % k])
    for i in range(NT):
        c = tile_cond[i]
        kb_ = (i % 2) * TOPK
        x1g = x1gs[i % 2]
        for k in range(TOPK):
            s.dma("pool", lambda e, i=i, k=k, kb_=kb_: e.indirect_dma_start(
                out=yk[kb_ + k][:], out_offset=None, in_=Y.ap(), in_offset=bass.IndirectOffsetOnAxis(ap=destki[:, i, k:k + 1], axis=0),
                bounds_check=BC(e, R - 1), oob_is_err=False), reads=y_toks + ["destki"], writes=["yk%d" % (kb_ + k)])
        s.dma("sp", lambda e, i=i, x1g=x1g: e.dma_start(out=x1g[:], in_=X1.ap()[i * 128:(i + 1) * 128, :]), writes=["x1g%d" % (i % 2)])
        s.op("dve", TS(acc[:], yk[kb_][:], wk[:, i, 0:1], None, ALU.mult), reads=["yk%d" % kb_, "wk"], writes=["acc"])
        for k in range(1, TOPK):
            s.op("dve", STT(acc[:], yk[kb_ + k][:], wk[:, i, k:k + 1], acc[:], ALU.mult, ALU.add), reads=["yk%d" % (kb_ + k), "wk", "acc"], writes=["acc"])
        s.op("dve", TT(acc[:], acc[:], g2row[c][:], ALU.mult), reads=["acc", "g2row%d" % c], writes=["acc"])
        s.op("dve", TT(acc[:], acc[:], x1g[:], ALU.add), reads=["acc", "x1g%d" % (i % 2)], writes=["acc"])
        sc, tk = rms_rstd(acc[:], "acc", 4 + i % 2, D)
        yo = yout[i % 2]
        s.op("dve", STT(yo[:], acc[:], sc, nfrow[:], ALU.mult, ALU.mult), reads=["acc", tk, "nfrow"], writes=["yout%d" % (i % 2)])
        s.dma("sp", lambda e, yo=yo, i=i: e.dma_start(out=y_o.ap()[i * 128:(i + 1) * 128, :], in_=yo[:]), reads=["yout%d" % (i % 2)], writes=["y_%d" % i])
    s.finish()
    nc._cfg = cfg
    nc._ninst = s.ninst
    nc._peak = A.peak - A.base
    nc._dbg = dbg_outs
    return nc


def prepare_core_inputs(inp, cfg, core):
    cfg = derive(cfg)
    D, H, KT, NP = cfg["D"], cfg["H"], cfg["KT"], cfg["NP"]
    f = np.float32
    xp = np.asarray(inp["x_prompt"], f)
    xs = np.asarray(inp["x_sample"], f)
    x = np.concatenate([xp[core * NP + p] for p in range(NP)] + [xs[core]], axis=0)
    c_ctx = np.asarray(inp["c_ctx"], f)
    c = np.asarray(inp["c"], f)[core]
    cT = np.stack([c_ctx.reshape(KT, 128).T, c.reshape(KT, 128).T], axis=-1)

    def fm(v, n):
        return np.ascontiguousarray(np.asarray(v, f).reshape(n, 128).T)

    lbf = np.stack([fm(inp["hg_lb_fwd"][0], H), fm(inp["hg_lb_fwd"][1], H)], axis=1)
    lbb = np.stack([fm(inp["hg_lb_bwd"][0], H), fm(inp["hg_lb_bwd"][1], H)], axis=1)
    m = {
        "x": np.ascontiguousarray(x),
        "shf": np.ascontiguousarray(np.asarray(inp["state_hgrn_fwd"], f)[core, 0]),
        "shb": np.ascontiguousarray(np.asarray(inp["state_hgrn_bwd"], f)[core, 0]),
        "srf": np.ascontiguousarray(np.asarray(inp["state_ret_fwd"], f)[core, 0]),
        "srb": np.ascontiguousarray(np.asarray(inp["state_ret_bwd"], f)[core, 0]),
        "cT": np.ascontiguousarray(cT),
        "lbf": np.ascontiguousarray(lbf), "lbb": np.ascontiguousarray(lbb),
    }
    return m


def prepare_shared_inputs(inp, cfg):
    cfg = derive(cfg)
    D, H, KT, E = cfg["D"], cfg["H"], cfg["KT"], cfg["E"]
    f = np.float32
    perm = w_in_perm_index(cfg)
    sh = {
        "ada_w": np.ascontiguousarray(np.asarray(inp["ada_w"], f)[0]),
        "ada_b2": np.ascontiguousarray(np.broadcast_to(np.asarray(inp["ada_b"], f)[0][None, :], (2, 6 * D))),
        "n1g": np.asarray(inp["norm1_g"], f)[0][None, :].copy(),
        "n2g": np.asarray(inp["norm2_g"], f)[0][None, :].copy(),
        "nfg": np.asarray(inp["final_norm_g"], f)[None, :].copy(),
        "w_in": np.ascontiguousarray(np.asarray(inp["w_in"], f)[0][:, perm]),
        "hgng": np.asarray(inp["hg_norm_g"], f)[0].reshape(128, 1).copy(),
        "retg": np.ascontiguousarray(np.asarray(inp["ret_norm_g"], f)[0].reshape(2, 128).T),
        "rl2f": np.asarray(inp["ret_log2_fwd"], f)[0][None, :].copy(),
        "rl2b": np.asarray(inp["ret_log2_bwd"], f)[0][None, :].copy(),
        "w_pa": np.ascontiguousarray(np.asarray(inp["w_proj_hgrn"], f)[0]),
        "w_pb": np.ascontiguousarray(np.asarray(inp["w_proj_ret"], f)[0]),
        "w_out": np.ascontiguousarray(np.asarray(inp["w_out"], f)[0]),
        "rw": np.ascontiguousarray(np.asarray(inp["router_w"], f)[0]),
        "rb": np.asarray(inp["router_b"], f)[0][None, :].copy(),
        "w_gu": np.ascontiguousarray(np.asarray(inp["moe_w_gu"], f)[0].reshape(E, KT, 128, cfg["NCJG"], cfg["CWG"]).transpose(0, 3, 2, 1, 4)).reshape(E * cfg["NCJG"] * 128, KT * cfg["CWG"]),
        "b_gu": np.ascontiguousarray(np.asarray(inp["moe_b_gu"], f)[0]).reshape(E * cfg["NCJG"], cfg["CWG"]),
        "w_dn": np.ascontiguousarray(np.asarray(inp["moe_w_dn"], f)[0].reshape(E, KT, 128, cfg["NCJD"], cfg["CWD"]).transpose(0, 3, 2, 1, 4)).reshape(E * cfg["NCJD"] * 128, KT * cfg["CWD"]),
        "b_dn": np.ascontiguousarray(np.asarray(inp["moe_b_dn"], f)[0]).reshape(E * cfg["NCJD"], cfg["CWD"]),
        "cst": make_consts(cfg)[0],
        "rope": make_consts(cfg)[1],
    }
    return sh


def run(inp, cfg, runner=None, debug=False):
    cfgd = derive(cfg)
    ncores = cfgd["NCORES"]
    nc = build(cfg, debug=debug)
    shared = prepare_shared_inputs(inp, cfg)
    in_maps = []
    for core in range(ncores):
        m = dict(shared)
        m.update(prepare_core_inputs(inp, cfg, core))
        in_maps.append(m)
    if runner is None:
        res = run_bass_kernel_spmd(nc, in_maps, core_ids=list(range(ncores))).results
    else:
        res = runner(nc, in_maps)
    NP, TP, TS_, D, H = cfgd["NP"], cfgd["TP"], cfgd["TS"], cfgd["D"], cfgd["H"]
    yp = np.stack([res[c]["y"][p * TP:(p + 1) * TP] for c in range(ncores) for p in range(NP)], axis=0)
    ys = np.stack([res[c]["y"][NP * TP:] for c in range(ncores)], axis=0)
    hf = np.concatenate([res[c]["hf"] for c in range(ncores)], axis=0)[:, None]
    hb = np.concatenate([res[c]["hb"] for c in range(ncores)], axis=0)[:, None]
    rf = np.concatenate([res[c]["rf"] for c in range(ncores)], axis=0)[:, None]
    rb = np.concatenate([res[c]["rb_o"] for c in range(ncores)], axis=0)[:, None]
    outs = tuple(np.ascontiguousarray(a.astype(np.float32)) for a in (yp, ys, hf, hb, rf, rb))
    return outs, res, nc


def kernel(**inputs):
    outs, _, _ = run(inputs, FULL_CFG)
    return outs
```

```python
import math
from contextlib import ExitStack
import numpy as np
import ml_dtypes
import concourse.bass as bass
import concourse.mybir as mybir
from concourse.bass_utils import run_bass_kernel_spmd

F32 = mybir.dt.float32
BF16 = mybir.dt.bfloat16
I32 = mybir.dt.int32
AF = mybir.ActivationFunctionType
ALU = mybir.AluOpType
AX = mybir.AxisListType
EPS = 1e-6
CHUNK = 32
GRID_W = 64
ROPE_PAIRS = 32
TOPK = 4

FULL_CFG = dict(D=2048, H=8, TP=256, NP=2, TS=1024, E=32, NCORES=8)


class Sched:
    CE = ("pe", "act", "dve", "pool")

    def __init__(self, nc, nring=8):
        self.nc = nc
        self.q = {k: [] for k in ("pe", "act", "dve", "pool", "sp")}
        self.psem = {k: nc.alloc_semaphore(name="ps_" + k) for k in self.CE}
        self.pcnt = {k: 0 for k in self.CE}
        self.waited = {}
        self.ring = {}
        for qn in ("sp", "pool"):
            self.ring[qn] = dict(sems=[nc.alloc_semaphore(name="d_%s_%d" % (qn, i)) for i in range(nring)],
                                 vals=[0] * nring, nxt=0)
        self.lw = {}
        self.rd = {}
        self.ninst = 0
        self.dummy = None
        self.dummy_ctr = 0

    def _wait(self, engn, ev):
        if ev is None:
            return
        sem, val = ev
        key = (engn, id(sem))
        if self.waited.get(key, 0) >= val:
            return
        self.waited[key] = val
        self.q[engn].append(lambda e, sem=sem, val=val: e.wait_ge(sem, val))
        self.ninst += 1
        for g in getattr(self, "_gstack", []):
            if val <= g["snap"].get(id(sem), 0):
                d = g["waits"].setdefault(engn, {})
                if d.get(id(sem), (None, 0))[1] < val:
                    d[id(sem)] = (sem, val)

    def _deps(self, engn, reads, writes):
        for t in reads:
            self._wait(engn, self.lw.get(t))
        for t in writes:
            self._wait(engn, self.lw.get(t))
            for ev in self.rd.get(t, {}).values():
                self._wait(engn, ev)

    def _commit(self, ev, reads, writes):
        sem, val = ev
        for t in writes:
            self.lw[t] = ev
            self.rd[t] = {}
        for t in reads:
            d = self.rd.setdefault(t, {})
            old = d.get(id(sem))
            if old is None or old[1] < val:
                d[id(sem)] = ev

    @staticmethod
    def _excl(reads, writes):
        pr = [t for t in reads if t.startswith("ps")]
        if not pr:
            return list(reads), list(writes)
        return [t for t in reads if not t.startswith("ps")], list(writes) + [t for t in pr if t not in writes]

    def op(self, engn, fn, reads=(), writes=(), inc=True):
        reads, writes = self._excl(reads, writes)
        self._deps(engn, reads, writes)
        self.ninst += 1
        if inc:
            self.pcnt[engn] += 1
            sem = self.psem[engn]
            val = self.pcnt[engn]
            self.q[engn].append(lambda e, fn=fn, sem=sem: fn(e).then_inc(sem, 1))
            ev = (sem, val)
            self._commit(ev, reads, writes)
            return ev
        self.q[engn].append(lambda e, fn=fn: fn(e))
        return None

    def dma(self, qn, fn, reads=(), writes=()):
        r = self.ring[qn]
        i = r["nxt"]
        r["nxt"] = (i + 1) % len(r["sems"])
        sem = r["sems"][i]
        if r["vals"][i] > 0:
            self._wait(qn, (sem, r["vals"][i]))
        self._deps(qn, reads, writes)
        r["vals"][i] += 16
        val = r["vals"][i]
        self.q[qn].append(lambda e, fn=fn, sem=sem: fn(e).then_inc(sem, 16))
        self.ninst += 1
        ev = (sem, val)
        self._commit(ev, reads, writes)
        return ev

    def group_begin(self, flag_ap):
        self.waited.clear()
        if not hasattr(self, "_gstack"):
            self._gstack = []
        snap = {id(self.psem[k]): self.pcnt[k] for k in self.CE}
        for r in self.ring.values():
            for sem, v in zip(r["sems"], r["vals"]):
                snap[id(sem)] = v
        self._gstack.append(dict(flag=flag_ap, pcnt=dict(self.pcnt), rings={qn: list(r["vals"]) for qn, r in self.ring.items()},
                                 snap=snap, waits={}))
        for qn in self.q:
            self.q[qn].append(("IF", flag_ap))

    def group_end(self, else_dmas=None):
        g = self._gstack.pop()
        else_dmas = else_dmas or {}
        for qn in self.q:
            comp = []
            if qn in self.CE and self.pcnt[qn] > g["pcnt"][qn]:
                comp.append(("drain_inc", self.psem[qn], self.pcnt[qn] - g["pcnt"][qn]))
            if qn in self.ring:
                r = self.ring[qn]
                for sem, v0, v1 in zip(r["sems"], g["rings"][qn], r["vals"]):
                    d = v1 - v0
                    first = True
                    while d > 0:
                        k = min(32, d)
                        comp.append(("wait_inc", sem, v0 if first else 0, k))
                        first = False
                        d -= k
            self.q[qn].append(("ELSE_END", comp, list(else_dmas.get(qn, [])), list(g["waits"].get(qn, {}).values())))
        self.waited.clear()

    def _replay(self, qn, e):
        regs = []
        stack = []
        for t in self.q[qn]:
            if isinstance(t, tuple):
                if t[0] == "IF":
                    lvl = len(stack)
                    while len(regs) <= lvl:
                        regs.append(e.alloc_register("flag_%s_%d" % (qn, len(regs))))
                    e.reg_load(regs[lvl], t[1])
                    ctx = e.If_eq(regs[lvl], 1)
                    ctx.__enter__()
                    stack.append(ctx)
                else:
                    ctx = stack.pop()
                    ctx.__exit__(None, None, None)
                    if t[1] or t[3]:
                        c2 = e.Else()
                        c2.__enter__()
                        for (wsem, wval) in t[3]:
                            e.wait_ge(wsem, wval)
                        useful = list(t[2])
                        for c in t[1]:
                            if c[0] == "drain_inc":
                                e.drain().then_inc(c[1], c[2])
                            else:
                                if c[2] > 0:
                                    e.wait_ge(c[1], c[2])
                                if useful:
                                    o_, i_ = useful.pop(0)
                                    e.dma_start(out=o_, in_=i_).then_inc(c[1], c[3])
                                else:
                                    k = self.dummy_ctr
                                    self.dummy_ctr += 1
                                    e.dma_start(out=self.dummy[1][k:k + 1, :], in_=self.dummy[0]).then_inc(c[1], c[3])
                        c2.__exit__(None, None, None)
            else:
                t(e)

    def barrier(self):
        evs = [(self.psem[k], self.pcnt[k]) for k in self.CE if self.pcnt[k] > 0]
        for r in self.ring.values():
            evs += [(sem, v) for sem, v in zip(r["sems"], r["vals"]) if v > 0]
        for qn in self.q:
            for ev in evs:
                self._wait(qn, ev)

    def finish(self):
        self.barrier()
        with self.nc.Block() as block:
            @block.tensor
            def _(e):
                self._replay("pe", e)

            @block.scalar
            def _(e):
                self._replay("act", e)

            @block.vector
            def _(e):
                self._replay("dve", e)

            @block.gpsimd
            def _(e):
                self._replay("pool", e)

            @block.sync
            def _(e):
                self._replay("sp", e)


class _Stop(Exception):
    pass


class Arena:
    def __init__(self, nc):
        self.nc = nc
        self.base = (nc.sbuf_base + 31) // 32 * 32
        self.top = nc.sbuf_top // 32 * 32
        self.cur = self.base
        self.n = 0
        self.peak = self.cur

    def alloc(self, name, shape, dtype):
        sz = {F32: 4, BF16: 2, I32: 4}[dtype]
        nbytes = int(np.prod(shape[1:])) * sz
        nbytes = (nbytes + 31) // 32 * 32
        assert self.cur + nbytes <= self.top, "SBUF overflow at %s: need %d have %d" % (name, nbytes, self.top - self.cur)
        self.n += 1
        t = self.nc.alloc_sbuf_tensor_at("%s_%d" % (name, self.n), list(shape), dtype, offset=self.cur)
        self.cur += nbytes
        self.peak = max(self.peak, self.cur)
        return t

    def mark(self):
        return self.cur

    def release(self, m):
        self.cur = m


def TS(out, in0, s1, s2, op0, op1=None):
    if op1 is None:
        return lambda e: e.tensor_scalar(out=out, in0=in0, scalar1=s1, scalar2=None, op0=op0)
    return lambda e: e.tensor_scalar(out=out, in0=in0, scalar1=s1, scalar2=s2, op0=op0, op1=op1)


def TT(out, a, b, op):
    return lambda e: e.tensor_tensor(out=out, in0=a, in1=b, op=op)


def STT(out, in0, sc, in1, op0, op1):
    return lambda e: e.scalar_tensor_tensor(out=out, in0=in0, scalar=sc, in1=in1, op0=op0, op1=op1)


def ACT(out, in_, func, bias=None, scale=None, accum=None):
    kw = {}
    if bias is not None:
        kw["bias"] = bias
    if scale is not None:
        kw["scale"] = scale
    if accum is not None:
        kw["accum_out"] = accum
    return lambda e: e.activation(out=out, in_=in_, func=func, **kw)


def CP(out, in_):
    return lambda e: e.tensor_copy(out=out, in_=in_)


def const_layout(cfg):
    T, TS_, E, KT, NB = cfg["T"], cfg["TS"], cfg["E"], cfg["KT"], cfg["NB"]
    NT = T // 128
    names = [("ident", 128), ("mf", 128), ("mb", 128), ("slf", 128), ("slb", 128), ("i2", 128),
             ("dpos", 128), ("dneg", 128), ("iota1", 128), ("iotar", 128), ("ones", 128), ("psw", 128),
             ("c127", 1), ("cp", 1), ("cmask", 4), ("reset", cfg["CH"]), ("iotae", E),
             ("tokid", NT), ("iotaw", KT), ("bbase", NB), ("pbase", cfg["NPAIR"]), ("onesrow", max(E, 8))]
    off = {}
    c = 0
    for n, w in names:
        off[n] = (c, w)
        c += w
    return off, c


def make_consts(cfg):
    off, ncol = const_layout(cfg)
    T, TS_, E, KT, NB = cfg["T"], cfg["TS"], cfg["E"], cfg["KT"], cfg["NB"]
    NT = T // 128
    C = np.zeros((128, ncol), np.float32)

    def put(n, a):
        o, w = off[n]
        C[:, o:o + w] = a

    s = np.arange(128)[:, None]
    t = np.arange(128)[None, :]
    same = (s // CHUNK) == (t // CHUNK)
    put("ident", (s == t))
    put("mf", same & (s <= t))
    put("mb", same & (s >= t))
    put("slf", s < t)
    put("slb", s > t)
    put("i2", 2.0 * (s == t))
    put("dpos", np.maximum(t - s, 0))
    put("dneg", np.maximum(s - t, 0))
    put("iota1", np.broadcast_to(t + 1, (128, 128)))
    put("iotar", np.broadcast_to(128 - t, (128, 128)))
    put("ones", 1.0)
    d = np.arange(128)
    partner = np.where((d % 64) < 32, d + 32, d - 32)
    psw = np.zeros((128, 128), np.float32)
    psw[partner, d] = 1.0
    put("psw", psw)
    put("c127", 127 - s)
    put("cp", s)
    put("cmask", (s // CHUNK) == np.arange(4)[None, :])
    put("reset", np.broadcast_to((np.arange(cfg["CH"]) % CHUNK != 0).astype(np.float32), (128, cfg["CH"])))
    tok = np.arange(TS_)
    rows = (tok // GRID_W).astype(np.float32)
    cols = (tok % GRID_W).astype(np.float32)
    inv = (10000.0 ** (-np.arange(ROPE_PAIRS, dtype=np.float32) / ROPE_PAIRS)).astype(np.float32)
    pos = np.where((d[:, None] // 64) == 0, rows[None, :], cols[None, :]).astype(np.float32)
    ang = (pos * inv[d % 32][:, None]).astype(np.float32)
    sign = np.where((d % 64) < 32, -1.0, 1.0)[:, None]
    rope = np.concatenate([np.cos(ang), np.sin(ang) * sign], axis=1).astype(np.float32)
    put("iotae", np.broadcast_to(np.arange(E), (128, E)))
    put("tokid", np.arange(NT)[None, :] * 128 + s)
    put("iotaw", np.arange(KT)[None, :] * 128 + s)
    put("bbase", np.broadcast_to(np.arange(NB) * 128, (128, NB)))
    put("pbase", np.broadcast_to(np.arange(cfg["NPAIR"]) * 128 * cfg["GH"], (128, cfg["NPAIR"])))
    put("onesrow", 1.0)
    return C, rope


def derive(cfg):
    cfg = dict(cfg)
    D, H = cfg["D"], cfg["H"]
    cfg["KT"] = D // 128
    cfg["T"] = cfg["NP"] * cfg["TP"] + cfg["TS"]
    cfg["NT"] = cfg["T"] // 128
    cfg["CH"] = min(512, cfg["T"])
    assert cfg["T"] % cfg["CH"] == 0
    cfg["GH"] = cfg.get("GH", 4)
    cfg["NPAIR"] = cfg["T"] * TOPK // (128 * cfg["GH"]) + cfg["E"]
    cfg["NB"] = cfg["GH"] * cfg["NPAIR"]
    cap = cfg.get("CWCAP", 1024)
    cfg["CWG"] = min(cap, 2 * cfg["D"])
    cfg["CWD"] = min(cap, cfg["D"])
    cfg["NCJG"] = 2 * cfg["D"] // cfg["CWG"]
    cfg["NCJD"] = cfg["D"] // cfg["CWD"]
    cfg["HGC"] = 640
    cfg["RTC"] = 768
    cfg["MG0"] = H * 640 + H * 768
    cfg["INC"] = cfg["MG0"] + 2 * D
    return cfg


def w_in_perm_index(cfg):
    D, H = cfg["D"], cfg["H"]
    HK = H * 128
    RW = H * 256
    o_hq, o_zf, o_zb, o_hi, o_hg = 0, HK, 2 * HK, 3 * HK, 4 * HK
    o_rq = 5 * HK
    o_rk = o_rq + HK
    o_rv = o_rk + HK
    o_rg = o_rv + RW
    o_ma = o_rg + RW
    o_mb = o_ma + D
    idx = []
    for h in range(H):
        r = np.arange(128)
        idx += [o_hq + h * 128 + r, o_zf + h * 128 + r, o_zb + h * 128 + r, o_hg + h * 128 + r, o_hi + h * 128 + r]
    for h in range(H):
        r = np.arange(128)
        r2 = np.arange(256)
        idx += [o_rq + h * 128 + r, o_rk + h * 128 + r, o_rg + h * 256 + r2, o_rv + h * 256 + r2]
    idx += [o_ma + np.arange(D), o_mb + np.arange(D)]
    return np.concatenate(idx)


def build(cfg, debug=False):
    cfg = derive(cfg)
    D, H, KT, T, NT, E, NB, CH = cfg["D"], cfg["H"], cfg["KT"], cfg["T"], cfg["NT"], cfg["E"], cfg["NB"], cfg["CH"]
    TP, NP, TS_ = cfg["TP"], cfg["NP"], cfg["TS"]
    DFF = D
    NCHK = T // CH
    TPC = CH // 128
    NCK = T // CHUNK
    R = NB * 128
    GW = min(512, D)
    NG = D // GW
    JT = GW // 128
    coff, ncst = const_layout(cfg)

    nc = bass.Bass("TRN2", target_bir_lowering=False)
    s = Sched(nc)
    A = Arena(nc)
    dbg_outs = {}

    def din(name, shape, dt=F32):
        return nc.dram_tensor(name, list(shape), dt, kind="ExternalInput")

    def dscr(name, shape, dt=F32):
        return nc.dram_tensor(name, list(shape), dt, kind="Internal")

    def dout(name, shape, dt=F32):
        return nc.dram_tensor(name, list(shape), dt, kind="ExternalOutput")

    x_d = din("x", [T, D])
    shf_d, shb_d = din("shf", [H, 128, 128]), din("shb", [H, 128, 128])
    srf_d, srb_d = din("srf", [H, 128, 256]), din("srb", [H, 128, 256])
    cT_d = din("cT", [128, KT, 2])
    adaw_d = din("ada_w", [D, 6 * D])
    adab_d = din("ada_b2", [2, 6 * D])
    n1g_d, n2g_d, nfg_d = din("n1g", [1, D]), din("n2g", [1, D]), din("nfg", [1, D])
    win_d = din("w_in", [D, cfg["INC"]])
    lbf_d, lbb_d = din("lbf", [128, 2, H]), din("lbb", [128, 2, H])
    hgng_d = din("hgng", [128, 1])
    retg_d = din("retg", [128, 2])
    rl2f_d, rl2b_d = din("rl2f", [1, H]), din("rl2b", [1, H])
    wpa_d, wpb_d, wout_d = din("w_pa", [H * 128, D]), din("w_pb", [H * 256, D]), din("w_out", [D, D])
    rw_d, rb_d = din("rw", [D, E]), din("rb", [1, E])
    CWG, CWD, NCJG, NCJD, NPAIR = cfg["CWG"], cfg["CWD"], cfg["NCJG"], cfg["NCJD"], cfg["NPAIR"]
    wgu_d, bgu_d = din("w_gu", [E * NCJG * 128, KT * CWG]), din("b_gu", [E * NCJG, CWG])
    wdn_d, bdn_d = din("w_dn", [E * NCJD * 128, KT * CWD]), din("b_dn", [E * NCJD, CWD])
    cst_d = din("cst", [128, ncst])
    rope_d = din("rope", [128, 2 * TS_])

    y_o = dout("y", [T, D])
    hf_o, hb_o = dout("hf", [NP, H, 128, 128]), dout("hb", [NP, H, 128, 128])
    rf_o, rb_o = dout("rf", [NP, H, 128, 256]), dout("rb_o", [NP, H, 128, 256])

    MOD = dscr("MOD", [2, 6 * D])
    OAB = dscr("OAB", [3 * H, 128, T], BF16)
    X1 = dscr("X1", [T, D])
    XM2 = dscr("XM2", [T, D], BF16)
    ROWTOK = dscr("ROWTOK", [R, 128], I32)
    Y = dscr("Y", [R, D])

    def dump(name, ap, reads):
        if not debug:
            return
        shp = list(ap.shape)
        o = dout("dbg_" + name, shp, ap.dtype)
        dbg_outs[name] = shp
        s.dma("sp", lambda e: e.dma_start(out=o.ap(), in_=ap), reads=reads, writes=["dbg_" + name])

    psA = [nc.alloc_psum_tensor("psA%d" % i, [128, 512], F32) for i in range(4)]
    psS = [nc.alloc_psum_tensor("psS%d" % i, [128, 512], F32) for i in range(2)]
    psTbs = [nc.alloc_psum_tensor("psTb%d" % i, [128, 8, 128], BF16) for i in range(2)]

    cst = A.alloc("cst", [128, ncst], F32)
    dmy = A.alloc("dmy", [1, 32], F32)

    def C(name):
        o, w = coff[name]
        return cst[:, o:o + w]

    identb = A.alloc("identb", [128, 128], BF16)
    onesb = A.alloc("onesb", [128, 128], BF16)
    slfb = A.alloc("slfb", [128, 128], BF16)
    small = A.alloc("small", [128, 64], F32)
    lb = A.alloc("lb", [128, 2, 2, H], F32)
    lg = A.alloc("lg", [128, 2, H], F32)
    hgng = A.alloc("hgng", [128, 1], F32)
    retg = A.alloc("retg", [128, 2], F32)
    oab_o = [A.alloc("oabo%d" % i, [128, CH], BF16) for i in range(2)]
    oab_ctr = [0]
    rw = A.alloc("rw", [128, KT, E], F32)
    rbrow = A.alloc("rbrow", [128, E], F32)
    Mf = A.alloc("Mf", [128, NT, E], F32)
    Mb = A.alloc("Mb", [128, NT, E], BF16)
    Wd = A.alloc("Wd", [128, NT, E], F32)
    lgt = A.alloc("lgt", [128, E], F32)
    pex = A.alloc("pex", [128, E], F32)
    top8 = A.alloc("top8", [128, 8], F32)
    lbraw = A.alloc("lbraw", [128, 2, 2, H], F32)
    scb = A.alloc("scb", [128, KT, 2], BF16)
    mP = A.mark()
    WS = 768
    wslots = [A.alloc("wslot%d" % i, [128, max(KT, 2 * H), WS], BF16) for i in range(2)]
    wctr = [0]
    xmT = A.alloc("xmT", [128, KT, T], BF16)

    s.op("dve", lambda e: e.memset(dmy[:], 0.0), writes=["dmy"])
    DUMMY = dscr("DUMMY", [4096, 16])
    s.dummy = (dmy[0:1, 0:16], DUMMY.ap())
    s.dma("sp", lambda e: e.dma_start(out=cst[:], in_=cst_d.ap()), writes=["cst"])
    s.op("dve", CP(identb[:], C("ident")), reads=["cst"], writes=["identb"])
    s.op("dve", CP(onesb[:], C("ones")), reads=["cst"], writes=["onesb"])
    s.op("dve", CP(slfb[:], C("slf")), reads=["cst"], writes=["slfb"])
    s.dma("sp", lambda e: e.dma_start(out=hgng[:], in_=hgng_d.ap()), writes=["hgng"])
    s.dma("sp", lambda e: e.dma_start(out=retg[:], in_=retg_d.ap()), writes=["retg"])

    def wload(src2d, kt_n, ncols):
        i = wctr[0] % 2
        wctr[0] += 1
        slot = wslots[i]
        tok = "w%d" % i
        s.dma("pool", lambda e: e.dma_start(out=slot[:, 0:kt_n, 0:ncols],
                                            in_=src2d.rearrange("(kt p) n -> p kt n", p=128)), writes=[tok])
        return slot, tok

    _bc = {}

    def BC(e, v):
        if v not in _bc:
            _bc[v] = e.to_reg(v)
        return _bc[v]

    def mms(items, reads, writes):
        n = len(items)
        for i, (o, l, r, st, sp_) in enumerate(items):
            s.op("pe", lambda e, o=o, l=l, r=r, st=st, sp_=sp_: e.matmul(o, lhsT=l, rhs=r, start=st, stop=sp_),
                 reads=reads, writes=writes, inc=(i == n - 1))

    def acc_mm(out, pairs, reads, writes):
        n = len(pairs)
        mms([(out, l, r, i == 0, i == n - 1) for i, (l, r) in enumerate(pairs)], reads, writes)

    def transposes(srcs, dst_fn, reads, dst_tok_fn, evac_eng="act", f32=False, extra=None):
        n = len(srcs)
        g = 0
        half = 0
        while g < n:
            m = min(4, n - g)
            if f32:
                pt, ptok = psS[1][:].rearrange("p (c v) -> p c v", v=128), "psS1"
                view = pt[:, 0:m, :]
            else:
                pt, ptok = psTbs[half], "psTb%d" % half
                view = pt[:, 0:m, :]
            for j in range(m):
                src = srcs[g + j]
                o = pt[:, j, :]
                idn = C("ident") if f32 else identb[:]
                s.op("pe", lambda e, o=o, src=src, idn=idn: e.transpose(out=o, in_=src, identity=idn),
                     reads=list(reads) + ["cst", "identb"], writes=[ptok], inc=(j == m - 1))
            dst = dst_fn(g, m)
            if evac_eng == "act":
                s.op("act", lambda e, dst=dst, view=view: e.copy(out=dst, in_=view), reads=[ptok], writes=dst_tok_fn(g, m))
            else:
                s.op("dve", CP(dst, view), reads=[ptok], writes=dst_tok_fn(g, m))
            if extra is not None:
                extra(pt, 0, g, m, ptok)
            g += m
            half ^= 1

    s.dma("sp", lambda e: e.dma_start(out=lbraw[:, 0], in_=lbf_d.ap()), writes=["lbraw0"])
    s.dma("sp", lambda e: e.dma_start(out=lbraw[:, 1], in_=lbb_d.ap()), writes=["lbraw1"])
    for d_ in range(2):
        s.op("dve", TT(lbraw[:, d_, 0, :], lbraw[:, d_, 0, :], lbraw[:, d_, 1, :], ALU.subtract),
             reads=["lbraw%d" % d_], writes=["lbraw%d" % d_])
        s.op("act", ACT(lb[:, d_, 0, :], lbraw[:, d_, 0, :], AF.Sigmoid), reads=["lbraw%d" % d_], writes=["lb%d" % d_])
        s.op("dve", TS(lb[:, d_, 1, :], lb[:, d_, 0, :], -1.0, 1.0, ALU.mult, ALU.add), reads=["lb%d" % d_], writes=["lb%d" % d_])
    for d_, src in enumerate((rl2f_d, rl2b_d)):
        s.dma("sp", lambda e, d_=d_, src=src: e.dma_start(out=lg[:, d_, :], in_=src.ap().partition_broadcast(128)),
              writes=["lg%d" % d_])
        s.op("act", ACT(lg[:, d_, :], lg[:, d_, :], AF.Exp, scale=math.log(2.0)), reads=["lg%d" % d_], writes=["lg%d" % d_])
        s.op("dve", TS(lg[:, d_, :], lg[:, d_, :], -1.0, 1.0, ALU.mult, ALU.add), reads=["lg%d" % d_], writes=["lg%d" % d_])
        s.op("act", ACT(lg[:, d_, :], lg[:, d_, :], AF.Ln), reads=["lg%d" % d_], writes=["lg%d" % d_])

    if cfg.get("STOP") == "0":
        s.finish(); nc._cfg = cfg; nc._ninst = s.ninst; nc._peak = 0; nc._dbg = dbg_outs
        return nc
    mA2 = A.mark()
    cT = A.alloc("cT", [128, KT, 2], F32)
    s.dma("sp", lambda e: e.dma_start(out=cT[:], in_=cT_d.ap()), writes=["cT"])
    s.op("act", ACT(scb[:], cT[:], AF.Silu), reads=["cT"], writes=["scb"])
    modt = [A.alloc("modt%d" % i, [2, 512], F32) for i in range(2)]
    adab = [A.alloc("adab%d" % i, [2, 512], F32) for i in range(2)]
    NMC = (6 * D) // 512
    def ada_chunk(j, scb_, modt_, adab_):
        slot, wt = wload(adaw_d.ap()[:, j * 512:(j + 1) * 512], KT, 512)
        ps = psA[j % 4]
        acc_mm(ps[0:2, :], [(scb_[:, kt, :], slot[:, kt, 0:512]) for kt in range(KT)], ["scb", wt], ["psA%d" % (j % 4)])
        jj = j % len(adab_)
        tag = "c" if len(adab_) == 1 else ""
        ab = adab_[jj]
        mt = modt_[jj]
        s.dma("sp", lambda e: e.dma_start(out=ab[:], in_=adab_d.ap()[:, j * 512:(j + 1) * 512]), writes=["adab%s%d" % (tag, jj)])
        s.op("dve", TT(mt[:], ps[0:2, :], ab[:], ALU.add), reads=["psA%d" % (j % 4), "adab%s%d" % (tag, jj)], writes=["modt%s%d" % (tag, jj)])
        s.dma("sp", lambda e: e.dma_start(out=MOD.ap()[:, j * 512:(j + 1) * 512], in_=mt[:]),
              reads=["modt%s%d" % (tag, jj)], writes=["MOD%d" % j])

    NMA = min(NMC, (2 * D + 511) // 512)
    for j in range(NMA):
        ada_chunk(j, scb, modt, adab)

    def modtoks(c0, c1):
        return ["MOD%d" % j for j in range(c0 // 512, (c1 - 1) // 512 + 1)]

    def load_row(dst, cond, which, tok):
        c0 = which * D
        s.dma("sp", lambda e: e.dma_start(out=dst, in_=MOD.ap()[cond:cond + 1, c0:c0 + D].partition_broadcast(128)),
              reads=modtoks(c0, c0 + D), writes=[tok])

    def load_vec_row(dst, src_d, tok):
        s.dma("sp", lambda e: e.dma_start(out=dst, in_=src_d.ap().partition_broadcast(128)), writes=[tok])

    tile_cond = [0] * (NP * TP // 128) + [1] * (TS_ // 128)
    segs = [(p * TP // 128, TP // 128, "p", p) for p in range(NP)] + [(NP * TP // 128, TS_ // 128, "s", 0)]

    rows_a = {}
    n1row = A.alloc("n1row", [128, D], F32)
    load_vec_row(n1row[:], n1g_d, "n1row")
    for c in range(2):
        a1 = A.alloc("a1row%d" % c, [128, D], F32)
        sh = A.alloc("sh1row%d" % c, [128, D], F32)
        load_row(a1[:], c, 1, "a1row%d" % c)
        load_row(sh[:], c, 0, "sh1row%d" % c)
        s.op("dve", STT(a1[:], a1[:], 1.0, n1row[:], ALU.add, ALU.mult), reads=["a1row%d" % c, "n1row"], writes=["a1row%d" % c])
        rows_a[c] = (a1, sh)
    xbuf = [A.alloc("xbuf%d" % i, [128, D], F32) for i in range(2)]
    t32 = A.alloc("t32", [128, D], F32)
    xmb = [A.alloc("xmb%d" % i, [128, D], BF16) for i in range(2)]
    junk = A.alloc("junk", [128, D], BF16)

    def rms_rstd(src, src_tok, col, n):
        sc = small[:, col:col + 1]
        tk = "small%d" % col
        s.op("dve", lambda e: e.memset(sc, 0.0), writes=[tk])
        s.op("act", ACT(junk[:, 0:n], src, AF.Square, accum=sc), reads=[src_tok, tk], writes=["junk", tk])
        s.op("dve", TS(sc, sc, 1.0 / n, EPS, ALU.mult, ALU.add), reads=[tk], writes=[tk])
        s.op("act", ACT(sc, sc, AF.Sqrt), reads=[tk], writes=[tk])
        s.op("dve", lambda e: e.reciprocal(out=sc, in_=sc), reads=[tk], writes=[tk])
        return sc, tk

    for i in range(NT):
        c = tile_cond[i]
        xt = xbuf[i % 2]
        xtok = "xbuf%d" % (i % 2)
        s.dma("sp", lambda e, xt=xt, i=i: e.dma_start(out=xt[:], in_=x_d.ap()[i * 128:(i + 1) * 128, :]), writes=[xtok])
        sc, tk = rms_rstd(xt[:], xtok, i % 2, D)
        a1, sh = rows_a[c]
        s.op("dve", STT(t32[:], xt[:], sc, a1[:], ALU.mult, ALU.mult), reads=[xtok, tk, "a1row%d" % c], writes=["t32"])
        xm = xmb[i % 2]
        s.op("dve", TT(xm[:], t32[:], sh[:], ALU.add), reads=["t32", "sh1row%d" % c], writes=["xmb%d" % (i % 2)])
        transposes([xm[:, kt * 128:(kt + 1) * 128] for kt in range(KT)],
                   lambda g, m, i=i: xmT[:, g:g + m, i * 128:(i + 1) * 128],
                   ["xmb%d" % (i % 2)], lambda g, m, i=i: ["xmT_%d" % i])
    xmT_toks = ["xmT_%d" % i for i in range(NT)]
    if debug:
        for kt in range(KT):
            dump("xmT%d" % kt, xmT[:, kt, :], xmT_toks)
    s.barrier()
    A.release(mA2)

    if cfg.get("STOP") == "A":
        s.finish(); nc._cfg = cfg; nc._ninst = s.ninst; nc._peak = 0; nc._dbg = dbg_outs
        return nc
    mB = A.mark()
    q32 = A.alloc("q32", [128, T], BF16)
    fdir = [A.alloc("f%d" % d_, [128, T], F32) for d_ in range(2)]
    s1 = A.alloc("s1", [128, T], F32)
    s2 = A.alloc("s2", [128, T], F32)
    gcol = A.alloc("gcol", [128, NCK], F32)
    sgate = A.alloc("sgate", [128, T], BF16)
    vTM = A.alloc("vTM", [128, NT, 128], BF16)
    qp = [A.alloc("qp%d" % d_, [128, T], BF16) for d_ in range(2)]
    kp = [A.alloc("kp%d" % d_, [128, T], BF16) for d_ in range(2)]
    kpTM = [A.alloc("kpTM%d" % d_, [128, NT, 128], BF16) for d_ in range(2)]
    vexp = A.alloc("vexp", [128, NT, 4, 128], BF16)
    Rpb = [A.alloc("Rpb%d" % d_, [128, NCK, 128], BF16) for d_ in range(2)]
    decay = [A.alloc("decay%d" % d_, [128, NCK], F32) for d_ in range(2)]
    R32 = [[A.alloc("R32_%d_%d" % (d_, sg), [128, 128], F32) for sg in range(len(segs))] for d_ in range(2)]
    msk = [A.alloc("msk%d" % i, [128, 128], BF16) for i in range(2)]
    sqb = A.alloc("sqb", [128, CH], BF16)
    Mdir = [C("mf"), C("mb")]

    SB = cfg.get("SB", 99)
    for h in range(H if SB == 99 else 1):
        slot, wt = wload(win_d.ap()[:, h * 640:(h + 1) * 640], KT, 640)
        for ch in range(NCHK):
            cs = slice(ch * CH, (ch + 1) * CH)
            for j in range(4):
                acc_mm(psA[j][:, 0:CH], [(slot[:, kt, j * 128:(j + 1) * 128], xmT[:, kt, cs]) for kt in range(KT)],
                       [wt] + xmT_toks, ["psA%d" % j])
            s.op("act", ACT(q32[:, cs], psA[0][:, 0:CH], AF.Silu), reads=["psA0"], writes=["q32"])
            for d_ in range(2):
                s.op("act", ACT(s1[:, cs], psA[1 + d_][:, 0:CH], AF.Sigmoid), reads=["psA%d" % (1 + d_)], writes=["s1"])
                s.op("dve", TS(fdir[d_][:, cs], s1[:, cs], lb[:, d_, 1, h:h + 1], lb[:, d_, 0, h:h + 1], ALU.mult, ALU.add),
                     reads=["s1", "lb%d" % d_], writes=["f%d" % d_])
            s.op("act", ACT(sgate[:, cs], psA[3][:, 0:CH], AF.Sigmoid), reads=["psA3"], writes=["sgate"])
        for i in range(NT):
            ps = psS[i % 2]
            acc_mm(ps[:, 0:128], [(xmT[:, kt, i * 128:(i + 1) * 128], slot[:, kt, 512:640]) for kt in range(KT)],
                   [wt] + xmT_toks, ["psS%d" % (i % 2)])
            s.op("act", lambda e, ps=ps, i=i: e.copy(out=vTM[:, i, :], in_=ps[:, 0:128]), reads=["psS%d" % (i % 2)], writes=["vTM"])
        cmo, _ = coff["cmask"]
        for c in range(4):
            s.op("dve", TS(vexp[:, :, c, :], vTM[:], cst[:, cmo + c:cmo + c + 1], None, ALU.mult), reads=["vTM", "cst"], writes=["vexp"])
        if SB == 1:
            break
        for d_ in range(2):
            f = fdir[d_]
            ftok = "f%d" % d_
            s.op("act", ACT(s1[:], f[:], AF.Ln), reads=[ftok], writes=["s1"])
            s.op("dve", TS(f[:], f[:], -1.0, 1.0, ALU.mult, ALU.add), reads=[ftok, "s1"], writes=[ftok])
            for ch in range(NCHK):
                cs = slice(ch * CH, (ch + 1) * CH)
                s.op("dve", lambda e, cs=cs: e.tensor_tensor_scan(out=s2[:, cs], data0=C("reset"), data1=s1[:, cs], initial=0.0,
                                                                  op0=ALU.mult, op1=ALU.add), reads=["s1", "cst"], writes=["s2"])
            gi3 = s2[:].rearrange("p (c k) -> p c k", k=CHUNK)
            s.op("act", ACT(decay[d_][:], gi3[:, :, CHUNK - 1], AF.Exp), reads=["s2"], writes=["decay%d" % d_])
            if d_ == 0:
                s.op("dve", CP(gcol[:], gi3[:, :, CHUNK - 1]), reads=["s2"], writes=["gcol"])
                s.op("dve", TT(gi3, gcol[:].unsqueeze(2).to_broadcast([128, NCK, CHUNK]), gi3, ALU.subtract),
                     reads=["s2", "gcol", "decay%d" % d_], writes=["s2"])
            else:
                s.op("dve", TT(s2[:], s2[:], s1[:], ALU.subtract), reads=["s2", "s1", "decay%d" % d_], writes=["s2"])
            s.op("act", ACT(s1[:], s2[:], AF.Exp), reads=["s2"], writes=["s1"])
            s.op("dve", TT(kp[d_][:], f[:], s1[:], ALU.mult), reads=[ftok, "s1"], writes=["kp%d" % d_])
            s.op("act", ACT(s1[:], s2[:], AF.Exp, scale=-1.0), reads=["s2", "kp%d" % d_], writes=["s1"])
            s.op("dve", TT(qp[d_][:], q32[:], s1[:], ALU.mult), reads=["q32", "s1"], writes=["qp%d" % d_])
            if SB == 2:
                continue
            transposes([kp[d_][:, i * 128:(i + 1) * 128] for i in range(NT)],
                       lambda g, m, d_=d_: kpTM[d_][:, g:g + m, :], ["kp%d" % d_], lambda g, m, d_=d_: ["kpTM%d" % d_])
        banks = [(psS[0], "psS0"), (psS[1], "psS1"), (psA[1], "psA1"), (psA[2], "psA2"), (psA[3], "psA3"), (psA[0], "psA0")]
        chains = []
        for d_ in range(2):
            for sgi, (t0, nt, kind, sidx) in enumerate(segs):
                Rr = R32[d_][sgi]
                rtok = "R32_%d_%d" % (d_, sgi)
                if kind == "p":
                    s.op("dve", lambda e, Rr=Rr: e.memset(Rr[:], 0.0), writes=[rtok])
                else:
                    src = (shf_d, shb_d)[d_]
                    s.dma("sp", lambda e, Rr=Rr, src=src, h=h: e.dma_start(out=Rr[:], in_=src.ap()[h]), writes=[rtok])
                tiles = list(range(t0, t0 + nt))
                if d_ == 1:
                    tiles = tiles[::-1]
                chains.append(dict(d=d_, Rr=Rr, rtok=rtok, tiles=tiles, kind=kind, sidx=sidx))
        for ci, cn in enumerate(chains):
            cn["bank"], cn["btok"] = banks[ci % len(banks)]
        for r in range(max(len(cn["tiles"]) for cn in chains)):
            act_ch = [cn for cn in chains if r < len(cn["tiles"])]
            for cn in act_ch:
                i = cn["tiles"][r]
                d_ = cn["d"]
                mms([(cn["bank"][:, 0:512], kpTM[d_][:, i, :], vexp[:, i].rearrange("p c v -> p (c v)"), True, True)],
                    ["kpTM%d" % d_, "vexp"], [cn["btok"]])
            for st in range(4):
                for cn in act_ch:
                    i = cn["tiles"][r]
                    d_ = cn["d"]
                    c = st if d_ == 0 else 3 - st
                    cg = i * 4 + c
                    Rr, rtok = cn["Rr"], cn["rtok"]
                    pv = cn["bank"][:].rearrange("p (c v) -> p c v", v=128)
                    s.op("act", ACT(Rpb[d_][:, cg, :], Rr[:], AF.Copy, scale=decay[d_][:, cg:cg + 1]),
                         reads=[rtok, "decay%d" % d_], writes=["Rpb%d_%d" % (d_, i)])
                    s.op("dve", STT(Rr[:], Rr[:], decay[d_][:, cg:cg + 1], pv[:, c, :], ALU.mult, ALU.add),
                         reads=[rtok, "decay%d" % d_, cn["btok"]], writes=[rtok])
        for cn in chains:
            if cn["kind"] == "p":
                dst = (hf_o, hb_o)[cn["d"]]
                s.dma("sp", lambda e, Rr=cn["Rr"], dst=dst, sidx=cn["sidx"], h=h: e.dma_start(out=dst.ap()[sidx, h], in_=Rr[:]),
                      reads=[cn["rtok"]], writes=["hout%d_%d_%d" % (cn["d"], cn["sidx"], h)])
        if SB in (2, 3, 4):
            break
        for ch in range(NCHK):
            its = []
            for tl in range(TPC):
                i = ch * TPC + tl
                tsl = slice(i * 128, (i + 1) * 128)
                reg = psA[0][:, tl * 128:(tl + 1) * 128]
                first = True
                for d_ in range(2):
                    ps = psS[d_]
                    s.op("pe", lambda e, ps=ps, d_=d_, tsl=tsl: e.matmul(ps[:, 0:128], lhsT=kp[d_][:, tsl], rhs=qp[d_][:, tsl], start=True, stop=True),
                         reads=["kp%d" % d_, "qp%d" % d_], writes=["psS%d" % d_])
                    mk = msk[d_]
                    s.op("dve", TT(mk[:], ps[:, 0:128], Mdir[d_], ALU.mult), reads=["psS%d" % d_, "cst"], writes=["msk%d" % d_])
                    items = [(reg, vTM[:, i, :], mk[:], first, (SB == 5 and d_ == 1))]
                    first = False
                    for c in range(4 if SB != 5 else 0):
                        cg = i * 4 + c
                        items.append((psA[0][:, tl * 128 + c * 32: tl * 128 + (c + 1) * 32], Rpb[d_][:, cg, :],
                                      qp[d_][:, i * 128 + c * 32:i * 128 + (c + 1) * 32], False, (d_ == 1 and c == 3)))
                    mms(items, ["vTM", "msk%d" % d_, "Rpb%d_%d" % (d_, i), "qp%d" % d_], ["psA0"])
            cs = slice(ch * CH, (ch + 1) * CH)
            if SB in (5, 6):
                continue
            s.op("dve", CP(s1[:, 0:CH], psA[0][:, 0:CH]), reads=["psA0"], writes=["s1"])
            s.op("act", ACT(sqb[:], psA[0][:, 0:CH], AF.Square), reads=["psA0"], writes=["sqb"])
            acc_mm(psA[1][:, 0:CH], [(onesb[:], sqb[:])], ["onesb", "sqb"], ["psA1"])
            s.op("dve", TS(s2[:, 0:CH], psA[1][:, 0:CH], 1.0 / 128, EPS, ALU.mult, ALU.add), reads=["psA1"], writes=["s2"])
            s.op("act", ACT(s2[:, 0:CH], s2[:, 0:CH], AF.Sqrt), reads=["s2"], writes=["s2"])
            s.op("dve", lambda e: e.reciprocal(out=s2[:, 0:CH], in_=s2[:, 0:CH]), reads=["s2"], writes=["s2"])
            if SB == 7:
                continue
            s.op("dve", STT(s1[:, 0:CH], s1[:, 0:CH], hgng[:, 0:1], s2[:, 0:CH], ALU.mult, ALU.mult), reads=["s1", "s2", "hgng"], writes=["s1"])
            ob = oab_o[oab_ctr[0] % 2]
            otok = "oabo%d" % (oab_ctr[0] % 2)
            oab_ctr[0] += 1
            s.op("dve", TT(ob[:], s1[:, 0:CH], sgate[:, cs], ALU.mult), reads=["s1", "sgate"], writes=[otok])
            if SB == 8:
                continue
            s.dma("sp", lambda e, ob=ob, h=h, cs=cs: e.dma_start(out=OAB.ap()[h, :, cs], in_=ob[:]), reads=[otok], writes=["OAB_%d_%d" % (h, ch)])
    s.barrier()
    A.release(mB)

    if cfg.get("STOP") == "B":
        s.finish(); nc._cfg = cfg; nc._ninst = s.ninst; nc._peak = 0; nc._dbg = dbg_outs
        return nc
    mC = A.mark()
    q32 = A.alloc("rq32", [128, T], F32)
    k32 = A.alloc("rk32", [128, T], F32)
    qb = A.alloc("rqb", [128, T], BF16)
    kb = A.alloc("rkb", [128, T], BF16)
    qin = [A.alloc("qin%d" % d_, [128, T], BF16) for d_ in range(2)]
    kend = [A.alloc("kend%d" % d_, [128, NT, 128], BF16) for d_ in range(2)]
    rvTM = A.alloc("rvTM", [128, NT, 256], BF16)
    Rsb = [A.alloc("Rsb%d" % d_, [128, NT, 256], BF16) for d_ in range(2)]
    rg = A.alloc("rg", [128, 2, T], BF16)
    RR = [[A.alloc("RR_%d_%d" % (d_, sg), [128, 256], F32) for sg in range(len(segs))] for d_ in range(2)]
    tA = A.alloc("tA", [128, 128], F32)
    tB = A.alloc("tB", [128, 128], F32)
    Ds = A.alloc("Ds", [128, 128], F32)
    rowt = [A.alloc("rowt%d" % d_, [128, 128], F32) for d_ in range(2)]
    colt = A.alloc("colt", [128, 4], F32)
    t1 = A.alloc("t1", [128, CH], F32)
    t2 = A.alloc("t2", [128, CH], F32)
    rmsk = [A.alloc("rmsk%d" % i, [128, 128], BF16) for i in range(2)]
    ro32 = [A.alloc("ro32_%d" % v, [128, CH], F32) for v in range(2)]
    rob = [A.alloc("rob_%d" % v, [128, CH], BF16) for v in range(2)]
    rsq = [A.alloc("rsq_%d" % v, [128, CH], BF16) for v in range(2)]
    mean = A.alloc("mean", [128, CH], F32)
    var = A.alloc("var", [128, CH], F32)
    ts0 = NP * TP
    modt_c = [A.alloc("modtc0", [2, 512], F32)]
    adab_c = [A.alloc("adabc0", [2, 512], F32)]
    ada_rest = list(range(NMA, NMC))
    ropet = A.alloc("ropet", [128, 2 * TS_], F32)
    s.dma("sp", lambda e: e.dma_start(out=ropet[:], in_=rope_d.ap()), writes=["ropet"])

    for h in range(H):
        c0 = H * 640 + h * 768
        slot, wt = wload(win_d.ap()[:, c0:c0 + 768], KT, 768)
        nrest = (len(ada_rest) + (H - h) - 1) // (H - h)
        ada_now, ada_rest = ada_rest[:nrest], ada_rest[nrest:]
        for ch in range(NCHK):
            cs = slice(ch * CH, (ch + 1) * CH)
            for j in range(4):
                acc_mm(psA[j][:, 0:CH], [(slot[:, kt, j * 128:(j + 1) * 128], xmT[:, kt, cs]) for kt in range(KT)],
                       [wt] + xmT_toks, ["psA%d" % j])
            s.op("act", lambda e, cs=cs: e.copy(out=q32[:, cs], in_=psA[0][:, 0:CH]), reads=["psA0"], writes=["rq32"])
            s.op("act", ACT(k32[:, cs], psA[1][:, 0:CH], AF.Copy, scale=128.0 ** -0.5), reads=["psA1"], writes=["rk32"])
            for v in range(2):
                s.op("act", ACT(rg[:, v, cs], psA[2 + v][:, 0:CH], AF.Silu), reads=["psA%d" % (2 + v)], writes=["rg"])
        for i in range(NT):
            ps = psS[i % 2]
            acc_mm(ps[:, 0:256], [(xmT[:, kt, i * 128:(i + 1) * 128], slot[:, kt, 512:768]) for kt in range(KT)],
                   [wt] + xmT_toks, ["psS%d" % (i % 2)])
            s.op("act", lambda e, ps=ps, i=i: e.copy(out=rvTM[:, i, :], in_=ps[:, 0:256]), reads=["psS%d" % (i % 2)], writes=["rvTM"])
        for j in ada_now:
            ada_chunk(j, scb, modt_c, adab_c)
        if ts0 > 0:
            s.op("dve", CP(qb[:, 0:ts0], q32[:, 0:ts0]), reads=["rq32"], writes=["rqb"])
            s.op("dve", CP(kb[:, 0:ts0], k32[:, 0:ts0]), reads=["rk32"], writes=["rkb"])
        RC = min(512, TS_)
        for rc in range(TS_ // RC):
            cs = slice(ts0 + rc * RC, ts0 + (rc + 1) * RC)
            cosv = ropet[:, rc * RC:(rc + 1) * RC]
            sinv = ropet[:, TS_ + rc * RC: TS_ + (rc + 1) * RC]
            for src, dstb, stok, dtok, pi in ((q32, qb, "rq32", "rqb", 2), (k32, kb, "rk32", "rkb", 3)):
                acc_mm(psA[pi][:, 0:RC], [(C("psw"), src[:, cs])], ["cst", stok], ["psA%d" % pi])
                s.op("dve", TT(t1[:, 0:RC], src[:, cs], cosv, ALU.mult), reads=[stok, "ropet"], writes=["t1"])
                s.op("dve", TT(t2[:, 0:RC], psA[pi][:, 0:RC], sinv, ALU.mult), reads=["psA%d" % pi, "ropet"], writes=["t2"])
                s.op("dve", TT(dstb[:, cs], t1[:, 0:RC], t2[:, 0:RC], ALU.add), reads=["t1", "t2"], writes=[dtok])
        lgf = lg[:, 0, h:h + 1]
        lgb = lg[:, 1, h:h + 1]
        s.op("act", ACT(tA[:], C("dpos"), AF.Exp, scale=lgf), reads=["cst", "lg0"], writes=["tA"])
        s.op("dve", TT(tA[:], tA[:], C("slf"), ALU.mult), reads=["tA", "cst"], writes=["tA"])
        s.op("act", ACT(tB[:], C("dneg"), AF.Exp, scale=lgb), reads=["cst", "lg1"], writes=["tB"])
        s.op("dve", TT(tB[:], tB[:], C("slb"), ALU.mult), reads=["tB", "cst"], writes=["tB"])
        s.op("dve", TT(Ds[:], tA[:], tB[:], ALU.add), reads=["tA", "tB"], writes=["Ds"])
        s.op("dve", TT(Ds[:], Ds[:], C("i2"), ALU.add), reads=["Ds", "cst"], writes=["Ds"])
        s.op("act", ACT(rowt[0][:], C("iota1"), AF.Exp, scale=lgf), reads=["cst", "lg0"], writes=["rowt0"])
        s.op("act", ACT(rowt[1][:], C("iotar"), AF.Exp, scale=lgb), reads=["cst", "lg1"], writes=["rowt1"])
        s.op("act", ACT(colt[:, 0:1], C("c127"), AF.Exp, scale=lgf), reads=["cst", "lg0"], writes=["colt"])
        s.op("act", ACT(colt[:, 1:2], C("cp"), AF.Exp, scale=lgb), reads=["cst", "lg1"], writes=["colt"])
        s.op("act", ACT(colt[:, 2:3], lgf, AF.Exp, scale=128.0), reads=["lg0"], writes=["colt"])
        s.op("act", ACT(colt[:, 3:4], lgb, AF.Exp, scale=128.0), reads=["lg1"], writes=["colt"])
        for i in range(NT):
            tsl = slice(i * 128, (i + 1) * 128)
            for d_ in range(2):
                s.op("dve", TT(qin[d_][:, tsl], qb[:, tsl], rowt[d_][:], ALU.mult), reads=["rqb", "rowt%d" % d_], writes=["qin%d" % d_])
        g = 0
        half = 0
        while g < NT:
            m = min(4, NT - g)
            ptok = "psTb%d" % half
            for j in range(m):
                o = psTbs[half][:, j, :]
                src = kb[:, (g + j) * 128:(g + j + 1) * 128]
                s.op("pe", lambda e, o=o, src=src: e.transpose(out=o, in_=src, identity=identb[:]),
                     reads=["rkb", "identb"], writes=[ptok], inc=(j == m - 1))
            view = psTbs[half][:, 0:m, :]
            for d_ in range(2):
                s.op("act", ACT(kend[d_][:, g:g + m, :], view, AF.Copy, scale=colt[:, d_:d_ + 1]), reads=[ptok, "colt"], writes=["kend%d" % d_])
            g += m
            half ^= 1
        banks = [(psS[0], "psS0"), (psS[1], "psS1"), (psA[1], "psA1"), (psA[2], "psA2"), (psA[3], "psA3"), (psA[0], "psA0")]
        chains = []
        for d_ in range(2):
            for sgi, (t0, nt, kind, sidx) in enumerate(segs):
                Rr = RR[d_][sgi]
                rtok = "RR_%d_%d" % (d_, sgi)
                if kind == "p":
                    s.op("dve", lambda e, Rr=Rr: e.memset(Rr[:], 0.0), writes=[rtok])
                else:
                    src = (srf_d, srb_d)[d_]
                    s.dma("sp", lambda e, Rr=Rr, src=src, h=h: e.dma_start(out=Rr[:], in_=src.ap()[h]), writes=[rtok])
                tiles = list(range(t0, t0 + nt))
                if d_ == 1:
                    tiles = tiles[::-1]
                chains.append(dict(d=d_, Rr=Rr, rtok=rtok, tiles=tiles, kind=kind, sidx=sidx))
        for ci, cn in enumerate(chains):
            cn["bank"], cn["btok"] = banks[ci % len(banks)]
        for r in range(max(len(cn["tiles"]) for cn in chains)):
            act_ch = [cn for cn in chains if r < len(cn["tiles"])]
            for cn in act_ch:
                i = cn["tiles"][r]
                d_ = cn["d"]
                acc_mm(cn["bank"][:, 0:256], [(kend[d_][:, i, :], rvTM[:, i, :])], ["kend%d" % d_, "rvTM"], [cn["btok"]])
            for cn in act_ch:
                i = cn["tiles"][r]
                d_ = cn["d"]
                Rr, rtok = cn["Rr"], cn["rtok"]
                s.op("act", lambda e, d_=d_, i=i, Rr=Rr: e.copy(out=Rsb[d_][:, i, :], in_=Rr[:]), reads=[rtok], writes=["Rsb%d_%d" % (d_, i)])
                s.op("dve", STT(Rr[:], Rr[:], colt[:, 2 + d_:3 + d_], cn["bank"][:, 0:256], ALU.mult, ALU.add),
                     reads=[rtok, "colt", cn["btok"]], writes=[rtok])
        for cn in chains:
            if cn["kind"] == "p":
                dst = (rf_o, rb_o)[cn["d"]]
                s.dma("sp", lambda e, Rr=cn["Rr"], dst=dst, sidx=cn["sidx"], h=h: e.dma_start(out=dst.ap()[sidx, h], in_=Rr[:]),
                      reads=[cn["rtok"]], writes=["rout%d_%d_%d" % (cn["d"], cn["sidx"], h)])
        for ch in range(NCHK):
            cs = slice(ch * CH, (ch + 1) * CH)
            for tl in range(TPC):
                i = ch * TPC + tl
                tsl = slice(i * 128, (i + 1) * 128)
                ps = psS[i % 2]
                s.op("pe", lambda e, ps=ps, tsl=tsl: e.matmul(ps[:, 0:128], lhsT=kb[:, tsl], rhs=qb[:, tsl], start=True, stop=True),
                     reads=["rkb", "rqb"], writes=["psS%d" % (i % 2)])
                mk = rmsk[i % 2]
                s.op("dve", TT(mk[:], ps[:, 0:128], Ds[:], ALU.mult), reads=["psS%d" % (i % 2), "Ds"], writes=["rmsk%d" % (i % 2)])
                for v in range(2):
                    reg = psA[v][:, tl * 128:(tl + 1) * 128]
                    vs = slice(v * 128, (v + 1) * 128)
                    mms([(reg, rvTM[:, i, vs], mk[:], True, False),
                         (reg, Rsb[0][:, i, vs], qin[0][:, tsl], False, False),
                         (reg, Rsb[1][:, i, vs], qin[1][:, tsl], False, True)],
                        ["rvTM", "rmsk%d" % (i % 2), "Rsb0_%d" % i, "Rsb1_%d" % i, "qin0", "qin1"], ["psA%d" % v])
            for v in range(2):
                s.op("dve", CP(ro32[v][:], psA[v][:, 0:CH]), reads=["psA%d" % v], writes=["ro32_%d" % v])
                s.op("act", lambda e, v=v: e.copy(out=rob[v][:], in_=psA[v][:, 0:CH]), reads=["psA%d" % v], writes=["rob%d" % v])
                s.op("act", ACT(rsq[v][:], psA[v][:, 0:CH], AF.Square), reads=["psA%d" % v], writes=["rsq%d" % v])
            acc_mm(psA[2][:, 0:CH], [(onesb[:], rob[0][:]), (onesb[:], rob[1][:])], ["onesb", "rob0", "rob1"], ["psA2"])
            acc_mm(psA[3][:, 0:CH], [(onesb[:], rsq[0][:]), (onesb[:], rsq[1][:])], ["onesb", "rsq0", "rsq1"], ["psA3"])
            s.op("dve", TS(mean[:], psA[2][:, 0:CH], 1.0 / 256, None, ALU.mult), reads=["psA2"], writes=["mean"])
            s.op("dve", TT(var[:], mean[:], mean[:], ALU.mult), reads=["mean"], writes=["var"])
            s.op("dve", STT(var[:], psA[3][:, 0:CH], 1.0 / 256, var[:], ALU.mult, ALU.subtract), reads=["psA3", "var"], writes=["var"])
            s.op("dve", TS(var[:], var[:], EPS, None, ALU.add), reads=["var"], writes=["var"])
            s.op("act", ACT(var[:], var[:], AF.Sqrt), reads=["var"], writes=["var"])
            s.op("dve", lambda e: e.reciprocal(out=var[:], in_=var[:]), reads=["var"], writes=["var"])
            for v in range(2):
                s.op("dve", TT(ro32[v][:], ro32[v][:], mean[:], ALU.subtract), reads=["ro32_%d" % v, "mean"], writes=["ro32_%d" % v])
                s.op("dve", STT(ro32[v][:], ro32[v][:], retg[:, v:v + 1], var[:], ALU.mult, ALU.mult),
                     reads=["ro32_%d" % v, "retg", "var"], writes=["ro32_%d" % v])
                ob = oab_o[oab_ctr[0] % 2]
                otok = "oabo%d" % (oab_ctr[0] % 2)
                oab_ctr[0] += 1
                s.op("dve", TT(ob[:], ro32[v][:], rg[:, v, cs], ALU.mult), reads=["ro32_%d" % v, "rg"], writes=[otok])
                ci = H + 2 * h + v
                s.dma("sp", lambda e, ob=ob, ci=ci, cs=cs: e.dma_start(out=OAB.ap()[ci, :, cs], in_=ob[:]), reads=[otok], writes=["OAB_%d_%d" % (ci, ch)])
    s.barrier()
    A.release(mC)

    if cfg.get("STOP") == "C":
        s.finish(); nc._cfg = cfg; nc._ninst = s.ninst; nc._peak = 0; nc._dbg = dbg_outs
        return nc
    mD = A.mark()
    s.dma("sp", lambda e: e.dma_start(out=rw[:], in_=rw_d.ap().rearrange("(kt p) n -> p kt n", p=128)), writes=["rw"])
    load_vec_row(rbrow[:], rb_d, "rbrow")
    mD1 = A.mark()
    for ch in range(NCHK):
        cs = slice(ch * CH, (ch + 1) * CH)
        conds = sorted(set(tile_cond[ch * TPC:(ch + 1) * TPC]))
        A.release(mD1)
        oab = A.alloc("oab", [128, 3 * H, CH], BF16)
        s.dma("sp", lambda e, cs=cs: e.dma_start(out=oab[:], in_=OAB.ap()[:, :, cs].rearrange("c p t -> p c t")),
              reads=["OAB_%d_%d" % (ci, ch) for ci in range(3 * H)], writes=["oab"])
        merged = A.alloc("merged", [128, KT, CH], BF16)
        mD2 = A.mark()
        sga = A.alloc("sga", [128, JT, CH], F32)
        sgb = A.alloc("sgb", [128, JT, CH], F32)
        m1 = A.alloc("m1", [128, JT, CH], F32)
        for g in range(NG):
            slot, wt = wload(win_d.ap()[:, cfg["MG0"] + g * GW: cfg["MG0"] + (g + 1) * GW], KT, GW)
            for j in range(JT):
                acc_mm(psA[j][:, 0:CH], [(slot[:, kt, j * 128:(j + 1) * 128], xmT[:, kt, cs]) for kt in range(KT)], [wt] + xmT_toks, ["psA%d" % j])
                s.op("act", ACT(sga[:, j, :], psA[j][:, 0:CH], AF.Sigmoid), reads=["psA%d" % j], writes=["sga"])
            slot, wt = wload(win_d.ap()[:, cfg["MG0"] + D + g * GW: cfg["MG0"] + D + (g + 1) * GW], KT, GW)
            for j in range(JT):
                acc_mm(psA[j][:, 0:CH], [(slot[:, kt, j * 128:(j + 1) * 128], xmT[:, kt, cs]) for kt in range(KT)], [wt] + xmT_toks, ["psA%d" % j])
                s.op("act", ACT(sgb[:, j, :], psA[j][:, 0:CH], AF.Sigmoid), reads=["psA%d" % j], writes=["sgb"])
            slot, wt = wload(wpa_d.ap()[:, g * GW:(g + 1) * GW], H, GW)
            for j in range(JT):
                acc_mm(psA[j][:, 0:CH], [(slot[:, c, j * 128:(j + 1) * 128], oab[:, c, :]) for c in range(H)], [wt, "oab"], ["psA%d" % j])
                s.op("dve", TT(m1[:, j, :], sga[:, j, :], psA[j][:, 0:CH], ALU.mult), reads=["sga", "psA%d" % j], writes=["m1"])
            slot, wt = wload(wpb_d.ap()[:, g * GW:(g + 1) * GW], 2 * H, GW)
            for j in range(JT):
                acc_mm(psA[j][:, 0:CH], [(slot[:, c, j * 128:(j + 1) * 128], oab[:, H + c, :]) for c in range(2 * H)], [wt, "oab"], ["psA%d" % j])
                s.op("dve", TT(sgb[:, j, :], sgb[:, j, :], psA[j][:, 0:CH], ALU.mult), reads=["sgb", "psA%d" % j], writes=["sgb"])
                s.op("dve", TT(merged[:, g * JT + j, :], m1[:, j, :], sgb[:, j, :], ALU.add), reads=["m1", "sgb"], writes=["merged"])
        if debug:
            for kt in range(KT):
                dump("merged%d_%d" % (ch, kt), merged[:, kt, :], ["merged"])
        s.barrier()
        A.release(mD2)
        g1row = {}
        for c in conds:
            g1row[c] = A.alloc("g1row%d" % c, [128, D], F32)
            load_row(g1row[c][:], c, 2, "g1row%d" % c)
        xp = [A.alloc("xp%d" % i, [128, GW], F32) for i in range(2)]
        tp = A.alloc("tp", [128, GW], F32)
        xpc = 0
        for g in range(NG):
            slot, wt = wload(wout_d.ap()[:, g * GW:(g + 1) * GW], KT, GW)
            for tl in range(TPC):
                i = ch * TPC + tl
                c = tile_cond[i]
                ps = psA[tl % 4]
                acc_mm(ps[:, 0:GW], [(merged[:, kt, tl * 128:(tl + 1) * 128], slot[:, kt, 0:GW]) for kt in range(KT)], ["merged", wt], ["psA%d" % (tl % 4)])
                xq = xp[xpc % 2]
                xtok = "xp%d" % (xpc % 2)
                xpc += 1
                s.dma("sp", lambda e, xq=xq, i=i, g=g: e.dma_start(out=xq[:], in_=x_d.ap()[i * 128:(i + 1) * 128, g * GW:(g + 1) * GW]), writes=[xtok])
                s.op("dve", TT(tp[:], ps[:, 0:GW], g1row[c][:, g * GW:(g + 1) * GW], ALU.mult), reads=["psA%d" % (tl % 4), "g1row%d" % c], writes=["tp"])
                s.op("dve", TT(xq[:], tp[:], xq[:], ALU.add), reads=["tp", xtok], writes=[xtok])
                s.dma("sp", lambda e, xq=xq, i=i, g=g: e.dma_start(out=X1.ap()[i * 128:(i + 1) * 128, g * GW:(g + 1) * GW], in_=xq[:]),
                      reads=[xtok], writes=["X1_%d_%d" % (i, g)])
        s.barrier()
        A.release(mD1)
        rows2 = {}
        n2row = A.alloc("n2row", [128, D], F32)
        load_vec_row(n2row[:], n2g_d, "n2row")
        for c in conds:
            a2 = A.alloc("a2row%d" % c, [128, D], F32)
            sh2 = A.alloc("sh2row%d" % c, [128, D], F32)
            load_row(a2[:], c, 4, "a2row%d" % c)
            load_row(sh2[:], c, 3, "sh2row%d" % c)
            s.op("dve", STT(a2[:], a2[:], 1.0, n2row[:], ALU.add, ALU.mult), reads=["a2row%d" % c, "n2row"], writes=["a2row%d" % c])
            rows2[c] = (a2, sh2)
        x1t = [A.alloc("x1t%d" % i, [128, D], F32) for i in range(2)]
        xm2 = A.alloc("xm2", [128, D], F32)
        xm2b = [A.alloc("xm2b%d" % i, [128, D], BF16) for i in range(2)]
        xm2T = A.alloc("xm2T", [128, KT, 128], F32)
        junk = A.alloc("junk2", [128, D], BF16)
        for tl in range(TPC):
            i = ch * TPC + tl
            c = tile_cond[i]
            xt = x1t[i % 2]
            xtok = "x1t%d" % (i % 2)
            s.dma("sp", lambda e, xt=xt, i=i: e.dma_start(out=xt[:], in_=X1.ap()[i * 128:(i + 1) * 128, :]),
                  reads=["X1_%d_%d" % (i, g) for g in range(NG)], writes=[xtok])
            sc, tk = rms_rstd(xt[:], xtok, 2 + i % 2, D)
            a2, sh2 = rows2[c]
            s.op("dve", STT(xm2[:], xt[:], sc, a2[:], ALU.mult, ALU.mult), reads=[xtok, tk, "a2row%d" % c], writes=["xm2"])
            s.op("dve", TT(xm2[:], xm2[:], sh2[:], ALU.add), reads=["xm2", "sh2row%d" % c], writes=["xm2"])
            xb_ = xm2b[i % 2]
            s.op("act", lambda e, xb_=xb_: e.copy(out=xb_[:], in_=xm2[:]), reads=["xm2"], writes=["xm2b%d" % (i % 2)])
            s.dma("sp", lambda e, xb_=xb_, i=i: e.dma_start(out=XM2.ap()[i * 128:(i + 1) * 128, :], in_=xb_[:]),
                  reads=["xm2b%d" % (i % 2)], writes=["XM2_%d" % i])
            transposes([xm2[:, kt * 128:(kt + 1) * 128] for kt in range(KT)], lambda g, m: xm2T[:, g:g + m, :], ["xm2"],
                       lambda g, m: ["xm2T"], evac_eng="dve", f32=True)
            acc_mm(psS[0][:, 0:E], [(xm2T[:, kt, :], rw[:, kt, :]) for kt in range(KT)], ["xm2T", "rw"], ["psS0"])
            s.op("dve", TT(lgt[:], psS[0][:, 0:E], rbrow[:], ALU.add), reads=["psS0", "rbrow"], writes=["lgt"])
            if debug:
                dump("logits%d" % i, lgt[:], ["lgt"])
            s.op("dve", lambda e: e.max(out=top8[:], in_=lgt[:]), reads=["lgt"], writes=["top8"])
            s.op("dve", TS(Mf[:, i, :], lgt[:], top8[:, 3:4], None, ALU.is_ge), reads=["lgt", "top8"], writes=["Mf"])
            s.op("dve", TS(top8[:, 7:8], top8[:, 0:1], -1.0, None, ALU.mult), reads=["top8"], writes=["top8"])
            s.op("act", ACT(pex[:], lgt[:], AF.Exp, bias=top8[:, 7:8]), reads=["lgt", "top8"], writes=["pex"])
            s.op("dve", TT(pex[:], pex[:], Mf[:, i, :], ALU.mult), reads=["pex", "Mf"], writes=["pex"])
            s.op("dve", lambda e: e.reduce_sum(out=top8[:, 6:7], in_=pex[:], axis=AX.X), reads=["pex"], writes=["top8"])
            s.op("dve", lambda e: e.reciprocal(out=top8[:, 6:7], in_=top8[:, 6:7]), reads=["top8"], writes=["top8"])
            s.op("dve", TS(Wd[:, i, :], pex[:], top8[:, 6:7], None, ALU.mult), reads=["pex", "top8"], writes=["Wd"])
            s.op("dve", CP(Mb[:, i, :], Mf[:, i, :]), reads=["Mf"], writes=["Mb"])
        s.barrier()
    A.release(mP)

    if cfg.get("STOP") == "D":
        s.finish(); nc._cfg = cfg; nc._ninst = s.ninst; nc._peak = 0; nc._dbg = dbg_outs
        return nc
    destk = A.alloc("destk", [128, NT, TOPK], F32)
    destki = A.alloc("destki", [128, NT, TOPK], I32)
    wk = A.alloc("wk", [128, NT, TOPK], F32)
    SEGCAP = cfg.get("SEGCAP", 2048)
    SEGG, SEGD = min(SEGCAP, KT * CWG), min(SEGCAP, KT * CWD)
    AG, AD = KT * CWG // SEGG, KT * CWD // SEGD
    idxg = A.alloc("idxg", [128, NCJG, AG, NPAIR], I32)
    idxd = A.alloc("idxd", [128, NCJD, AD, NPAIR], I32)
    beig = A.alloc("beig", [128, NCJG, NPAIR], I32)
    beid = A.alloc("beid", [128, NCJD, NPAIR], I32)
    usedi = A.alloc("usedi", [128, NPAIR], I32)
    usedh = A.alloc("usedh", [128, NB], I32)
    GH = cfg["GH"]
    GR = 128 * GH
    mE2 = A.mark()
    rank = A.alloc("rank", [128, NT, E], F32)
    dest = A.alloc("dest", [128, NT, E], F32)
    csel = A.alloc("csel", [128, NT, E], F32)
    oh = A.alloc("oh", [128, NT, E], F32)
    tmpE = A.alloc("tmpE", [128, NT, E], F32)
    cnt = A.alloc("cnt", [128, E], F32)
    NT2 = (NT + GH - 1) // GH
    cmpc = A.alloc("cmpc", [128, E, NT2], F32)
    hc1 = A.alloc("hc1", [128, NB, E], F32)
    hc2 = A.alloc("hc2", [128, NB, E], F32)
    endr = A.alloc("endr", [128, E], F32)
    uhf = A.alloc("uhf", [128, NB], F32)
    padf = A.alloc("padf", [128, E], F32)
    pend = A.alloc("pend", [128, E], F32)
    pstart = A.alloc("pstart", [128, E], F32)
    tokidi = A.alloc("tokidi", [128, NT], I32)
    pe_ = A.alloc("pe", [128, NPAIR], F32)
    tpe = A.alloc("tpe", [128, NPAIR], F32)
    cmpb = A.alloc("cmpb", [128, NPAIR, E], F32)
    rtinit = A.alloc("rtinit", [128, 128], I32)
    tokrep = A.alloc("tokrep", [128, NT, 128], I32)

    for i in range(NT):
        pairs = [(onesb[:], Mb[:, j, :]) for j in range(i)] + [(slfb[:], Mb[:, i, :])]
        acc_mm(psS[i % 2][:, 0:E], pairs, ["onesb", "slfb", "Mb"], ["psS%d" % (i % 2)])
        s.op("dve", CP(rank[:, i, :], psS[i % 2][:, 0:E]), reads=["psS%d" % (i % 2)], writes=["rank"])
    acc_mm(psS[0][:, 0:E], [(onesb[:], Mb[:, j, :]) for j in range(NT)], ["onesb", "Mb"], ["psS0"])
    s.op("dve", CP(cnt[:], psS[0][:, 0:E]), reads=["psS0"], writes=["cnt"])
    pbo, _ = coff["pbase"]
    s.op("dve", TT(cmpc[:], cnt[:].unsqueeze(2).to_broadcast([128, E, NT2]), cst[:, pbo:pbo + NT2].unsqueeze(1).to_broadcast([128, E, NT2]), ALU.is_gt),
         reads=["cnt", "cst"], writes=["cmpc"])
    s.op("dve", lambda e: e.tensor_reduce(out=padf[:], in_=cmpc[:], axis=AX.X, op=ALU.add), reads=["cmpc"], writes=["padf"])
    s.op("dve", TS(padf[:], padf[:], float(GR), None, ALU.mult), reads=["padf"], writes=["padf"])
    oro, _ = coff["onesrow"]
    s.op("dve", lambda e: e.tensor_tensor_scan(out=pend[:], data0=cst[:, oro:oro + E], data1=padf[:], initial=0.0, op0=ALU.mult, op1=ALU.add),
         reads=["padf", "cst"], writes=["pend"])
    s.op("dve", TT(pstart[:], pend[:], padf[:], ALU.subtract), reads=["pend", "padf"], writes=["pstart"])
    s.op("dve", TT(endr[:], pstart[:], cnt[:], ALU.add), reads=["pstart", "cnt"], writes=["endr"])
    bbo_, _ = coff["bbase"]
    bbv = cst[:, bbo_:bbo_ + NB].unsqueeze(2).to_broadcast([128, NB, E])
    s.op("dve", TT(hc1[:], pstart[:].unsqueeze(1).to_broadcast([128, NB, E]), bbv, ALU.is_le), reads=["pstart", "cst"], writes=["hc1"])
    s.op("dve", TT(hc2[:], endr[:].unsqueeze(1).to_broadcast([128, NB, E]), bbv, ALU.is_gt), reads=["endr", "cst"], writes=["hc2"])
    s.op("dve", TT(hc1[:], hc1[:], hc2[:], ALU.mult), reads=["hc1", "hc2"], writes=["hc1"])
    s.op("dve", lambda e: e.tensor_reduce(out=uhf[:], in_=hc1[:], axis=AX.X, op=ALU.add), reads=["hc1"], writes=["uhf"])
    s.op("dve", CP(usedh[:], uhf[:]), reads=["uhf"], writes=["usedi"])
    for i in range(NT):
        s.op("dve", TT(dest[:, i, :], rank[:, i, :], pstart[:], ALU.add), reads=["rank", "pstart"], writes=["dest"])
        s.op("dve", lambda e, i=i: e.tensor_tensor_scan(out=csel[:, i, :], data0=cst[:, oro:oro + E], data1=Mf[:, i, :], initial=0.0,
                                                          op0=ALU.mult, op1=ALU.add), reads=["Mf", "cst"], writes=["csel"])
    for k in range(TOPK):
        s.op("dve", STT(oh[:], csel[:], float(k + 1), Mf[:], ALU.is_equal, ALU.mult), reads=["csel", "Mf"], writes=["oh"])
        s.op("dve", TT(tmpE[:], oh[:], dest[:], ALU.mult), reads=["oh", "dest"], writes=["tmpE"])
        s.op("dve", lambda e, k=k: e.tensor_reduce(out=destk[:, :, k], in_=tmpE[:], axis=AX.X, op=ALU.add), reads=["tmpE"], writes=["destk"])
        s.op("dve", TT(tmpE[:], oh[:], Wd[:], ALU.mult), reads=["oh", "Wd", "destk"], writes=["tmpE"])
        s.op("dve", lambda e, k=k: e.tensor_reduce(out=wk[:, :, k], in_=tmpE[:], axis=AX.X, op=ALU.add), reads=["tmpE"], writes=["wk"])
    s.op("dve", CP(destki[:], destk[:]), reads=["destk"], writes=["destki"])
    s.op("dve", CP(tokidi[:], C("tokid")), reads=["cst"], writes=["tokidi"])
    s.op("dve", TT(cmpb[:], pend[:].unsqueeze(1).to_broadcast([128, NPAIR, E]), cst[:, pbo:pbo + NPAIR].unsqueeze(2).to_broadcast([128, NPAIR, E]), ALU.is_le),
         reads=["pend", "cst"], writes=["cmpb"])
    s.op("dve", lambda e: e.tensor_reduce(out=pe_[:], in_=cmpb[:], axis=AX.X, op=ALU.add), reads=["cmpb"], writes=["pe"])
    cpo, _ = coff["cp"]
    tpe2 = A.alloc("tpe2", [128, NPAIR], F32)
    s.op("dve", TS(tpe2[:], pe_[:], float(E), None, ALU.is_lt), reads=["pe"], writes=["tpe2"])
    s.op("dve", CP(usedi[:], tpe2[:]), reads=["tpe2"], writes=["usedi"])
    for (ncj, na, idx_t, be_t) in ((NCJG, AG, idxg, beig), (NCJD, AD, idxd, beid)):
        for cj in range(ncj):
            s.op("dve", TS(tpe[:], pe_[:], float(ncj), float(cj), ALU.mult, ALU.add), reads=["pe", "idxtabs"], writes=["tpe"])
            s.op("dve", CP(be_t[:, cj, :], tpe[:]), reads=["tpe"], writes=["idxtabs"])
            s.op("dve", TS(tpe[:], tpe[:], 128.0, cst[:, cpo:cpo + 1], ALU.mult, ALU.add), reads=["tpe", "cst", "idxtabs"], writes=["tpe"])
            for a_ in range(na):
                s.op("dve", TS(tpe2[:], tpe[:], float(na), float(a_), ALU.mult, ALU.add), reads=["tpe", "idxtabs"], writes=["tpe2"])
                s.op("dve", CP(idx_t[:, cj, a_, :], tpe2[:]), reads=["tpe2"], writes=["idxtabs"])
    s.op("dve", lambda e: e.memset(rtinit[:], T), writes=["rtinit"])
    for b in range(NB):
        s.dma("sp", lambda e, b=b: e.dma_start(out=ROWTOK.ap()[b * 128:(b + 1) * 128, :], in_=rtinit[:]), reads=["rtinit"], writes=["ROWTOK%d" % b])
    for i in range(NT):
        s.op("dve", CP(tokrep[:, i, :], tokidi[:, i:i + 1].to_broadcast([128, 128])), reads=["tokidi"], writes=["tokrep"])
    rt_toks = []
    for i in range(NT):
        for k in range(TOPK):
            tk = "RT_%d_%d" % (i, k)
            rt_toks.append(tk)
            s.dma("pool", lambda e, i=i, k=k: e.indirect_dma_start(
                out=ROWTOK.ap(), out_offset=bass.IndirectOffsetOnAxis(ap=destki[:, i, k:k + 1], axis=0),
                in_=tokrep[:, i, :], in_offset=None, bounds_check=BC(e, R - 1), oob_is_err=False),
                reads=["ROWTOK%d" % b for b in range(NB)] + ["destki", "tokrep"], writes=[tk])
    if debug:
        dump("destk", destk[:].rearrange("p t k -> p (t k)"), ["destk"])
        dump("wk", wk[:].rearrange("p t k -> p (t k)"), ["wk"])
    s.barrier()
    if cfg.get("STOP") == "E":
        s.finish(); nc._cfg = cfg; nc._ninst = s.ninst; nc._peak = 0; nc._dbg = dbg_outs
        return nc
    A.release(mE2)
    mF = A.mark()
    FT = DFF // 128
    CWM = max(CWG, CWD)
    NSLOT = 2
    mslots = [A.alloc("mslot%d" % i, [128, KT * CWM], BF16) for i in range(NSLOT)]
    mctr = [0]
    rtb = [A.alloc("rtb%d" % i, [128, 16], I32) for i in range(GH)]
    xg = [A.alloc("xg%d" % i, [128, D], BF16) for i in range(GH)]
    xgT = [A.alloc("xgT%d" % i, [128, KT, 128], BF16) for i in range(GH)]
    bch = [A.alloc("bch%d" % i, [128, CWM], BF16) for i in range(2)]
    bctr = [0]
    PWG = min(512, CWG)
    PWD = min(512, CWD)
    hb = [A.alloc("hb%d" % i, [128, PWG], F32) for i in range(2)]
    gt = A.alloc("gt", [128, PWG // 2], F32)
    ut = A.alloc("ut", [128, PWG // 2], F32)
    sgt = A.alloc("sgt", [128, PWG // 2], F32)
    actb = [A.alloc("actb%d" % i, [128, DFF], BF16) for i in range(GH)]
    actT = [A.alloc("actT%d" % i, [128, FT, 128], BF16) for i in range(GH)]
    yp = [A.alloc("yp%d" % i, [128, CWD], F32) for i in range(2)]
    ypc = [0]
    zt = A.alloc("zt", [128, CWD], F32)
    s.op("dve", lambda e: e.memset(zt[:], 0.0), writes=["zt"])
    for i in range(GH):
        s.op("dve", lambda e, i=i: e.memset(xg[i][:], 0.0), writes=["xg%d" % i])
    for i in range(NSLOT):
        s.op("dve", lambda e, i=i: e.memset(mslots[i][:], 0.0), writes=["mslot%d_%d" % (i, a_) for a_ in range(max(AG, AD))])

    def mload(src_d, idx_tab, cj, g, nrows, ncols, seg, na):
        i = mctr[0] % NSLOT
        mctr[0] += 1
        slot = mslots[i]
        tok = "mslot%d" % i
        n = KT * ncols
        srcv = src_d.ap().rearrange("r (a s) -> (r a) s", s=seg)
        toks = []
        for a_ in range(na):
            s.dma("pool", lambda e, a_=a_: e.indirect_dma_start(
                out=slot[:, a_ * seg:(a_ + 1) * seg], out_offset=None, in_=srcv,
                in_offset=bass.IndirectOffsetOnAxis(ap=idx_tab[:, cj, a_, g:g + 1], axis=0),
                bounds_check=BC(e, nrows * na - 1), oob_is_err=False), reads=["idxtabs"], writes=[tok + "_%d" % a_])
            toks.append(tok + "_%d" % a_)
        return slot[:, 0:n].rearrange("p (k c) -> p k c", c=ncols), toks

    def bload(src_d, idx_ap, nrows, ncols):
        i = bctr[0] % 2
        bctr[0] += 1
        bt = bch[i]
        tok = "bch%d" % i
        s.dma("pool", lambda e: e.indirect_dma_start(
            out=bt[:, 0:ncols], out_offset=None, in_=src_d.ap(), in_offset=bass.IndirectOffsetOnAxis(ap=idx_ap, axis=0),
            bounds_check=BC(e, nrows - 1), oob_is_err=False), reads=["idxtabs"], writes=[tok])
        return bt, tok

    USE_CF = cfg.get("CF", True)

    def hbeg(g, hf):
        if USE_CF:
            s.group_begin(usedh[0:1, g * GH + hf:g * GH + hf + 1])

    def hend(else_dmas=None):
        if USE_CF:
            s.group_end(else_dmas)

    for g in range(NPAIR):
        outer = USE_CF and g >= cfg.get("CF_MIN", T * TOPK // GR)
        if outer:
            s.group_begin(usedi[0:1, g:g + 1])
        for hf in range(GH):
            b = GH * g + hf
            hbeg(g, hf)
            s.dma("sp", lambda e, b=b, hf=hf: e.dma_start(out=rtb[hf][:], in_=ROWTOK.ap()[b * 128:(b + 1) * 128, 0:16]),
                  reads=rt_toks + ["ROWTOK%d" % b], writes=["rtb%d" % hf])
            s.dma("pool", lambda e, hf=hf: e.indirect_dma_start(
                out=xg[hf][:], out_offset=None, in_=XM2.ap(), in_offset=bass.IndirectOffsetOnAxis(ap=rtb[hf][:, 0:1], axis=0),
                bounds_check=BC(e, T - 1), oob_is_err=False), reads=["rtb%d" % hf] + ["XM2_%d" % i for i in range(NT)], writes=["xg%d" % hf])
            transposes([xg[hf][:, kt * 128:(kt + 1) * 128] for kt in range(KT)], lambda g_, m, hf=hf: xgT[hf][:, g_:g_ + m, :], ["xg%d" % hf],
                       lambda g_, m, hf=hf: ["xgT%d" % hf])
            hend()
        for cj in range(NCJG):
            slot, stoks = mload(wgu_d, idxg, cj, g, E * NCJG * 128, CWG, SEGG, AG)
            bt, btok = bload(bgu_d, beig[:, cj, g:g + 1], E * NCJG, CWG)
            npc = CWG // PWG
            for hf in range(GH):
                hbeg(g, hf)
                for pc in range(npc):
                    bank = (hf * npc + pc) % 4
                    acc_mm(psA[bank][:, 0:PWG], [(xgT[hf][:, kt, :], slot[:, kt, pc * PWG:(pc + 1) * PWG]) for kt in range(KT)],
                           ["xgT%d" % hf] + stoks, ["psA%d" % bank])
                    hbb = hb[pc % 2]
                    htok = "hb%d" % (pc % 2)
                    HW_ = PWG // 2
                    s.op("dve", TT(hbb[:], psA[bank][:, 0:PWG], bt[:, pc * PWG:(pc + 1) * PWG], ALU.add), reads=["psA%d" % bank, btok], writes=[htok])
                    hv = hbb[:].rearrange("p (f two) -> p f two", two=2)
                    s.op("dve", TS(gt[:], hv[:, :, 0], 7.0, None, ALU.min), reads=[htok], writes=["gt"])
                    s.op("dve", TS(ut[:], hv[:, :, 1], -7.0, 7.0, ALU.max, ALU.min), reads=[htok], writes=["ut"])
                    s.op("act", ACT(sgt[:], gt[:], AF.Sigmoid, scale=1.702), reads=["gt"], writes=["sgt"])
                    s.op("dve", TT(gt[:], gt[:], sgt[:], ALU.mult), reads=["gt", "sgt"], writes=["gt"])
                    f0 = (cj * CWG + pc * PWG) // 2
                    s.op("dve", STT(actb[hf][:, f0:f0 + HW_], ut[:], 1.0, gt[:], ALU.add, ALU.mult), reads=["ut", "gt"], writes=["actb%d" % hf])
                hend()
        for hf in range(GH):
            hbeg(g, hf)
            transposes([actb[hf][:, ft * 128:(ft + 1) * 128] for ft in range(FT)], lambda g_, m, hf=hf: actT[hf][:, g_:g_ + m, :], ["actb%d" % hf],
                       lambda g_, m, hf=hf: ["actT%d" % hf])
            hend()
        for cj in range(NCJD):
            slot, stoks = mload(wdn_d, idxd, cj, g, E * NCJD * 128, CWD, SEGD, AD)
            bt, btok = bload(bdn_d, beid[:, cj, g:g + 1], E * NCJD, CWD)
            npc = CWD // PWD
            for hf in range(GH):
                b = GH * g + hf
                hbeg(g, hf)
                ypb = yp[ypc[0] % 2]
                ytok = "yp%d" % (ypc[0] % 2)
                ypc[0] += 1
                for pc in range(npc):
                    bank = (hf * npc + pc) % 4
                    acc_mm(psA[bank][:, 0:PWD], [(actT[hf][:, kt, :], slot[:, kt, pc * PWD:(pc + 1) * PWD]) for kt in range(KT)],
                           ["actT%d" % hf] + stoks, ["psA%d" % bank])
                    s.op("dve", TT(ypb[:, pc * PWD:(pc + 1) * PWD], psA[bank][:, 0:PWD], bt[:, pc * PWD:(pc + 1) * PWD], ALU.add),
                         reads=["psA%d" % bank, btok], writes=[ytok])
                ydst = Y.ap()[b * 128:(b + 1) * 128, cj * CWD:(cj + 1) * CWD]
                s.dma("sp", lambda e, ypb=ypb, ydst=ydst: e.dma_start(out=ydst, in_=ypb[:]), reads=[ytok], writes=["Y_%d_%d" % (b, cj)])
                hend(else_dmas={"sp": [(ydst, zt[:])]})
        if outer:
            s.group_end(else_dmas={"sp": [(Y.ap()[(GH * g + hf) * 128:(GH * g + hf + 1) * 128, cj * CWD:(cj + 1) * CWD], zt[:])
                                          for hf in range(GH) for cj in range(NCJD)]})
    s.barrier()
    A.release(mF)

    if cfg.get("STOP") == "F":
        s.finish(); nc._cfg = cfg; nc._ninst = s.ninst; nc._peak = 0; nc._dbg = dbg_outs
        return nc
    y_toks = ["Y_%d_%d" % (b, cj) for b in range(NB) for cj in range(NCJD)]
    yk = [A.alloc("yk%d" % k, [128, D], F32) for k in range(2 * TOPK)]
    acc = A.alloc("acc", [128, D], F32)
    x1gs = [A.alloc("x1g%d" % i, [128, D], F32) for i in range(2)]
    g2row = {}
    for c in range(2):
        g2row[c] = A.alloc("g2row%d" % c, [128, D], F32)
        load_row(g2row[c][:], c, 5, "g2row%d" % c)
    nfrow = A.alloc("nfrow", [128, D], F32)
    load_vec_row(nfrow[:], nfg_d, "nfrow")
    junk = A.alloc("junk3", [128, D], BF16)
    yout = [A.alloc("yout%d" % i, [128, D], F32) for i in range(2)]
    for k in range(2 * TOPK):
        s.op("dve", lambda e, k=k: e.memset(yk[k][:], 0.0), writes=["yk%d" % k])
    for i in range(NT):
        c = tile_cond[i]
        kb_ = (i % 2) * TOPK
        x1g = x1gs[i % 2]
        for k in range(TOPK):
            s.dma("pool", lambda e, i=i, k=k, kb_=kb_: e.indirect_dma_start(
                out=yk[kb_ + k][:], out_offset=None, in_=Y.ap(), in_offset=bass.IndirectOffsetOnAxis(ap=destki[:, i, k:k + 1], axis=0),
                bounds_check=BC(e, R - 1), oob_is_err=False), reads=y_toks + ["destki"], writes=["yk%d" % (kb_ + k)])
        s.dma("sp", lambda e, i=i, x1g=x1g: e.dma_start(out=x1g[:], in_=X1.ap()[i * 128:(i + 1) * 128, :]), writes=["x1g%d" % (i % 2)])
        s.op("dve", TS(acc[:], yk[kb_][:], wk[:, i, 0:1], None, ALU.mult), reads=["yk%d" % kb_, "wk"], writes=["acc"])
        for k in range(1, TOPK):
            s.op("dve", STT(acc[:], yk[kb_ + k][:], wk[:, i, k:k + 1], acc[:], ALU.mult, ALU.add), reads=["yk%d" % (kb_ + k), "wk", "acc"], writes=["acc"])
        s.op("dve", TT(acc[:], acc[:], g2row[c][:], ALU.mult), reads=["acc", "g2row%d" % c], writes=["acc"])
        s.op("dve", TT(acc[:], acc[:], x1g[:], ALU.add), reads=["acc", "x1g%d" % (i % 2)], writes=["acc"])
        sc, tk = rms_rstd(acc[:], "acc", 4 + i % 2, D)
        yo = yout[i % 2]
        s.op("dve", STT(yo[:], acc[:], sc, nfrow[:], ALU.mult, ALU.mult), reads=["acc", tk, "nfrow"], writes=["yout%d" % (i % 2)])
        s.dma("sp", lambda e, yo=yo, i=i: e.dma_start(out=y_o.ap()[i * 128:(i + 1) * 128, :], in_=yo[:]), reads=["yout%d" % (i % 2)], writes=["y_%d" % i])
    s.finish()
    nc._cfg = cfg
    nc._ninst = s.ninst
    nc._peak = A.peak - A.base
    nc._dbg = dbg_outs
    return nc


def prepare_core_inputs(inp, cfg, core):
    cfg = derive(cfg)
    D, H, KT, NP = cfg["D"], cfg["H"], cfg["KT"], cfg["NP"]
    f = np.float32
    xp = np.asarray(inp["x_prompt"], f)
    xs = np.asarray(inp["x_sample"], f)
    x = np.concatenate([xp[core * NP + p] for p in range(NP)] + [xs[core]], axis=0)
    c_ctx = np.asarray(inp["c_ctx"], f)
    c = np.asarray(inp["c"], f)[core]
    cT = np.stack([c_ctx.reshape(KT, 128).T, c.reshape(KT, 128).T], axis=-1)

    def fm(v, n):
        return np.ascontiguousarray(np.asarray(v, f).reshape(n, 128).T)

    lbf = np.stack([fm(inp["hg_lb_fwd"][0], H), fm(inp["hg_lb_fwd"][1], H)], axis=1)
    lbb = np.stack([fm(inp["hg_lb_bwd"][0], H), fm(inp["hg_lb_bwd"][1], H)], axis=1)
    m = {
        "x": np.ascontiguousarray(x),
        "shf": np.ascontiguousarray(np.asarray(inp["state_hgrn_fwd"], f)[core, 0]),
        "shb": np.ascontiguousarray(np.asarray(inp["state_hgrn_bwd"], f)[core, 0]),
        "srf": np.ascontiguousarray(np.asarray(inp["state_ret_fwd"], f)[core, 0]),
        "srb": np.ascontiguousarray(np.asarray(inp["state_ret_bwd"], f)[core, 0]),
        "cT": np.ascontiguousarray(cT),
        "lbf": np.ascontiguousarray(lbf), "lbb": np.ascontiguousarray(lbb),
    }
    return m


def prepare_shared_inputs(inp, cfg):
    cfg = derive(cfg)
    D, H, KT, E = cfg["D"], cfg["H"], cfg["KT"], cfg["E"]
    f = np.float32
    perm = w_in_perm_index(cfg)
    sh = {
        "ada_w": np.ascontiguousarray(np.asarray(inp["ada_w"], f)[0]),
        "ada_b2": np.ascontiguousarray(np.broadcast_to(np.asarray(inp["ada_b"], f)[0][None, :], (2, 6 * D))),
        "n1g": np.asarray(inp["norm1_g"], f)[0][None, :].copy(),
        "n2g": np.asarray(inp["norm2_g"], f)[0][None, :].copy(),
        "nfg": np.asarray(inp["final_norm_g"], f)[None, :].copy(),
        "w_in": np.ascontiguousarray(np.asarray(inp["w_in"], f)[0][:, perm]),
        "hgng": np.asarray(inp["hg_norm_g"], f)[0].reshape(128, 1).copy(),
        "retg": np.ascontiguousarray(np.asarray(inp["ret_norm_g"], f)[0].reshape(2, 128).T),
        "rl2f": np.asarray(inp["ret_log2_fwd"], f)[0][None, :].copy(),
        "rl2b": np.asarray(inp["ret_log2_bwd"], f)[0][None, :].copy(),
        "w_pa": np.ascontiguousarray(np.asarray(inp["w_proj_hgrn"], f)[0]),
        "w_pb": np.ascontiguousarray(np.asarray(inp["w_proj_ret"], f)[0]),
        "w_out": np.ascontiguousarray(np.asarray(inp["w_out"], f)[0]),
        "rw": np.ascontiguousarray(np.asarray(inp["router_w"], f)[0]),
        "rb": np.asarray(inp["router_b"], f)[0][None, :].copy(),
        "w_gu": np.ascontiguousarray(np.asarray(inp["moe_w_gu"], f)[0].reshape(E, KT, 128, cfg["NCJG"], cfg["CWG"]).transpose(0, 3, 2, 1, 4)).reshape(E * cfg["NCJG"] * 128, KT * cfg["CWG"]),
        "b_gu": np.ascontiguousarray(np.asarray(inp["moe_b_gu"], f)[0]).reshape(E * cfg["NCJG"], cfg["CWG"]),
        "w_dn": np.ascontiguousarray(np.asarray(inp["moe_w_dn"], f)[0].reshape(E, KT, 128, cfg["NCJD"], cfg["CWD"]).transpose(0, 3, 2, 1, 4)).reshape(E * cfg["NCJD"] * 128, KT * cfg["CWD"]),
        "b_dn": np.ascontiguousarray(np.asarray(inp["moe_b_dn"], f)[0]).reshape(E * cfg["NCJD"], cfg["CWD"]),
        "cst": make_consts(cfg)[0],
        "rope": make_consts(cfg)[1],
    }
    return sh


def run(inp, cfg, runner=None, debug=False):
    cfgd = derive(cfg)
    ncores = cfgd["NCORES"]
    nc = build(cfg, debug=debug)
    shared = prepare_shared_inputs(inp, cfg)
    in_maps = []
    for core in range(ncores):
        m = dict(shared)
        m.update(prepare_core_inputs(inp, cfg, core))
        in_maps.append(m)
    if runner is None:
        res = run_bass_kernel_spmd(nc, in_maps, core_ids=list(range(ncores))).results
    else:
        res = runner(nc, in_maps)
    NP, TP, TS_, D, H = cfgd["NP"], cfgd["TP"], cfgd["TS"], cfgd["D"], cfgd["H"]
    yp = np.stack([res[c]["y"][p * TP:(p + 1) * TP] for c in range(ncores) for p in range(NP)], axis=0)
    ys = np.stack([res[c]["y"][NP * TP:] for c in range(ncores)], axis=0)
    hf = np.concatenate([res[c]["hf"] for c in range(ncores)], axis=0)[:, None]
    hb = np.concatenate([res[c]["hb"] for c in range(ncores)], axis=0)[:, None]
    rf = np.concatenate([res[c]["rf"] for c in range(ncores)], axis=0)[:, None]
    rb = np.concatenate([res[c]["rb_o"] for c in range(ncores)], axis=0)[:, None]
    outs = tuple(np.ascontiguousarray(a.astype(np.float32)) for a in (yp, ys, hf, hb, rf, rb))
    return outs, res, nc


def kernel(**inputs):
    outs, _, _ = run(inputs, FULL_CFG)
    return outs
```
